# Optimizing a Trainium2 kernel written in Bass

```python
import math
import jax
import jax.numpy as jnp
from jax import lax
import numpy as np

D_MODEL = 2048
BATCH = 2
SEQ = 8192
DEPTH = 4

HEAD_DIM = 128
N_HEADS = 4
BRANCH_WIDTH = N_HEADS * HEAD_DIM
N_BRANCHES = 4
DIFF_DIM = HEAD_DIM // 2
IDX_HEADS = 16
IDX_DIM = 64
TOPK_MAX = 256
DILATED_CONFIGS = ((128, 1), (512, 4), (2048, 16))
N_BUCKETS = 32
MAX_DISTANCE = 2048
BLOCK_Q = 128
RMS_EPS = 1e-6
N_BIAS_HEADS = 3 * N_HEADS
QKV_COLS = 3 * BRANCH_WIDTH
IN_SIZES = (N_BRANCHES * BRANCH_WIDTH, N_BRANCHES * D_MODEL, QKV_COLS, IDX_HEADS * IDX_DIM,
            IDX_DIM, IDX_HEADS, QKV_COLS, N_HEADS, QKV_COLS, QKV_COLS)
IN_OFFSETS = tuple(int(o) for o in np.cumsum(IN_SIZES)[:-1])
N_IN = int(sum(IN_SIZES))

kernel_name = 'hybrid_dsa_fox_diff_dilated_gated'


def rms_norm(x, w):
    xf = x.astype(jnp.float32)
    y = xf * lax.rsqrt(jnp.mean(xf * xf, axis=-1, keepdims=True) + RMS_EPS)
    return (y * w.astype(jnp.float32)).astype(x.dtype)


def t5_bucket(dist):
    dist = jnp.maximum(dist, 0)
    max_exact = N_BUCKETS // 2
    d = jnp.maximum(dist, max_exact).astype(jnp.float32)
    large = max_exact + (jnp.log(d / max_exact) / math.log(MAX_DISTANCE / max_exact)
                         * (N_BUCKETS - max_exact)).astype(jnp.int32)
    large = jnp.minimum(large, N_BUCKETS - 1)
    return jnp.where(dist < max_exact, dist, large)


def sweep_blocks(block_fn, seq):
    out = lax.map(block_fn, jnp.arange(seq // BLOCK_Q))
    n, b, q, h, d = out.shape
    return out.transpose(1, 0, 2, 3, 4).reshape(b, n * q, h, d)


def dsa_attention(q, k, v, q_idx, k_idx, w_idx, bias_table):
    seq = q.shape[1]
    topk = min(TOPK_MAX, seq // 4)
    key_pos = jnp.arange(seq)
    gather = jax.vmap(lambda a, i: a[i])
    k_idx_f = k_idx.astype(jnp.float32)

    def block(i):
        t0 = i * BLOCK_Q
        tq = t0 + jnp.arange(BLOCK_Q)
        qi = lax.dynamic_slice_in_dim(q_idx, t0, BLOCK_Q, axis=1).astype(jnp.float32)
        wi = lax.dynamic_slice_in_dim(w_idx, t0, BLOCK_Q, axis=1).astype(jnp.float32) * IDX_HEADS ** -0.5
        rel = jax.nn.relu(jnp.einsum('bqhd,bsd->bqhs', qi, k_idx_f) * IDX_DIM ** -0.5)
        score = jnp.einsum('bqh,bqhs->bqs', wi, rel)
        causal = key_pos[None, :] <= tq[:, None]
        score = jnp.where(causal, score, -jnp.inf)
        _, sel = lax.top_k(score, topk)
        valid = sel <= tq[None, :, None]
        kg = gather(k, sel)
        vg = gather(v, sel)
        qb = lax.dynamic_slice_in_dim(q, t0, BLOCK_Q, axis=1)
        logits = jnp.einsum('bqhd,bqkhd->bhqk', qb, kg).astype(jnp.float32) * HEAD_DIM ** -0.5
        bias = bias_table[t5_bucket(tq[None, :, None] - sel)].transpose(0, 3, 1, 2)
        logits = jnp.where(valid[:, None], logits + bias, -jnp.inf)
        probs = jax.nn.softmax(logits, axis=-1)
        return jnp.einsum('bhqk,bqkhd->bqhd', probs, vg)

    return sweep_blocks(block, seq)


def forgetting_attention(q, k, v, f_logit):
    seq = q.shape[1]
    key_pos = jnp.arange(seq)
    cum = lax.cumsum(jax.nn.log_sigmoid(f_logit.astype(jnp.float32)), axis=1).transpose(0, 2, 1)

    def block(i):
        t0 = i * BLOCK_Q
        tq = t0 + jnp.arange(BLOCK_Q)
        qb = lax.dynamic_slice_in_dim(q, t0, BLOCK_Q, axis=1)
        cq = lax.dynamic_slice_in_dim(cum, t0, BLOCK_Q, axis=2)
        logits = (jnp.einsum('bqhd,bshd->bhqs', qb, k).astype(jnp.float32) * HEAD_DIM ** -0.5
                  + cq[..., None] - cum[:, :, None, :])
        causal = key_pos[None, :] <= tq[:, None]
        probs = jax.nn.softmax(jnp.where(causal, logits, -jnp.inf), axis=-1)
        return jnp.einsum('bhqs,bshd->bqhd', probs, v)

    return sweep_blocks(block, seq)


def differential_attention(q, k, v, lam, subln_w, bias_table, lambda_init):
    seq = q.shape[1]
    key_pos = jnp.arange(seq)

    def block(i):
        t0 = i * BLOCK_Q
        tq = t0 + jnp.arange(BLOCK_Q)
        qb = lax.dynamic_slice_in_dim(q, t0, BLOCK_Q, axis=1)
        logits = jnp.einsum('bqhcd,bshcd->bhcqs', qb, k).astype(jnp.float32) * DIFF_DIM ** -0.5
        bias = bias_table[t5_bucket(tq[:, None] - key_pos[None, :])].transpose(2, 0, 1)
        causal = key_pos[None, :] <= tq[:, None]
        logits = jnp.where(causal, logits + bias[None, :, None], -jnp.inf)
        probs = jax.nn.softmax(logits, axis=-1)
        attn = probs[:, :, 0] - lam * probs[:, :, 1]
        return jnp.einsum('bhqs,bshd->bqhd', attn, v)

    out = sweep_blocks(block, seq)
    return rms_norm(out, subln_w) * (1.0 - lambda_init)


def dilated_attention(q, k, v, bias_table):
    seq = q.shape[1]

    def block(i):
        t0 = i * BLOCK_Q
        tq = t0 + jnp.arange(BLOCK_Q)
        qb = lax.dynamic_slice_in_dim(q, t0, BLOCK_Q, axis=1)
        outs, lses = [], []
        for window, dilation in DILATED_CONFIGS:
            dist = dilation * jnp.arange(window // dilation + 1)
            idx = tq[:, None] - dist[None, :]
            valid = idx >= 0
            idx = jnp.maximum(idx, 0)
            kg = k[:, idx]
            vg = v[:, idx]
            logits = (jnp.einsum('bqhd,bqkhd->bhqk', qb, kg).astype(jnp.float32) * HEAD_DIM ** -0.5
                      + bias_table[t5_bucket(dist)].T[None, :, None, :])
            logits = jnp.where(valid, logits, -jnp.inf)
            m = jnp.max(logits, axis=-1, keepdims=True)
            e = jnp.exp(logits - m)
            s = jnp.sum(e, axis=-1, keepdims=True)
            outs.append(jnp.einsum('bhqk,bqkhd->bqhd', e / s, vg))
            lses.append(m + jnp.log(s))
        wts = jax.nn.softmax(jnp.stack(lses), axis=0).transpose(0, 1, 3, 2, 4)
        return jnp.sum(wts * jnp.stack(outs), axis=0)

    return sweep_blocks(block, seq)


def hybrid_layer(x, layer, norm_w, w_in, fox_b_f, lq1, lk1, lq2, lk2, subln_w, w_branch, w_out, rel_bias):
    bsz, seq, _ = x.shape
    h = rms_norm(x, norm_w)
    p = h @ w_in
    (z, gate, dsa_qkv, idx_q, idx_k, idx_w, fox_qkv, fox_f, diff_qkv, dil_qkv) = jnp.split(p, IN_OFFSETS, axis=-1)

    def heads3(t):
        return [a.reshape(bsz, seq, N_HEADS, HEAD_DIM) for a in jnp.split(t, 3, axis=-1)]

    bias_dsa, bias_diff, bias_dil = jnp.split(rel_bias, 3, axis=-1)

    q, k, v = heads3(dsa_qkv)
    o_dsa = dsa_attention(q, k, v, idx_q.reshape(bsz, seq, IDX_HEADS, IDX_DIM), idx_k, idx_w, bias_dsa)

    q, k, v = heads3(fox_qkv)
    o_fox = forgetting_attention(q, k, v, fox_f + fox_b_f)

    q, k, v = heads3(diff_qkv)
    q = q.reshape(bsz, seq, N_HEADS, 2, DIFF_DIM)
    k = k.reshape(bsz, seq, N_HEADS, 2, DIFF_DIM)
    lambda_init = 0.8 - 0.6 * math.exp(-0.3 * layer)
    lam = (jnp.exp(jnp.sum((lq1 * lk1).astype(jnp.float32)))
           - jnp.exp(jnp.sum((lq2 * lk2).astype(jnp.float32))) + lambda_init)
    o_diff = differential_attention(q, k, v, lam, subln_w, bias_diff, lambda_init)

    q, k, v = heads3(dil_qkv)
    o_dil = dilated_attention(q, k, v, bias_dil)

    branches = (o_dsa, o_fox, o_diff, o_dil)
    z_parts = jnp.split(z, N_BRANCHES, axis=-1)
    g_parts = jnp.split(gate, N_BRANCHES, axis=-1)
    merged = None
    for b in range(N_BRANCHES):
        y = branches[b].reshape(bsz, seq, BRANCH_WIDTH).astype(x.dtype) * jax.nn.silu(z_parts[b])
        term = jax.nn.sigmoid(g_parts[b]) * (y @ w_branch[b])
        merged = term if merged is None else merged + term
    return (x + merged @ w_out).astype(x.dtype)


def setup_inputs(seed: int = 0) -> dict:
    key = jax.random.key(seed)
    ks = jax.random.split(key, 13)

    def nrm(k, shape, scale):
        return scale * jax.random.normal(k, shape, jnp.float32)

    return {
        'x': nrm(ks[0], (BATCH, SEQ, D_MODEL), 1.0),
        'norm_w': 1.0 + nrm(ks[1], (DEPTH, D_MODEL), 0.05),
        'w_in': nrm(ks[2], (DEPTH, D_MODEL, N_IN), D_MODEL ** -0.5),
        'fox_b_f': 4.0 + nrm(ks[3], (DEPTH, N_HEADS), 1.0),
        'diff_lq1': nrm(ks[4], (DEPTH, DIFF_DIM), 0.1),
        'diff_lk1': nrm(ks[5], (DEPTH, DIFF_DIM), 0.1),
        'diff_lq2': nrm(ks[6], (DEPTH, DIFF_DIM), 0.1),
        'diff_lk2': nrm(ks[7], (DEPTH, DIFF_DIM), 0.1),
        'diff_subln_w': 1.0 + nrm(ks[8], (DEPTH, HEAD_DIM), 0.05),
        'w_branch': nrm(ks[9], (DEPTH, N_BRANCHES, BRANCH_WIDTH, D_MODEL), BRANCH_WIDTH ** -0.5),
        'w_out': nrm(ks[10], (DEPTH, D_MODEL, D_MODEL), D_MODEL ** -0.5),
        'rel_bias': nrm(ks[11], (N_BUCKETS, N_BIAS_HEADS), 0.5),
        'final_norm_w': 1.0 + nrm(ks[12], (D_MODEL,), 0.05),
    }


def reference(x, norm_w, w_in, fox_b_f, diff_lq1, diff_lk1, diff_lq2, diff_lk2, diff_subln_w,
              w_branch, w_out, rel_bias, final_norm_w):
    for layer in range(DEPTH):
        x = hybrid_layer(x, layer, norm_w[layer], w_in[layer], fox_b_f[layer],
                         diff_lq1[layer], diff_lk1[layer], diff_lq2[layer], diff_lk2[layer],
                         diff_subln_w[layer], w_branch[layer], w_out[layer], rel_bias)
    return rms_norm(x, final_norm_w)
```

```python
import math
from contextlib import ExitStack

import numpy as np
import concourse.bass as bass
import concourse.mybir as mybir
from concourse.bass_utils import run_bass_kernel_spmd

F32 = mybir.dt.float32
BF16 = mybir.dt.bfloat16
ALU = mybir.AluOpType
AF = mybir.ActivationFunctionType
AX = mybir.AxisListType

D = 2048
S = 8192
NT = 2048
DEPTH = 4
N_IN = 17492
NEG = -30000.0
GL = 4608
GOFF = 1920
DFAR = 2176
C_Z, C_G = 0, 2048
C_DSA, C_IQ, C_IK, C_IW, C_FOX, C_FF, C_DIFF, C_DIL = 10240, 11776, 12800, 12864, 12880, 14416, 14420, 15956
S128 = 128 ** -0.5
S64 = 64 ** -0.5
TOPK = 256
NBIS = 24


class Buf:
    __slots__ = ("w", "r")

    def __init__(self):
        self.w = {}
        self.r = {}


class Prog:
    ENG = ("sync", "scalar", "vector", "gpsimd", "tensor")

    def __init__(self, ndma=24):
        self.streams = {e: [] for e in self.ENG}
        self.count = {e: 0 for e in self.ENG}
        self.seen = {e: {} for e in self.ENG}
        self.ndma = ndma
        self.dma_i = 0
        self.dma_cnt = [0] * ndma
        self.cc_cnt = 0
        self.nops = 0

    def _deps(self, eng, reads, writes, acc=False):
        deps = {}

        def add(k, v):
            if deps.get(k, 0) < v:
                deps[k] = v
        for b in reads:
            for k, v in b.w.items():
                add(k, v)
        for b in writes:
            if not acc:
                for k, v in b.w.items():
                    add(k, v)
            for k, v in b.r.items():
                add(k, v)
        waits = []
        for k, v in deps.items():
            if k == eng and eng == "tensor":
                continue
            if self.seen[eng].get(k, 0) < v:
                self.seen[eng][k] = v
                waits.append((k, v))
        return waits

    def _record(self, me, reads, writes, acc=False):
        for b in reads:
            if b.r.get(me[0], 0) < me[1]:
                b.r[me[0]] = me[1]
        for b in writes:
            if acc:
                b.w[me[0]] = me[1]
            else:
                b.w = {me[0]: me[1]}
                b.r = {}
        self.nops += 1

    def op(self, eng, fn, reads=(), writes=(), acc=False):
        waits = self._deps(eng, reads, writes, acc)
        self.count[eng] += 1
        me = (eng, self.count[eng])
        self.streams[eng].append((waits, fn, eng, 1))
        self._record(me, reads, writes, acc)

    def dma(self, eng, fn, reads=(), writes=(), acc=False):
        i = self.dma_i % self.ndma
        self.dma_i += 1
        key = "dma%d" % i
        waits = self._deps(eng, reads, writes, acc)
        prev = self.dma_cnt[i]
        if prev and self.seen[eng].get(key, 0) < prev:
            self.seen[eng][key] = prev
            waits.append((key, prev))
        self.dma_cnt[i] += 16
        me = (key, self.dma_cnt[i])
        self.streams[eng].append((waits, fn, key, 16))
        self._record(me, reads, writes, acc)

    def cc(self, fn, reads=(), writes=()):
        eng = "gpsimd"
        waits = self._deps(eng, reads, writes)
        self.cc_cnt += 1
        me = ("cc", self.cc_cnt)
        self.streams[eng].append((waits, fn, "cc", None))
        self._record(me, reads, writes)

    def finish(self, eng, bufs):
        waits = self._deps(eng, bufs, ())
        self.streams[eng].append((waits, None, None, 0))

    def emit(self, block, sems):
        def mk(e):
            def body(engh):
                for waits, fn, key, inc in self.streams[e]:
                    for k, v in waits:
                        engh.wait_ge(sems[k], v)
                    if fn is not None:
                        ins = fn(engh)
                        if inc is None:
                            ins.then_inc(sems[key])
                        else:
                            ins.then_inc(sems[key], inc)
            return body
        block.sync(mk("sync"))
        block.scalar(mk("scalar"))
        block.vector(mk("vector"))
        block.gpsimd(mk("gpsimd"))
        block.tensor(mk("tensor"))


def t5_bucket_np(dist):
    dist = np.maximum(dist, 0)
    d = np.maximum(dist, 16).astype(np.float32)
    large = 16 + (np.log(d / np.float32(16)) / np.float32(math.log(2048 / 16)) * np.float32(16)).astype(np.int32)
    large = np.minimum(large, 31)
    return np.where(dist < 16, dist, large)


def build(depth=DEPTH):
    nc = bass.Bass("TRN2", target_bir_lowering=False)
    P = Prog()
    L = depth

    def din(name, shape, dt=F32):
        return nc.dram_tensor(name, list(shape), dt, kind="ExternalInput").ap()

    def dscr(name, shape, dt):
        return nc.dram_tensor(name, list(shape), dt)

    xT_in = din("xT", [D, NT])
    w_in_a = din("w_in", [L, D, N_IN])
    w_br_a = din("w_br", [L, D, D])
    w_out_a = din("w_out", [L, D, D])
    normw_in = din("normw", [128, L * 16])
    fnormw_in = din("fnormw", [128, 16])
    foxb_in = din("foxb", [128, L * 4])
    lq_in = din("lq", [128, L * 4 * 64])
    subln_in = din("subln", [128, L])
    gt_in = din("gt", [12, 128, GL])
    mneg_in = din("mneg", [128, GL], BF16)
    cdil_in = din("cdil", [128, GL], BF16)
    b31_in = din("b31", [128, 12])
    mt_in = din("mt", [128, 2432], BF16)
    sel_in = din("sel", [128, 16 * 64])
    ident_in = din("ident", [128, 128], BF16)
    tri_in = din("tri", [128, 128])
    yT_out = nc.dram_tensor("yT", [D, NT], F32, kind="ExternalOutput").ap()

    wf_in = [w_in_a[l] for l in range(L)]
    wf_br = [w_br_a[l] for l in range(L)]
    wf_out = [w_out_a[l] for l in range(L)]
    xs_dd = [dscr("xs_d%d" % i, [D, NT], F32).ap() for i in range(2)]
    hT_d = dscr("hT_d", [128, 16, NT], BF16).ap()
    qs_d = dscr("qs_d", [24, 128, NT], BF16).ap()
    kT_loc = [dscr("kT_loc%d" % q, [128, NT], BF16) for q in range(17)]
    kT_all = [dscr("kT_all%d" % q, [512, NT], BF16) for q in range(17)]
    v_loc = [dscr("v_loc%d" % q, [128, 2048], BF16) for q in range(16)]
    v_all = [dscr("v_all%d" % q, [512, 2048], BF16) for q in range(16)]
    fa_loc = dscr("fa_loc", [128, 64], F32)
    fa_all = dscr("fa_all", [512, 64], F32)
    ao_d = dscr("ao_d", [16, 128, NT], F32).ap()
    A_d = dscr("A_d", [16, 128, S], BF16).ap()

    B = {}

    def buf(name):
        if name not in B:
            B[name] = Buf()
        return B[name]

    groups = [[0, 1, 2, 3], [4, 5, 6, 7]]
    es = ExitStack()
    with es:
        def sb(name, shape, dt):
            return es.enter_context(nc.sbuf_tensor("sb_" + name, list(shape), dt))

        arena = sb("arena", [128, 16, NT], BF16)
        kT_s = sb("kT_s", [128, S], BF16)
        v_s = sb("v_s", [128, 64, 128], BF16)
        wb = [sb("wb%d" % i, [128, 16, 256], BF16) for i in range(2)]
        G_s = sb("G_s", [128, GL], F32)
        Gadd = sb("Gadd", [128, GL], BF16)
        qT_s = sb("qT_s", [128, NT], BF16)
        pT = [sb("pT%d" % i, [128, 512], BF16) for i in range(3)]
        tmpf = [sb("tmpf%d" % i, [128, 512], F32) for i in range(2)]
        rbuf = [sb("rbuf%d" % i, [128, 512], F32) for i in range(2)]
        ot = [sb("ot%d" % i, [128, 512], F32) for i in range(5)]
        stg = [sb("stg%d" % i, [128, 512], BF16) for i in range(4)]
        bm = sb("bm", [128, 16, 64], F32)
        negcum = sb("negcum", [128, 4, 64], F32)
        totf = sb("totf", [128, 4, 64], F32)
        inclf = sb("inclf", [128, 4, 64], F32)
        agf = sb("agf", [128, 4, 64], F32)
        fa4 = sb("fa4", [128, 4, 64], F32)
        cqref = sb("cqref", [128, 4, 16], F32)
        seltmp = sb("seltmp", [128, 16, 64], F32)
        sel_s = sb("sel_s", [128, 16, 64], F32)
        ones1 = sb("ones1", [128, 64], F32)
        ff_s = sb("ff_s", [128, 16, 4], F32)
        fa_s = sb("fa_s", [128, 16, 4], F32)
        wI_s = sb("wI_s", [128, 16, 16], F32)
        qi_s = [sb("qi_s%d" % i, [128, 8, 128], BF16) for i in range(2)]
        ident = sb("ident", [128, 128], BF16)
        onesb = sb("onesb", [128, 128], BF16)
        tri = sb("tri", [128, 128], F32)
        onesf = sb("onesf", [128, 128], F32)
        mt_s = sb("mt_s", [128, 2432], BF16)
        normw = sb("normw", [128, L * 16], F32)
        fnormw = sb("fnormw", [128, 16], F32)
        foxb = sb("foxb", [128, L * 4], F32)
        lq_s = sb("lq_s", [128, L * 4 * 64], F32)
        subln = sb("subln", [128, L], F32)
        b31 = sb("b31", [128, 12], F32)
        sm = sb("sm", [128, 64], F32)
        whalf = sb("whalf", [128, NBIS], F32)
        pw2 = sb("pw2", [128, NBIS], F32)
        epsc = sb("epsc", [128, 1], F32)
        onec = sb("onec", [128, 1], F32)

        ps = [es.enter_context(nc.psum_tensor("ps%d" % i, [128, 512], F32)) for i in range(8)]
        sems = {e: es.enter_context(nc.semaphore("s_" + e)) for e in Prog.ENG}
        for i in range(P.ndma):
            sems["dma%d" % i] = es.enter_context(nc.semaphore("s_dma%d" % i))
        sems["cc"] = es.enter_context(nc.semaphore("s_cc"))
        block = es.enter_context(nc.Block())
        print("sbuf bytes remaining", nc.sbuf_bytes_remaining)

        Bps = [buf("ps%d" % i) for i in range(8)]
        rr = {"stg": 0, "pT": 0, "tmpf": 0, "rbuf": 0, "psL": 0, "psA": 0, "wb": 0, "q": 0, "ev": 0}

        def nxt(k, n):
            v = rr[k] % n
            rr[k] += 1
            return v

        def ld(eng, dst, src, name):
            P.dma(eng, lambda e: e.dma_start(out=dst, in_=src), writes=[buf(name)])
        ld("sync", normw[:], normw_in, "normw")
        ld("sync", fnormw[:], fnormw_in, "fnormw")
        ld("sync", foxb[:], foxb_in, "foxb")
        ld("sync", lq_s[:], lq_in, "lq")
        ld("sync", subln[:], subln_in, "subln")
        ld("sync", b31[:], b31_in, "b31")
        ld("sync", mt_s[:], mt_in, "mt")
        ld("sync", sel_s[:].rearrange("p a b -> p (a b)"), sel_in, "sel")
        ld("sync", ident[:], ident_in, "ident")
        ld("sync", tri[:], tri_in, "tri")
        P.op("vector", lambda e: e.memset(onesb[:], 1.0), writes=[buf("onesb")])
        P.op("vector", lambda e: e.memset(onesf[:], 1.0), writes=[buf("onesf")])
        P.op("vector", lambda e: e.memset(ones1[:], 1.0), writes=[buf("ones1")])
        P.op("vector", lambda e: e.memset(epsc[:], 1e-6), writes=[buf("epsc")])
        P.op("vector", lambda e: e.memset(onec[:], 1.0), writes=[buf("onec")])
        for i in range(NBIS):
            P.op("vector", lambda e, i=i: e.memset(pw2[:, i:i + 1], 2.0 ** -(i + 1)), writes=[buf("pw2")])

        def load_w(wfull, bname, col0, ncols, dup=False):
            i = nxt("wb", 2)
            t = wb[i]
            bw = buf("wb%d" % i)
            src = wfull[:, col0:col0 + ncols].rearrange("(k p) n -> p k n", p=128)
            P.dma("gpsimd", lambda e: e.dma_start(out=t[:, :, 0:ncols], in_=src), reads=[buf(bname)], writes=[bw])
            if dup:
                P.dma("gpsimd", lambda e: e.dma_start(out=t[:, :, ncols:2 * ncols], in_=src), reads=[buf(bname)], writes=[bw])
            return t, bw

        hT = arena
        Bar = [buf("arena%d" % j) for j in range(4)]

        def rstd_from_ssq(ps_ap, dst, n, width, bps, bdst):
            P.op("scalar", lambda e: e.activation(out=dst[:, :width], in_=ps_ap, func=AF.Ln, bias=epsc[:, 0:1], scale=1.0 / n),
                 reads=[bps, buf("epsc")], writes=[bdst])
            P.op("scalar", lambda e: e.activation(out=dst[:, :width], in_=dst[:, :width], func=AF.Exp, scale=-0.5),
                 reads=[bdst], writes=[bdst])

        def evac(dst_ap, src_ap, scale, reads, writes):
            i = nxt("ev", 2)
            if i == 0:
                P.op("scalar", lambda e: e.mul(dst_ap, src_ap, float(scale)), reads=reads, writes=writes)
            else:
                P.op("vector", lambda e: e.tensor_scalar(out=dst_ap, in0=src_ap, scalar1=float(scale), scalar2=None, op0=ALU.mult), reads=reads, writes=writes)

        def do_layer(layer):
            Bwin = "wf_in%d" % layer
            Wl = wf_in[layer]
            xsrc = xT_in if layer == 0 else xs_dd[(layer - 1) % 2]
            xs_d = xs_dd[layer % 2]
            Bx = buf("xin") if layer == 0 else buf("xs_d%d" % ((layer - 1) % 2))
            Bxo = buf("xs_d%d" % (layer % 2))
            for j in range(4):
                xt = arena
                pss = ps[6]
                for k in range(16):
                    o = ot[k % 2]
                    bo = buf("ot%d" % (k % 2))
                    P.dma("sync", lambda e, k=k, j=j, o=o: e.dma_start(out=o[:], in_=xsrc[k * 128:(k + 1) * 128, j * 512:(j + 1) * 512]), reads=[Bx], writes=[bo])
                    s_ = stg[k % 2]
                    bs_ = buf("stg%d" % (k % 2))
                    P.op("scalar", lambda e, o=o, s_=s_: e.activation(out=s_[:], in_=o[:], func=AF.Square), reads=[bo], writes=[bs_])
                    P.op("tensor", lambda e, s_=s_, k=k: e.matmul(pss[:, :], lhsT=onesb[:], rhs=s_[:], start=(k == 0), stop=(k == 15)),
                         reads=[bs_, buf("onesb")], writes=[Bps[6]])
                rstd_from_ssq(pss[:, :], ot[4], D, 512, Bps[6], buf("ot4"))
                for k in range(16):
                    o = ot[k % 2]
                    bo = buf("ot%d" % (k % 2))
                    P.dma("sync", lambda e, k=k, j=j, o=o: e.dma_start(out=o[:], in_=xsrc[k * 128:(k + 1) * 128, j * 512:(j + 1) * 512]), reads=[Bx], writes=[bo])
                    P.op("vector", lambda e, k=k, j=j, o=o: e.scalar_tensor_tensor(out=hT[:, k, j * 512:(j + 1) * 512], in0=o[:], scalar=normw[:, layer * 16 + k:layer * 16 + k + 1],
                                                                                   in1=ot[4][:], op0=ALU.mult, op1=ALU.mult),
                         reads=[bo, buf("ot4"), buf("normw")], writes=Bar, acc=True)
            P.dma("sync", lambda e: e.dma_start(out=hT_d, in_=hT[:]), reads=Bar, writes=[buf("hT_d")])

            def fm_group(col0, ncols, dest_fn, scale, dup=False):
                t, bw = load_w(Wl, Bwin, col0, ncols, dup=dup)
                nc_eff = 2 * ncols if dup else ncols
                for q in range((nc_eff + 127) // 128):
                    m = min(128, nc_eff - q * 128)
                    for j in range(4):
                        pi = 4 + nxt("psA", 2)
                        for k in range(16):
                            P.op("tensor", lambda e, pi=pi, k=k, q=q, j=j, m=m: e.matmul(ps[pi][:m, :], lhsT=t[:, k, q * 128:q * 128 + m], rhs=hT[:, k, j * 512:(j + 1) * 512],
                                                                                        start=(k == 0), stop=(k == 15)),
                                 reads=[bw] + Bar, writes=[Bps[pi]])
                        si = nxt("stg", 4)
                        evac(stg[si][:m, :], ps[pi][:m, :], scale, [Bps[pi]], [buf("stg%d" % si)])
                        dst, bd = dest_fn(q, j, m)
                        P.dma("sync", lambda e, dst=dst, si=si, m=m: e.dma_start(out=dst, in_=stg[si][:m, :]), reads=[buf("stg%d" % si)], writes=[bd], acc=True)

            def qdest(base):
                return lambda q, j, m: (qs_d[base + q, 0:m, j * 512:(j + 1) * 512], buf("qs_d"))

            def kdest(base):
                return lambda q, j, m: (kT_loc[(base + q) * 128:(base + q) * 128 + m, j * 512:(j + 1) * 512], buf("kT_loc"))

            def tm_group(col0, ncols, dest_fn, kind):
                t, bw = load_w(Wl, Bwin, col0, ncols)
                for blk in range(16):
                    pi = 4 + nxt("psA", 2)
                    for k in range(16):
                        P.op("tensor", lambda e, pi=pi, k=k, blk=blk: e.matmul(ps[pi][:, 0:ncols], lhsT=hT[:, k, blk * 128:(blk + 1) * 128], rhs=t[:, k, 0:ncols],
                                                                                start=(k == 0), stop=(k == 15)),
                             reads=[bw] + Bar, writes=[Bps[pi]])
                    dest_fn(blk, pi)

            for (mx, c0, s_q) in ((0, C_DSA, S128), (1, C_FOX, S128), (2, C_DIFF, S64), (3, C_DIL, S128)):
                for hh in range(2):
                    fm_group(c0 + hh * 256, 256, (lambda q, j, m, mx=mx, hh=hh: (qs_d[mx * 4 + hh * 2 + q, 0:m, j * 512:(j + 1) * 512], buf("qs_d"))), s_q)
                for hh in range(2):
                    fm_group(c0 + 512 + hh * 256, 256, (lambda q, j, m, mx=mx, hh=hh: (kT_loc[mx * 4 + hh * 2 + q][0:m, j * 512:(j + 1) * 512], buf("kT_loc"))), 1.0)
                for hh in range(2):
                    def vdest(blk, pi, mx=mx, hh=hh):
                        si = nxt("stg", 4)
                        evac(stg[si][:, 0:256], ps[pi][:, 0:256], 1.0, [Bps[pi]], [buf("stg%d" % si)])
                        P.dma("sync", lambda e, si=si: e.dma_start(out=v_loc[blk][:, mx * 512 + hh * 256:mx * 512 + hh * 256 + 256], in_=stg[si][:, 0:256]),
                              reads=[buf("stg%d" % si)], writes=[buf("v_loc")], acc=True)
                    tm_group(c0 + 1024 + hh * 256, 256, vdest, "v")
            for hh in range(4):
                fm_group(C_IQ + hh * 256, 256, (lambda q, j, m, hh=hh: (qs_d[16 + hh * 2 + q, 0:m, j * 512:(j + 1) * 512], buf("qs_d"))), 1.0)
            fm_group(C_IK, 64, (lambda q, j, m: (kT_loc[16][0:m, j * 512:(j + 1) * 512], buf("kT_loc"))), 1.0, dup=True)

            def wdest(blk, pi):
                P.op("vector", lambda e: e.tensor_copy(out=wI_s[:, blk, :], in_=ps[pi][:, 0:16]), reads=[Bps[pi]], writes=[buf("wI")], acc=True)
            tm_group(C_IW, 16, wdest, "w")

            def fdest(blk, pi):
                P.op("vector", lambda e: e.tensor_scalar(out=ff_s[:, blk, :], in0=ps[pi][:, 0:4], scalar1=1.0, scalar2=None, op0=ALU.mult), reads=[Bps[pi]], writes=[buf("ff")], acc=True)
            tm_group(C_FF, 4, fdest, "f")

            for h in range(4):
                P.op("vector", lambda e, h=h: e.tensor_scalar(out=fa_s[:, :, h], in0=ff_s[:, :, h], scalar1=foxb[:, layer * 4 + h:layer * 4 + h + 1], scalar2=None, op0=ALU.add),
                     reads=[buf("ff"), buf("foxb")], writes=[buf("fa")])
            P.op("scalar", lambda e: e.activation(out=fa_s[:], in_=fa_s[:], func=AF.Exp, scale=-1.0), reads=[buf("fa")], writes=[buf("fa")])
            P.op("scalar", lambda e: e.activation(out=fa_s[:], in_=fa_s[:], func=AF.Ln, bias=onec[:, 0:1], scale=1.0), reads=[buf("fa"), buf("onec")], writes=[buf("fa")])
            P.op("vector", lambda e: e.tensor_scalar(out=fa_s[:], in0=fa_s[:], scalar1=-1.0, scalar2=None, op0=ALU.mult), reads=[buf("fa")], writes=[buf("fa")])
            P.dma("sync", lambda e: e.dma_start(out=fa_loc[:, :], in_=fa_s[:].rearrange("p a b -> p (a b)")), reads=[buf("fa")], writes=[buf("fa_loc")])

            for q in range(17):
                P.cc(lambda e, q=q: e.collective_compute("AllGather", ALU.bypass, replica_groups=groups, ins=[kT_loc[q].ap().opt()], outs=[kT_all[q].ap().opt()]),
                     reads=[buf("kT_loc")], writes=[buf("kT_all%d" % q)])
            for q in range(16):
                P.cc(lambda e, q=q: e.collective_compute("AllGather", ALU.bypass, replica_groups=groups, ins=[v_loc[q].ap().opt()], outs=[v_all[q].ap().opt()]),
                     reads=[buf("v_loc")], writes=[buf("v_all%d" % q)])
            P.cc(lambda e: e.collective_compute("AllGather", ALU.bypass, replica_groups=groups, ins=[fa_loc.ap().opt()], outs=[fa_all.ap().opt()]),
                 reads=[buf("fa_loc")], writes=[buf("fa_all")])

            P.dma("sync", lambda e: e.dma_start(out=fa4[:], in_=fa_all.ap().rearrange("(c p) f -> p c f", p=128)), reads=[buf("fa_all")], writes=[buf("fa4")])
            for c in range(4):
                for h in range(4):
                    src = fa4[:, c, :].rearrange("p (j u h) -> p j u h", j=4, u=4, h=4)[:, :, :, h]
                    dst = agf[:, h, :].rearrange("p (j c u) -> p j c u", j=4, c=4, u=4)[:, :, c, :]
                    P.op("vector", lambda e, src=src, dst=dst: e.tensor_copy(out=dst, in_=src), reads=[buf("fa4")], writes=[buf("agf")], acc=True)
            agf2 = agf[:].rearrange("p a b -> p (a b)")
            P.op("tensor", lambda e: e.matmul(ps[6][:, 0:256], lhsT=tri[:], rhs=agf2, start=True, stop=True), reads=[buf("agf"), buf("tri")], writes=[Bps[6]])
            P.op("tensor", lambda e: e.matmul(ps[7][:, 0:256], lhsT=onesf[:], rhs=agf2, start=True, stop=True), reads=[buf("agf"), buf("onesf")], writes=[Bps[7]])
            P.op("vector", lambda e: e.tensor_copy(out=totf[:].rearrange("p a b -> p (a b)"), in_=ps[7][:, 0:256]), reads=[Bps[7]], writes=[buf("totf")])
            for h in range(4):
                P.op("vector", lambda e, h=h: e.tensor_tensor_scan(out=inclf[:, h, :], data0=ones1[:, :], data1=totf[:, h, :], initial=0.0, op0=ALU.mult, op1=ALU.add),
                     reads=[buf("totf"), buf("ones1")], writes=[buf("inclf")])
            P.op("vector", lambda e: e.tensor_tensor(out=negcum[:], in0=totf[:], in1=inclf[:], op=ALU.subtract), reads=[buf("totf"), buf("inclf")], writes=[buf("negcum")])
            P.op("vector", lambda e: e.tensor_tensor(out=negcum[:].rearrange("p a b -> p (a b)"), in0=negcum[:].rearrange("p a b -> p (a b)"), in1=ps[6][:, 0:256], op=ALU.subtract),
                 reads=[buf("negcum"), Bps[6]], writes=[buf("negcum")])
            for h in range(4):
                for blk in range(16):
                    P.op("vector", lambda e, h=h, blk=blk: e.tensor_tensor(out=seltmp[:, blk, :], in0=sel_s[:, blk, :], in1=totf[:, h, :], op=ALU.mult),
                         reads=[buf("sel"), buf("totf")], writes=[buf("seltmp")], acc=True)
                P.op("vector", lambda e, h=h: e.tensor_reduce(out=cqref[:, h, :], in_=seltmp[:], axis=AX.X, op=ALU.add), reads=[buf("seltmp")], writes=[buf("cqref")])

            lam0 = 0.8 - 0.6 * math.exp(-0.3 * layer)
            lb = layer * 256
            P.op("vector", lambda e: e.tensor_tensor(out=tmpf[0][:, 0:64], in0=lq_s[:, lb:lb + 64], in1=lq_s[:, lb + 64:lb + 128], op=ALU.mult), reads=[buf("lq")], writes=[buf("tmpf0")])
            P.op("vector", lambda e: e.tensor_reduce(out=sm[:, 0:1], in_=tmpf[0][:, 0:64], axis=AX.X, op=ALU.add), reads=[buf("tmpf0")], writes=[buf("sm")])
            P.op("vector", lambda e: e.tensor_tensor(out=tmpf[0][:, 0:64], in0=lq_s[:, lb + 128:lb + 192], in1=lq_s[:, lb + 192:lb + 256], op=ALU.mult), reads=[buf("lq")], writes=[buf("tmpf0")])
            P.op("vector", lambda e: e.tensor_reduce(out=sm[:, 1:2], in_=tmpf[0][:, 0:64], axis=AX.X, op=ALU.add), reads=[buf("tmpf0")], writes=[buf("sm")])
            P.op("scalar", lambda e: e.activation(out=sm[:, 2:4], in_=sm[:, 0:2], func=AF.Exp), reads=[buf("sm")], writes=[buf("sm")])
            P.op("vector", lambda e: e.tensor_tensor(out=sm[:, 4:5], in0=sm[:, 3:4], in1=sm[:, 2:3], op=ALU.subtract), reads=[buf("sm")], writes=[buf("sm")])
            P.op("vector", lambda e: e.tensor_scalar(out=sm[:, 5:6], in0=sm[:, 4:5], scalar1=-lam0, scalar2=None, op0=ALU.add), reads=[buf("sm")], writes=[buf("sm")])
            P.op("vector", lambda e: e.tensor_scalar(out=sm[:, 6:7], in0=subln[:, layer:layer + 1], scalar1=1.0 - lam0, scalar2=None, op0=ALU.mult), reads=[buf("subln")], writes=[buf("sm")])

            def load_kv(mx, hh):
                ch = mx * 4 + hh
                src = kT_all[ch].ap().rearrange("(c r) (j i) -> r j c i", c=4, i=512)
                P.dma("sync", lambda e: e.dma_start(out=kT_s[:].rearrange("p (j c i) -> p j c i", j=4, c=4, i=512), in_=src), reads=[buf("kT_all%d" % ch)], writes=[buf("kT_s")])
                col = mx * 512 + hh * 128
                for c in range(4):
                    for blk in range(16):
                        srcv = v_all[blk][c * 128:(c + 1) * 128, col:col + 128]
                        kb = 16 * (blk // 4) + 4 * c + (blk % 4)
                        dstv = v_s[:, kb, :]
                        P.dma("gpsimd" if (blk % 2) else "sync", lambda e, srcv=srcv, dstv=dstv: e.dma_start(out=dstv, in_=srcv), reads=[buf("v_all%d" % blk)], writes=[buf("v_s")], acc=True)
                P.dma("sync", lambda e: e.dma_start(out=qT_s[:], in_=qs_d[ch]), reads=[buf("qs_d")], writes=[buf("qT_s")])

            def load_G(kind, gi):
                if kind == "mask":
                    P.dma("sync", lambda e: e.dma_start(out=Gadd[:], in_=mneg_in), writes=[buf("Gadd")])
                    P.op("vector", lambda e: e.tensor_copy(out=G_s[:], in_=Gadd[:]), reads=[buf("Gadd")], writes=[buf("G_s")])
                    return
                P.dma("sync", lambda e: e.dma_start(out=G_s[:], in_=gt_in[gi]), writes=[buf("G_s")])
                P.dma("sync", lambda e: e.dma_start(out=Gadd[:], in_=(mneg_in if kind == "bias" else cdil_in)), writes=[buf("Gadd")])
                P.op("vector", lambda e: e.tensor_tensor(out=G_s[:], in0=G_s[:], in1=Gadd[:], op=ALU.add), reads=[buf("Gadd"), buf("G_s")], writes=[buf("G_s")])

            def flash(j, krows, kb_lo, mode, h, gi, dsa_A=None):
                kbs = list(range(kb_lo, 16 * j + 16))
                for n, kb in enumerate(kbs):
                    ds_ = 2048 * j - 128 * kb
                    li = nxt("psL", 2)
                    Ls = ps[li]
                    P.op("tensor", lambda e, Ls=Ls, kb=kb: e.matmul(Ls[:, :], lhsT=kT_s[krows[0]:krows[1], kb * 128:(kb + 1) * 128], rhs=qT_s[krows[0]:krows[1], j * 512:(j + 1) * 512],
                                                                      start=True, stop=(dsa_A is None)),
                         reads=[buf("kT_s"), buf("qT_s")], writes=[Bps[li]])
                    if dsa_A is not None:
                        At, bA, off = dsa_A(kb)
                        for u in range(4):
                            P.op("tensor", lambda e, Ls=Ls, u=u, At=At, off=off: e.matmul(Ls[:, u * 128:(u + 1) * 128], lhsT=At[:, u, off:off + 128], rhs=ident[:], start=False, stop=True,
                                                                                          skip_group_check=True),
                                 reads=[bA, buf("ident")], writes=[Bps[li]])
                    src, bsrc = Ls, Bps[li]
                    need_add = (ds_ < 128) if mode == "fox" else (ds_ < DFAR)
                    if need_add:
                        ti = nxt("tmpf", 2)
                        u0 = min(ds_, DFAR) + GOFF
                        P.op("vector", lambda e, ti=ti, Ls=Ls, u0=u0: e.tensor_tensor(out=tmpf[ti][:], in0=Ls[:, :], in1=G_s[:, u0:u0 + 512], op=ALU.add),
                             reads=[Bps[li], buf("G_s")], writes=[buf("tmpf%d" % ti)])
                        src, bsrc = tmpf[ti], buf("tmpf%d" % ti)
                    pi = nxt("pT", 3)
                    bp = buf("pT%d" % pi)
                    if mode == "fox":
                        for u in range(4):
                            P.op("scalar", lambda e, pi=pi, src=src, u=u, kb=kb: e.activation(out=pT[pi][:, u * 128:(u + 1) * 128], in_=src[:, u * 128:(u + 1) * 128], func=AF.Exp,
                                                                                                bias=bm[:, 4 * j + u, kb:kb + 1], scale=1.0),
                                 reads=[bsrc, buf("bm")], writes=[bp])
                    elif need_add:
                        P.op("scalar", lambda e, pi=pi, src=src: e.activation(out=pT[pi][:], in_=src[:, :], func=AF.Exp), reads=[bsrc], writes=[bp])
                    else:
                        P.op("scalar", lambda e, pi=pi, src=src: e.activation(out=pT[pi][:], in_=src[:, :], func=AF.Exp, bias=b31[:, gi:gi + 1], scale=1.0),
                             reads=[bsrc, buf("b31")], writes=[bp])
                    first, last = (n == 0), (n == len(kbs) - 1)
                    P.op("tensor", lambda e, pi=pi, kb=kb, first=first, last=last: e.matmul(ps[2][:, :], lhsT=v_s[:, kb, :], rhs=pT[pi][:], start=first, stop=last),
                         reads=[buf("v_s"), bp], writes=[Bps[2]])
                    P.op("tensor", lambda e, pi=pi, first=first, last=last: e.matmul(ps[3][:, :], lhsT=onesb[:], rhs=pT[pi][:], start=first, stop=last),
                         reads=[buf("onesb"), bp], writes=[Bps[3]])

            def normalize(dst, bdst):
                P.op("vector", lambda e: e.reciprocal(out=ot[4][:], in_=ps[3][:, :]), reads=[Bps[3]], writes=[buf("ot4")])
                P.op("vector", lambda e: e.tensor_tensor(out=dst[:], in0=ps[2][:, :], in1=ot[4][:], op=ALU.mult), reads=[Bps[2], buf("ot4")], writes=[bdst])

            def store_o(src, bsrc, ch, j):
                P.dma("sync", lambda e: e.dma_start(out=ao_d[ch, :, j * 512:(j + 1) * 512], in_=src[:]), reads=[bsrc], writes=[buf("ao_d")], acc=True)

            load_G("mask", 0)
            for h in range(4):
                load_kv(1, h)
                for blk in range(16):
                    P.op("vector", lambda e, h=h, blk=blk: e.tensor_scalar(out=bm[:, blk, :], in0=negcum[:, h, :], scalar1=cqref[:, h, blk:blk + 1], scalar2=None, op0=ALU.add),
                         reads=[buf("negcum"), buf("cqref")], writes=[buf("bm")], acc=True)
                for j in range(4):
                    flash(j, (0, 128), 0, "fox", h, 0)
                    normalize(ot[0], buf("ot0"))
                    store_o(ot[0], buf("ot0"), 4 + h, j)
            for h in range(4):
                load_G("dil", 8 + h)
                load_kv(3, h)
                for j in range(4):
                    kb_lo = max(0, 16 * j - 16)
                    flash(j, (0, 128), kb_lo, "dil", h, 8 + h)
                    normalize(ot[0], buf("ot0"))
                    store_o(ot[0], buf("ot0"), 12 + h, j)
            for h in range(4):
                load_G("bias", 4 + h)
                load_kv(2, h)
                for j in range(4):
                    flash(j, (0, 64), 0, "bias", h, 4 + h)
                    normalize(ot[0], buf("ot0"))
                    flash(j, (64, 128), 0, "bias", h, 4 + h)
                    normalize(ot[1], buf("ot1"))
                    P.op("vector", lambda e: e.scalar_tensor_tensor(out=ot[2][:], in0=ot[1][:], scalar=sm[:, 5:6], in1=ot[0][:], op0=ALU.mult, op1=ALU.add),
                         reads=[buf("ot0"), buf("ot1"), buf("sm")], writes=[buf("ot2")])
                    si = nxt("stg", 4)
                    P.op("scalar", lambda e, si=si: e.activation(out=stg[si][:], in_=ot[2][:], func=AF.Square), reads=[buf("ot2")], writes=[buf("stg%d" % si)])
                    P.op("tensor", lambda e, si=si: e.matmul(ps[6][:, :], lhsT=onesb[:], rhs=stg[si][:], start=True, stop=True), reads=[buf("stg%d" % si), buf("onesb")], writes=[Bps[6]])
                    rstd_from_ssq(ps[6][:, :], ot[3], 128, 512, Bps[6], buf("ot3"))
                    P.op("vector", lambda e: e.scalar_tensor_tensor(out=ot[0][:], in0=ot[2][:], scalar=sm[:, 6:7], in1=ot[3][:], op0=ALU.mult, op1=ALU.mult),
                         reads=[buf("ot2"), buf("ot3"), buf("sm")], writes=[buf("ot0")])
                    store_o(ot[0], buf("ot0"), 8 + h, j)

            I_s = arena[:].rearrange("p a b -> p (a b)")[:, 0:16384].bitcast(F32)
            At_s = arena[:].rearrange("p a b -> p (a b)")[:, 16384:24576]
            ki_s = arena[:].rearrange("p a b -> p (a b)")[:, 24576:32768]
            BI = [Bar[0], Bar[1]]
            srck = kT_all[16].ap().rearrange("(c r) (j i) -> r j c i", c=4, i=512)
            P.dma("sync", lambda e: e.dma_start(out=ki_s.rearrange("p (j c i) -> p j c i", j=4, c=4, i=512), in_=srck), reads=[buf("kT_all16")], writes=[Bar[3]])
            for blk in range(16):
                j, u = blk // 4, blk % 4
                nkc = 4 * j + 4
                nk = nkc * 512
                qi = qi_s[blk % 2]
                bq = buf("qi%d" % (blk % 2))
                P.dma("sync", lambda e, qi=qi, blk=blk: e.dma_start(out=qi[:], in_=qs_d[16:24, :, blk * 128:(blk + 1) * 128].rearrange("m p t -> p m t")), reads=[buf("qs_d")], writes=[bq])
                for kc in range(nkc):
                    for ih in range(16):
                        pi = 6 + nxt("psA", 2)
                        r0 = (ih % 2) * 64
                        P.op("tensor", lambda e, pi=pi, ih=ih, r0=r0, kc=kc, qi=qi: e.matmul(ps[pi][:, :], lhsT=qi[r0:r0 + 64, ih // 2, :], rhs=ki_s[r0:r0 + 64, kc * 512:(kc + 1) * 512],
                                                                                              start=True, stop=True),
                             reads=[bq, Bar[3]], writes=[Bps[pi]])
                        ri = nxt("rbuf", 2)
                        P.op("scalar", lambda e, pi=pi, ri=ri: e.activation(out=rbuf[ri][:], in_=ps[pi][:, :], func=AF.Relu), reads=[Bps[pi]], writes=[buf("rbuf%d" % ri)])
                        if ih == 0:
                            P.op("vector", lambda e, ri=ri, kc=kc, blk=blk: e.tensor_scalar(out=I_s[:, kc * 512:(kc + 1) * 512], in0=rbuf[ri][:], scalar1=wI_s[:, blk, 0:1], scalar2=None, op0=ALU.mult),
                                 reads=[buf("rbuf%d" % ri), buf("wI")], writes=BI)
                        else:
                            P.op("vector", lambda e, ri=ri, kc=kc, blk=blk, ih=ih: e.scalar_tensor_tensor(out=I_s[:, kc * 512:(kc + 1) * 512], in0=rbuf[ri][:], scalar=wI_s[:, blk, ih:ih + 1],
                                                                                                          in1=I_s[:, kc * 512:(kc + 1) * 512], op0=ALU.mult, op1=ALU.add),
                                 reads=[buf("rbuf%d" % ri), buf("wI")] + BI, writes=BI)
                P.op("vector", lambda e, nk=nk: e.tensor_reduce(out=sm[:, 8:9], in_=I_s[:, 0:nk], axis=AX.X, op=ALU.min), reads=BI, writes=[buf("sm")])
                for kc in range(4 * j, nkc):
                    v0 = 512 * kc - 2048 * j - 128 * u + 384
                    P.op("vector", lambda e, kc=kc, v0=v0: e.tensor_tensor(out=I_s[:, kc * 512:(kc + 1) * 512], in0=I_s[:, kc * 512:(kc + 1) * 512], in1=mt_s[:, v0:v0 + 512], op=ALU.add),
                         reads=BI + [buf("mt")], writes=BI)
                P.op("vector", lambda e, nk=nk: e.tensor_reduce(out=sm[:, 9:10], in_=I_s[:, 0:nk], axis=AX.X, op=ALU.max), reads=BI, writes=[buf("sm")])
                P.op("vector", lambda e: e.tensor_scalar(out=sm[:, 10:11], in0=sm[:, 8:9], scalar1=-1.0, scalar2=None, op0=ALU.add), reads=[buf("sm")], writes=[buf("sm")])
                P.op("vector", lambda e: e.tensor_tensor(out=sm[:, 11:12], in0=sm[:, 9:10], in1=sm[:, 8:9], op=ALU.subtract), reads=[buf("sm")], writes=[buf("sm")])
                P.op("vector", lambda e: e.tensor_scalar(out=sm[:, 11:12], in0=sm[:, 11:12], scalar1=2.0, scalar2=None, op0=ALU.add), reads=[buf("sm")], writes=[buf("sm")])
                P.op("vector", lambda e: e.tensor_scalar(out=whalf[:], in0=pw2[:], scalar1=sm[:, 11:12], scalar2=None, op0=ALU.mult), reads=[buf("sm"), buf("pw2")], writes=[buf("whalf")])
                for it in range(NBIS):
                    P.op("vector", lambda e, it=it: e.tensor_tensor(out=sm[:, 12:13], in0=sm[:, 10:11], in1=whalf[:, it:it + 1], op=ALU.add), reads=[buf("sm"), buf("whalf")], writes=[buf("sm")])
                    P.op("vector", lambda e, nk=nk: e.tensor_scalar(out=At_s[:, 0:nk], in0=I_s[:, 0:nk], scalar1=sm[:, 12:13], scalar2=None, op0=ALU.is_ge, op1=ALU.add, accum_out=sm[:, 13:14]),
                         reads=BI + [buf("sm")], writes=[Bar[2], buf("sm")])
                    P.op("vector", lambda e, it=it: e.tensor_scalar(out=sm[:, 14:15], in0=sm[:, 13:14], scalar1=TOPK - 0.5, scalar2=whalf[:, it:it + 1], op0=ALU.is_ge, op1=ALU.mult),
                         reads=[buf("sm"), buf("whalf")], writes=[buf("sm")])
                    P.op("vector", lambda e: e.tensor_tensor(out=sm[:, 10:11], in0=sm[:, 10:11], in1=sm[:, 14:15], op=ALU.add), reads=[buf("sm")], writes=[buf("sm")])
                P.op("vector", lambda e, nk=nk: e.tensor_scalar(out=At_s[:, 0:nk], in0=I_s[:, 0:nk], scalar1=sm[:, 10:11], scalar2=NEG, op0=ALU.is_lt, op1=ALU.mult),
                     reads=BI + [buf("sm")], writes=[Bar[2]])
                P.dma("sync", lambda e, blk=blk, nk=nk: e.dma_start(out=A_d[blk, :, 0:nk], in_=At_s[:, 0:nk]), reads=[Bar[2]], writes=[buf("A_d")], acc=True)
            Apc = [arena[:].rearrange("p a b -> p (a b)")[:, i * 8192:(i + 1) * 8192].rearrange("p (u s) -> p u s", u=4) for i in range(2)]
            for h in range(4):
                load_G("bias", h)
                load_kv(0, h)
                for j in range(4):
                    state = {}

                    def dsa_A(kb, j=j, state=state):
                        pc = kb // 16
                        if state.get("pc") != pc:
                            ai = nxt("q", 2)
                            state["pc"], state["ai"] = pc, ai
                            for u in range(4):
                                P.dma("sync", lambda e, ai=ai, u=u, pc=pc: e.dma_start(out=Apc[ai][:, u, :], in_=A_d[4 * j + u, :, pc * 2048:(pc + 1) * 2048]), reads=[buf("A_d")], writes=[Bar[ai]], acc=True)
                        ai = state["ai"]
                        return Apc[ai], Bar[ai], (kb % 16) * 128
                    flash(j, (0, 128), 0, "bias", h, h, dsa_A=dsa_A)
                    normalize(ot[0], buf("ot0"))
                    store_o(ot[0], buf("ot0"), h, j)

            Wbr, Wout = wf_br[layer], wf_out[layer]
            Bbr, Bout = "wf_br%d" % layer, "wf_out%d" % layer
            hTt = arena[:, :, 0:512]
            def reg(r):
                return arena[:, 4 * r:4 * r + 4, :].rearrange("p a (b t) -> p (a b) t", t=512)
            hTt, yTt, mTt = reg(0), reg(1), reg(2)
            for j in range(4):
                P.dma("sync", lambda e, j=j: e.dma_start(out=hTt, in_=hT_d[:, :, j * 512:(j + 1) * 512]), reads=[buf("hT_d")], writes=[Bar[0]])
                for zg in range(8):
                    t, bw = load_w(Wl, Bwin, C_Z + zg * 256, 256)
                    for q in range(2):
                        ch = zg * 2 + q
                        pi = 4 + nxt("psA", 2)
                        for k in range(16):
                            P.op("tensor", lambda e, pi=pi, k=k, q=q, t=t: e.matmul(ps[pi][:, :], lhsT=t[:, k, q * 128:(q + 1) * 128], rhs=hTt[:, k, :], start=(k == 0), stop=(k == 15)),
                                 reads=[bw, Bar[0]], writes=[Bps[pi]])
                        ti = nxt("tmpf", 2)
                        P.op("scalar", lambda e, pi=pi, ti=ti: e.activation(out=tmpf[ti][:], in_=ps[pi][:, :], func=AF.Silu), reads=[Bps[pi]], writes=[buf("tmpf%d" % ti)])
                        oi = nxt("rbuf", 2)
                        P.dma("sync", lambda e, oi=oi, ch=ch, j=j: e.dma_start(out=rbuf[oi][:], in_=ao_d[ch, :, j * 512:(j + 1) * 512]), reads=[buf("ao_d")], writes=[buf("rbuf%d" % oi)])
                        P.op("vector", lambda e, ti=ti, oi=oi, ch=ch: e.tensor_tensor(out=yTt[:, ch, :], in0=tmpf[ti][:], in1=rbuf[oi][:], op=ALU.mult),
                             reads=[buf("tmpf%d" % ti), buf("rbuf%d" % oi)], writes=[Bar[1]], acc=True)
                for ng in range(8):
                    wts = []
                    for b in range(4):
                        pass
                    for b in range(4):
                        tg, bwg = load_w(Wl, Bwin, C_G + b * 2048 + ng * 256, 256)
                        i2 = nxt("wb", 2)
                        tb_, bwb = wb[i2], buf("wb%d" % i2)
                        srcb = Wbr[b * 512:(b + 1) * 512, ng * 256:(ng + 1) * 256].rearrange("(k p) n -> p k n", p=128)
                        P.dma("gpsimd", lambda e, tb_=tb_, srcb=srcb: e.dma_start(out=tb_[:, 0:4, :], in_=srcb), reads=[buf(Bbr)], writes=[bwb])
                        for q in range(2):
                            n = ng * 2 + q
                            pg = 4 + nxt("psA", 2)
                            for k in range(16):
                                P.op("tensor", lambda e, pg=pg, k=k, q=q, tg=tg: e.matmul(ps[pg][:, :], lhsT=tg[:, k, q * 128:(q + 1) * 128], rhs=hTt[:, k, :], start=(k == 0), stop=(k == 15)),
                                     reads=[bwg, Bar[0]], writes=[Bps[pg]])
                            pu = 6 + nxt("psL", 2)
                            for k in range(4):
                                P.op("tensor", lambda e, pu=pu, k=k, q=q, tb_=tb_, b=b: e.matmul(ps[pu][:, :], lhsT=tb_[:, k, q * 128:(q + 1) * 128], rhs=yTt[:, b * 4 + k, :], start=(k == 0), stop=(k == 3)),
                                     reads=[bwb, Bar[1]], writes=[Bps[pu]])
                            ti = nxt("tmpf", 2)
                            P.op("scalar", lambda e, pg=pg, ti=ti: e.activation(out=tmpf[ti][:], in_=ps[pg][:, :], func=AF.Sigmoid), reads=[Bps[pg]], writes=[buf("tmpf%d" % ti)])
                            acc = ot[q]
                            bacc = buf("ot%d" % q)
                            if b == 0:
                                P.op("vector", lambda e, ti=ti, pu=pu, acc=acc: e.tensor_tensor(out=acc[:], in0=ps[pu][:, :], in1=tmpf[ti][:], op=ALU.mult),
                                     reads=[Bps[pu], buf("tmpf%d" % ti)], writes=[bacc])
                            else:
                                P.op("vector", lambda e, ti=ti, pu=pu: e.tensor_tensor(out=tmpf[ti][:], in0=ps[pu][:, :], in1=tmpf[ti][:], op=ALU.mult),
                                     reads=[Bps[pu], buf("tmpf%d" % ti)], writes=[buf("tmpf%d" % ti)])
                                P.op("gpsimd", lambda e, ti=ti, acc=acc: e.tensor_tensor(out=acc[:], in0=acc[:], in1=tmpf[ti][:], op=ALU.add),
                                     reads=[buf("tmpf%d" % ti), bacc], writes=[bacc])
                            if b == 3:
                                P.op("scalar", lambda e, n=n, acc=acc: e.activation(out=mTt[:, n, :], in_=acc[:], func=AF.Copy), reads=[bacc], writes=[Bar[2]], acc=True)
                for ng in range(8):
                    to, bwo = load_w(Wout, Bout, ng * 256, 256)
                    for q in range(2):
                        n = ng * 2 + q
                        po = 4 + nxt("psA", 2)
                        for k in range(16):
                            P.op("tensor", lambda e, po=po, k=k, q=q, to=to: e.matmul(ps[po][:, :], lhsT=to[:, k, q * 128:(q + 1) * 128], rhs=mTt[:, k, :], start=(k == 0), stop=(k == 15)),
                                 reads=[bwo, Bar[2]], writes=[Bps[po]])
                        oi = nxt("rbuf", 2)
                        P.dma("sync", lambda e, oi=oi, n=n, j=j: e.dma_start(out=rbuf[oi][:], in_=xsrc[n * 128:(n + 1) * 128, j * 512:(j + 1) * 512]), reads=[Bx], writes=[buf("rbuf%d" % oi)])
                        P.op("vector", lambda e, oi=oi, po=po: e.tensor_tensor(out=rbuf[oi][:], in0=ps[po][:, :], in1=rbuf[oi][:], op=ALU.add),
                             reads=[Bps[po], buf("rbuf%d" % oi)], writes=[buf("rbuf%d" % oi)])
                        P.dma("sync", lambda e, oi=oi, n=n, j=j: e.dma_start(out=xs_d[n * 128:(n + 1) * 128, j * 512:(j + 1) * 512], in_=rbuf[oi][:]), reads=[buf("rbuf%d" % oi)], writes=[Bxo], acc=True)

        for layer_ in range(L):
            do_layer(layer_)

        xs_d = xs_dd[(L - 1) % 2]
        Bx = buf("xs_d%d" % ((L - 1) % 2))
        Bout_ = buf("yT")
        for j in range(4):
            for k in range(16):
                o = ot[k % 2]
                bo = buf("ot%d" % (k % 2))
                P.dma("sync", lambda e, k=k, j=j, o=o: e.dma_start(out=o[:], in_=xs_d[k * 128:(k + 1) * 128, j * 512:(j + 1) * 512]), reads=[Bx], writes=[bo])
                s_ = stg[k % 2]
                bs_ = buf("stg%d" % (k % 2))
                P.op("scalar", lambda e, o=o, s_=s_: e.activation(out=s_[:], in_=o[:], func=AF.Square), reads=[bo], writes=[bs_])
                P.op("tensor", lambda e, s_=s_, k=k: e.matmul(ps[6][:, :], lhsT=onesb[:], rhs=s_[:], start=(k == 0), stop=(k == 15)), reads=[bs_, buf("onesb")], writes=[Bps[6]])
            rstd_from_ssq(ps[6][:, :], ot[4], D, 512, Bps[6], buf("ot4"))
            for k in range(16):
                o = ot[k % 2]
                bo = buf("ot%d" % (k % 2))
                P.dma("sync", lambda e, k=k, j=j, o=o: e.dma_start(out=o[:], in_=xs_d[k * 128:(k + 1) * 128, j * 512:(j + 1) * 512]), reads=[Bx], writes=[bo])
                ri = nxt("rbuf", 2)
                P.op("vector", lambda e, k=k, o=o, ri=ri: e.scalar_tensor_tensor(out=rbuf[ri][:], in0=o[:], scalar=fnormw[:, k:k + 1], in1=ot[4][:], op0=ALU.mult, op1=ALU.mult),
                     reads=[bo, buf("ot4"), buf("fnormw")], writes=[buf("rbuf%d" % ri)])
                P.dma("sync", lambda e, k=k, j=j, ri=ri: e.dma_start(out=yT_out[k * 128:(k + 1) * 128, j * 512:(j + 1) * 512], in_=rbuf[ri][:]), reads=[buf("rbuf%d" % ri)], writes=[Bout_], acc=True)
        P.finish("sync", [Bout_])
        P.emit(block, sems)
    print("ops", P.nops)
    return nc


def host_prep(inputs, depth=DEPTH):
    x = np.asarray(inputs["x"], np.float32)
    L = depth
    rel = np.asarray(inputs["rel_bias"], np.float32)
    maps = []
    sl = np.arange(128)[:, None]
    uu = np.arange(GL)[None, :]
    import ml_dtypes
    bf = ml_dtypes.bfloat16
    ident = np.eye(128, dtype=np.float32).astype(bf)
    tri = (np.arange(128)[:, None] <= np.arange(128)[None, :]).astype(np.float32)
    normw = np.ascontiguousarray(np.asarray(inputs["norm_w"], np.float32)[:L].reshape(L, 16, 128).transpose(2, 0, 1).reshape(128, L * 16))
    fnormw = np.ascontiguousarray(np.asarray(inputs["final_norm_w"], np.float32).reshape(16, 128).T)
    foxb = np.ascontiguousarray(np.broadcast_to(np.asarray(inputs["fox_b_f"], np.float32)[:L].reshape(1, L * 4), (128, L * 4)))
    lq = np.stack([np.asarray(inputs[k], np.float32)[:L] for k in ("diff_lq1", "diff_lk1", "diff_lq2", "diff_lk2")], axis=1)
    lq = np.ascontiguousarray(np.broadcast_to(lq.reshape(1, L * 256), (128, L * 256)))
    subln = np.ascontiguousarray(np.asarray(inputs["diff_subln_w"], np.float32)[:L].T)
    b31 = np.ascontiguousarray(np.broadcast_to(rel[31:32, :], (128, 12)))
    w_in = np.asarray(inputs["w_in"], np.float32)
    w_br = np.asarray(inputs["w_branch"], np.float32).reshape(DEPTH, 2048, 2048)
    w_out = np.asarray(inputs["w_out"], np.float32)
    for core in range(8):
        b, c = core // 4, core % 4
        toks = np.concatenate([np.arange(512 * (4 * j + c), 512 * (4 * j + c + 1)) for j in range(4)])
        xT = np.ascontiguousarray(x[b, toks, :].T)
        dist = uu - GOFF + 512 * c - sl
        bidx = t5_bucket_np(dist)
        gt = np.ascontiguousarray(rel[bidx, :].transpose(2, 0, 1))
        mneg = np.where(dist >= 0, 0.0, NEG).astype(np.float32).astype(bf)
        nval = ((dist >= 0) & (dist <= 128)).astype(np.int32) + ((dist >= 0) & (dist % 4 == 0) & (dist <= 512)).astype(np.int32) \
            + ((dist >= 0) & (dist % 16 == 0) & (dist <= 2048)).astype(np.int32)
        cdil = np.where(nval > 0, np.log(np.maximum(nval, 1).astype(np.float32)), NEG).astype(np.float32).astype(bf)
        vv = np.arange(2432)[None, :]
        mt = np.where((vv - 384 - 512 * c - sl) > 0, -1e9, 0.0).astype(np.float32).astype(bf)
        blk = np.arange(16)
        kb_own = 16 * (blk // 4) + 4 * c + (blk % 4)
        sel = (np.arange(64)[None, :] <= kb_own[:, None]).astype(np.float32)
        sel = np.ascontiguousarray(np.broadcast_to(sel.reshape(1, 1024), (128, 1024)))
        maps.append({
            "xT": xT,
            "w_in": w_in[:L], "w_br": w_br[:L], "w_out": w_out[:L],
            "normw": normw, "fnormw": fnormw, "foxb": foxb, "lq": lq, "subln": subln,
            "gt": gt, "mneg": mneg, "cdil": cdil, "b31": b31, "mt": mt, "sel": sel, "ident": ident, "tri": tri,
        })
    return maps


def run(inputs, depth=DEPTH):
    nc = build(depth)
    maps = host_prep(inputs, depth)
    res = run_bass_kernel_spmd(nc, maps, core_ids=list(range(8)))
    out = np.zeros((2, S, D), np.float32)
    for core in range(8):
        b, c = core // 4, core % 4
        toks = np.concatenate([np.arange(512 * (4 * j + c), 512 * (4 * j + c + 1)) for j in range(4)])
        out[b, toks, :] = res.results[core]["yT"].T
    return out


def kernel(**inputs):
    return run(inputs, DEPTH)
```

```python
import math
from contextlib import ExitStack

import numpy as np
import concourse.bass as bass
import concourse.mybir as mybir
from concourse.bass_utils import run_bass_kernel_spmd

F32 = mybir.dt.float32
BF16 = mybir.dt.bfloat16
ALU = mybir.AluOpType
AF = mybir.ActivationFunctionType
AX = mybir.AxisListType

D = 2048
S = 8192
NT = 2048
DEPTH = 4
N_IN = 17492
NEG = -30000.0
GL = 4608
GOFF = 1920
DFAR = 2176
C_Z, C_G = 0, 2048
C_DSA, C_IQ, C_IK, C_IW, C_FOX, C_FF, C_DIFF, C_DIL = 10240, 11776, 12800, 12864, 12880, 14416, 14420, 15956
S128 = 128 ** -0.5
S64 = 64 ** -0.5
TOPK = 256
NBIS = 18


class Buf:
    __slots__ = ("w", "r")

    def __init__(self):
        self.w = {}
        self.r = {}


class Prog:
    ENG = ("sync", "scalar", "vector", "gpsimd", "tensor")

    def __init__(self, ndma=24):
        self.streams = {e: [] for e in self.ENG}
        self.count = {e: 0 for e in self.ENG}
        self.seen = {e: {} for e in self.ENG}
        self.ndma = ndma
        self.dma_i = 0
        self.dma_cnt = [0] * ndma
        self.cc_cnt = 0
        self.nops = 0

    def _deps(self, eng, reads, writes, acc=False):
        deps = {}

        def add(k, v):
            if deps.get(k, 0) < v:
                deps[k] = v
        for b in reads:
            for k, v in b.w.items():
                add(k, v)
        for b in writes:
            if not acc:
                for k, v in b.w.items():
                    add(k, v)
            for k, v in b.r.items():
                add(k, v)
        waits = []
        for k, v in deps.items():
            if k == eng and eng == "tensor":
                continue
            if self.seen[eng].get(k, 0) < v:
                self.seen[eng][k] = v
                waits.append((k, v))
        return waits

    def _record(self, me, reads, writes, acc=False):
        for b in reads:
            if b.r.get(me[0], 0) < me[1]:
                b.r[me[0]] = me[1]
        for b in writes:
            if acc:
                b.w[me[0]] = me[1]
            else:
                b.w = {me[0]: me[1]}
                b.r = {}
        self.nops += 1

    def op(self, eng, fn, reads=(), writes=(), acc=False):
        waits = self._deps(eng, reads, writes, acc)
        self.count[eng] += 1
        me = (eng, self.count[eng])
        self.streams[eng].append((waits, fn, eng, 1))
        self._record(me, reads, writes, acc)

    def dma(self, eng, fn, reads=(), writes=(), acc=False):
        i = self.dma_i % self.ndma
        self.dma_i += 1
        key = "dma%d" % i
        waits = self._deps(eng, reads, writes, acc)
        prev = self.dma_cnt[i]
        if prev and self.seen[eng].get(key, 0) < prev:
            self.seen[eng][key] = prev
            waits.append((key, prev))
        self.dma_cnt[i] += 16
        me = (key, self.dma_cnt[i])
        self.streams[eng].append((waits, fn, key, 16))
        self._record(me, reads, writes, acc)

    def cc(self, fn, reads=(), writes=()):
        eng = "gpsimd"
        waits = self._deps(eng, reads, writes)
        self.cc_cnt += 1
        me = ("cc", self.cc_cnt)
        self.streams[eng].append((waits, fn, "cc", None))
        self._record(me, reads, writes)

    def finish(self, eng, bufs):
        waits = self._deps(eng, bufs, ())
        self.streams[eng].append((waits, None, None, 0))

    def emit(self, block, sems):
        def mk(e):
            def body(engh):
                for waits, fn, key, inc in self.streams[e]:
                    for k, v in waits:
                        engh.wait_ge(sems[k], v)
                    if fn is not None:
                        ins = fn(engh)
                        if inc is None:
                            ins.then_inc(sems[key])
                        else:
                            ins.then_inc(sems[key], inc)
            return body
        block.sync(mk("sync"))
        block.scalar(mk("scalar"))
        block.vector(mk("vector"))
        block.gpsimd(mk("gpsimd"))
        block.tensor(mk("tensor"))


def t5_bucket_np(dist):
    dist = np.maximum(dist, 0)
    d = np.maximum(dist, 16).astype(np.float32)
    large = 16 + (np.log(d / np.float32(16)) / np.float32(math.log(2048 / 16)) * np.float32(16)).astype(np.int32)
    large = np.minimum(large, 31)
    return np.where(dist < 16, dist, large)


def build(depth=DEPTH):
    nc = bass.Bass("TRN2", target_bir_lowering=False)
    P = Prog()
    L = depth

    def din(name, shape, dt=F32):
        return nc.dram_tensor(name, list(shape), dt, kind="ExternalInput").ap()

    def dscr(name, shape, dt):
        return nc.dram_tensor(name, list(shape), dt)

    xT_in = din("xT", [D, NT])
    w_in_a = din("w_in", [L, D, N_IN])
    w_br_a = din("w_br", [L, D, D])
    w_out_a = din("w_out", [L, D, D])
    normw_in = din("normw", [128, L * 16])
    fnormw_in = din("fnormw", [128, 16])
    foxb_in = din("foxb", [128, L * 4])
    lq_in = din("lq", [128, L * 4 * 64])
    subln_in = din("subln", [128, L])
    gt_in = din("gt", [12, 128, GL])
    mneg_in = din("mneg", [128, GL], BF16)
    cdil_in = din("cdil", [128, GL], BF16)
    b31_in = din("b31", [128, 12])
    mt_in = din("mt", [128, 2432], BF16)
    sel_in = din("sel", [128, 16 * 64])
    ident_in = din("ident", [128, 128], BF16)
    tri_in = din("tri", [128, 128])
    yT_out = nc.dram_tensor("yT", [D, NT], F32, kind="ExternalOutput").ap()

    wf_in = [w_in_a[l] for l in range(L)]
    wf_br = [w_br_a[l] for l in range(L)]
    wf_out = [w_out_a[l] for l in range(L)]
    xs_dd = [dscr("xs_d%d" % i, [D, NT], F32).ap() for i in range(2)]
    hT_d = dscr("hT_d", [128, 16, NT], BF16).ap()
    qs_d = dscr("qs_d", [24, 128, NT], BF16).ap()
    kT_loc = [dscr("kT_loc%d" % q, [128, NT], BF16) for q in range(17)]
    kT_all = [dscr("kT_all%d" % q, [512, NT], BF16) for q in range(17)]
    v_loc = [dscr("v_loc%d" % q, [128, 2048], BF16) for q in range(16)]
    v_all = [dscr("v_all%d" % q, [512, 2048], BF16) for q in range(16)]
    fa_loc = dscr("fa_loc", [128, 64], F32)
    fa_all = dscr("fa_all", [512, 64], F32)
    ao_d = dscr("ao_d", [16, 128, NT], F32).ap()
    A_d = dscr("A_d", [16, 128, S], BF16).ap()

    B = {}

    def buf(name):
        if name not in B:
            B[name] = Buf()
        return B[name]

    groups = [[0, 1, 2, 3], [4, 5, 6, 7]]
    es = ExitStack()
    with es:
        def sb(name, shape, dt):
            return es.enter_context(nc.sbuf_tensor("sb_" + name, list(shape), dt))

        arena = sb("arena", [128, 16, NT], BF16)
        kT_s = sb("kT_s", [128, S], BF16)
        v_s = sb("v_s", [128, 64, 128], BF16)
        wb = [sb("wb%d" % i, [128, 16, 256], BF16) for i in range(2)]
        G_s = sb("G_s", [128, GL], F32)
        Gadd = sb("Gadd", [128, GL], BF16)
        qT_s = sb("qT_s", [128, NT], BF16)
        pT = [sb("pT%d" % i, [128, 512], BF16) for i in range(4)]
        tmpf = [sb("tmpf%d" % i, [128, 512], F32) for i in range(2)]
        rbuf = [sb("rbuf%d" % i, [128, 512], F32) for i in range(2)]
        ot = [sb("ot%d" % i, [128, 512], F32) for i in range(5)]
        stg = [sb("stg%d" % i, [128, 512], BF16) for i in range(4)]
        bm = sb("bm", [128, 16, 64], F32)
        negcum = sb("negcum", [128, 4, 64], F32)
        totf = sb("totf", [128, 4, 64], F32)
        inclf = sb("inclf", [128, 4, 64], F32)
        agf = sb("agf", [128, 4, 64], F32)
        fa4 = sb("fa4", [128, 4, 64], F32)
        cqref = sb("cqref", [128, 4, 16], F32)
        seltmp = sb("seltmp", [128, 16, 64], F32)
        sel_s = sb("sel_s", [128, 16, 64], F32)
        ones1 = sb("ones1", [128, 64], F32)
        ff_s = sb("ff_s", [128, 16, 4], F32)
        fa_s = sb("fa_s", [128, 16, 4], F32)
        wI_s = sb("wI_s", [128, 16, 16], F32)
        qi_s = [sb("qi_s%d" % i, [128, 8, 128], BF16) for i in range(2)]
        ident = sb("ident", [128, 128], BF16)
        onesb = sb("onesb", [128, 128], BF16)
        tri = sb("tri", [128, 128], F32)
        onesf = sb("onesf", [128, 128], F32)
        mt_s = sb("mt_s", [128, 2432], BF16)
        normw = sb("normw", [128, L * 16], F32)
        fnormw = sb("fnormw", [128, 16], F32)
        foxb = sb("foxb", [128, L * 4], F32)
        lq_s = sb("lq_s", [128, L * 4 * 64], F32)
        subln = sb("subln", [128, L], F32)
        b31 = sb("b31", [128, 12], F32)
        sm = sb("sm", [128, 64], F32)
        whalf = sb("whalf", [128, NBIS], F32)
        pw2 = sb("pw2", [128, NBIS], F32)
        epsc = sb("epsc", [128, 1], F32)
        onec = sb("onec", [128, 1], F32)

        ps = [es.enter_context(nc.psum_tensor("ps%d" % i, [128, 512], F32)) for i in range(8)]
        sems = {e: es.enter_context(nc.semaphore("s_" + e)) for e in Prog.ENG}
        for i in range(P.ndma):
            sems["dma%d" % i] = es.enter_context(nc.semaphore("s_dma%d" % i))
        sems["cc"] = es.enter_context(nc.semaphore("s_cc"))
        block = es.enter_context(nc.Block())
        print("sbuf bytes remaining", nc.sbuf_bytes_remaining)

        Bps = [buf("ps%d" % i) for i in range(8)]
        rr = {"stg": 0, "pT": 0, "tmpf": 0, "rbuf": 0, "psL": 0, "psA": 0, "wb": 0, "q": 0, "ev": 0, "oz": 0, "psU": 0}

        def nxt(k, n):
            v = rr[k] % n
            rr[k] += 1
            return v

        def ld(eng, dst, src, name):
            P.dma(eng, lambda e: e.dma_start(out=dst, in_=src), writes=[buf(name)])
        ld("sync", normw[:], normw_in, "normw")
        ld("sync", fnormw[:], fnormw_in, "fnormw")
        ld("sync", foxb[:], foxb_in, "foxb")
        ld("sync", lq_s[:], lq_in, "lq")
        ld("sync", subln[:], subln_in, "subln")
        ld("sync", b31[:], b31_in, "b31")
        ld("sync", mt_s[:], mt_in, "mt")
        ld("sync", sel_s[:].rearrange("p a b -> p (a b)"), sel_in, "sel")
        ld("sync", ident[:], ident_in, "ident")
        ld("sync", tri[:], tri_in, "tri")
        P.op("vector", lambda e: e.memset(onesb[:], 1.0), writes=[buf("onesb")])
        P.op("vector", lambda e: e.memset(onesf[:], 1.0), writes=[buf("onesf")])
        P.op("vector", lambda e: e.memset(ones1[:], 1.0), writes=[buf("ones1")])
        P.op("vector", lambda e: e.memset(epsc[:], 1e-6), writes=[buf("epsc")])
        P.op("vector", lambda e: e.memset(onec[:], 1.0), writes=[buf("onec")])
        for i in range(NBIS):
            P.op("vector", lambda e, i=i: e.memset(pw2[:, i:i + 1], 2.0 ** -(i + 1)), writes=[buf("pw2")])

        def load_w(wfull, bname, col0, ncols, dup=False):
            i = nxt("wb", 2)
            t = wb[i]
            bw = buf("wb%d" % i)
            src = wfull[:, col0:col0 + ncols].rearrange("(k p) n -> p k n", p=128)
            P.dma("gpsimd", lambda e: e.dma_start(out=t[:, :, 0:ncols], in_=src), reads=[buf(bname)], writes=[bw])
            if dup:
                P.dma("gpsimd", lambda e: e.dma_start(out=t[:, :, ncols:2 * ncols], in_=src), reads=[buf(bname)], writes=[bw])
            return t, bw

        hT = arena
        Bar = [buf("arena%d" % j) for j in range(4)]

        def rstd_from_ssq(ps_ap, dst, n, width, bps, bdst):
            P.op("scalar", lambda e: e.activation(out=dst[:, :width], in_=ps_ap, func=AF.Ln, bias=epsc[:, 0:1], scale=1.0 / n),
                 reads=[bps, buf("epsc")], writes=[bdst])
            P.op("scalar", lambda e: e.activation(out=dst[:, :width], in_=dst[:, :width], func=AF.Exp, scale=-0.5),
                 reads=[bdst], writes=[bdst])

        def evac(dst_ap, src_ap, scale, reads, writes):
            i = nxt("ev", 2)
            if i == 0:
                P.op("scalar", lambda e: e.mul(dst_ap, src_ap, float(scale)), reads=reads, writes=writes)
            else:
                P.op("vector", lambda e: e.tensor_scalar(out=dst_ap, in0=src_ap, scalar1=float(scale), scalar2=None, op0=ALU.mult), reads=reads, writes=writes)

        def do_layer(layer):
            Bwin = "wf_in%d" % layer
            Wl = wf_in[layer]
            xsrc = xT_in if layer == 0 else xs_dd[(layer - 1) % 2]
            xs_d = xs_dd[layer % 2]
            Bx = buf("xin") if layer == 0 else buf("xs_d%d" % ((layer - 1) % 2))
            Bxo = buf("xs_d%d" % (layer % 2))
            for j in range(4):
                xt = arena
                pss = ps[6]
                for k in range(16):
                    o = ot[k % 2]
                    bo = buf("ot%d" % (k % 2))
                    P.dma("sync", lambda e, k=k, j=j, o=o: e.dma_start(out=o[:], in_=xsrc[k * 128:(k + 1) * 128, j * 512:(j + 1) * 512]), reads=[Bx], writes=[bo])
                    s_ = stg[k % 2]
                    bs_ = buf("stg%d" % (k % 2))
                    P.op("scalar", lambda e, o=o, s_=s_: e.activation(out=s_[:], in_=o[:], func=AF.Square), reads=[bo], writes=[bs_])
                    P.op("tensor", lambda e, s_=s_, k=k: e.matmul(pss[:, :], lhsT=onesb[:], rhs=s_[:], start=(k == 0), stop=(k == 15)),
                         reads=[bs_, buf("onesb")], writes=[Bps[6]])
                rstd_from_ssq(pss[:, :], ot[4], D, 512, Bps[6], buf("ot4"))
                for k in range(16):
                    o = ot[k % 2]
                    bo = buf("ot%d" % (k % 2))
                    P.dma("sync", lambda e, k=k, j=j, o=o: e.dma_start(out=o[:], in_=xsrc[k * 128:(k + 1) * 128, j * 512:(j + 1) * 512]), reads=[Bx], writes=[bo])
                    P.op("vector", lambda e, k=k, j=j, o=o: e.scalar_tensor_tensor(out=hT[:, k, j * 512:(j + 1) * 512], in0=o[:], scalar=normw[:, layer * 16 + k:layer * 16 + k + 1],
                                                                                   in1=ot[4][:], op0=ALU.mult, op1=ALU.mult),
                         reads=[bo, buf("ot4"), buf("normw")], writes=Bar, acc=True)
            P.dma("sync", lambda e: e.dma_start(out=hT_d, in_=hT[:]), reads=Bar, writes=[buf("hT_d")])

            def fm_group(col0, ncols, dest_fn, scale, dup=False):
                t, bw = load_w(Wl, Bwin, col0, ncols, dup=dup)
                nc_eff = 2 * ncols if dup else ncols
                for q in range((nc_eff + 127) // 128):
                    m = min(128, nc_eff - q * 128)
                    for j in range(4):
                        pi = 4 + nxt("psA", 2)
                        for k in range(16):
                            P.op("tensor", lambda e, pi=pi, k=k, q=q, j=j, m=m: e.matmul(ps[pi][:m, :], lhsT=t[:, k, q * 128:q * 128 + m], rhs=hT[:, k, j * 512:(j + 1) * 512],
                                                                                        start=(k == 0), stop=(k == 15)),
                                 reads=[bw] + Bar, writes=[Bps[pi]])
                        si = nxt("stg", 4)
                        evac(stg[si][:m, :], ps[pi][:m, :], scale, [Bps[pi]], [buf("stg%d" % si)])
                        dst, bd = dest_fn(q, j, m)
                        P.dma("sync", lambda e, dst=dst, si=si, m=m: e.dma_start(out=dst, in_=stg[si][:m, :]), reads=[buf("stg%d" % si)], writes=[bd], acc=True)

            def qdest(base):
                return lambda q, j, m: (qs_d[base + q, 0:m, j * 512:(j + 1) * 512], buf("qs_d"))

            def kdest(base):
                return lambda q, j, m: (kT_loc[(base + q) * 128:(base + q) * 128 + m, j * 512:(j + 1) * 512], buf("kT_loc"))

            def tm_group(col0, ncols, dest_fn, kind):
                t, bw = load_w(Wl, Bwin, col0, ncols)
                for blk in range(16):
                    pi = 4 + nxt("psA", 2)
                    for k in range(16):
                        P.op("tensor", lambda e, pi=pi, k=k, blk=blk: e.matmul(ps[pi][:, 0:ncols], lhsT=hT[:, k, blk * 128:(blk + 1) * 128], rhs=t[:, k, 0:ncols],
                                                                                start=(k == 0), stop=(k == 15)),
                             reads=[bw] + Bar, writes=[Bps[pi]])
                    dest_fn(blk, pi)

            for (mx, c0, s_q) in ((0, C_DSA, S128), (1, C_FOX, S128), (2, C_DIFF, S64), (3, C_DIL, S128)):
                for hh in range(2):
                    fm_group(c0 + hh * 256, 256, (lambda q, j, m, mx=mx, hh=hh: (qs_d[mx * 4 + hh * 2 + q, 0:m, j * 512:(j + 1) * 512], buf("qs_d"))), s_q)
                for hh in range(2):
                    fm_group(c0 + 512 + hh * 256, 256, (lambda q, j, m, mx=mx, hh=hh: (kT_loc[mx * 4 + hh * 2 + q][0:m, j * 512:(j + 1) * 512], buf("kT_loc"))), 1.0)
                for hh in range(2):
                    def vdest(blk, pi, mx=mx, hh=hh):
                        si = nxt("stg", 4)
                        evac(stg[si][:, 0:256], ps[pi][:, 0:256], 1.0, [Bps[pi]], [buf("stg%d" % si)])
                        P.dma("sync", lambda e, si=si: e.dma_start(out=v_loc[blk][:, mx * 512 + hh * 256:mx * 512 + hh * 256 + 256], in_=stg[si][:, 0:256]),
                              reads=[buf("stg%d" % si)], writes=[buf("v_loc")], acc=True)
                    tm_group(c0 + 1024 + hh * 256, 256, vdest, "v")
            for hh in range(4):
                fm_group(C_IQ + hh * 256, 256, (lambda q, j, m, hh=hh: (qs_d[16 + hh * 2 + q, 0:m, j * 512:(j + 1) * 512], buf("qs_d"))), 1.0)
            fm_group(C_IK, 64, (lambda q, j, m: (kT_loc[16][0:m, j * 512:(j + 1) * 512], buf("kT_loc"))), 1.0, dup=True)

            def wdest(blk, pi):
                P.op("vector", lambda e: e.tensor_copy(out=wI_s[:, blk, :], in_=ps[pi][:, 0:16]), reads=[Bps[pi]], writes=[buf("wI")], acc=True)
            tm_group(C_IW, 16, wdest, "w")

            def fdest(blk, pi):
                P.op("vector", lambda e: e.tensor_scalar(out=ff_s[:, blk, :], in0=ps[pi][:, 0:4], scalar1=1.0, scalar2=None, op0=ALU.mult), reads=[Bps[pi]], writes=[buf("ff")], acc=True)
            tm_group(C_FF, 4, fdest, "f")

            for h in range(4):
                P.op("vector", lambda e, h=h: e.tensor_scalar(out=fa_s[:, :, h], in0=ff_s[:, :, h], scalar1=foxb[:, layer * 4 + h:layer * 4 + h + 1], scalar2=None, op0=ALU.add),
                     reads=[buf("ff"), buf("foxb")], writes=[buf("fa")])
            P.op("scalar", lambda e: e.activation(out=fa_s[:], in_=fa_s[:], func=AF.Exp, scale=-1.0), reads=[buf("fa")], writes=[buf("fa")])
            P.op("scalar", lambda e: e.activation(out=fa_s[:], in_=fa_s[:], func=AF.Ln, bias=onec[:, 0:1], scale=1.0), reads=[buf("fa"), buf("onec")], writes=[buf("fa")])
            P.op("vector", lambda e: e.tensor_scalar(out=fa_s[:], in0=fa_s[:], scalar1=-1.0, scalar2=None, op0=ALU.mult), reads=[buf("fa")], writes=[buf("fa")])
            P.dma("sync", lambda e: e.dma_start(out=fa_loc[:, :], in_=fa_s[:].rearrange("p a b -> p (a b)")), reads=[buf("fa")], writes=[buf("fa_loc")])

            for q in range(17):
                P.cc(lambda e, q=q: e.collective_compute("AllGather", ALU.bypass, replica_groups=groups, ins=[kT_loc[q].ap().opt()], outs=[kT_all[q].ap().opt()]),
                     reads=[buf("kT_loc")], writes=[buf("kT_all%d" % q)])
            for q in range(16):
                P.cc(lambda e, q=q: e.collective_compute("AllGather", ALU.bypass, replica_groups=groups, ins=[v_loc[q].ap().opt()], outs=[v_all[q].ap().opt()]),
                     reads=[buf("v_loc")], writes=[buf("v_all%d" % q)])
            P.cc(lambda e: e.collective_compute("AllGather", ALU.bypass, replica_groups=groups, ins=[fa_loc.ap().opt()], outs=[fa_all.ap().opt()]),
                 reads=[buf("fa_loc")], writes=[buf("fa_all")])

            P.dma("sync", lambda e: e.dma_start(out=fa4[:], in_=fa_all.ap().rearrange("(c p) f -> p c f", p=128)), reads=[buf("fa_all")], writes=[buf("fa4")])
            for c in range(4):
                for h in range(4):
                    src = fa4[:, c, :].rearrange("p (j u h) -> p j u h", j=4, u=4, h=4)[:, :, :, h]
                    dst = agf[:, h, :].rearrange("p (j c u) -> p j c u", j=4, c=4, u=4)[:, :, c, :]
                    P.op("vector", lambda e, src=src, dst=dst: e.tensor_copy(out=dst, in_=src), reads=[buf("fa4")], writes=[buf("agf")], acc=True)
            agf2 = agf[:].rearrange("p a b -> p (a b)")
            P.op("tensor", lambda e: e.matmul(ps[6][:, 0:256], lhsT=tri[:], rhs=agf2, start=True, stop=True), reads=[buf("agf"), buf("tri")], writes=[Bps[6]])
            P.op("tensor", lambda e: e.matmul(ps[7][:, 0:256], lhsT=onesf[:], rhs=agf2, start=True, stop=True), reads=[buf("agf"), buf("onesf")], writes=[Bps[7]])
            P.op("vector", lambda e: e.tensor_copy(out=totf[:].rearrange("p a b -> p (a b)"), in_=ps[7][:, 0:256]), reads=[Bps[7]], writes=[buf("totf")])
            for h in range(4):
                P.op("vector", lambda e, h=h: e.tensor_tensor_scan(out=inclf[:, h, :], data0=ones1[:, :], data1=totf[:, h, :], initial=0.0, op0=ALU.mult, op1=ALU.add),
                     reads=[buf("totf"), buf("ones1")], writes=[buf("inclf")])
            P.op("vector", lambda e: e.tensor_tensor(out=negcum[:], in0=totf[:], in1=inclf[:], op=ALU.subtract), reads=[buf("totf"), buf("inclf")], writes=[buf("negcum")])
            P.op("vector", lambda e: e.tensor_tensor(out=negcum[:].rearrange("p a b -> p (a b)"), in0=negcum[:].rearrange("p a b -> p (a b)"), in1=ps[6][:, 0:256], op=ALU.subtract),
                 reads=[buf("negcum"), Bps[6]], writes=[buf("negcum")])
            for h in range(4):
                for blk in range(16):
                    P.op("vector", lambda e, h=h, blk=blk: e.tensor_tensor(out=seltmp[:, blk, :], in0=sel_s[:, blk, :], in1=totf[:, h, :], op=ALU.mult),
                         reads=[buf("sel"), buf("totf")], writes=[buf("seltmp")], acc=True)
                P.op("vector", lambda e, h=h: e.tensor_reduce(out=cqref[:, h, :], in_=seltmp[:], axis=AX.X, op=ALU.add), reads=[buf("seltmp")], writes=[buf("cqref")])

            lam0 = 0.8 - 0.6 * math.exp(-0.3 * layer)
            lb = layer * 256
            P.op("vector", lambda e: e.tensor_tensor(out=tmpf[0][:, 0:64], in0=lq_s[:, lb:lb + 64], in1=lq_s[:, lb + 64:lb + 128], op=ALU.mult), reads=[buf("lq")], writes=[buf("tmpf0")])
            P.op("vector", lambda e: e.tensor_reduce(out=sm[:, 0:1], in_=tmpf[0][:, 0:64], axis=AX.X, op=ALU.add), reads=[buf("tmpf0")], writes=[buf("sm")])
            P.op("vector", lambda e: e.tensor_tensor(out=tmpf[0][:, 0:64], in0=lq_s[:, lb + 128:lb + 192], in1=lq_s[:, lb + 192:lb + 256], op=ALU.mult), reads=[buf("lq")], writes=[buf("tmpf0")])
            P.op("vector", lambda e: e.tensor_reduce(out=sm[:, 1:2], in_=tmpf[0][:, 0:64], axis=AX.X, op=ALU.add), reads=[buf("tmpf0")], writes=[buf("sm")])
            P.op("scalar", lambda e: e.activation(out=sm[:, 2:4], in_=sm[:, 0:2], func=AF.Exp), reads=[buf("sm")], writes=[buf("sm")])
            P.op("vector", lambda e: e.tensor_tensor(out=sm[:, 4:5], in0=sm[:, 3:4], in1=sm[:, 2:3], op=ALU.subtract), reads=[buf("sm")], writes=[buf("sm")])
            P.op("vector", lambda e: e.tensor_scalar(out=sm[:, 5:6], in0=sm[:, 4:5], scalar1=-lam0, scalar2=None, op0=ALU.add), reads=[buf("sm")], writes=[buf("sm")])
            P.op("vector", lambda e: e.tensor_scalar(out=sm[:, 6:7], in0=subln[:, layer:layer + 1], scalar1=1.0 - lam0, scalar2=None, op0=ALU.mult), reads=[buf("subln")], writes=[buf("sm")])

            def load_kv(mx, hh):
                ch = mx * 4 + hh
                src = kT_all[ch].ap().rearrange("(c r) (j i) -> r j c i", c=4, i=512)
                P.dma("sync", lambda e: e.dma_start(out=kT_s[:].rearrange("p (j c i) -> p j c i", j=4, c=4, i=512), in_=src), reads=[buf("kT_all%d" % ch)], writes=[buf("kT_s")])
                col = mx * 512 + hh * 128
                for c in range(4):
                    for blk in range(16):
                        srcv = v_all[blk][c * 128:(c + 1) * 128, col:col + 128]
                        kb = 16 * (blk // 4) + 4 * c + (blk % 4)
                        dstv = v_s[:, kb, :]
                        P.dma("gpsimd" if (blk % 2) else "sync", lambda e, srcv=srcv, dstv=dstv: e.dma_start(out=dstv, in_=srcv), reads=[buf("v_all%d" % blk)], writes=[buf("v_s")], acc=True)
                P.dma("sync", lambda e: e.dma_start(out=qT_s[:], in_=qs_d[ch]), reads=[buf("qs_d")], writes=[buf("qT_s")])

            def load_G(kind, gi):
                if kind == "mask":
                    P.dma("sync", lambda e: e.dma_start(out=Gadd[:], in_=mneg_in), writes=[buf("Gadd")])
                    P.op("vector", lambda e: e.tensor_copy(out=G_s[:], in_=Gadd[:]), reads=[buf("Gadd")], writes=[buf("G_s")])
                    return
                P.dma("sync", lambda e: e.dma_start(out=G_s[:], in_=gt_in[gi]), writes=[buf("G_s")])
                P.dma("sync", lambda e: e.dma_start(out=Gadd[:], in_=(mneg_in if kind == "bias" else cdil_in)), writes=[buf("Gadd")])
                P.op("vector", lambda e: e.tensor_tensor(out=G_s[:], in0=G_s[:], in1=Gadd[:], op=ALU.add), reads=[buf("Gadd"), buf("G_s")], writes=[buf("G_s")])

            LB = (0, 1, 7)

            def flash(j, krows, kb_lo, mode, h, gi, dsa_A=None):
                kbs = list(range(kb_lo, 16 * j + 16))
                ob = 2 + 2 * nxt("oz", 2)
                zb = ob + 1
                pend = None

                def pvz(pi, kb, first, last):
                    bp = buf("pT%d" % pi)
                    P.op("tensor", lambda e: e.matmul(ps[ob][:, :], lhsT=v_s[:, kb, :], rhs=pT[pi][:], start=first, stop=last),
                         reads=[buf("v_s"), bp], writes=[Bps[ob]])
                    P.op("tensor", lambda e: e.matmul(ps[zb][:, :], lhsT=onesb[:], rhs=pT[pi][:], start=first, stop=last),
                         reads=[buf("onesb"), bp], writes=[Bps[zb]])
                for n, kb in enumerate(kbs):
                    ds_ = 2048 * j - 128 * kb
                    li = LB[nxt("psL", 3)]
                    Ls = ps[li]
                    P.op("tensor", lambda e, Ls=Ls, kb=kb: e.matmul(Ls[:, :], lhsT=kT_s[krows[0]:krows[1], kb * 128:(kb + 1) * 128], rhs=qT_s[krows[0]:krows[1], j * 512:(j + 1) * 512],
                                                                      start=True, stop=(dsa_A is None)),
                         reads=[buf("kT_s"), buf("qT_s")], writes=[Bps[li]])
                    if dsa_A is not None:
                        At, bA, off = dsa_A(kb)
                        for u in range(4):
                            P.op("tensor", lambda e, Ls=Ls, u=u, At=At, off=off: e.matmul(Ls[:, u * 128:(u + 1) * 128], lhsT=At[:, u, off:off + 128], rhs=ident[:], start=False, stop=True,
                                                                                          skip_group_check=True),
                                 reads=[bA, buf("ident")], writes=[Bps[li]])
                    src, bsrc = Ls, Bps[li]
                    need_add = (ds_ < 128) if mode == "fox" else (ds_ < DFAR)
                    if need_add:
                        ti = nxt("tmpf", 2)
                        u0 = min(ds_, DFAR) + GOFF
                        P.op("vector", lambda e, ti=ti, Ls=Ls, u0=u0: e.tensor_tensor(out=tmpf[ti][:], in0=Ls[:, :], in1=G_s[:, u0:u0 + 512], op=ALU.add),
                             reads=[Bps[li], buf("G_s")], writes=[buf("tmpf%d" % ti)])
                        src, bsrc = tmpf[ti], buf("tmpf%d" % ti)
                    pi = nxt("pT", 4)
                    bp = buf("pT%d" % pi)
                    if mode == "fox":
                        for u in range(4):
                            P.op("scalar", lambda e, pi=pi, src=src, u=u, kb=kb: e.activation(out=pT[pi][:, u * 128:(u + 1) * 128], in_=src[:, u * 128:(u + 1) * 128], func=AF.Exp,
                                                                                                bias=bm[:, 4 * j + u, kb:kb + 1], scale=1.0),
                                 reads=[bsrc, buf("bm")], writes=[bp], acc=(u > 0))
                    elif need_add:
                        P.op("scalar", lambda e, pi=pi, src=src: e.activation(out=pT[pi][:], in_=src[:, :], func=AF.Exp), reads=[bsrc], writes=[bp])
                    else:
                        P.op("scalar", lambda e, pi=pi, src=src: e.activation(out=pT[pi][:], in_=src[:, :], func=AF.Exp, bias=b31[:, gi:gi + 1], scale=1.0),
                             reads=[bsrc, buf("b31")], writes=[bp])
                    if pend is not None:
                        pvz(*pend)
                    pend = (pi, kb, n == 0, n == len(kbs) - 1)
                pvz(*pend)
                return ob, zb

            def normalize(dst, bdst, oz):
                ob, zb = oz
                P.op("vector", lambda e: e.reciprocal(out=ot[4][:], in_=ps[zb][:, :]), reads=[Bps[zb]], writes=[buf("ot4")])
                P.op("vector", lambda e: e.tensor_tensor(out=dst[:], in0=ps[ob][:, :], in1=ot[4][:], op=ALU.mult), reads=[Bps[ob], buf("ot4")], writes=[bdst])

            def store_o(src, bsrc, ch, j):
                P.dma("sync", lambda e: e.dma_start(out=ao_d[ch, :, j * 512:(j + 1) * 512], in_=src[:]), reads=[bsrc], writes=[buf("ao_d")], acc=True)

            load_G("mask", 0)
            for h in range(4):
                load_kv(1, h)
                for blk in range(16):
                    P.op("vector", lambda e, h=h, blk=blk: e.tensor_scalar(out=bm[:, blk, :], in0=negcum[:, h, :], scalar1=cqref[:, h, blk:blk + 1], scalar2=None, op0=ALU.add),
                         reads=[buf("negcum"), buf("cqref")], writes=[buf("bm")], acc=True)
                for j in range(4):
                    oz = flash(j, (0, 128), 0, "fox", h, 0)
                    normalize(ot[0], buf("ot0"), oz)
                    store_o(ot[0], buf("ot0"), 4 + h, j)
            for h in range(4):
                load_G("dil", 8 + h)
                load_kv(3, h)
                for j in range(4):
                    kb_lo = max(0, 16 * j - 16)
                    oz = flash(j, (0, 128), kb_lo, "dil", h, 8 + h)
                    normalize(ot[0], buf("ot0"), oz)
                    store_o(ot[0], buf("ot0"), 12 + h, j)
            for h in range(4):
                load_G("bias", 4 + h)
                load_kv(2, h)
                for j in range(4):
                    oz = flash(j, (0, 64), 0, "bias", h, 4 + h)
                    normalize(ot[0], buf("ot0"), oz)
                    oz = flash(j, (64, 128), 0, "bias", h, 4 + h)
                    normalize(ot[1], buf("ot1"), oz)
                    P.op("vector", lambda e: e.scalar_tensor_tensor(out=ot[2][:], in0=ot[1][:], scalar=sm[:, 5:6], in1=ot[0][:], op0=ALU.mult, op1=ALU.add),
                         reads=[buf("ot0"), buf("ot1"), buf("sm")], writes=[buf("ot2")])
                    si = nxt("stg", 4)
                    P.op("scalar", lambda e, si=si: e.activation(out=stg[si][:], in_=ot[2][:], func=AF.Square), reads=[buf("ot2")], writes=[buf("stg%d" % si)])
                    P.op("tensor", lambda e, si=si: e.matmul(ps[6][:, :], lhsT=onesb[:], rhs=stg[si][:], start=True, stop=True), reads=[buf("stg%d" % si), buf("onesb")], writes=[Bps[6]])
                    rstd_from_ssq(ps[6][:, :], ot[3], 128, 512, Bps[6], buf("ot3"))
                    P.op("vector", lambda e: e.scalar_tensor_tensor(out=ot[0][:], in0=ot[2][:], scalar=sm[:, 6:7], in1=ot[3][:], op0=ALU.mult, op1=ALU.mult),
                         reads=[buf("ot2"), buf("ot3"), buf("sm")], writes=[buf("ot0")])
                    store_o(ot[0], buf("ot0"), 8 + h, j)

            I_s = arena[:].rearrange("p a b -> p (a b)")[:, 0:16384].bitcast(F32)
            At_s = arena[:].rearrange("p a b -> p (a b)")[:, 16384:24576]
            ki_s = arena[:].rearrange("p a b -> p (a b)")[:, 24576:32768]
            BI = [Bar[0], Bar[1]]
            srck = kT_all[16].ap().rearrange("(c r) (j i) -> r j c i", c=4, i=512)
            P.dma("sync", lambda e: e.dma_start(out=ki_s.rearrange("p (j c i) -> p j c i", j=4, c=4, i=512), in_=srck), reads=[buf("kT_all16")], writes=[Bar[3]])
            for blk in range(16):
                j, u = blk // 4, blk % 4
                nkc = 4 * j + 4
                nk = nkc * 512
                qi = qi_s[blk % 2]
                bq = buf("qi%d" % (blk % 2))
                P.dma("sync", lambda e, qi=qi, blk=blk: e.dma_start(out=qi[:], in_=qs_d[16:24, :, blk * 128:(blk + 1) * 128].rearrange("m p t -> p m t")), reads=[buf("qs_d")], writes=[bq])
                for kc in range(nkc):
                    for ih in range(16):
                        pi = 6 + nxt("psA", 2)
                        r0 = (ih % 2) * 64
                        P.op("tensor", lambda e, pi=pi, ih=ih, r0=r0, kc=kc, qi=qi: e.matmul(ps[pi][:, :], lhsT=qi[r0:r0 + 64, ih // 2, :], rhs=ki_s[r0:r0 + 64, kc * 512:(kc + 1) * 512],
                                                                                              start=True, stop=True),
                             reads=[bq, Bar[3]], writes=[Bps[pi]])
                        ri = nxt("rbuf", 2)
                        P.op("scalar", lambda e, pi=pi, ri=ri: e.activation(out=rbuf[ri][:], in_=ps[pi][:, :], func=AF.Relu), reads=[Bps[pi]], writes=[buf("rbuf%d" % ri)])
                        if ih == 0:
                            P.op("vector", lambda e, ri=ri, kc=kc, blk=blk: e.tensor_scalar(out=I_s[:, kc * 512:(kc + 1) * 512], in0=rbuf[ri][:], scalar1=wI_s[:, blk, 0:1], scalar2=None, op0=ALU.mult),
                                 reads=[buf("rbuf%d" % ri), buf("wI")], writes=BI)
                        else:
                            P.op("vector", lambda e, ri=ri, kc=kc, blk=blk, ih=ih: e.scalar_tensor_tensor(out=I_s[:, kc * 512:(kc + 1) * 512], in0=rbuf[ri][:], scalar=wI_s[:, blk, ih:ih + 1],
                                                                                                          in1=I_s[:, kc * 512:(kc + 1) * 512], op0=ALU.mult, op1=ALU.add),
                                 reads=[buf("rbuf%d" % ri), buf("wI")] + BI, writes=BI)
                P.op("vector", lambda e, nk=nk: e.tensor_reduce(out=sm[:, 8:9], in_=I_s[:, 0:nk], axis=AX.X, op=ALU.min), reads=BI, writes=[buf("sm")])
                for kc in range(4 * j, nkc):
                    v0 = 512 * kc - 2048 * j - 128 * u + 384
                    P.op("vector", lambda e, kc=kc, v0=v0: e.tensor_tensor(out=I_s[:, kc * 512:(kc + 1) * 512], in0=I_s[:, kc * 512:(kc + 1) * 512], in1=mt_s[:, v0:v0 + 512], op=ALU.add),
                         reads=BI + [buf("mt")], writes=BI)
                P.op("vector", lambda e, nk=nk: e.tensor_reduce(out=sm[:, 9:10], in_=I_s[:, 0:nk], axis=AX.X, op=ALU.max), reads=BI, writes=[buf("sm")])
                P.op("vector", lambda e: e.tensor_scalar(out=sm[:, 10:11], in0=sm[:, 8:9], scalar1=-1.0, scalar2=None, op0=ALU.add), reads=[buf("sm")], writes=[buf("sm")])
                P.op("vector", lambda e: e.tensor_tensor(out=sm[:, 11:12], in0=sm[:, 9:10], in1=sm[:, 8:9], op=ALU.subtract), reads=[buf("sm")], writes=[buf("sm")])
                P.op("vector", lambda e: e.tensor_scalar(out=sm[:, 11:12], in0=sm[:, 11:12], scalar1=2.0, scalar2=None, op0=ALU.add), reads=[buf("sm")], writes=[buf("sm")])
                P.op("vector", lambda e: e.tensor_scalar(out=whalf[:], in0=pw2[:], scalar1=sm[:, 11:12], scalar2=None, op0=ALU.mult), reads=[buf("sm"), buf("pw2")], writes=[buf("whalf")])
                for it in range(NBIS):
                    P.op("vector", lambda e, it=it: e.tensor_tensor(out=sm[:, 12:13], in0=sm[:, 10:11], in1=whalf[:, it:it + 1], op=ALU.add), reads=[buf("sm"), buf("whalf")], writes=[buf("sm")])
                    P.op("vector", lambda e, nk=nk: e.tensor_scalar(out=At_s[:, 0:nk], in0=I_s[:, 0:nk], scalar1=sm[:, 12:13], scalar2=None, op0=ALU.is_ge, op1=ALU.add, accum_out=sm[:, 13:14]),
                         reads=BI + [buf("sm")], writes=[Bar[2], buf("sm")])
                    P.op("vector", lambda e, it=it: e.tensor_scalar(out=sm[:, 14:15], in0=sm[:, 13:14], scalar1=TOPK - 0.5, scalar2=whalf[:, it:it + 1], op0=ALU.is_ge, op1=ALU.mult),
                         reads=[buf("sm"), buf("whalf")], writes=[buf("sm")])
                    P.op("vector", lambda e: e.tensor_tensor(out=sm[:, 10:11], in0=sm[:, 10:11], in1=sm[:, 14:15], op=ALU.add), reads=[buf("sm")], writes=[buf("sm")])
                P.op("vector", lambda e, nk=nk: e.tensor_scalar(out=At_s[:, 0:nk], in0=I_s[:, 0:nk], scalar1=sm[:, 10:11], scalar2=NEG, op0=ALU.is_lt, op1=ALU.mult),
                     reads=BI + [buf("sm")], writes=[Bar[2]])
                P.dma("sync", lambda e, blk=blk, nk=nk: e.dma_start(out=A_d[blk, :, 0:nk], in_=At_s[:, 0:nk]), reads=[Bar[2]], writes=[buf("A_d")], acc=True)
            Apc = [arena[:].rearrange("p a b -> p (a b)")[:, i * 8192:(i + 1) * 8192].rearrange("p (u s) -> p u s", u=4) for i in range(2)]
            for h in range(4):
                load_G("bias", h)
                load_kv(0, h)
                for j in range(4):
                    state = {}

                    def dsa_A(kb, j=j, state=state):
                        pc = kb // 16
                        if state.get("pc") != pc:
                            ai = nxt("q", 2)
                            state["pc"], state["ai"] = pc, ai
                            for u in range(4):
                                P.dma("sync", lambda e, ai=ai, u=u, pc=pc: e.dma_start(out=Apc[ai][:, u, :], in_=A_d[4 * j + u, :, pc * 2048:(pc + 1) * 2048]), reads=[buf("A_d")], writes=[Bar[ai]], acc=True)
                        ai = state["ai"]
                        return Apc[ai], Bar[ai], (kb % 16) * 128
                    oz = flash(j, (0, 128), 0, "bias", h, h, dsa_A=dsa_A)
                    normalize(ot[0], buf("ot0"), oz)
                    store_o(ot[0], buf("ot0"), h, j)

            Wbr, Wout = wf_br[layer], wf_out[layer]
            Bbr, Bout = "wf_br%d" % layer, "wf_out%d" % layer
            hTt = arena[:, :, 0:512]
            def reg(r):
                return arena[:, 4 * r:4 * r + 4, :].rearrange("p a (b t) -> p (a b) t", t=512)
            hTt, yTt, mTt = reg(0), reg(1), reg(2)
            for j in range(4):
                P.dma("sync", lambda e, j=j: e.dma_start(out=hTt, in_=hT_d[:, :, j * 512:(j + 1) * 512]), reads=[buf("hT_d")], writes=[Bar[0]])
                for zg in range(8):
                    t, bw = load_w(Wl, Bwin, C_Z + zg * 256, 256)
                    for q in range(2):
                        ch = zg * 2 + q
                        pi = 4 + nxt("psA", 2)
                        for k in range(16):
                            P.op("tensor", lambda e, pi=pi, k=k, q=q, t=t: e.matmul(ps[pi][:, :], lhsT=t[:, k, q * 128:(q + 1) * 128], rhs=hTt[:, k, :], start=(k == 0), stop=(k == 15)),
                                 reads=[bw, Bar[0]], writes=[Bps[pi]])
                        ti = nxt("tmpf", 2)
                        P.op("scalar", lambda e, pi=pi, ti=ti: e.activation(out=tmpf[ti][:], in_=ps[pi][:, :], func=AF.Silu), reads=[Bps[pi]], writes=[buf("tmpf%d" % ti)])
                        oi = nxt("rbuf", 2)
                        P.dma("sync", lambda e, oi=oi, ch=ch, j=j: e.dma_start(out=rbuf[oi][:], in_=ao_d[ch, :, j * 512:(j + 1) * 512]), reads=[buf("ao_d")], writes=[buf("rbuf%d" % oi)])
                        P.op("vector", lambda e, ti=ti, oi=oi, ch=ch: e.tensor_tensor(out=yTt[:, ch, :], in0=tmpf[ti][:], in1=rbuf[oi][:], op=ALU.mult),
                             reads=[buf("tmpf%d" % ti), buf("rbuf%d" % oi)], writes=[Bar[1]], acc=True)
                for ng in range(8):
                    wts = []
                    for b in range(4):
                        pass
                    for b in range(4):
                        tg, bwg = load_w(Wl, Bwin, C_G + b * 2048 + ng * 256, 256)
                        i2 = nxt("wb", 2)
                        tb_, bwb = wb[i2], buf("wb%d" % i2)
                        srcb = Wbr[b * 512:(b + 1) * 512, ng * 256:(ng + 1) * 256].rearrange("(k p) n -> p k n", p=128)
                        P.dma("gpsimd", lambda e, tb_=tb_, srcb=srcb: e.dma_start(out=tb_[:, 0:4, :], in_=srcb), reads=[buf(Bbr)], writes=[bwb])
                        for q in range(2):
                            n = ng * 2 + q
                            pg = 4 + nxt("psA", 2)
                            for k in range(16):
                                P.op("tensor", lambda e, pg=pg, k=k, q=q, tg=tg: e.matmul(ps[pg][:, :], lhsT=tg[:, k, q * 128:(q + 1) * 128], rhs=hTt[:, k, :], start=(k == 0), stop=(k == 15)),
                                     reads=[bwg, Bar[0]], writes=[Bps[pg]])
                            pu = 6 + nxt("psU", 2)
                            for k in range(4):
                                P.op("tensor", lambda e, pu=pu, k=k, q=q, tb_=tb_, b=b: e.matmul(ps[pu][:, :], lhsT=tb_[:, k, q * 128:(q + 1) * 128], rhs=yTt[:, b * 4 + k, :], start=(k == 0), stop=(k == 3)),
                                     reads=[bwb, Bar[1]], writes=[Bps[pu]])
                            ti = nxt("tmpf", 2)
                            P.op("scalar", lambda e, pg=pg, ti=ti: e.activation(out=tmpf[ti][:], in_=ps[pg][:, :], func=AF.Sigmoid), reads=[Bps[pg]], writes=[buf("tmpf%d" % ti)])
                            acc = ot[q]
                            bacc = buf("ot%d" % q)
                            if b == 0:
                                P.op("vector", lambda e, ti=ti, pu=pu, acc=acc: e.tensor_tensor(out=acc[:], in0=ps[pu][:, :], in1=tmpf[ti][:], op=ALU.mult),
                                     reads=[Bps[pu], buf("tmpf%d" % ti)], writes=[bacc])
                            else:
                                P.op("vector", lambda e, ti=ti, pu=pu: e.tensor_tensor(out=tmpf[ti][:], in0=ps[pu][:, :], in1=tmpf[ti][:], op=ALU.mult),
                                     reads=[Bps[pu], buf("tmpf%d" % ti)], writes=[buf("tmpf%d" % ti)])
                                P.op("gpsimd", lambda e, ti=ti, acc=acc: e.tensor_tensor(out=acc[:], in0=acc[:], in1=tmpf[ti][:], op=ALU.add),
                                     reads=[buf("tmpf%d" % ti), bacc], writes=[bacc])
                            if b == 3:
                                P.op("scalar", lambda e, n=n, acc=acc: e.activation(out=mTt[:, n, :], in_=acc[:], func=AF.Copy), reads=[bacc], writes=[Bar[2]], acc=True)
                for ng in range(8):
                    to, bwo = load_w(Wout, Bout, ng * 256, 256)
                    for q in range(2):
                        n = ng * 2 + q
                        po = 4 + nxt("psA", 2)
                        for k in range(16):
                            P.op("tensor", lambda e, po=po, k=k, q=q, to=to: e.matmul(ps[po][:, :], lhsT=to[:, k, q * 128:(q + 1) * 128], rhs=mTt[:, k, :], start=(k == 0), stop=(k == 15)),
                                 reads=[bwo, Bar[2]], writes=[Bps[po]])
                        oi = nxt("rbuf", 2)
                        P.dma("sync", lambda e, oi=oi, n=n, j=j: e.dma_start(out=rbuf[oi][:], in_=xsrc[n * 128:(n + 1) * 128, j * 512:(j + 1) * 512]), reads=[Bx], writes=[buf("rbuf%d" % oi)])
                        P.op("vector", lambda e, oi=oi, po=po: e.tensor_tensor(out=rbuf[oi][:], in0=ps[po][:, :], in1=rbuf[oi][:], op=ALU.add),
                             reads=[Bps[po], buf("rbuf%d" % oi)], writes=[buf("rbuf%d" % oi)])
                        P.dma("sync", lambda e, oi=oi, n=n, j=j: e.dma_start(out=xs_d[n * 128:(n + 1) * 128, j * 512:(j + 1) * 512], in_=rbuf[oi][:]), reads=[buf("rbuf%d" % oi)], writes=[Bxo], acc=True)

        for layer_ in range(L):
            do_layer(layer_)

        xs_d = xs_dd[(L - 1) % 2]
        Bx = buf("xs_d%d" % ((L - 1) % 2))
        Bout_ = buf("yT")
        for j in range(4):
            for k in range(16):
                o = ot[k % 2]
                bo = buf("ot%d" % (k % 2))
                P.dma("sync", lambda e, k=k, j=j, o=o: e.dma_start(out=o[:], in_=xs_d[k * 128:(k + 1) * 128, j * 512:(j + 1) * 512]), reads=[Bx], writes=[bo])
                s_ = stg[k % 2]
                bs_ = buf("stg%d" % (k % 2))
                P.op("scalar", lambda e, o=o, s_=s_: e.activation(out=s_[:], in_=o[:], func=AF.Square), reads=[bo], writes=[bs_])
                P.op("tensor", lambda e, s_=s_, k=k: e.matmul(ps[6][:, :], lhsT=onesb[:], rhs=s_[:], start=(k == 0), stop=(k == 15)), reads=[bs_, buf("onesb")], writes=[Bps[6]])
            rstd_from_ssq(ps[6][:, :], ot[4], D, 512, Bps[6], buf("ot4"))
            for k in range(16):
                o = ot[k % 2]
                bo = buf("ot%d" % (k % 2))
                P.dma("sync", lambda e, k=k, j=j, o=o: e.dma_start(out=o[:], in_=xs_d[k * 128:(k + 1) * 128, j * 512:(j + 1) * 512]), reads=[Bx], writes=[bo])
                ri = nxt("rbuf", 2)
                P.op("vector", lambda e, k=k, o=o, ri=ri: e.scalar_tensor_tensor(out=rbuf[ri][:], in0=o[:], scalar=fnormw[:, k:k + 1], in1=ot[4][:], op0=ALU.mult, op1=ALU.mult),
                     reads=[bo, buf("ot4"), buf("fnormw")], writes=[buf("rbuf%d" % ri)])
                P.dma("sync", lambda e, k=k, j=j, ri=ri: e.dma_start(out=yT_out[k * 128:(k + 1) * 128, j * 512:(j + 1) * 512], in_=rbuf[ri][:]), reads=[buf("rbuf%d" % ri)], writes=[Bout_], acc=True)
        P.finish("sync", [Bout_])
        P.emit(block, sems)
    print("ops", P.nops)
    return nc


def host_prep(inputs, depth=DEPTH):
    x = np.asarray(inputs["x"], np.float32)
    L = depth
    rel = np.asarray(inputs["rel_bias"], np.float32)
    maps = []
    sl = np.arange(128)[:, None]
    uu = np.arange(GL)[None, :]
    import ml_dtypes
    bf = ml_dtypes.bfloat16
    ident = np.eye(128, dtype=np.float32).astype(bf)
    tri = (np.arange(128)[:, None] <= np.arange(128)[None, :]).astype(np.float32)
    normw = np.ascontiguousarray(np.asarray(inputs["norm_w"], np.float32)[:L].reshape(L, 16, 128).transpose(2, 0, 1).reshape(128, L * 16))
    fnormw = np.ascontiguousarray(np.asarray(inputs["final_norm_w"], np.float32).reshape(16, 128).T)
    foxb = np.ascontiguousarray(np.broadcast_to(np.asarray(inputs["fox_b_f"], np.float32)[:L].reshape(1, L * 4), (128, L * 4)))
    lq = np.stack([np.asarray(inputs[k], np.float32)[:L] for k in ("diff_lq1", "diff_lk1", "diff_lq2", "diff_lk2")], axis=1)
    lq = np.ascontiguousarray(np.broadcast_to(lq.reshape(1, L * 256), (128, L * 256)))
    subln = np.ascontiguousarray(np.asarray(inputs["diff_subln_w"], np.float32)[:L].T)
    b31 = np.ascontiguousarray(np.broadcast_to(rel[31:32, :], (128, 12)))
    w_in = np.asarray(inputs["w_in"], np.float32)
    w_br = np.asarray(inputs["w_branch"], np.float32).reshape(DEPTH, 2048, 2048)
    w_out = np.asarray(inputs["w_out"], np.float32)
    for core in range(8):
        b, c = core // 4, core % 4
        toks = np.concatenate([np.arange(512 * (4 * j + c), 512 * (4 * j + c + 1)) for j in range(4)])
        xT = np.ascontiguousarray(x[b, toks, :].T)
        dist = uu - GOFF + 512 * c - sl
        bidx = t5_bucket_np(dist)
        gt = np.ascontiguousarray(rel[bidx, :].transpose(2, 0, 1))
        mneg = np.where(dist >= 0, 0.0, NEG).astype(np.float32).astype(bf)
        nval = ((dist >= 0) & (dist <= 128)).astype(np.int32) + ((dist >= 0) & (dist % 4 == 0) & (dist <= 512)).astype(np.int32) \
            + ((dist >= 0) & (dist % 16 == 0) & (dist <= 2048)).astype(np.int32)
        cdil = np.where(nval > 0, np.log(np.maximum(nval, 1).astype(np.float32)), NEG).astype(np.float32).astype(bf)
        vv = np.arange(2432)[None, :]
        mt = np.where((vv - 384 - 512 * c - sl) > 0, -1e9, 0.0).astype(np.float32).astype(bf)
        blk = np.arange(16)
        kb_own = 16 * (blk // 4) + 4 * c + (blk % 4)
        sel = (np.arange(64)[None, :] <= kb_own[:, None]).astype(np.float32)
        sel = np.ascontiguousarray(np.broadcast_to(sel.reshape(1, 1024), (128, 1024)))
        maps.append({
            "xT": xT,
            "w_in": w_in[:L], "w_br": w_br[:L], "w_out": w_out[:L],
            "normw": normw, "fnormw": fnormw, "foxb": foxb, "lq": lq, "subln": subln,
            "gt": gt, "mneg": mneg, "cdil": cdil, "b31": b31, "mt": mt, "sel": sel, "ident": ident, "tri": tri,
        })
    return maps


def run(inputs, depth=DEPTH):
    nc = build(depth)
    maps = host_prep(inputs, depth)
    res = run_bass_kernel_spmd(nc, maps, core_ids=list(range(8)))
    out = np.zeros((2, S, D), np.float32)
    for core in range(8):
        b, c = core // 4, core % 4
        toks = np.concatenate([np.arange(512 * (4 * j + c), 512 * (4 * j + c + 1)) for j in range(4)])
        out[b, toks, :] = res.results[core]["yT"].T
    return out


def kernel(**inputs):
    return run(inputs, DEPTH)
```

```python
import math
from contextlib import ExitStack

import numpy as np
import concourse.bass as bass
import concourse.mybir as mybir
from concourse.bass_utils import run_bass_kernel_spmd

F32 = mybir.dt.float32
BF16 = mybir.dt.bfloat16
ALU = mybir.AluOpType
AF = mybir.ActivationFunctionType
AX = mybir.AxisListType

D = 2048
S = 8192
NT = 2048
DEPTH = 4
N_IN = 17492
NEG = -30000.0
GL = 4608
GOFF = 1920
DFAR = 2176
C_Z, C_G = 0, 2048
C_DSA, C_IQ, C_IK, C_IW, C_FOX, C_FF, C_DIFF, C_DIL = 10240, 11776, 12800, 12864, 12880, 14416, 14420, 15956
S128 = 128 ** -0.5
S64 = 64 ** -0.5
TOPK = 256
NBIS = 18


class Buf:
    __slots__ = ("w", "r")

    def __init__(self):
        self.w = {}
        self.r = {}


class Prog:
    ENG = ("sync", "scalar", "vector", "gpsimd", "tensor")

    def __init__(self, ndma=24):
        self.streams = {e: [] for e in self.ENG}
        self.count = {e: 0 for e in self.ENG}
        self.seen = {e: {} for e in self.ENG}
        self.ndma = ndma
        self.dma_i = 0
        self.dma_cnt = [0] * ndma
        self.cc_cnt = 0
        self.nops = 0

    def _deps(self, eng, reads, writes, acc=False):
        deps = {}

        def add(k, v):
            if deps.get(k, 0) < v:
                deps[k] = v
        for b in reads:
            for k, v in b.w.items():
                add(k, v)
        for b in writes:
            if not acc:
                for k, v in b.w.items():
                    add(k, v)
            for k, v in b.r.items():
                add(k, v)
        waits = []
        for k, v in deps.items():
            if k == eng and eng == "tensor":
                continue
            if self.seen[eng].get(k, 0) < v:
                self.seen[eng][k] = v
                waits.append((k, v))
        return waits

    def _record(self, me, reads, writes, acc=False):
        for b in reads:
            if b.r.get(me[0], 0) < me[1]:
                b.r[me[0]] = me[1]
        for b in writes:
            if acc:
                b.w[me[0]] = me[1]
            else:
                b.w = {me[0]: me[1]}
                b.r = {}
        self.nops += 1

    def op(self, eng, fn, reads=(), writes=(), acc=False):
        waits = self._deps(eng, reads, writes, acc)
        self.count[eng] += 1
        me = (eng, self.count[eng])
        self.streams[eng].append((waits, fn, eng, 1))
        self._record(me, reads, writes, acc)

    def dma(self, eng, fn, reads=(), writes=(), acc=False):
        i = self.dma_i % self.ndma
        self.dma_i += 1
        key = "dma%d" % i
        waits = self._deps(eng, reads, writes, acc)
        prev = self.dma_cnt[i]
        if prev and self.seen[eng].get(key, 0) < prev:
            self.seen[eng][key] = prev
            waits.append((key, prev))
        self.dma_cnt[i] += 16
        me = (key, self.dma_cnt[i])
        self.streams[eng].append((waits, fn, key, 16))
        self._record(me, reads, writes, acc)

    def cc(self, fn, reads=(), writes=()):
        eng = "gpsimd"
        waits = self._deps(eng, reads, writes)
        self.cc_cnt += 1
        me = ("cc", self.cc_cnt)
        self.streams[eng].append((waits, fn, "cc", None))
        self._record(me, reads, writes)

    def finish(self, eng, bufs):
        waits = self._deps(eng, bufs, ())
        self.streams[eng].append((waits, None, None, 0))

    def emit(self, block, sems):
        def mk(e):
            def body(engh):
                for waits, fn, key, inc in self.streams[e]:
                    for k, v in waits:
                        engh.wait_ge(sems[k], v)
                    if fn is not None:
                        ins = fn(engh)
                        if inc is None:
                            ins.then_inc(sems[key])
                        else:
                            ins.then_inc(sems[key], inc)
            return body
        block.sync(mk("sync"))
        block.scalar(mk("scalar"))
        block.vector(mk("vector"))
        block.gpsimd(mk("gpsimd"))
        block.tensor(mk("tensor"))


def t5_bucket_np(dist):
    dist = np.maximum(dist, 0)
    d = np.maximum(dist, 16).astype(np.float32)
    large = 16 + (np.log(d / np.float32(16)) / np.float32(math.log(2048 / 16)) * np.float32(16)).astype(np.int32)
    large = np.minimum(large, 31)
    return np.where(dist < 16, dist, large)


def build(depth=DEPTH):
    nc = bass.Bass("TRN2", target_bir_lowering=False)
    P = Prog()
    L = depth

    def din(name, shape, dt=F32):
        return nc.dram_tensor(name, list(shape), dt, kind="ExternalInput").ap()

    def dscr(name, shape, dt):
        return nc.dram_tensor(name, list(shape), dt)

    xT_in = din("xT", [D, NT])
    w_in_a = din("w_in", [L, D, N_IN])
    w_br_a = din("w_br", [L, D, D])
    w_out_a = din("w_out", [L, D, D])
    normw_in = din("normw", [128, L * 16])
    fnormw_in = din("fnormw", [128, 16])
    foxb_in = din("foxb", [128, L * 4])
    lq_in = din("lq", [128, L * 4 * 64])
    subln_in = din("subln", [128, L])
    gt_in = din("gt", [12, 128, GL])
    mneg_in = din("mneg", [128, GL], BF16)
    cdil_in = din("cdil", [128, GL], BF16)
    b31_in = din("b31", [128, 12])
    mt_in = din("mt", [128, 2432], BF16)
    sel_in = din("sel", [128, 16 * 64])
    ident_in = din("ident", [128, 128], BF16)
    tri_in = din("tri", [128, 128])
    yT_out = nc.dram_tensor("yT", [D, NT], F32, kind="ExternalOutput").ap()

    wf_in = [w_in_a[l] for l in range(L)]
    wf_br = [w_br_a[l] for l in range(L)]
    wf_out = [w_out_a[l] for l in range(L)]
    xs_dd = [dscr("xs_d%d" % i, [D, NT], F32).ap() for i in range(2)]
    hT_d = dscr("hT_d", [128, 16, NT], BF16).ap()
    qs_d = dscr("qs_d", [24, 128, NT], BF16).ap()
    yT_d = dscr("yT_d", [16, 128, NT], BF16).ap()
    mT_d = dscr("mT_d", [16, 128, NT], BF16).ap()
    kT_loc = [dscr("kT_loc%d" % q, [128, NT], BF16) for q in range(17)]
    kT_all = [dscr("kT_all%d" % q, [512, NT], BF16) for q in range(17)]
    v_loc = [dscr("v_loc%d" % q, [128, 2048], BF16) for q in range(16)]
    v_all = [dscr("v_all%d" % q, [512, 2048], BF16) for q in range(16)]
    fa_loc = dscr("fa_loc", [128, 64], F32)
    fa_all = dscr("fa_all", [512, 64], F32)
    ao_d = dscr("ao_d", [16, 128, NT], F32).ap()
    A_d = dscr("A_d", [16, 128, S], BF16).ap()

    B = {}

    def buf(name):
        if name not in B:
            B[name] = Buf()
        return B[name]

    groups = [[0, 1, 2, 3], [4, 5, 6, 7]]
    es = ExitStack()
    with es:
        def sb(name, shape, dt):
            return es.enter_context(nc.sbuf_tensor("sb_" + name, list(shape), dt))

        arena = sb("arena", [128, 16, NT], BF16)
        kT_s = sb("kT_s", [128, S], BF16)
        v_s = sb("v_s", [128, 64, 128], BF16)
        wb = [sb("wb%d" % i, [128, 16, 256], BF16) for i in range(2)]
        G_s = sb("G_s", [128, GL], F32)
        Gadd = sb("Gadd", [128, GL], BF16)
        qT_s = sb("qT_s", [128, NT], BF16)
        pT = [sb("pT%d" % i, [128, 512], BF16) for i in range(4)]
        tmpf = [sb("tmpf%d" % i, [128, 512], F32) for i in range(2)]
        rbuf = [sb("rbuf%d" % i, [128, 512], F32) for i in range(2)]
        ot = [sb("ot%d" % i, [128, 512], F32) for i in range(5)]
        stg = [sb("stg%d" % i, [128, 512], BF16) for i in range(4)]
        bm = sb("bm", [128, 16, 64], F32)
        negcum = sb("negcum", [128, 4, 64], F32)
        totf = sb("totf", [128, 4, 64], F32)
        inclf = sb("inclf", [128, 4, 64], F32)
        agf = sb("agf", [128, 4, 64], F32)
        fa4 = sb("fa4", [128, 4, 64], F32)
        cqref = sb("cqref", [128, 4, 16], F32)
        seltmp = sb("seltmp", [128, 16, 64], F32)
        sel_s = sb("sel_s", [128, 16, 64], F32)
        ones1 = sb("ones1", [128, 64], F32)
        ff_s = sb("ff_s", [128, 16, 4], F32)
        fa_s = sb("fa_s", [128, 16, 4], F32)
        wI_s = sb("wI_s", [128, 16, 16], F32)
        qi_s = [sb("qi_s%d" % i, [128, 8, 128], BF16) for i in range(2)]
        ident = sb("ident", [128, 128], BF16)
        onesb = sb("onesb", [128, 128], BF16)
        tri = sb("tri", [128, 128], F32)
        onesf = sb("onesf", [128, 128], F32)
        mt_s = sb("mt_s", [128, 2432], BF16)
        normw = sb("normw", [128, L * 16], F32)
        fnormw = sb("fnormw", [128, 16], F32)
        foxb = sb("foxb", [128, L * 4], F32)
        lq_s = sb("lq_s", [128, L * 4 * 64], F32)
        subln = sb("subln", [128, L], F32)
        b31 = sb("b31", [128, 12], F32)
        sm = sb("sm", [128, 64], F32)
        whalf = sb("whalf", [128, NBIS], F32)
        pw2 = sb("pw2", [128, NBIS], F32)
        epsc = sb("epsc", [128, 1], F32)
        onec = sb("onec", [128, 1], F32)

        ps = [es.enter_context(nc.psum_tensor("ps%d" % i, [128, 512], F32)) for i in range(8)]
        sems = {e: es.enter_context(nc.semaphore("s_" + e)) for e in Prog.ENG}
        for i in range(P.ndma):
            sems["dma%d" % i] = es.enter_context(nc.semaphore("s_dma%d" % i))
        sems["cc"] = es.enter_context(nc.semaphore("s_cc"))
        block = es.enter_context(nc.Block())
        print("sbuf bytes remaining", nc.sbuf_bytes_remaining)

        Bps = [buf("ps%d" % i) for i in range(8)]
        rr = {"stg": 0, "pT": 0, "tmpf": 0, "rbuf": 0, "psL": 0, "psA": 0, "wb": 0, "q": 0, "ev": 0, "oz": 0, "psU": 0, "yb": 0}

        def nxt(k, n):
            v = rr[k] % n
            rr[k] += 1
            return v

        def ld(eng, dst, src, name):
            P.dma(eng, lambda e: e.dma_start(out=dst, in_=src), writes=[buf(name)])
        ld("sync", normw[:], normw_in, "normw")
        ld("sync", fnormw[:], fnormw_in, "fnormw")
        ld("sync", foxb[:], foxb_in, "foxb")
        ld("sync", lq_s[:], lq_in, "lq")
        ld("sync", subln[:], subln_in, "subln")
        ld("sync", b31[:], b31_in, "b31")
        ld("sync", mt_s[:], mt_in, "mt")
        ld("sync", sel_s[:].rearrange("p a b -> p (a b)"), sel_in, "sel")
        ld("sync", ident[:], ident_in, "ident")
        ld("sync", tri[:], tri_in, "tri")
        P.op("vector", lambda e: e.memset(onesb[:], 1.0), writes=[buf("onesb")])
        P.op("vector", lambda e: e.memset(onesf[:], 1.0), writes=[buf("onesf")])
        P.op("vector", lambda e: e.memset(ones1[:], 1.0), writes=[buf("ones1")])
        P.op("vector", lambda e: e.memset(epsc[:], 1e-6), writes=[buf("epsc")])
        P.op("vector", lambda e: e.memset(onec[:], 1.0), writes=[buf("onec")])
        for i in range(NBIS):
            P.op("vector", lambda e, i=i: e.memset(pw2[:, i:i + 1], 2.0 ** -(i + 1)), writes=[buf("pw2")])

        def load_w(wfull, bname, col0, ncols, dup=False):
            i = nxt("wb", 2)
            t = wb[i]
            bw = buf("wb%d" % i)
            src = wfull[:, col0:col0 + ncols].rearrange("(k p) n -> p k n", p=128)
            P.dma("gpsimd", lambda e: e.dma_start(out=t[:, :, 0:ncols], in_=src), reads=[buf(bname)], writes=[bw])
            if dup:
                P.dma("gpsimd", lambda e: e.dma_start(out=t[:, :, ncols:2 * ncols], in_=src), reads=[buf(bname)], writes=[bw])
            return t, bw

        hT = arena
        Bar = [buf("arena%d" % j) for j in range(4)]

        def rstd_from_ssq(ps_ap, dst, n, width, bps, bdst):
            P.op("scalar", lambda e: e.activation(out=dst[:, :width], in_=ps_ap, func=AF.Ln, bias=epsc[:, 0:1], scale=1.0 / n),
                 reads=[bps, buf("epsc")], writes=[bdst])
            P.op("scalar", lambda e: e.activation(out=dst[:, :width], in_=dst[:, :width], func=AF.Exp, scale=-0.5),
                 reads=[bdst], writes=[bdst])

        def evac(dst_ap, src_ap, scale, reads, writes):
            i = nxt("ev", 2)
            if i == 0:
                P.op("scalar", lambda e: e.mul(dst_ap, src_ap, float(scale)), reads=reads, writes=writes)
            else:
                P.op("vector", lambda e: e.tensor_scalar(out=dst_ap, in0=src_ap, scalar1=float(scale), scalar2=None, op0=ALU.mult), reads=reads, writes=writes)

        def do_layer(layer):
            Bwin = "wf_in%d" % layer
            Wl = wf_in[layer]
            xsrc = xT_in if layer == 0 else xs_dd[(layer - 1) % 2]
            xs_d = xs_dd[layer % 2]
            Bx = buf("xin") if layer == 0 else buf("xs_d%d" % ((layer - 1) % 2))
            Bxo = buf("xs_d%d" % (layer % 2))
            for j in range(4):
                xt = arena
                pss = ps[6]
                for k in range(16):
                    o = ot[k % 2]
                    bo = buf("ot%d" % (k % 2))
                    P.dma("sync", lambda e, k=k, j=j, o=o: e.dma_start(out=o[:], in_=xsrc[k * 128:(k + 1) * 128, j * 512:(j + 1) * 512]), reads=[Bx], writes=[bo])
                    s_ = stg[k % 2]
                    bs_ = buf("stg%d" % (k % 2))
                    P.op("scalar", lambda e, o=o, s_=s_: e.activation(out=s_[:], in_=o[:], func=AF.Square), reads=[bo], writes=[bs_])
                    P.op("tensor", lambda e, s_=s_, k=k: e.matmul(pss[:, :], lhsT=onesb[:], rhs=s_[:], start=(k == 0), stop=(k == 15)),
                         reads=[bs_, buf("onesb")], writes=[Bps[6]])
                rstd_from_ssq(pss[:, :], ot[4], D, 512, Bps[6], buf("ot4"))
                for k in range(16):
                    o = ot[k % 2]
                    bo = buf("ot%d" % (k % 2))
                    P.dma("sync", lambda e, k=k, j=j, o=o: e.dma_start(out=o[:], in_=xsrc[k * 128:(k + 1) * 128, j * 512:(j + 1) * 512]), reads=[Bx], writes=[bo])
                    P.op("vector", lambda e, k=k, j=j, o=o: e.scalar_tensor_tensor(out=hT[:, k, j * 512:(j + 1) * 512], in0=o[:], scalar=normw[:, layer * 16 + k:layer * 16 + k + 1],
                                                                                   in1=ot[4][:], op0=ALU.mult, op1=ALU.mult),
                         reads=[bo, buf("ot4"), buf("normw")], writes=Bar, acc=True)
            P.dma("sync", lambda e: e.dma_start(out=hT_d, in_=hT[:]), reads=Bar, writes=[buf("hT_d")])

            def fm_group(col0, ncols, dest_fn, scale, dup=False):
                t, bw = load_w(Wl, Bwin, col0, ncols, dup=dup)
                nc_eff = 2 * ncols if dup else ncols
                for q in range((nc_eff + 127) // 128):
                    m = min(128, nc_eff - q * 128)
                    for j in range(4):
                        pi = 4 + nxt("psA", 2)
                        for k in range(16):
                            P.op("tensor", lambda e, pi=pi, k=k, q=q, j=j, m=m: e.matmul(ps[pi][:m, :], lhsT=t[:, k, q * 128:q * 128 + m], rhs=hT[:, k, j * 512:(j + 1) * 512],
                                                                                        start=(k == 0), stop=(k == 15)),
                                 reads=[bw] + Bar, writes=[Bps[pi]])
                        si = nxt("stg", 4)
                        evac(stg[si][:m, :], ps[pi][:m, :], scale, [Bps[pi]], [buf("stg%d" % si)])
                        dst, bd = dest_fn(q, j, m)
                        P.dma("sync", lambda e, dst=dst, si=si, m=m: e.dma_start(out=dst, in_=stg[si][:m, :]), reads=[buf("stg%d" % si)], writes=[bd], acc=True)

            def qdest(base):
                return lambda q, j, m: (qs_d[base + q, 0:m, j * 512:(j + 1) * 512], buf("qs_d"))

            def kdest(base):
                return lambda q, j, m: (kT_loc[(base + q) * 128:(base + q) * 128 + m, j * 512:(j + 1) * 512], buf("kT_loc"))

            def tm_group(col0, ncols, dest_fn, kind):
                t, bw = load_w(Wl, Bwin, col0, ncols)
                for blk in range(16):
                    pi = 4 + nxt("psA", 2)
                    for k in range(16):
                        P.op("tensor", lambda e, pi=pi, k=k, blk=blk: e.matmul(ps[pi][:, 0:ncols], lhsT=hT[:, k, blk * 128:(blk + 1) * 128], rhs=t[:, k, 0:ncols],
                                                                                start=(k == 0), stop=(k == 15)),
                             reads=[bw] + Bar, writes=[Bps[pi]])
                    dest_fn(blk, pi)

            for (mx, c0, s_q) in ((0, C_DSA, S128), (1, C_FOX, S128), (2, C_DIFF, S64), (3, C_DIL, S128)):
                for hh in range(2):
                    fm_group(c0 + hh * 256, 256, (lambda q, j, m, mx=mx, hh=hh: (qs_d[mx * 4 + hh * 2 + q, 0:m, j * 512:(j + 1) * 512], buf("qs_d"))), s_q)
                for hh in range(2):
                    fm_group(c0 + 512 + hh * 256, 256, (lambda q, j, m, mx=mx, hh=hh: (kT_loc[mx * 4 + hh * 2 + q][0:m, j * 512:(j + 1) * 512], buf("kT_loc"))), 1.0)
                for hh in range(2):
                    def vdest(blk, pi, mx=mx, hh=hh):
                        si = nxt("stg", 4)
                        evac(stg[si][:, 0:256], ps[pi][:, 0:256], 1.0, [Bps[pi]], [buf("stg%d" % si)])
                        P.dma("sync", lambda e, si=si: e.dma_start(out=v_loc[blk][:, mx * 512 + hh * 256:mx * 512 + hh * 256 + 256], in_=stg[si][:, 0:256]),
                              reads=[buf("stg%d" % si)], writes=[buf("v_loc")], acc=True)
                    tm_group(c0 + 1024 + hh * 256, 256, vdest, "v")
            for hh in range(4):
                fm_group(C_IQ + hh * 256, 256, (lambda q, j, m, hh=hh: (qs_d[16 + hh * 2 + q, 0:m, j * 512:(j + 1) * 512], buf("qs_d"))), 1.0)
            fm_group(C_IK, 64, (lambda q, j, m: (kT_loc[16][0:m, j * 512:(j + 1) * 512], buf("kT_loc"))), 1.0, dup=True)

            def wdest(blk, pi):
                P.op("vector", lambda e: e.tensor_copy(out=wI_s[:, blk, :], in_=ps[pi][:, 0:16]), reads=[Bps[pi]], writes=[buf("wI")], acc=True)
            tm_group(C_IW, 16, wdest, "w")

            def fdest(blk, pi):
                P.op("vector", lambda e: e.tensor_scalar(out=ff_s[:, blk, :], in0=ps[pi][:, 0:4], scalar1=1.0, scalar2=None, op0=ALU.mult), reads=[Bps[pi]], writes=[buf("ff")], acc=True)
            tm_group(C_FF, 4, fdest, "f")

            for h in range(4):
                P.op("vector", lambda e, h=h: e.tensor_scalar(out=fa_s[:, :, h], in0=ff_s[:, :, h], scalar1=foxb[:, layer * 4 + h:layer * 4 + h + 1], scalar2=None, op0=ALU.add),
                     reads=[buf("ff"), buf("foxb")], writes=[buf("fa")])
            P.op("scalar", lambda e: e.activation(out=fa_s[:], in_=fa_s[:], func=AF.Exp, scale=-1.0), reads=[buf("fa")], writes=[buf("fa")])
            P.op("scalar", lambda e: e.activation(out=fa_s[:], in_=fa_s[:], func=AF.Ln, bias=onec[:, 0:1], scale=1.0), reads=[buf("fa"), buf("onec")], writes=[buf("fa")])
            P.op("vector", lambda e: e.tensor_scalar(out=fa_s[:], in0=fa_s[:], scalar1=-1.0, scalar2=None, op0=ALU.mult), reads=[buf("fa")], writes=[buf("fa")])
            P.dma("sync", lambda e: e.dma_start(out=fa_loc[:, :], in_=fa_s[:].rearrange("p a b -> p (a b)")), reads=[buf("fa")], writes=[buf("fa_loc")])

            for q in range(17):
                P.cc(lambda e, q=q: e.collective_compute("AllGather", ALU.bypass, replica_groups=groups, ins=[kT_loc[q].ap().opt()], outs=[kT_all[q].ap().opt()]),
                     reads=[buf("kT_loc")], writes=[buf("kT_all%d" % q)])
            for q in range(16):
                P.cc(lambda e, q=q: e.collective_compute("AllGather", ALU.bypass, replica_groups=groups, ins=[v_loc[q].ap().opt()], outs=[v_all[q].ap().opt()]),
                     reads=[buf("v_loc")], writes=[buf("v_all%d" % q)])
            P.cc(lambda e: e.collective_compute("AllGather", ALU.bypass, replica_groups=groups, ins=[fa_loc.ap().opt()], outs=[fa_all.ap().opt()]),
                 reads=[buf("fa_loc")], writes=[buf("fa_all")])

            P.dma("sync", lambda e: e.dma_start(out=fa4[:], in_=fa_all.ap().rearrange("(c p) f -> p c f", p=128)), reads=[buf("fa_all")], writes=[buf("fa4")])
            for c in range(4):
                for h in range(4):
                    src = fa4[:, c, :].rearrange("p (j u h) -> p j u h", j=4, u=4, h=4)[:, :, :, h]
                    dst = agf[:, h, :].rearrange("p (j c u) -> p j c u", j=4, c=4, u=4)[:, :, c, :]
                    P.op("vector", lambda e, src=src, dst=dst: e.tensor_copy(out=dst, in_=src), reads=[buf("fa4")], writes=[buf("agf")], acc=True)
            agf2 = agf[:].rearrange("p a b -> p (a b)")
            P.op("tensor", lambda e: e.matmul(ps[6][:, 0:256], lhsT=tri[:], rhs=agf2, start=True, stop=True), reads=[buf("agf"), buf("tri")], writes=[Bps[6]])
            P.op("tensor", lambda e: e.matmul(ps[7][:, 0:256], lhsT=onesf[:], rhs=agf2, start=True, stop=True), reads=[buf("agf"), buf("onesf")], writes=[Bps[7]])
            P.op("vector", lambda e: e.tensor_copy(out=totf[:].rearrange("p a b -> p (a b)"), in_=ps[7][:, 0:256]), reads=[Bps[7]], writes=[buf("totf")])
            for h in range(4):
                P.op("vector", lambda e, h=h: e.tensor_tensor_scan(out=inclf[:, h, :], data0=ones1[:, :], data1=totf[:, h, :], initial=0.0, op0=ALU.mult, op1=ALU.add),
                     reads=[buf("totf"), buf("ones1")], writes=[buf("inclf")])
            P.op("vector", lambda e: e.tensor_tensor(out=negcum[:], in0=totf[:], in1=inclf[:], op=ALU.subtract), reads=[buf("totf"), buf("inclf")], writes=[buf("negcum")])
            P.op("vector", lambda e: e.tensor_tensor(out=negcum[:].rearrange("p a b -> p (a b)"), in0=negcum[:].rearrange("p a b -> p (a b)"), in1=ps[6][:, 0:256], op=ALU.subtract),
                 reads=[buf("negcum"), Bps[6]], writes=[buf("negcum")])
            for h in range(4):
                for blk in range(16):
                    P.op("vector", lambda e, h=h, blk=blk: e.tensor_tensor(out=seltmp[:, blk, :], in0=sel_s[:, blk, :], in1=totf[:, h, :], op=ALU.mult),
                         reads=[buf("sel"), buf("totf")], writes=[buf("seltmp")], acc=True)
                P.op("vector", lambda e, h=h: e.tensor_reduce(out=cqref[:, h, :], in_=seltmp[:], axis=AX.X, op=ALU.add), reads=[buf("seltmp")], writes=[buf("cqref")])

            lam0 = 0.8 - 0.6 * math.exp(-0.3 * layer)
            lb = layer * 256
            P.op("vector", lambda e: e.tensor_tensor(out=tmpf[0][:, 0:64], in0=lq_s[:, lb:lb + 64], in1=lq_s[:, lb + 64:lb + 128], op=ALU.mult), reads=[buf("lq")], writes=[buf("tmpf0")])
            P.op("vector", lambda e: e.tensor_reduce(out=sm[:, 0:1], in_=tmpf[0][:, 0:64], axis=AX.X, op=ALU.add), reads=[buf("tmpf0")], writes=[buf("sm")])
            P.op("vector", lambda e: e.tensor_tensor(out=tmpf[0][:, 0:64], in0=lq_s[:, lb + 128:lb + 192], in1=lq_s[:, lb + 192:lb + 256], op=ALU.mult), reads=[buf("lq")], writes=[buf("tmpf0")])
            P.op("vector", lambda e: e.tensor_reduce(out=sm[:, 1:2], in_=tmpf[0][:, 0:64], axis=AX.X, op=ALU.add), reads=[buf("tmpf0")], writes=[buf("sm")])
            P.op("scalar", lambda e: e.activation(out=sm[:, 2:4], in_=sm[:, 0:2], func=AF.Exp), reads=[buf("sm")], writes=[buf("sm")])
            P.op("vector", lambda e: e.tensor_tensor(out=sm[:, 4:5], in0=sm[:, 3:4], in1=sm[:, 2:3], op=ALU.subtract), reads=[buf("sm")], writes=[buf("sm")])
            P.op("vector", lambda e: e.tensor_scalar(out=sm[:, 5:6], in0=sm[:, 4:5], scalar1=-lam0, scalar2=None, op0=ALU.add), reads=[buf("sm")], writes=[buf("sm")])
            P.op("vector", lambda e: e.tensor_scalar(out=sm[:, 6:7], in0=subln[:, layer:layer + 1], scalar1=1.0 - lam0, scalar2=None, op0=ALU.mult), reads=[buf("subln")], writes=[buf("sm")])

            P.op("vector", lambda e: e.memset(sm[:, 40:41], 0.0),
                 writes=[buf("kT_s"), buf("v_s"), buf("fence")] + [buf("yb%d" % i) for i in range(4)] + [buf("acc%d" % i) for i in range(8)])
            def load_kv(mx, hh):
                ch = mx * 4 + hh
                src = kT_all[ch].ap().rearrange("(c r) (j i) -> r j c i", c=4, i=512)
                P.dma("sync", lambda e: e.dma_start(out=kT_s[:].rearrange("p (j c i) -> p j c i", j=4, c=4, i=512), in_=src), reads=[buf("kT_all%d" % ch)], writes=[buf("kT_s")])
                col = mx * 512 + hh * 128
                for c in range(4):
                    for blk in range(16):
                        srcv = v_all[blk][c * 128:(c + 1) * 128, col:col + 128]
                        kb = 16 * (blk // 4) + 4 * c + (blk % 4)
                        dstv = v_s[:, kb, :]
                        P.dma("gpsimd" if (blk % 2) else "sync", lambda e, srcv=srcv, dstv=dstv: e.dma_start(out=dstv, in_=srcv), reads=[buf("v_all%d" % blk)], writes=[buf("v_s")], acc=True)
                P.dma("sync", lambda e: e.dma_start(out=qT_s[:], in_=qs_d[ch]), reads=[buf("qs_d")], writes=[buf("qT_s")])

            def load_G(kind, gi):
                if kind == "mask":
                    P.dma("sync", lambda e: e.dma_start(out=Gadd[:], in_=mneg_in), writes=[buf("Gadd")])
                    P.op("vector", lambda e: e.tensor_copy(out=G_s[:], in_=Gadd[:]), reads=[buf("Gadd")], writes=[buf("G_s")])
                    return
                P.dma("sync", lambda e: e.dma_start(out=G_s[:], in_=gt_in[gi]), writes=[buf("G_s")])
                P.dma("sync", lambda e: e.dma_start(out=Gadd[:], in_=(mneg_in if kind == "bias" else cdil_in)), writes=[buf("Gadd")])
                P.op("vector", lambda e: e.tensor_tensor(out=G_s[:], in0=G_s[:], in1=Gadd[:], op=ALU.add), reads=[buf("Gadd"), buf("G_s")], writes=[buf("G_s")])

            LB = (0, 1, 7)

            def flash(j, krows, kb_lo, mode, h, gi, dsa_A=None):
                kbs = list(range(kb_lo, 16 * j + 16))
                ob = 2 + 2 * nxt("oz", 2)
                zb = ob + 1
                pend = None

                def pvz(pi, kb, first, last):
                    bp = buf("pT%d" % pi)
                    P.op("tensor", lambda e: e.matmul(ps[ob][:, :], lhsT=v_s[:, kb, :], rhs=pT[pi][:], start=first, stop=last),
                         reads=[buf("v_s"), bp], writes=[Bps[ob]])
                    P.op("tensor", lambda e: e.matmul(ps[zb][:, :], lhsT=onesb[:], rhs=pT[pi][:], start=first, stop=last),
                         reads=[buf("onesb"), bp], writes=[Bps[zb]])
                for n, kb in enumerate(kbs):
                    ds_ = 2048 * j - 128 * kb
                    li = LB[nxt("psL", 3)]
                    Ls = ps[li]
                    P.op("tensor", lambda e, Ls=Ls, kb=kb: e.matmul(Ls[:, :], lhsT=kT_s[krows[0]:krows[1], kb * 128:(kb + 1) * 128], rhs=qT_s[krows[0]:krows[1], j * 512:(j + 1) * 512],
                                                                      start=True, stop=(dsa_A is None)),
                         reads=[buf("kT_s"), buf("qT_s")], writes=[Bps[li]])
                    if dsa_A is not None:
                        At, bA, off = dsa_A(kb)
                        for u in range(4):
                            P.op("tensor", lambda e, Ls=Ls, u=u, At=At, off=off: e.matmul(Ls[:, u * 128:(u + 1) * 128], lhsT=At[:, u, off:off + 128], rhs=ident[:], start=False, stop=True,
                                                                                          skip_group_check=True),
                                 reads=[bA, buf("ident")], writes=[Bps[li]])
                    src, bsrc = Ls, Bps[li]
                    need_add = (ds_ < 128) if mode == "fox" else (ds_ < DFAR)
                    if need_add:
                        ti = nxt("tmpf", 2)
                        u0 = min(ds_, DFAR) + GOFF
                        P.op("vector", lambda e, ti=ti, Ls=Ls, u0=u0: e.tensor_tensor(out=tmpf[ti][:], in0=Ls[:, :], in1=G_s[:, u0:u0 + 512], op=ALU.add),
                             reads=[Bps[li], buf("G_s")], writes=[buf("tmpf%d" % ti)])
                        src, bsrc = tmpf[ti], buf("tmpf%d" % ti)
                    pi = nxt("pT", 4)
                    bp = buf("pT%d" % pi)
                    if mode == "fox":
                        for u in range(4):
                            P.op("scalar", lambda e, pi=pi, src=src, u=u, kb=kb: e.activation(out=pT[pi][:, u * 128:(u + 1) * 128], in_=src[:, u * 128:(u + 1) * 128], func=AF.Exp,
                                                                                                bias=bm[:, 4 * j + u, kb:kb + 1], scale=1.0),
                                 reads=[bsrc, buf("bm")], writes=[bp], acc=(u > 0))
                    elif need_add:
                        P.op("scalar", lambda e, pi=pi, src=src: e.activation(out=pT[pi][:], in_=src[:, :], func=AF.Exp), reads=[bsrc], writes=[bp])
                    else:
                        P.op("scalar", lambda e, pi=pi, src=src: e.activation(out=pT[pi][:], in_=src[:, :], func=AF.Exp, bias=b31[:, gi:gi + 1], scale=1.0),
                             reads=[bsrc, buf("b31")], writes=[bp])
                    if pend is not None:
                        pvz(*pend)
                    pend = (pi, kb, n == 0, n == len(kbs) - 1)
                pvz(*pend)
                return ob, zb

            def normalize(dst, bdst, oz):
                ob, zb = oz
                P.op("vector", lambda e: e.reciprocal(out=ot[4][:], in_=ps[zb][:, :]), reads=[Bps[zb]], writes=[buf("ot4")])
                P.op("vector", lambda e: e.tensor_tensor(out=dst[:], in0=ps[ob][:, :], in1=ot[4][:], op=ALU.mult), reads=[Bps[ob], buf("ot4")], writes=[bdst])

            def store_o(src, bsrc, ch, j):
                P.dma("sync", lambda e: e.dma_start(out=ao_d[ch, :, j * 512:(j + 1) * 512], in_=src[:]), reads=[bsrc], writes=[buf("ao_d")], acc=True)

            load_G("mask", 0)
            for h in range(4):
                load_kv(1, h)
                for blk in range(16):
                    P.op("vector", lambda e, h=h, blk=blk: e.tensor_scalar(out=bm[:, blk, :], in0=negcum[:, h, :], scalar1=cqref[:, h, blk:blk + 1], scalar2=None, op0=ALU.add),
                         reads=[buf("negcum"), buf("cqref")], writes=[buf("bm")], acc=True)
                for j in range(4):
                    oz = flash(j, (0, 128), 0, "fox", h, 0)
                    normalize(ot[0], buf("ot0"), oz)
                    store_o(ot[0], buf("ot0"), 4 + h, j)
            for h in range(4):
                load_G("dil", 8 + h)
                load_kv(3, h)
                for j in range(4):
                    kb_lo = max(0, 16 * j - 16)
                    oz = flash(j, (0, 128), kb_lo, "dil", h, 8 + h)
                    normalize(ot[0], buf("ot0"), oz)
                    store_o(ot[0], buf("ot0"), 12 + h, j)
            for h in range(4):
                load_G("bias", 4 + h)
                load_kv(2, h)
                for j in range(4):
                    oz = flash(j, (0, 64), 0, "bias", h, 4 + h)
                    normalize(ot[0], buf("ot0"), oz)
                    oz = flash(j, (64, 128), 0, "bias", h, 4 + h)
                    normalize(ot[1], buf("ot1"), oz)
                    P.op("vector", lambda e: e.scalar_tensor_tensor(out=ot[2][:], in0=ot[1][:], scalar=sm[:, 5:6], in1=ot[0][:], op0=ALU.mult, op1=ALU.add),
                         reads=[buf("ot0"), buf("ot1"), buf("sm")], writes=[buf("ot2")])
                    si = nxt("stg", 4)
                    P.op("scalar", lambda e, si=si: e.activation(out=stg[si][:], in_=ot[2][:], func=AF.Square), reads=[buf("ot2")], writes=[buf("stg%d" % si)])
                    P.op("tensor", lambda e, si=si: e.matmul(ps[6][:, :], lhsT=onesb[:], rhs=stg[si][:], start=True, stop=True), reads=[buf("stg%d" % si), buf("onesb")], writes=[Bps[6]])
                    rstd_from_ssq(ps[6][:, :], ot[3], 128, 512, Bps[6], buf("ot3"))
                    P.op("vector", lambda e: e.scalar_tensor_tensor(out=ot[0][:], in0=ot[2][:], scalar=sm[:, 6:7], in1=ot[3][:], op0=ALU.mult, op1=ALU.mult),
                         reads=[buf("ot2"), buf("ot3"), buf("sm")], writes=[buf("ot0")])
                    store_o(ot[0], buf("ot0"), 8 + h, j)

            I_s = arena[:].rearrange("p a b -> p (a b)")[:, 0:16384].bitcast(F32)
            At_s = arena[:].rearrange("p a b -> p (a b)")[:, 16384:24576]
            ki_s = arena[:].rearrange("p a b -> p (a b)")[:, 24576:32768]
            BI = [Bar[0], Bar[1]]
            srck = kT_all[16].ap().rearrange("(c r) (j i) -> r j c i", c=4, i=512)
            P.dma("sync", lambda e: e.dma_start(out=ki_s.rearrange("p (j c i) -> p j c i", j=4, c=4, i=512), in_=srck), reads=[buf("kT_all16")], writes=[Bar[3]])
            for blk in range(16):
                j, u = blk // 4, blk % 4
                nkc = 4 * j + 4
                nk = nkc * 512
                qi = qi_s[blk % 2]
                bq = buf("qi%d" % (blk % 2))
                P.dma("sync", lambda e, qi=qi, blk=blk: e.dma_start(out=qi[:], in_=qs_d[16:24, :, blk * 128:(blk + 1) * 128].rearrange("m p t -> p m t")), reads=[buf("qs_d")], writes=[bq])
                for kc in range(nkc):
                    for ih in range(16):
                        pi = 6 + nxt("psA", 2)
                        r0 = (ih % 2) * 64
                        P.op("tensor", lambda e, pi=pi, ih=ih, r0=r0, kc=kc, qi=qi: e.matmul(ps[pi][:, :], lhsT=qi[r0:r0 + 64, ih // 2, :], rhs=ki_s[r0:r0 + 64, kc * 512:(kc + 1) * 512],
                                                                                              start=True, stop=True),
                             reads=[bq, Bar[3]], writes=[Bps[pi]])
                        ri = nxt("rbuf", 2)
                        P.op("scalar", lambda e, pi=pi, ri=ri: e.activation(out=rbuf[ri][:], in_=ps[pi][:, :], func=AF.Relu), reads=[Bps[pi]], writes=[buf("rbuf%d" % ri)])
                        if ih == 0:
                            P.op("vector", lambda e, ri=ri, kc=kc, blk=blk: e.tensor_scalar(out=I_s[:, kc * 512:(kc + 1) * 512], in0=rbuf[ri][:], scalar1=wI_s[:, blk, 0:1], scalar2=None, op0=ALU.mult),
                                 reads=[buf("rbuf%d" % ri), buf("wI")], writes=BI)
                        else:
                            P.op("vector", lambda e, ri=ri, kc=kc, blk=blk, ih=ih: e.scalar_tensor_tensor(out=I_s[:, kc * 512:(kc + 1) * 512], in0=rbuf[ri][:], scalar=wI_s[:, blk, ih:ih + 1],
                                                                                                          in1=I_s[:, kc * 512:(kc + 1) * 512], op0=ALU.mult, op1=ALU.add),
                                 reads=[buf("rbuf%d" % ri), buf("wI")] + BI, writes=BI)
                P.op("vector", lambda e, nk=nk: e.tensor_reduce(out=sm[:, 8:9], in_=I_s[:, 0:nk], axis=AX.X, op=ALU.min), reads=BI, writes=[buf("sm")])
                for kc in range(4 * j, nkc):
                    v0 = 512 * kc - 2048 * j - 128 * u + 384
                    P.op("vector", lambda e, kc=kc, v0=v0: e.tensor_tensor(out=I_s[:, kc * 512:(kc + 1) * 512], in0=I_s[:, kc * 512:(kc + 1) * 512], in1=mt_s[:, v0:v0 + 512], op=ALU.add),
                         reads=BI + [buf("mt")], writes=BI)
                P.op("vector", lambda e, nk=nk: e.tensor_reduce(out=sm[:, 9:10], in_=I_s[:, 0:nk], axis=AX.X, op=ALU.max), reads=BI, writes=[buf("sm")])
                P.op("vector", lambda e: e.tensor_scalar(out=sm[:, 10:11], in0=sm[:, 8:9], scalar1=-1.0, scalar2=None, op0=ALU.add), reads=[buf("sm")], writes=[buf("sm")])
                P.op("vector", lambda e: e.tensor_tensor(out=sm[:, 11:12], in0=sm[:, 9:10], in1=sm[:, 8:9], op=ALU.subtract), reads=[buf("sm")], writes=[buf("sm")])
                P.op("vector", lambda e: e.tensor_scalar(out=sm[:, 11:12], in0=sm[:, 11:12], scalar1=2.0, scalar2=None, op0=ALU.add), reads=[buf("sm")], writes=[buf("sm")])
                P.op("vector", lambda e: e.tensor_scalar(out=whalf[:], in0=pw2[:], scalar1=sm[:, 11:12], scalar2=None, op0=ALU.mult), reads=[buf("sm"), buf("pw2")], writes=[buf("whalf")])
                for it in range(NBIS):
                    P.op("vector", lambda e, it=it: e.tensor_tensor(out=sm[:, 12:13], in0=sm[:, 10:11], in1=whalf[:, it:it + 1], op=ALU.add), reads=[buf("sm"), buf("whalf")], writes=[buf("sm")])
                    P.op("vector", lambda e, nk=nk: e.tensor_scalar(out=At_s[:, 0:nk], in0=I_s[:, 0:nk], scalar1=sm[:, 12:13], scalar2=None, op0=ALU.is_ge, op1=ALU.add, accum_out=sm[:, 13:14]),
                         reads=BI + [buf("sm")], writes=[Bar[2], buf("sm")])
                    P.op("vector", lambda e, it=it: e.tensor_scalar(out=sm[:, 14:15], in0=sm[:, 13:14], scalar1=TOPK - 0.5, scalar2=whalf[:, it:it + 1], op0=ALU.is_ge, op1=ALU.mult),
                         reads=[buf("sm"), buf("whalf")], writes=[buf("sm")])
                    P.op("vector", lambda e: e.tensor_tensor(out=sm[:, 10:11], in0=sm[:, 10:11], in1=sm[:, 14:15], op=ALU.add), reads=[buf("sm")], writes=[buf("sm")])
                P.op("vector", lambda e, nk=nk: e.tensor_scalar(out=At_s[:, 0:nk], in0=I_s[:, 0:nk], scalar1=sm[:, 10:11], scalar2=NEG, op0=ALU.is_lt, op1=ALU.mult),
                     reads=BI + [buf("sm")], writes=[Bar[2]])
                P.dma("sync", lambda e, blk=blk, nk=nk: e.dma_start(out=A_d[blk, :, 0:nk], in_=At_s[:, 0:nk]), reads=[Bar[2]], writes=[buf("A_d")], acc=True)
            Apc = [arena[:].rearrange("p a b -> p (a b)")[:, i * 8192:(i + 1) * 8192].rearrange("p (u s) -> p u s", u=4) for i in range(2)]
            for h in range(4):
                load_G("bias", h)
                load_kv(0, h)
                for j in range(4):
                    state = {}

                    def dsa_A(kb, j=j, state=state):
                        pc = kb // 16
                        if state.get("pc") != pc:
                            ai = nxt("q", 2)
                            state["pc"], state["ai"] = pc, ai
                            for u in range(4):
                                P.dma("sync", lambda e, ai=ai, u=u, pc=pc: e.dma_start(out=Apc[ai][:, u, :], in_=A_d[4 * j + u, :, pc * 2048:(pc + 1) * 2048]), reads=[buf("A_d")], writes=[Bar[ai]], acc=True)
                        ai = state["ai"]
                        return Apc[ai], Bar[ai], (kb % 16) * 128
                    oz = flash(j, (0, 128), 0, "bias", h, h, dsa_A=dsa_A)
                    normalize(ot[0], buf("ot0"), oz)
                    store_o(ot[0], buf("ot0"), h, j)

            P.op("vector", lambda e: e.memset(sm[:, 40:41], 0.0),
                 writes=[buf("kT_s"), buf("v_s"), buf("fence")] + [buf("yb%d" % i) for i in range(4)] + [buf("acc%d" % i) for i in range(8)])
            Wbr, Wout = wf_br[layer], wf_out[layer]
            Bbr, Bout = "wf_br%d" % layer, "wf_out%d" % layer
            P.dma("sync", lambda e: e.dma_start(out=hT[:], in_=hT_d), reads=[buf("hT_d")], writes=Bar)
            ybufs = [kT_s[:, i * 2048:(i + 1) * 2048].rearrange("p (k t) -> p k t", k=4) for i in range(4)]
            accs = [v_s[:].rearrange("p a b -> p (a b)")[:, i * 1024:(i + 1) * 1024].bitcast(F32) for i in range(8)]
            mbuf = G_s[:].bitcast(BF16)[:, 0:8192].rearrange("p (k t) -> p k t", k=16)
            for zg in range(8):
                t, bw = load_w(Wl, Bwin, C_Z + zg * 256, 256)
                for q in range(2):
                    ch = zg * 2 + q
                    for j in range(4):
                        pi = 4 + nxt("psA", 2)
                        for k in range(16):
                            P.op("tensor", lambda e, pi=pi, k=k, q=q, t=t, j=j: e.matmul(ps[pi][:, :], lhsT=t[:, k, q * 128:(q + 1) * 128], rhs=hT[:, k, j * 512:(j + 1) * 512], start=(k == 0), stop=(k == 15)),
                                 reads=[bw] + Bar, writes=[Bps[pi]])
                        ti = nxt("tmpf", 2)
                        P.op("scalar", lambda e, pi=pi, ti=ti: e.activation(out=tmpf[ti][:], in_=ps[pi][:, :], func=AF.Silu), reads=[Bps[pi]], writes=[buf("tmpf%d" % ti)])
                        oi = nxt("rbuf", 2)
                        P.dma("sync", lambda e, oi=oi, ch=ch, j=j: e.dma_start(out=rbuf[oi][:], in_=ao_d[ch, :, j * 512:(j + 1) * 512]), reads=[buf("ao_d")], writes=[buf("rbuf%d" % oi)])
                        si = nxt("stg", 4)
                        P.op("vector", lambda e, ti=ti, oi=oi, si=si: e.tensor_tensor(out=stg[si][:], in0=tmpf[ti][:], in1=rbuf[oi][:], op=ALU.mult),
                             reads=[buf("tmpf%d" % ti), buf("rbuf%d" % oi)], writes=[buf("stg%d" % si)])
                        P.dma("sync", lambda e, si=si, ch=ch, j=j: e.dma_start(out=yT_d[ch, :, j * 512:(j + 1) * 512], in_=stg[si][:]), reads=[buf("stg%d" % si)], writes=[buf("yT_d")], acc=True)
            for ng in range(8):
                for b in range(4):
                    tg, bwg = load_w(Wl, Bwin, C_G + b * 2048 + ng * 256, 256)
                    i2 = nxt("wb", 2)
                    tb_, bwb = wb[i2], buf("wb%d" % i2)
                    srcb = Wbr[b * 512:(b + 1) * 512, ng * 256:(ng + 1) * 256].rearrange("(k p) n -> p k n", p=128)
                    P.dma("gpsimd", lambda e, tb_=tb_, srcb=srcb: e.dma_start(out=tb_[:, 0:4, :], in_=srcb), reads=[buf(Bbr)], writes=[bwb])
                    for j in range(4):
                        yi = nxt("yb", 4)
                        yb, byb = ybufs[yi], buf("yb%d" % yi)
                        P.dma("sync", lambda e, yb=yb, b=b, j=j: e.dma_start(out=yb, in_=yT_d[b * 4:(b + 1) * 4, :, j * 512:(j + 1) * 512].rearrange("k p t -> p k t")), reads=[buf("yT_d")], writes=[byb])
                        for q in range(2):
                            pg = 4 + nxt("psA", 2)
                            for k in range(16):
                                P.op("tensor", lambda e, pg=pg, k=k, q=q, tg=tg, j=j: e.matmul(ps[pg][:, :], lhsT=tg[:, k, q * 128:(q + 1) * 128], rhs=hT[:, k, j * 512:(j + 1) * 512], start=(k == 0), stop=(k == 15)),
                                     reads=[bwg] + Bar, writes=[Bps[pg]])
                            pu = 6 + nxt("psU", 2)
                            for k in range(4):
                                P.op("tensor", lambda e, pu=pu, k=k, q=q, tb_=tb_, yb=yb: e.matmul(ps[pu][:, :], lhsT=tb_[:, k, q * 128:(q + 1) * 128], rhs=yb[:, k, :], start=(k == 0), stop=(k == 3)),
                                     reads=[bwb, byb], writes=[Bps[pu]])
                            ti = nxt("tmpf", 2)
                            P.op("scalar", lambda e, pg=pg, ti=ti: e.activation(out=tmpf[ti][:], in_=ps[pg][:, :], func=AF.Sigmoid), reads=[Bps[pg]], writes=[buf("tmpf%d" % ti)])
                            ai = q * 4 + j
                            acc, bacc = accs[ai], buf("acc%d" % ai)
                            if b == 0:
                                P.op("vector", lambda e, ti=ti, pu=pu, acc=acc: e.tensor_tensor(out=acc, in0=ps[pu][:, :], in1=tmpf[ti][:], op=ALU.mult),
                                     reads=[Bps[pu], buf("tmpf%d" % ti)], writes=[bacc])
                            else:
                                P.op("vector", lambda e, ti=ti, pu=pu: e.tensor_tensor(out=tmpf[ti][:], in0=ps[pu][:, :], in1=tmpf[ti][:], op=ALU.mult),
                                     reads=[Bps[pu], buf("tmpf%d" % ti)], writes=[buf("tmpf%d" % ti)])
                                P.op("gpsimd", lambda e, ti=ti, acc=acc: e.tensor_tensor(out=acc, in0=acc, in1=tmpf[ti][:], op=ALU.add),
                                     reads=[buf("tmpf%d" % ti), bacc], writes=[bacc])
                            if b == 3:
                                n = ng * 2 + q
                                si = nxt("stg", 4)
                                P.op("scalar", lambda e, acc=acc, si=si: e.activation(out=stg[si][:], in_=acc, func=AF.Copy), reads=[bacc], writes=[buf("stg%d" % si)])
                                P.dma("sync", lambda e, si=si, n=n, j=j: e.dma_start(out=mT_d[n, :, j * 512:(j + 1) * 512], in_=stg[si][:]), reads=[buf("stg%d" % si)], writes=[buf("mT_d")], acc=True)
            for j in range(4):
                P.dma("sync", lambda e, j=j: e.dma_start(out=mbuf, in_=mT_d[:, :, j * 512:(j + 1) * 512].rearrange("k p t -> p k t")), reads=[buf("mT_d")], writes=[buf("G_s")])
                for ng in range(8):
                    to, bwo = load_w(Wout, Bout, ng * 256, 256)
                    for q in range(2):
                        n = ng * 2 + q
                        po = 4 + nxt("psA", 2)
                        for k in range(16):
                            P.op("tensor", lambda e, po=po, k=k, q=q, to=to: e.matmul(ps[po][:, :], lhsT=to[:, k, q * 128:(q + 1) * 128], rhs=mbuf[:, k, :], start=(k == 0), stop=(k == 15)),
                                 reads=[bwo, buf("G_s")], writes=[Bps[po]])
                        oi = nxt("rbuf", 2)
                        P.dma("sync", lambda e, oi=oi, n=n, j=j: e.dma_start(out=rbuf[oi][:], in_=xsrc[n * 128:(n + 1) * 128, j * 512:(j + 1) * 512]), reads=[Bx], writes=[buf("rbuf%d" % oi)])
                        P.op("vector", lambda e, oi=oi, po=po: e.tensor_tensor(out=rbuf[oi][:], in0=ps[po][:, :], in1=rbuf[oi][:], op=ALU.add),
                             reads=[Bps[po], buf("rbuf%d" % oi)], writes=[buf("rbuf%d" % oi)])
                        P.dma("sync", lambda e, oi=oi, n=n, j=j: e.dma_start(out=xs_d[n * 128:(n + 1) * 128, j * 512:(j + 1) * 512], in_=rbuf[oi][:]), reads=[buf("rbuf%d" % oi)], writes=[Bxo], acc=True)

        for layer_ in range(L):
            do_layer(layer_)

        xs_d = xs_dd[(L - 1) % 2]
        Bx = buf("xs_d%d" % ((L - 1) % 2))
        Bout_ = buf("yT")
        for j in range(4):
            for k in range(16):
                o = ot[k % 2]
                bo = buf("ot%d" % (k % 2))
                P.dma("sync", lambda e, k=k, j=j, o=o: e.dma_start(out=o[:], in_=xs_d[k * 128:(k + 1) * 128, j * 512:(j + 1) * 512]), reads=[Bx], writes=[bo])
                s_ = stg[k % 2]
                bs_ = buf("stg%d" % (k % 2))
                P.op("scalar", lambda e, o=o, s_=s_: e.activation(out=s_[:], in_=o[:], func=AF.Square), reads=[bo], writes=[bs_])
                P.op("tensor", lambda e, s_=s_, k=k: e.matmul(ps[6][:, :], lhsT=onesb[:], rhs=s_[:], start=(k == 0), stop=(k == 15)), reads=[bs_, buf("onesb")], writes=[Bps[6]])
            rstd_from_ssq(ps[6][:, :], ot[4], D, 512, Bps[6], buf("ot4"))
            for k in range(16):
                o = ot[k % 2]
                bo = buf("ot%d" % (k % 2))
                P.dma("sync", lambda e, k=k, j=j, o=o: e.dma_start(out=o[:], in_=xs_d[k * 128:(k + 1) * 128, j * 512:(j + 1) * 512]), reads=[Bx], writes=[bo])
                ri = nxt("rbuf", 2)
                P.op("vector", lambda e, k=k, o=o, ri=ri: e.scalar_tensor_tensor(out=rbuf[ri][:], in0=o[:], scalar=fnormw[:, k:k + 1], in1=ot[4][:], op0=ALU.mult, op1=ALU.mult),
                     reads=[bo, buf("ot4"), buf("fnormw")], writes=[buf("rbuf%d" % ri)])
                P.dma("sync", lambda e, k=k, j=j, ri=ri: e.dma_start(out=yT_out[k * 128:(k + 1) * 128, j * 512:(j + 1) * 512], in_=rbuf[ri][:]), reads=[buf("rbuf%d" % ri)], writes=[Bout_], acc=True)
        P.finish("sync", [Bout_])
        P.emit(block, sems)
    print("ops", P.nops)
    return nc


def host_prep(inputs, depth=DEPTH):
    x = np.asarray(inputs["x"], np.float32)
    L = depth
    rel = np.asarray(inputs["rel_bias"], np.float32)
    maps = []
    sl = np.arange(128)[:, None]
    uu = np.arange(GL)[None, :]
    import ml_dtypes
    bf = ml_dtypes.bfloat16
    ident = np.eye(128, dtype=np.float32).astype(bf)
    tri = (np.arange(128)[:, None] <= np.arange(128)[None, :]).astype(np.float32)
    normw = np.ascontiguousarray(np.asarray(inputs["norm_w"], np.float32)[:L].reshape(L, 16, 128).transpose(2, 0, 1).reshape(128, L * 16))
    fnormw = np.ascontiguousarray(np.asarray(inputs["final_norm_w"], np.float32).reshape(16, 128).T)
    foxb = np.ascontiguousarray(np.broadcast_to(np.asarray(inputs["fox_b_f"], np.float32)[:L].reshape(1, L * 4), (128, L * 4)))
    lq = np.stack([np.asarray(inputs[k], np.float32)[:L] for k in ("diff_lq1", "diff_lk1", "diff_lq2", "diff_lk2")], axis=1)
    lq = np.ascontiguousarray(np.broadcast_to(lq.reshape(1, L * 256), (128, L * 256)))
    subln = np.ascontiguousarray(np.asarray(inputs["diff_subln_w"], np.float32)[:L].T)
    b31 = np.ascontiguousarray(np.broadcast_to(rel[31:32, :], (128, 12)))
    w_in = np.asarray(inputs["w_in"], np.float32)
    w_br = np.asarray(inputs["w_branch"], np.float32).reshape(DEPTH, 2048, 2048)
    w_out = np.asarray(inputs["w_out"], np.float32)
    for core in range(8):
        b, c = core // 4, core % 4
        toks = np.concatenate([np.arange(512 * (4 * j + c), 512 * (4 * j + c + 1)) for j in range(4)])
        xT = np.ascontiguousarray(x[b, toks, :].T)
        dist = uu - GOFF + 512 * c - sl
        bidx = t5_bucket_np(dist)
        gt = np.ascontiguousarray(rel[bidx, :].transpose(2, 0, 1))
        mneg = np.where(dist >= 0, 0.0, NEG).astype(np.float32).astype(bf)
        nval = ((dist >= 0) & (dist <= 128)).astype(np.int32) + ((dist >= 0) & (dist % 4 == 0) & (dist <= 512)).astype(np.int32) \
            + ((dist >= 0) & (dist % 16 == 0) & (dist <= 2048)).astype(np.int32)
        cdil = np.where(nval > 0, np.log(np.maximum(nval, 1).astype(np.float32)), NEG).astype(np.float32).astype(bf)
        vv = np.arange(2432)[None, :]
        mt = np.where((vv - 384 - 512 * c - sl) > 0, -1e9, 0.0).astype(np.float32).astype(bf)
        blk = np.arange(16)
        kb_own = 16 * (blk // 4) + 4 * c + (blk % 4)
        sel = (np.arange(64)[None, :] <= kb_own[:, None]).astype(np.float32)
        sel = np.ascontiguousarray(np.broadcast_to(sel.reshape(1, 1024), (128, 1024)))
        maps.append({
            "xT": xT,
            "w_in": w_in[:L], "w_br": w_br[:L], "w_out": w_out[:L],
            "normw": normw, "fnormw": fnormw, "foxb": foxb, "lq": lq, "subln": subln,
            "gt": gt, "mneg": mneg, "cdil": cdil, "b31": b31, "mt": mt, "sel": sel, "ident": ident, "tri": tri,
        })
    return maps


def run(inputs, depth=DEPTH):
    nc = build(depth)
    maps = host_prep(inputs, depth)
    res = run_bass_kernel_spmd(nc, maps, core_ids=list(range(8)))
    out = np.zeros((2, S, D), np.float32)
    for core in range(8):
        b, c = core // 4, core % 4
        toks = np.concatenate([np.arange(512 * (4 * j + c), 512 * (4 * j + c + 1)) for j in range(4)])
        out[b, toks, :] = res.results[core]["yT"].T
    return out


def kernel(**inputs):
    return run(inputs, DEPTH)
```

```python
import math
from contextlib import ExitStack

import numpy as np
import concourse.bass as bass
import concourse.mybir as mybir
from concourse.bass_utils import run_bass_kernel_spmd

F32 = mybir.dt.float32
BF16 = mybir.dt.bfloat16
ALU = mybir.AluOpType
AF = mybir.ActivationFunctionType
AX = mybir.AxisListType

D = 2048
S = 8192
NT = 2048
DEPTH = 4
N_IN = 17492
NEG = -30000.0
GL = 4608
GOFF = 1920
DFAR = 2176
C_Z, C_G = 0, 2048
C_DSA, C_IQ, C_IK, C_IW, C_FOX, C_FF, C_DIFF, C_DIL = 10240, 11776, 12800, 12864, 12880, 14416, 14420, 15956
S128 = 128 ** -0.5
S64 = 64 ** -0.5
TOPK = 256
NBIS = 18


class Buf:
    __slots__ = ("w", "r")

    def __init__(self):
        self.w = {}
        self.r = {}


class Prog:
    ENG = ("sync", "scalar", "vector", "gpsimd", "tensor")

    def __init__(self, ndma=24):
        self.streams = {e: [] for e in self.ENG}
        self.count = {e: 0 for e in self.ENG}
        self.seen = {e: {} for e in self.ENG}
        self.ndma = ndma
        self.dma_i = 0
        self.dma_cnt = [0] * ndma
        self.cc_cnt = 0
        self.nops = 0

    def _deps(self, eng, reads, writes, acc=False):
        deps = {}

        def add(k, v):
            if deps.get(k, 0) < v:
                deps[k] = v
        for b in reads:
            for k, v in b.w.items():
                add(k, v)
        for b in writes:
            if not acc:
                for k, v in b.w.items():
                    add(k, v)
            for k, v in b.r.items():
                add(k, v)
        waits = []
        for k, v in deps.items():
            if k == eng and eng == "tensor":
                continue
            if self.seen[eng].get(k, 0) < v:
                self.seen[eng][k] = v
                waits.append((k, v))
        return waits

    def _record(self, me, reads, writes, acc=False):
        for b in reads:
            if b.r.get(me[0], 0) < me[1]:
                b.r[me[0]] = me[1]
        for b in writes:
            if acc:
                b.w[me[0]] = me[1]
            else:
                b.w = {me[0]: me[1]}
                b.r = {}
        self.nops += 1

    def op(self, eng, fn, reads=(), writes=(), acc=False):
        waits = self._deps(eng, reads, writes, acc)
        self.count[eng] += 1
        me = (eng, self.count[eng])
        self.streams[eng].append((waits, fn, eng, 1))
        self._record(me, reads, writes, acc)

    def dma(self, eng, fn, reads=(), writes=(), acc=False):
        i = self.dma_i % self.ndma
        self.dma_i += 1
        key = "dma%d" % i
        waits = self._deps(eng, reads, writes, acc)
        prev = self.dma_cnt[i]
        if prev and self.seen[eng].get(key, 0) < prev:
            self.seen[eng][key] = prev
            waits.append((key, prev))
        self.dma_cnt[i] += 16
        me = (key, self.dma_cnt[i])
        self.streams[eng].append((waits, fn, key, 16))
        self._record(me, reads, writes, acc)

    def cc(self, fn, reads=(), writes=()):
        eng = "gpsimd"
        waits = self._deps(eng, reads, writes)
        self.cc_cnt += 1
        me = ("cc", self.cc_cnt)
        self.streams[eng].append((waits, fn, "cc", None))
        self._record(me, reads, writes)

    def finish(self, eng, bufs):
        waits = self._deps(eng, bufs, ())
        self.streams[eng].append((waits, None, None, 0))

    def emit(self, block, sems):
        def mk(e):
            def body(engh):
                for waits, fn, key, inc in self.streams[e]:
                    for k, v in waits:
                        engh.wait_ge(sems[k], v)
                    if fn is not None:
                        ins = fn(engh)
                        if inc is None:
                            ins.then_inc(sems[key])
                        else:
                            ins.then_inc(sems[key], inc)
            return body
        block.sync(mk("sync"))
        block.scalar(mk("scalar"))
        block.vector(mk("vector"))
        block.gpsimd(mk("gpsimd"))
        block.tensor(mk("tensor"))


def t5_bucket_np(dist):
    dist = np.maximum(dist, 0)
    d = np.maximum(dist, 16).astype(np.float32)
    large = 16 + (np.log(d / np.float32(16)) / np.float32(math.log(2048 / 16)) * np.float32(16)).astype(np.int32)
    large = np.minimum(large, 31)
    return np.where(dist < 16, dist, large)


def build(depth=DEPTH):
    nc = bass.Bass("TRN2", target_bir_lowering=False)
    P = Prog()
    L = depth

    def din(name, shape, dt=F32):
        return nc.dram_tensor(name, list(shape), dt, kind="ExternalInput").ap()

    def dscr(name, shape, dt):
        return nc.dram_tensor(name, list(shape), dt)

    xT_in = din("xT", [D, NT])
    w_in_a = din("w_in", [L, D, N_IN])
    w_br_a = din("w_br", [L, D, D])
    w_out_a = din("w_out", [L, D, D])
    normw_in = din("normw", [128, L * 16])
    fnormw_in = din("fnormw", [128, 16])
    foxb_in = din("foxb", [128, L * 4])
    lq_in = din("lq", [128, L * 4 * 64])
    subln_in = din("subln", [128, L])
    gt_in = din("gt", [12, 128, GL])
    mneg_in = din("mneg", [128, GL], BF16)
    cdil_in = din("cdil", [128, GL], BF16)
    b31_in = din("b31", [128, 12])
    mt_in = din("mt", [128, 2432], BF16)
    sel_in = din("sel", [128, 16 * 64])
    ident_in = din("ident", [128, 128], BF16)
    tri_in = din("tri", [128, 128])
    yT_out = nc.dram_tensor("yT", [D, NT], F32, kind="ExternalOutput").ap()

    wf_in = [w_in_a[l] for l in range(L)]
    wf_br = [w_br_a[l] for l in range(L)]
    wf_out = [w_out_a[l] for l in range(L)]
    xs_dd = [dscr("xs_d%d" % i, [D, NT], F32).ap() for i in range(2)]
    hT_d = dscr("hT_d", [128, 16, NT], BF16).ap()
    qs_d = dscr("qs_d", [24, 128, NT], BF16).ap()
    yT_d = dscr("yT_d", [16, 128, NT], BF16).ap()
    mT_d = dscr("mT_d", [16, 128, NT], BF16).ap()
    kT_loc = [dscr("kT_loc%d" % q, [128, NT], BF16) for q in range(17)]
    kT_all = [dscr("kT_all%d" % q, [512, NT], BF16) for q in range(17)]
    v_loc = [dscr("v_loc%d" % q, [128, 2048], BF16) for q in range(16)]
    v_all = [dscr("v_all%d" % q, [512, 2048], BF16) for q in range(16)]
    fa_loc = dscr("fa_loc", [128, 64], F32)
    fa_all = dscr("fa_all", [512, 64], F32)
    ao_d = dscr("ao_d", [16, 128, NT], F32).ap()
    A_d = dscr("A_d", [16, 128, S], BF16).ap()

    B = {}

    def buf(name):
        if name not in B:
            B[name] = Buf()
        return B[name]

    groups = [[0, 1, 2, 3], [4, 5, 6, 7]]
    es = ExitStack()
    with es:
        def sb(name, shape, dt):
            return es.enter_context(nc.sbuf_tensor("sb_" + name, list(shape), dt))

        arena = sb("arena", [128, 16, NT], BF16)
        kT_s = sb("kT_s", [128, S], BF16)
        v_s = sb("v_s", [128, 64, 128], BF16)
        wb = [sb("wb%d" % i, [128, 16, 256], BF16) for i in range(2)]
        G_s = sb("G_s", [128, GL], F32)
        Gadd = sb("Gadd", [128, GL], BF16)
        qT_s = sb("qT_s", [128, NT], BF16)
        pT = [sb("pT%d" % i, [128, 512], BF16) for i in range(4)]
        tmpf = [sb("tmpf%d" % i, [128, 512], F32) for i in range(2)]
        rbuf = [sb("rbuf%d" % i, [128, 512], F32) for i in range(2)]
        ot = [sb("ot%d" % i, [128, 512], F32) for i in range(5)]
        stg = [sb("stg%d" % i, [128, 512], BF16) for i in range(4)]
        bm = sb("bm", [128, 16, 64], F32)
        negcum = sb("negcum", [128, 4, 64], F32)
        totf = sb("totf", [128, 4, 64], F32)
        inclf = sb("inclf", [128, 4, 64], F32)
        agf = sb("agf", [128, 4, 64], F32)
        fa4 = sb("fa4", [128, 4, 64], F32)
        cqref = sb("cqref", [128, 4, 16], F32)
        seltmp = sb("seltmp", [128, 16, 64], F32)
        sel_s = sb("sel_s", [128, 16, 64], F32)
        ones1 = sb("ones1", [128, 64], F32)
        ff_s = sb("ff_s", [128, 16, 4], F32)
        fa_s = sb("fa_s", [128, 16, 4], F32)
        wI_s = sb("wI_s", [128, 16, 16], F32)
        qi_s = [sb("qi_s%d" % i, [128, 8, 128], BF16) for i in range(2)]
        ident = sb("ident", [128, 128], BF16)
        onesb = sb("onesb", [128, 128], BF16)
        tri = sb("tri", [128, 128], F32)
        onesf = sb("onesf", [128, 128], F32)
        mt_s = sb("mt_s", [128, 2432], BF16)
        normw = sb("normw", [128, L * 16], F32)
        fnormw = sb("fnormw", [128, 16], F32)
        foxb = sb("foxb", [128, L * 4], F32)
        lq_s = sb("lq_s", [128, L * 4 * 64], F32)
        subln = sb("subln", [128, L], F32)
        b31 = sb("b31", [128, 12], F32)
        sm = sb("sm", [128, 64], F32)
        whalf = sb("whalf", [128, NBIS], F32)
        pw2 = sb("pw2", [128, NBIS], F32)
        epsc = sb("epsc", [128, 1], F32)
        onec = sb("onec", [128, 1], F32)

        ps = [es.enter_context(nc.psum_tensor("ps%d" % i, [128, 512], F32)) for i in range(8)]
        sems = {e: es.enter_context(nc.semaphore("s_" + e)) for e in Prog.ENG}
        for i in range(P.ndma):
            sems["dma%d" % i] = es.enter_context(nc.semaphore("s_dma%d" % i))
        sems["cc"] = es.enter_context(nc.semaphore("s_cc"))
        block = es.enter_context(nc.Block())
        print("sbuf bytes remaining", nc.sbuf_bytes_remaining)

        Bps = [buf("ps%d" % i) for i in range(8)]
        rr = {"stg": 0, "pT": 0, "tmpf": 0, "rbuf": 0, "psL": 0, "psA": 0, "wb": 0, "q": 0, "ev": 0, "oz": 0, "psU": 0, "yb": 0}

        def nxt(k, n):
            v = rr[k] % n
            rr[k] += 1
            return v

        def ld(eng, dst, src, name):
            P.dma(eng, lambda e: e.dma_start(out=dst, in_=src), writes=[buf(name)])
        ld("sync", normw[:], normw_in, "normw")
        ld("sync", fnormw[:], fnormw_in, "fnormw")
        ld("sync", foxb[:], foxb_in, "foxb")
        ld("sync", lq_s[:], lq_in, "lq")
        ld("sync", subln[:], subln_in, "subln")
        ld("sync", b31[:], b31_in, "b31")
        ld("sync", mt_s[:], mt_in, "mt")
        ld("sync", sel_s[:].rearrange("p a b -> p (a b)"), sel_in, "sel")
        ld("sync", ident[:], ident_in, "ident")
        ld("sync", tri[:], tri_in, "tri")
        P.op("vector", lambda e: e.memset(onesb[:], 1.0), writes=[buf("onesb")])
        P.op("vector", lambda e: e.memset(onesf[:], 1.0), writes=[buf("onesf")])
        P.op("vector", lambda e: e.memset(ones1[:], 1.0), writes=[buf("ones1")])
        P.op("vector", lambda e: e.memset(epsc[:], 1e-6), writes=[buf("epsc")])
        P.op("vector", lambda e: e.memset(onec[:], 1.0), writes=[buf("onec")])
        for i in range(NBIS):
            P.op("vector", lambda e, i=i: e.memset(pw2[:, i:i + 1], 2.0 ** -(i + 1)), writes=[buf("pw2")])

        def load_w(wfull, bname, col0, ncols, dup=False):
            i = nxt("wb", 2)
            t = wb[i]
            bw = buf("wb%d" % i)
            src = wfull[:, col0:col0 + ncols].rearrange("(k p) n -> p k n", p=128)
            P.dma("gpsimd", lambda e: e.dma_start(out=t[:, :, 0:ncols], in_=src), reads=[buf(bname)], writes=[bw])
            if dup:
                P.dma("gpsimd", lambda e: e.dma_start(out=t[:, :, ncols:2 * ncols], in_=src), reads=[buf(bname)], writes=[bw])
            return t, bw

        hT = arena
        Bar = [buf("arena%d" % j) for j in range(4)]

        def rstd_from_ssq(ps_ap, dst, n, width, bps, bdst):
            P.op("scalar", lambda e: e.activation(out=dst[:, :width], in_=ps_ap, func=AF.Ln, bias=epsc[:, 0:1], scale=1.0 / n),
                 reads=[bps, buf("epsc")], writes=[bdst])
            P.op("scalar", lambda e: e.activation(out=dst[:, :width], in_=dst[:, :width], func=AF.Exp, scale=-0.5),
                 reads=[bdst], writes=[bdst])

        def evac(dst_ap, src_ap, scale, reads, writes):
            i = nxt("ev", 2)
            if i == 0:
                P.op("scalar", lambda e: e.mul(dst_ap, src_ap, float(scale)), reads=reads, writes=writes)
            else:
                P.op("vector", lambda e: e.tensor_scalar(out=dst_ap, in0=src_ap, scalar1=float(scale), scalar2=None, op0=ALU.mult), reads=reads, writes=writes)

        def do_layer(layer):
            Bwin = "wf_in%d" % layer
            Wl = wf_in[layer]
            xsrc = xT_in if layer == 0 else xs_dd[(layer - 1) % 2]
            xs_d = xs_dd[layer % 2]
            Bx = buf("xin") if layer == 0 else buf("xs_d%d" % ((layer - 1) % 2))
            Bxo = buf("xs_d%d" % (layer % 2))
            for j in range(4):
                xt = arena
                pss = ps[6]
                for k in range(16):
                    o = ot[k % 2]
                    bo = buf("ot%d" % (k % 2))
                    P.dma("sync", lambda e, k=k, j=j, o=o: e.dma_start(out=o[:], in_=xsrc[k * 128:(k + 1) * 128, j * 512:(j + 1) * 512]), reads=[Bx], writes=[bo])
                    s_ = stg[k % 2]
                    bs_ = buf("stg%d" % (k % 2))
                    P.op("scalar", lambda e, o=o, s_=s_: e.activation(out=s_[:], in_=o[:], func=AF.Square), reads=[bo], writes=[bs_])
                    P.op("tensor", lambda e, s_=s_, k=k: e.matmul(pss[:, :], lhsT=onesb[:], rhs=s_[:], start=(k == 0), stop=(k == 15)),
                         reads=[bs_, buf("onesb")], writes=[Bps[6]])
                rstd_from_ssq(pss[:, :], ot[4], D, 512, Bps[6], buf("ot4"))
                for k in range(16):
                    o = ot[k % 2]
                    bo = buf("ot%d" % (k % 2))
                    P.dma("sync", lambda e, k=k, j=j, o=o: e.dma_start(out=o[:], in_=xsrc[k * 128:(k + 1) * 128, j * 512:(j + 1) * 512]), reads=[Bx], writes=[bo])
                    P.op("vector", lambda e, k=k, j=j, o=o: e.scalar_tensor_tensor(out=hT[:, k, j * 512:(j + 1) * 512], in0=o[:], scalar=normw[:, layer * 16 + k:layer * 16 + k + 1],
                                                                                   in1=ot[4][:], op0=ALU.mult, op1=ALU.mult),
                         reads=[bo, buf("ot4"), buf("normw")], writes=Bar, acc=True)
            P.dma("sync", lambda e: e.dma_start(out=hT_d, in_=hT[:]), reads=Bar, writes=[buf("hT_d")])

            def fm_group(col0, ncols, dest_fn, scale, dup=False):
                t, bw = load_w(Wl, Bwin, col0, ncols, dup=dup)
                nc_eff = 2 * ncols if dup else ncols
                for q in range((nc_eff + 127) // 128):
                    m = min(128, nc_eff - q * 128)
                    for j in range(4):
                        pi = 4 + nxt("psA", 2)
                        for k in range(16):
                            P.op("tensor", lambda e, pi=pi, k=k, q=q, j=j, m=m: e.matmul(ps[pi][:m, :], lhsT=t[:, k, q * 128:q * 128 + m], rhs=hT[:, k, j * 512:(j + 1) * 512],
                                                                                        start=(k == 0), stop=(k == 15)),
                                 reads=[bw] + Bar, writes=[Bps[pi]])
                        si = nxt("stg", 4)
                        evac(stg[si][:m, :], ps[pi][:m, :], scale, [Bps[pi]], [buf("stg%d" % si)])
                        dst, bd = dest_fn(q, j, m)
                        P.dma("sync", lambda e, dst=dst, si=si, m=m: e.dma_start(out=dst, in_=stg[si][:m, :]), reads=[buf("stg%d" % si)], writes=[bd], acc=True)

            def qdest(base):
                return lambda q, j, m: (qs_d[base + q, 0:m, j * 512:(j + 1) * 512], buf("qs_d"))

            def kdest(base):
                return lambda q, j, m: (kT_loc[(base + q) * 128:(base + q) * 128 + m, j * 512:(j + 1) * 512], buf("kT_loc"))

            def tm_group(col0, ncols, dest_fn, kind):
                t, bw = load_w(Wl, Bwin, col0, ncols)
                for blk in range(16):
                    pi = 4 + nxt("psA", 2)
                    for k in range(16):
                        P.op("tensor", lambda e, pi=pi, k=k, blk=blk: e.matmul(ps[pi][:, 0:ncols], lhsT=hT[:, k, blk * 128:(blk + 1) * 128], rhs=t[:, k, 0:ncols],
                                                                                start=(k == 0), stop=(k == 15)),
                             reads=[bw] + Bar, writes=[Bps[pi]])
                    dest_fn(blk, pi)

            for (mx, c0, s_q) in ((0, C_DSA, S128), (1, C_FOX, S128), (2, C_DIFF, S64), (3, C_DIL, S128)):
                for hh in range(2):
                    fm_group(c0 + hh * 256, 256, (lambda q, j, m, mx=mx, hh=hh: (qs_d[mx * 4 + hh * 2 + q, 0:m, j * 512:(j + 1) * 512], buf("qs_d"))), s_q)
                for hh in range(2):
                    fm_group(c0 + 512 + hh * 256, 256, (lambda q, j, m, mx=mx, hh=hh: (kT_loc[mx * 4 + hh * 2 + q][0:m, j * 512:(j + 1) * 512], buf("kT_loc"))), 1.0)
                for hh in range(2):
                    def vdest(blk, pi, mx=mx, hh=hh):
                        si = nxt("stg", 4)
                        evac(stg[si][:, 0:256], ps[pi][:, 0:256], 1.0, [Bps[pi]], [buf("stg%d" % si)])
                        P.dma("sync", lambda e, si=si: e.dma_start(out=v_loc[blk][:, mx * 512 + hh * 256:mx * 512 + hh * 256 + 256], in_=stg[si][:, 0:256]),
                              reads=[buf("stg%d" % si)], writes=[buf("v_loc")], acc=True)
                    tm_group(c0 + 1024 + hh * 256, 256, vdest, "v")
            for hh in range(4):
                fm_group(C_IQ + hh * 256, 256, (lambda q, j, m, hh=hh: (qs_d[16 + hh * 2 + q, 0:m, j * 512:(j + 1) * 512], buf("qs_d"))), 1.0)
            fm_group(C_IK, 64, (lambda q, j, m: (kT_loc[16][0:m, j * 512:(j + 1) * 512], buf("kT_loc"))), 1.0, dup=True)

            def wdest(blk, pi):
                P.op("vector", lambda e: e.tensor_copy(out=wI_s[:, blk, :], in_=ps[pi][:, 0:16]), reads=[Bps[pi]], writes=[buf("wI")], acc=True)
            tm_group(C_IW, 16, wdest, "w")

            def fdest(blk, pi):
                P.op("vector", lambda e: e.tensor_scalar(out=ff_s[:, blk, :], in0=ps[pi][:, 0:4], scalar1=1.0, scalar2=None, op0=ALU.mult), reads=[Bps[pi]], writes=[buf("ff")], acc=True)
            tm_group(C_FF, 4, fdest, "f")

            for h in range(4):
                P.op("vector", lambda e, h=h: e.tensor_scalar(out=fa_s[:, :, h], in0=ff_s[:, :, h], scalar1=foxb[:, layer * 4 + h:layer * 4 + h + 1], scalar2=None, op0=ALU.add),
                     reads=[buf("ff"), buf("foxb")], writes=[buf("fa")])
            P.op("scalar", lambda e: e.activation(out=fa_s[:], in_=fa_s[:], func=AF.Exp, scale=-1.0), reads=[buf("fa")], writes=[buf("fa")])
            P.op("scalar", lambda e: e.activation(out=fa_s[:], in_=fa_s[:], func=AF.Ln, bias=onec[:, 0:1], scale=1.0), reads=[buf("fa"), buf("onec")], writes=[buf("fa")])
            P.op("vector", lambda e: e.tensor_scalar(out=fa_s[:], in0=fa_s[:], scalar1=-1.0, scalar2=None, op0=ALU.mult), reads=[buf("fa")], writes=[buf("fa")])
            P.dma("sync", lambda e: e.dma_start(out=fa_loc[:, :], in_=fa_s[:].rearrange("p a b -> p (a b)")), reads=[buf("fa")], writes=[buf("fa_loc")])

            for q in range(17):
                P.cc(lambda e, q=q: e.collective_compute("AllGather", ALU.bypass, replica_groups=groups, ins=[kT_loc[q].ap().opt()], outs=[kT_all[q].ap().opt()]),
                     reads=[buf("kT_loc")], writes=[buf("kT_all%d" % q)])
            for q in range(16):
                P.cc(lambda e, q=q: e.collective_compute("AllGather", ALU.bypass, replica_groups=groups, ins=[v_loc[q].ap().opt()], outs=[v_all[q].ap().opt()]),
                     reads=[buf("v_loc")], writes=[buf("v_all%d" % q)])
            P.cc(lambda e: e.collective_compute("AllGather", ALU.bypass, replica_groups=groups, ins=[fa_loc.ap().opt()], outs=[fa_all.ap().opt()]),
                 reads=[buf("fa_loc")], writes=[buf("fa_all")])

            P.dma("sync", lambda e: e.dma_start(out=fa4[:], in_=fa_all.ap().rearrange("(c p) f -> p c f", p=128)), reads=[buf("fa_all")], writes=[buf("fa4")])
            for c in range(4):
                for h in range(4):
                    src = fa4[:, c, :].rearrange("p (j u h) -> p j u h", j=4, u=4, h=4)[:, :, :, h]
                    dst = agf[:, h, :].rearrange("p (j c u) -> p j c u", j=4, c=4, u=4)[:, :, c, :]
                    P.op("vector", lambda e, src=src, dst=dst: e.tensor_copy(out=dst, in_=src), reads=[buf("fa4")], writes=[buf("agf")], acc=True)
            agf2 = agf[:].rearrange("p a b -> p (a b)")
            P.op("tensor", lambda e: e.matmul(ps[6][:, 0:256], lhsT=tri[:], rhs=agf2, start=True, stop=True), reads=[buf("agf"), buf("tri")], writes=[Bps[6]])
            P.op("tensor", lambda e: e.matmul(ps[7][:, 0:256], lhsT=onesf[:], rhs=agf2, start=True, stop=True), reads=[buf("agf"), buf("onesf")], writes=[Bps[7]])
            P.op("vector", lambda e: e.tensor_copy(out=totf[:].rearrange("p a b -> p (a b)"), in_=ps[7][:, 0:256]), reads=[Bps[7]], writes=[buf("totf")])
            for h in range(4):
                P.op("vector", lambda e, h=h: e.tensor_tensor_scan(out=inclf[:, h, :], data0=ones1[:, :], data1=totf[:, h, :], initial=0.0, op0=ALU.mult, op1=ALU.add),
                     reads=[buf("totf"), buf("ones1")], writes=[buf("inclf")])
            P.op("vector", lambda e: e.tensor_tensor(out=negcum[:], in0=totf[:], in1=inclf[:], op=ALU.subtract), reads=[buf("totf"), buf("inclf")], writes=[buf("negcum")])
            P.op("vector", lambda e: e.tensor_tensor(out=negcum[:].rearrange("p a b -> p (a b)"), in0=negcum[:].rearrange("p a b -> p (a b)"), in1=ps[6][:, 0:256], op=ALU.subtract),
                 reads=[buf("negcum"), Bps[6]], writes=[buf("negcum")])
            for h in range(4):
                for blk in range(16):
                    P.op("vector", lambda e, h=h, blk=blk: e.tensor_tensor(out=seltmp[:, blk, :], in0=sel_s[:, blk, :], in1=totf[:, h, :], op=ALU.mult),
                         reads=[buf("sel"), buf("totf")], writes=[buf("seltmp")], acc=True)
                P.op("vector", lambda e, h=h: e.tensor_reduce(out=cqref[:, h, :], in_=seltmp[:], axis=AX.X, op=ALU.add), reads=[buf("seltmp")], writes=[buf("cqref")])

            lam0 = 0.8 - 0.6 * math.exp(-0.3 * layer)
            lb = layer * 256
            P.op("vector", lambda e: e.tensor_tensor(out=tmpf[0][:, 0:64], in0=lq_s[:, lb:lb + 64], in1=lq_s[:, lb + 64:lb + 128], op=ALU.mult), reads=[buf("lq")], writes=[buf("tmpf0")])
            P.op("vector", lambda e: e.tensor_reduce(out=sm[:, 0:1], in_=tmpf[0][:, 0:64], axis=AX.X, op=ALU.add), reads=[buf("tmpf0")], writes=[buf("sm")])
            P.op("vector", lambda e: e.tensor_tensor(out=tmpf[0][:, 0:64], in0=lq_s[:, lb + 128:lb + 192], in1=lq_s[:, lb + 192:lb + 256], op=ALU.mult), reads=[buf("lq")], writes=[buf("tmpf0")])
            P.op("vector", lambda e: e.tensor_reduce(out=sm[:, 1:2], in_=tmpf[0][:, 0:64], axis=AX.X, op=ALU.add), reads=[buf("tmpf0")], writes=[buf("sm")])
            P.op("scalar", lambda e: e.activation(out=sm[:, 2:4], in_=sm[:, 0:2], func=AF.Exp), reads=[buf("sm")], writes=[buf("sm")])
            P.op("vector", lambda e: e.tensor_tensor(out=sm[:, 4:5], in0=sm[:, 3:4], in1=sm[:, 2:3], op=ALU.subtract), reads=[buf("sm")], writes=[buf("sm")])
            P.op("vector", lambda e: e.tensor_scalar(out=sm[:, 5:6], in0=sm[:, 4:5], scalar1=-lam0, scalar2=None, op0=ALU.add), reads=[buf("sm")], writes=[buf("sm")])
            P.op("vector", lambda e: e.tensor_scalar(out=sm[:, 6:7], in0=subln[:, layer:layer + 1], scalar1=1.0 - lam0, scalar2=None, op0=ALU.mult), reads=[buf("subln")], writes=[buf("sm")])

            P.op("vector", lambda e: e.memset(sm[:, 40:41], 0.0),
                 writes=[buf("kT_s"), buf("v_s"), buf("fence")] + [buf("yb%d" % i) for i in range(4)] + [buf("acc%d" % i) for i in range(8)])
            def load_kv(mx, hh):
                ch = mx * 4 + hh
                src = kT_all[ch].ap().rearrange("(c r) (j i) -> r j c i", c=4, i=512)
                P.dma("sync", lambda e: e.dma_start(out=kT_s[:].rearrange("p (j c i) -> p j c i", j=4, c=4, i=512), in_=src), reads=[buf("kT_all%d" % ch)], writes=[buf("kT_s")])
                col = mx * 512 + hh * 128
                for c in range(4):
                    for blk in range(16):
                        srcv = v_all[blk][c * 128:(c + 1) * 128, col:col + 128]
                        kb = 16 * (blk // 4) + 4 * c + (blk % 4)
                        dstv = v_s[:, kb, :]
                        P.dma("gpsimd" if (blk % 2) else "sync", lambda e, srcv=srcv, dstv=dstv: e.dma_start(out=dstv, in_=srcv), reads=[buf("v_all%d" % blk)], writes=[buf("v_s")], acc=True)
                P.dma("sync", lambda e: e.dma_start(out=qT_s[:], in_=qs_d[ch]), reads=[buf("qs_d")], writes=[buf("qT_s")])

            def load_G(kind, gi):
                if kind == "mask":
                    P.dma("sync", lambda e: e.dma_start(out=Gadd[:], in_=mneg_in), writes=[buf("Gadd")])
                    P.op("vector", lambda e: e.tensor_copy(out=G_s[:], in_=Gadd[:]), reads=[buf("Gadd")], writes=[buf("G_s")])
                    return
                P.dma("sync", lambda e: e.dma_start(out=G_s[:], in_=gt_in[gi]), writes=[buf("G_s")])
                P.dma("sync", lambda e: e.dma_start(out=Gadd[:], in_=(mneg_in if kind == "bias" else cdil_in)), writes=[buf("Gadd")])
                P.op("vector", lambda e: e.tensor_tensor(out=G_s[:], in0=G_s[:], in1=Gadd[:], op=ALU.add), reads=[buf("Gadd"), buf("G_s")], writes=[buf("G_s")])

            def indexer_gen():
                I_s = arena[:].rearrange("p a b -> p (a b)")[:, 0:16384].bitcast(F32)
                At_s = arena[:].rearrange("p a b -> p (a b)")[:, 16384:24576]
                ki_s = arena[:].rearrange("p a b -> p (a b)")[:, 24576:32768]
                BI = [Bar[0], Bar[1]]
                srck = kT_all[16].ap().rearrange("(c r) (j i) -> r j c i", c=4, i=512)
                P.dma("sync", lambda e: e.dma_start(out=ki_s.rearrange("p (j c i) -> p j c i", j=4, c=4, i=512), in_=srck), reads=[buf("kT_all16")], writes=[Bar[3]])
                for blk in range(16):
                    j, u = blk // 4, blk % 4
                    nkc = 4 * j + 4
                    nk = nkc * 512
                    qi = qi_s[blk % 2]
                    bq = buf("qi%d" % (blk % 2))
                    P.dma("sync", lambda e, qi=qi, blk=blk: e.dma_start(out=qi[:], in_=qs_d[16:24, :, blk * 128:(blk + 1) * 128].rearrange("m p t -> p m t")), reads=[buf("qs_d")], writes=[bq])
                    for kc in range(nkc):
                        for ih in range(16):
                            pi = 6 + nxt("psA", 2)
                            r0 = (ih % 2) * 64
                            P.op("tensor", lambda e, pi=pi, ih=ih, r0=r0, kc=kc, qi=qi: e.matmul(ps[pi][:, :], lhsT=qi[r0:r0 + 64, ih // 2, :], rhs=ki_s[r0:r0 + 64, kc * 512:(kc + 1) * 512],
                                                                                                  start=True, stop=True),
                                 reads=[bq, Bar[3]], writes=[Bps[pi]])
                            ri = nxt("rbuf", 2)
                            P.op("scalar", lambda e, pi=pi, ri=ri: e.activation(out=rbuf[ri][:], in_=ps[pi][:, :], func=AF.Relu), reads=[Bps[pi]], writes=[buf("rbuf%d" % ri)])
                            if ih == 0:
                                P.op("vector", lambda e, ri=ri, kc=kc, blk=blk: e.tensor_scalar(out=I_s[:, kc * 512:(kc + 1) * 512], in0=rbuf[ri][:], scalar1=wI_s[:, blk, 0:1], scalar2=None, op0=ALU.mult),
                                     reads=[buf("rbuf%d" % ri), buf("wI")], writes=BI)
                            else:
                                P.op("vector", lambda e, ri=ri, kc=kc, blk=blk, ih=ih: e.scalar_tensor_tensor(out=I_s[:, kc * 512:(kc + 1) * 512], in0=rbuf[ri][:], scalar=wI_s[:, blk, ih:ih + 1],
                                                                                                              in1=I_s[:, kc * 512:(kc + 1) * 512], op0=ALU.mult, op1=ALU.add),
                                     reads=[buf("rbuf%d" % ri), buf("wI")] + BI, writes=BI)
                            yield
                    P.op("vector", lambda e, nk=nk: e.tensor_reduce(out=sm[:, 8:9], in_=I_s[:, 0:nk], axis=AX.X, op=ALU.min), reads=BI, writes=[buf("smI")])
                    for kc in range(4 * j, nkc):
                        v0 = 512 * kc - 2048 * j - 128 * u + 384
                        P.op("vector", lambda e, kc=kc, v0=v0: e.tensor_tensor(out=I_s[:, kc * 512:(kc + 1) * 512], in0=I_s[:, kc * 512:(kc + 1) * 512], in1=mt_s[:, v0:v0 + 512], op=ALU.add),
                             reads=BI + [buf("mt")], writes=BI)
                    P.op("vector", lambda e, nk=nk: e.tensor_reduce(out=sm[:, 9:10], in_=I_s[:, 0:nk], axis=AX.X, op=ALU.max), reads=BI, writes=[buf("smI")])
                    P.op("vector", lambda e: e.tensor_scalar(out=sm[:, 10:11], in0=sm[:, 8:9], scalar1=-1.0, scalar2=None, op0=ALU.add), reads=[buf("smI")], writes=[buf("smI")])
                    P.op("vector", lambda e: e.tensor_tensor(out=sm[:, 11:12], in0=sm[:, 9:10], in1=sm[:, 8:9], op=ALU.subtract), reads=[buf("smI")], writes=[buf("smI")])
                    P.op("vector", lambda e: e.tensor_scalar(out=sm[:, 11:12], in0=sm[:, 11:12], scalar1=2.0, scalar2=None, op0=ALU.add), reads=[buf("smI")], writes=[buf("smI")])
                    P.op("vector", lambda e: e.tensor_scalar(out=whalf[:], in0=pw2[:], scalar1=sm[:, 11:12], scalar2=None, op0=ALU.mult), reads=[buf("smI"), buf("pw2")], writes=[buf("whalf")])
                    yield
                    for it in range(NBIS):
                        P.op("vector", lambda e, it=it: e.tensor_tensor(out=sm[:, 12:13], in0=sm[:, 10:11], in1=whalf[:, it:it + 1], op=ALU.add), reads=[buf("smI"), buf("whalf")], writes=[buf("smI")])
                        P.op("vector", lambda e, nk=nk: e.tensor_scalar(out=At_s[:, 0:nk], in0=I_s[:, 0:nk], scalar1=sm[:, 12:13], scalar2=None, op0=ALU.is_ge, op1=ALU.add, accum_out=sm[:, 13:14]),
                             reads=BI + [buf("smI")], writes=[Bar[2], buf("smI")])
                        P.op("vector", lambda e, it=it: e.tensor_scalar(out=sm[:, 14:15], in0=sm[:, 13:14], scalar1=TOPK - 0.5, scalar2=whalf[:, it:it + 1], op0=ALU.is_ge, op1=ALU.mult),
                             reads=[buf("smI"), buf("whalf")], writes=[buf("smI")])
                        P.op("vector", lambda e: e.tensor_tensor(out=sm[:, 10:11], in0=sm[:, 10:11], in1=sm[:, 14:15], op=ALU.add), reads=[buf("smI")], writes=[buf("smI")])
                        yield
                    P.op("vector", lambda e, nk=nk: e.tensor_scalar(out=At_s[:, 0:nk], in0=I_s[:, 0:nk], scalar1=sm[:, 10:11], scalar2=NEG, op0=ALU.is_lt, op1=ALU.mult),
                         reads=BI + [buf("smI")], writes=[Bar[2]])
                    P.dma("sync", lambda e, blk=blk, nk=nk: e.dma_start(out=A_d[blk, :, 0:nk], in_=At_s[:, 0:nk]), reads=[Bar[2]], writes=[buf("A_d")], acc=True)
                    yield

            idx_it = indexer_gen()

            def idx_step(k):
                alive = True
                for _ in range(k):
                    try:
                        next(idx_it)
                    except StopIteration:
                        alive = False
                        break
                return alive

            LB = (0, 1)

            def flash(j, krows, kb_lo, mode, h, gi, dsa_A=None):
                kbs = list(range(kb_lo, 16 * j + 16))
                ob = 2 + 2 * nxt("oz", 2)
                zb = ob + 1
                pend = None

                def pvz(pi, kb, first, last):
                    bp = buf("pT%d" % pi)
                    P.op("tensor", lambda e: e.matmul(ps[ob][:, :], lhsT=v_s[:, kb, :], rhs=pT[pi][:], start=first, stop=last),
                         reads=[buf("v_s"), bp], writes=[Bps[ob]])
                    P.op("tensor", lambda e: e.matmul(ps[zb][:, :], lhsT=onesb[:], rhs=pT[pi][:], start=first, stop=last),
                         reads=[buf("onesb"), bp], writes=[Bps[zb]])
                for n, kb in enumerate(kbs):
                    ds_ = 2048 * j - 128 * kb
                    li = LB[nxt("psL", 2)]
                    Ls = ps[li]
                    P.op("tensor", lambda e, Ls=Ls, kb=kb: e.matmul(Ls[:, :], lhsT=kT_s[krows[0]:krows[1], kb * 128:(kb + 1) * 128], rhs=qT_s[krows[0]:krows[1], j * 512:(j + 1) * 512],
                                                                      start=True, stop=(dsa_A is None)),
                         reads=[buf("kT_s"), buf("qT_s")], writes=[Bps[li]])
                    if dsa_A is not None:
                        At, bA, off = dsa_A(kb)
                        for u in range(4):
                            P.op("tensor", lambda e, Ls=Ls, u=u, At=At, off=off: e.matmul(Ls[:, u * 128:(u + 1) * 128], lhsT=At[:, u, off:off + 128], rhs=ident[:], start=False, stop=True,
                                                                                          skip_group_check=True),
                                 reads=[bA, buf("ident")], writes=[Bps[li]])
                    src, bsrc = Ls, Bps[li]
                    need_add = (ds_ < 128) if mode == "fox" else (ds_ < DFAR)
                    if need_add:
                        ti = nxt("tmpf", 2)
                        u0 = min(ds_, DFAR) + GOFF
                        P.op("vector", lambda e, ti=ti, Ls=Ls, u0=u0: e.tensor_tensor(out=tmpf[ti][:], in0=Ls[:, :], in1=G_s[:, u0:u0 + 512], op=ALU.add),
                             reads=[Bps[li], buf("G_s")], writes=[buf("tmpf%d" % ti)])
                        src, bsrc = tmpf[ti], buf("tmpf%d" % ti)
                    pi = nxt("pT", 4)
                    bp = buf("pT%d" % pi)
                    if mode == "fox":
                        for u in range(4):
                            P.op("scalar", lambda e, pi=pi, src=src, u=u, kb=kb: e.activation(out=pT[pi][:, u * 128:(u + 1) * 128], in_=src[:, u * 128:(u + 1) * 128], func=AF.Exp,
                                                                                                bias=bm[:, 4 * j + u, kb:kb + 1], scale=1.0),
                                 reads=[bsrc, buf("bm")], writes=[bp], acc=(u > 0))
                    elif need_add:
                        P.op("scalar", lambda e, pi=pi, src=src: e.activation(out=pT[pi][:], in_=src[:, :], func=AF.Exp), reads=[bsrc], writes=[bp])
                    else:
                        P.op("scalar", lambda e, pi=pi, src=src: e.activation(out=pT[pi][:], in_=src[:, :], func=AF.Exp, bias=b31[:, gi:gi + 1], scale=1.0),
                             reads=[bsrc, buf("b31")], writes=[bp])
                    if pend is not None:
                        pvz(*pend)
                    pend = (pi, kb, n == 0, n == len(kbs) - 1)
                    if dsa_A is None:
                        idx_step(2 if (n % 3 == 0) else 1)
                pvz(*pend)
                return ob, zb

            def normalize(dst, bdst, oz):
                ob, zb = oz
                P.op("vector", lambda e: e.reciprocal(out=ot[4][:], in_=ps[zb][:, :]), reads=[Bps[zb]], writes=[buf("ot4")])
                P.op("vector", lambda e: e.tensor_tensor(out=dst[:], in0=ps[ob][:, :], in1=ot[4][:], op=ALU.mult), reads=[Bps[ob], buf("ot4")], writes=[bdst])

            def store_o(src, bsrc, ch, j):
                P.dma("sync", lambda e: e.dma_start(out=ao_d[ch, :, j * 512:(j + 1) * 512], in_=src[:]), reads=[bsrc], writes=[buf("ao_d")], acc=True)

            load_G("mask", 0)
            for h in range(4):
                load_kv(1, h)
                for blk in range(16):
                    P.op("vector", lambda e, h=h, blk=blk: e.tensor_scalar(out=bm[:, blk, :], in0=negcum[:, h, :], scalar1=cqref[:, h, blk:blk + 1], scalar2=None, op0=ALU.add),
                         reads=[buf("negcum"), buf("cqref")], writes=[buf("bm")], acc=True)
                for j in range(4):
                    oz = flash(j, (0, 128), 0, "fox", h, 0)
                    normalize(ot[0], buf("ot0"), oz)
                    store_o(ot[0], buf("ot0"), 4 + h, j)
            for h in range(4):
                load_G("dil", 8 + h)
                load_kv(3, h)
                for j in range(4):
                    kb_lo = max(0, 16 * j - 16)
                    oz = flash(j, (0, 128), kb_lo, "dil", h, 8 + h)
                    normalize(ot[0], buf("ot0"), oz)
                    store_o(ot[0], buf("ot0"), 12 + h, j)
            for h in range(4):
                load_G("bias", 4 + h)
                load_kv(2, h)
                for j in range(4):
                    oz = flash(j, (0, 64), 0, "bias", h, 4 + h)
                    normalize(ot[0], buf("ot0"), oz)
                    oz = flash(j, (64, 128), 0, "bias", h, 4 + h)
                    normalize(ot[1], buf("ot1"), oz)
                    P.op("vector", lambda e: e.scalar_tensor_tensor(out=ot[2][:], in0=ot[1][:], scalar=sm[:, 5:6], in1=ot[0][:], op0=ALU.mult, op1=ALU.add),
                         reads=[buf("ot0"), buf("ot1"), buf("sm")], writes=[buf("ot2")])
                    si = nxt("stg", 4)
                    P.op("scalar", lambda e, si=si: e.activation(out=stg[si][:], in_=ot[2][:], func=AF.Square), reads=[buf("ot2")], writes=[buf("stg%d" % si)])
                    P.op("tensor", lambda e, si=si: e.matmul(ps[6][:, :], lhsT=onesb[:], rhs=stg[si][:], start=True, stop=True), reads=[buf("stg%d" % si), buf("onesb")], writes=[Bps[6]])
                    rstd_from_ssq(ps[6][:, :], ot[3], 128, 512, Bps[6], buf("ot3"))
                    P.op("vector", lambda e: e.scalar_tensor_tensor(out=ot[0][:], in0=ot[2][:], scalar=sm[:, 6:7], in1=ot[3][:], op0=ALU.mult, op1=ALU.mult),
                         reads=[buf("ot2"), buf("ot3"), buf("sm")], writes=[buf("ot0")])
                    store_o(ot[0], buf("ot0"), 8 + h, j)

            while idx_step(64):
                pass
            Apc = [arena[:].rearrange("p a b -> p (a b)")[:, i * 8192:(i + 1) * 8192].rearrange("p (u s) -> p u s", u=4) for i in range(2)]
            for h in range(4):
                load_G("bias", h)
                load_kv(0, h)
                for j in range(4):
                    state = {}

                    def dsa_A(kb, j=j, state=state):
                        pc = kb // 16
                        if state.get("pc") != pc:
                            ai = nxt("q", 2)
                            state["pc"], state["ai"] = pc, ai
                            for u in range(4):
                                P.dma("sync", lambda e, ai=ai, u=u, pc=pc: e.dma_start(out=Apc[ai][:, u, :], in_=A_d[4 * j + u, :, pc * 2048:(pc + 1) * 2048]), reads=[buf("A_d")], writes=[Bar[ai]], acc=True)
                        ai = state["ai"]
                        return Apc[ai], Bar[ai], (kb % 16) * 128
                    oz = flash(j, (0, 128), 0, "bias", h, h, dsa_A=dsa_A)
                    normalize(ot[0], buf("ot0"), oz)
                    store_o(ot[0], buf("ot0"), h, j)

            P.op("vector", lambda e: e.memset(sm[:, 40:41], 0.0),
                 writes=[buf("kT_s"), buf("v_s"), buf("fence")] + [buf("yb%d" % i) for i in range(4)] + [buf("acc%d" % i) for i in range(8)])
            Wbr, Wout = wf_br[layer], wf_out[layer]
            Bbr, Bout = "wf_br%d" % layer, "wf_out%d" % layer
            P.dma("sync", lambda e: e.dma_start(out=hT[:], in_=hT_d), reads=[buf("hT_d")], writes=Bar)
            ybufs = [kT_s[:, i * 2048:(i + 1) * 2048].rearrange("p (k t) -> p k t", k=4) for i in range(4)]
            accs = [v_s[:].rearrange("p a b -> p (a b)")[:, i * 1024:(i + 1) * 1024].bitcast(F32) for i in range(8)]
            mbuf = G_s[:].bitcast(BF16)[:, 0:8192].rearrange("p (k t) -> p k t", k=16)
            for zg in range(8):
                t, bw = load_w(Wl, Bwin, C_Z + zg * 256, 256)
                for q in range(2):
                    ch = zg * 2 + q
                    for j in range(4):
                        pi = 4 + nxt("psA", 2)
                        for k in range(16):
                            P.op("tensor", lambda e, pi=pi, k=k, q=q, t=t, j=j: e.matmul(ps[pi][:, :], lhsT=t[:, k, q * 128:(q + 1) * 128], rhs=hT[:, k, j * 512:(j + 1) * 512], start=(k == 0), stop=(k == 15)),
                                 reads=[bw] + Bar, writes=[Bps[pi]])
                        ti = nxt("tmpf", 2)
                        P.op("scalar", lambda e, pi=pi, ti=ti: e.activation(out=tmpf[ti][:], in_=ps[pi][:, :], func=AF.Silu), reads=[Bps[pi]], writes=[buf("tmpf%d" % ti)])
                        oi = nxt("rbuf", 2)
                        P.dma("sync", lambda e, oi=oi, ch=ch, j=j: e.dma_start(out=rbuf[oi][:], in_=ao_d[ch, :, j * 512:(j + 1) * 512]), reads=[buf("ao_d")], writes=[buf("rbuf%d" % oi)])
                        si = nxt("stg", 4)
                        P.op("vector", lambda e, ti=ti, oi=oi, si=si: e.tensor_tensor(out=stg[si][:], in0=tmpf[ti][:], in1=rbuf[oi][:], op=ALU.mult),
                             reads=[buf("tmpf%d" % ti), buf("rbuf%d" % oi)], writes=[buf("stg%d" % si)])
                        P.dma("sync", lambda e, si=si, ch=ch, j=j: e.dma_start(out=yT_d[ch, :, j * 512:(j + 1) * 512], in_=stg[si][:]), reads=[buf("stg%d" % si)], writes=[buf("yT_d")], acc=True)
            for ng in range(8):
                for b in range(4):
                    tg, bwg = load_w(Wl, Bwin, C_G + b * 2048 + ng * 256, 256)
                    i2 = nxt("wb", 2)
                    tb_, bwb = wb[i2], buf("wb%d" % i2)
                    srcb = Wbr[b * 512:(b + 1) * 512, ng * 256:(ng + 1) * 256].rearrange("(k p) n -> p k n", p=128)
                    P.dma("gpsimd", lambda e, tb_=tb_, srcb=srcb: e.dma_start(out=tb_[:, 0:4, :], in_=srcb), reads=[buf(Bbr)], writes=[bwb])
                    for j in range(4):
                        yi = nxt("yb", 4)
                        yb, byb = ybufs[yi], buf("yb%d" % yi)
                        P.dma("sync", lambda e, yb=yb, b=b, j=j: e.dma_start(out=yb, in_=yT_d[b * 4:(b + 1) * 4, :, j * 512:(j + 1) * 512].rearrange("k p t -> p k t")), reads=[buf("yT_d")], writes=[byb])
                        for q in range(2):
                            pg = 4 + nxt("psA", 2)
                            for k in range(16):
                                P.op("tensor", lambda e, pg=pg, k=k, q=q, tg=tg, j=j: e.matmul(ps[pg][:, :], lhsT=tg[:, k, q * 128:(q + 1) * 128], rhs=hT[:, k, j * 512:(j + 1) * 512], start=(k == 0), stop=(k == 15)),
                                     reads=[bwg] + Bar, writes=[Bps[pg]])
                            pu = 6 + nxt("psU", 2)
                            for k in range(4):
                                P.op("tensor", lambda e, pu=pu, k=k, q=q, tb_=tb_, yb=yb: e.matmul(ps[pu][:, :], lhsT=tb_[:, k, q * 128:(q + 1) * 128], rhs=yb[:, k, :], start=(k == 0), stop=(k == 3)),
                                     reads=[bwb, byb], writes=[Bps[pu]])
                            ti = nxt("tmpf", 2)
                            P.op("scalar", lambda e, pg=pg, ti=ti: e.activation(out=tmpf[ti][:], in_=ps[pg][:, :], func=AF.Sigmoid), reads=[Bps[pg]], writes=[buf("tmpf%d" % ti)])
                            ai = q * 4 + j
                            acc, bacc = accs[ai], buf("acc%d" % ai)
                            if b == 0:
                                P.op("vector", lambda e, ti=ti, pu=pu, acc=acc: e.tensor_tensor(out=acc, in0=ps[pu][:, :], in1=tmpf[ti][:], op=ALU.mult),
                                     reads=[Bps[pu], buf("tmpf%d" % ti)], writes=[bacc])
                            else:
                                P.op("vector", lambda e, ti=ti, pu=pu: e.tensor_tensor(out=tmpf[ti][:], in0=ps[pu][:, :], in1=tmpf[ti][:], op=ALU.mult),
                                     reads=[Bps[pu], buf("tmpf%d" % ti)], writes=[buf("tmpf%d" % ti)])
                                P.op("gpsimd", lambda e, ti=ti, acc=acc: e.tensor_tensor(out=acc, in0=acc, in1=tmpf[ti][:], op=ALU.add),
                                     reads=[buf("tmpf%d" % ti), bacc], writes=[bacc])
                            if b == 3:
                                n = ng * 2 + q
                                si = nxt("stg", 4)
                                P.op("scalar", lambda e, acc=acc, si=si: e.activation(out=stg[si][:], in_=acc, func=AF.Copy), reads=[bacc], writes=[buf("stg%d" % si)])
                                P.dma("sync", lambda e, si=si, n=n, j=j: e.dma_start(out=mT_d[n, :, j * 512:(j + 1) * 512], in_=stg[si][:]), reads=[buf("stg%d" % si)], writes=[buf("mT_d")], acc=True)
            for j in range(4):
                P.dma("sync", lambda e, j=j: e.dma_start(out=mbuf, in_=mT_d[:, :, j * 512:(j + 1) * 512].rearrange("k p t -> p k t")), reads=[buf("mT_d")], writes=[buf("G_s")])
                for ng in range(8):
                    to, bwo = load_w(Wout, Bout, ng * 256, 256)
                    for q in range(2):
                        n = ng * 2 + q
                        po = 4 + nxt("psA", 2)
                        for k in range(16):
                            P.op("tensor", lambda e, po=po, k=k, q=q, to=to: e.matmul(ps[po][:, :], lhsT=to[:, k, q * 128:(q + 1) * 128], rhs=mbuf[:, k, :], start=(k == 0), stop=(k == 15)),
                                 reads=[bwo, buf("G_s")], writes=[Bps[po]])
                        oi = nxt("rbuf", 2)
                        P.dma("sync", lambda e, oi=oi, n=n, j=j: e.dma_start(out=rbuf[oi][:], in_=xsrc[n * 128:(n + 1) * 128, j * 512:(j + 1) * 512]), reads=[Bx], writes=[buf("rbuf%d" % oi)])
                        P.op("vector", lambda e, oi=oi, po=po: e.tensor_tensor(out=rbuf[oi][:], in0=ps[po][:, :], in1=rbuf[oi][:], op=ALU.add),
                             reads=[Bps[po], buf("rbuf%d" % oi)], writes=[buf("rbuf%d" % oi)])
                        P.dma("sync", lambda e, oi=oi, n=n, j=j: e.dma_start(out=xs_d[n * 128:(n + 1) * 128, j * 512:(j + 1) * 512], in_=rbuf[oi][:]), reads=[buf("rbuf%d" % oi)], writes=[Bxo], acc=True)

        for layer_ in range(L):
            do_layer(layer_)

        xs_d = xs_dd[(L - 1) % 2]
        Bx = buf("xs_d%d" % ((L - 1) % 2))
        Bout_ = buf("yT")
        for j in range(4):
            for k in range(16):
                o = ot[k % 2]
                bo = buf("ot%d" % (k % 2))
                P.dma("sync", lambda e, k=k, j=j, o=o: e.dma_start(out=o[:], in_=xs_d[k * 128:(k + 1) * 128, j * 512:(j + 1) * 512]), reads=[Bx], writes=[bo])
                s_ = stg[k % 2]
                bs_ = buf("stg%d" % (k % 2))
                P.op("scalar", lambda e, o=o, s_=s_: e.activation(out=s_[:], in_=o[:], func=AF.Square), reads=[bo], writes=[bs_])
                P.op("tensor", lambda e, s_=s_, k=k: e.matmul(ps[6][:, :], lhsT=onesb[:], rhs=s_[:], start=(k == 0), stop=(k == 15)), reads=[bs_, buf("onesb")], writes=[Bps[6]])
            rstd_from_ssq(ps[6][:, :], ot[4], D, 512, Bps[6], buf("ot4"))
            for k in range(16):
                o = ot[k % 2]
                bo = buf("ot%d" % (k % 2))
                P.dma("sync", lambda e, k=k, j=j, o=o: e.dma_start(out=o[:], in_=xs_d[k * 128:(k + 1) * 128, j * 512:(j + 1) * 512]), reads=[Bx], writes=[bo])
                ri = nxt("rbuf", 2)
                P.op("vector", lambda e, k=k, o=o, ri=ri: e.scalar_tensor_tensor(out=rbuf[ri][:], in0=o[:], scalar=fnormw[:, k:k + 1], in1=ot[4][:], op0=ALU.mult, op1=ALU.mult),
                     reads=[bo, buf("ot4"), buf("fnormw")], writes=[buf("rbuf%d" % ri)])
                P.dma("sync", lambda e, k=k, j=j, ri=ri: e.dma_start(out=yT_out[k * 128:(k + 1) * 128, j * 512:(j + 1) * 512], in_=rbuf[ri][:]), reads=[buf("rbuf%d" % ri)], writes=[Bout_], acc=True)
        P.finish("sync", [Bout_])
        P.emit(block, sems)
    print("ops", P.nops)
    return nc


def host_prep(inputs, depth=DEPTH):
    x = np.asarray(inputs["x"], np.float32)
    L = depth
    rel = np.asarray(inputs["rel_bias"], np.float32)
    maps = []
    sl = np.arange(128)[:, None]
    uu = np.arange(GL)[None, :]
    import ml_dtypes
    bf = ml_dtypes.bfloat16
    ident = np.eye(128, dtype=np.float32).astype(bf)
    tri = (np.arange(128)[:, None] <= np.arange(128)[None, :]).astype(np.float32)
    normw = np.ascontiguousarray(np.asarray(inputs["norm_w"], np.float32)[:L].reshape(L, 16, 128).transpose(2, 0, 1).reshape(128, L * 16))
    fnormw = np.ascontiguousarray(np.asarray(inputs["final_norm_w"], np.float32).reshape(16, 128).T)
    foxb = np.ascontiguousarray(np.broadcast_to(np.asarray(inputs["fox_b_f"], np.float32)[:L].reshape(1, L * 4), (128, L * 4)))
    lq = np.stack([np.asarray(inputs[k], np.float32)[:L] for k in ("diff_lq1", "diff_lk1", "diff_lq2", "diff_lk2")], axis=1)
    lq = np.ascontiguousarray(np.broadcast_to(lq.reshape(1, L * 256), (128, L * 256)))
    subln = np.ascontiguousarray(np.asarray(inputs["diff_subln_w"], np.float32)[:L].T)
    b31 = np.ascontiguousarray(np.broadcast_to(rel[31:32, :], (128, 12)))
    w_in = np.asarray(inputs["w_in"], np.float32)
    w_br = np.asarray(inputs["w_branch"], np.float32).reshape(DEPTH, 2048, 2048)
    w_out = np.asarray(inputs["w_out"], np.float32)
    for core in range(8):
        b, c = core // 4, core % 4
        toks = np.concatenate([np.arange(512 * (4 * j + c), 512 * (4 * j + c + 1)) for j in range(4)])
        xT = np.ascontiguousarray(x[b, toks, :].T)
        dist = uu - GOFF + 512 * c - sl
        bidx = t5_bucket_np(dist)
        gt = np.ascontiguousarray(rel[bidx, :].transpose(2, 0, 1))
        mneg = np.where(dist >= 0, 0.0, NEG).astype(np.float32).astype(bf)
        nval = ((dist >= 0) & (dist <= 128)).astype(np.int32) + ((dist >= 0) & (dist % 4 == 0) & (dist <= 512)).astype(np.int32) \
            + ((dist >= 0) & (dist % 16 == 0) & (dist <= 2048)).astype(np.int32)
        cdil = np.where(nval > 0, np.log(np.maximum(nval, 1).astype(np.float32)), NEG).astype(np.float32).astype(bf)
        vv = np.arange(2432)[None, :]
        mt = np.where((vv - 384 - 512 * c - sl) > 0, -1e9, 0.0).astype(np.float32).astype(bf)
        blk = np.arange(16)
        kb_own = 16 * (blk // 4) + 4 * c + (blk % 4)
        sel = (np.arange(64)[None, :] <= kb_own[:, None]).astype(np.float32)
        sel = np.ascontiguousarray(np.broadcast_to(sel.reshape(1, 1024), (128, 1024)))
        maps.append({
            "xT": xT,
            "w_in": w_in[:L], "w_br": w_br[:L], "w_out": w_out[:L],
            "normw": normw, "fnormw": fnormw, "foxb": foxb, "lq": lq, "subln": subln,
            "gt": gt, "mneg": mneg, "cdil": cdil, "b31": b31, "mt": mt, "sel": sel, "ident": ident, "tri": tri,
        })
    return maps


def run(inputs, depth=DEPTH):
    nc = build(depth)
    maps = host_prep(inputs, depth)
    res = run_bass_kernel_spmd(nc, maps, core_ids=list(range(8)))
    out = np.zeros((2, S, D), np.float32)
    for core in range(8):
        b, c = core // 4, core % 4
        toks = np.concatenate([np.arange(512 * (4 * j + c), 512 * (4 * j + c + 1)) for j in range(4)])
        out[b, toks, :] = res.results[core]["yT"].T
    return out


def kernel(**inputs):
    return run(inputs, DEPTH)
```

```python
import math
from contextlib import ExitStack

import numpy as np
import concourse.bass as bass
import concourse.mybir as mybir
from concourse.bass_utils import run_bass_kernel_spmd

F32 = mybir.dt.float32
BF16 = mybir.dt.bfloat16
ALU = mybir.AluOpType
AF = mybir.ActivationFunctionType
AX = mybir.AxisListType

D = 2048
S = 8192
NT = 2048
DEPTH = 4
N_IN = 17492
NEG = -30000.0
GL = 4608
GOFF = 1920
DFAR = 2176
C_Z, C_G = 0, 2048
C_DSA, C_IQ, C_IK, C_IW, C_FOX, C_FF, C_DIFF, C_DIL = 10240, 11776, 12800, 12864, 12880, 14416, 14420, 15956
S128 = 128 ** -0.5
S64 = 64 ** -0.5
TOPK = 256
NBIS = 18


class Buf:
    __slots__ = ("w", "r")

    def __init__(self):
        self.w = {}
        self.r = {}


class Prog:
    ENG = ("sync", "scalar", "vector", "gpsimd", "tensor")

    def __init__(self, ndma=24):
        self.streams = {e: [] for e in self.ENG}
        self.count = {e: 0 for e in self.ENG}
        self.seen = {e: {} for e in self.ENG}
        self.ndma = ndma
        self.dma_i = 0
        self.dma_cnt = [0] * ndma
        self.cc_cnt = 0
        self.nops = 0

    def _deps(self, eng, reads, writes, acc=False):
        deps = {}

        def add(k, v):
            if deps.get(k, 0) < v:
                deps[k] = v
        for b in reads:
            for k, v in b.w.items():
                add(k, v)
        for b in writes:
            if not acc:
                for k, v in b.w.items():
                    add(k, v)
            for k, v in b.r.items():
                add(k, v)
        waits = []
        for k, v in deps.items():
            if k == eng and eng == "tensor":
                continue
            if self.seen[eng].get(k, 0) < v:
                self.seen[eng][k] = v
                waits.append((k, v))
        return waits

    def _record(self, me, reads, writes, acc=False):
        for b in reads:
            if b.r.get(me[0], 0) < me[1]:
                b.r[me[0]] = me[1]
        for b in writes:
            if acc:
                b.w[me[0]] = me[1]
            else:
                b.w = {me[0]: me[1]}
                b.r = {}
        self.nops += 1

    def op(self, eng, fn, reads=(), writes=(), acc=False):
        waits = self._deps(eng, reads, writes, acc)
        self.count[eng] += 1
        me = (eng, self.count[eng])
        self.streams[eng].append((waits, fn, eng, 1))
        self._record(me, reads, writes, acc)

    def dma(self, eng, fn, reads=(), writes=(), acc=False):
        i = self.dma_i % self.ndma
        self.dma_i += 1
        key = "dma%d" % i
        waits = self._deps(eng, reads, writes, acc)
        prev = self.dma_cnt[i]
        if prev and self.seen[eng].get(key, 0) < prev:
            self.seen[eng][key] = prev
            waits.append((key, prev))
        self.dma_cnt[i] += 16
        me = (key, self.dma_cnt[i])
        self.streams[eng].append((waits, fn, key, 16))
        self._record(me, reads, writes, acc)

    def cc(self, fn, reads=(), writes=()):
        eng = "gpsimd"
        waits = self._deps(eng, reads, writes)
        self.cc_cnt += 1
        me = ("cc", self.cc_cnt)
        self.streams[eng].append((waits, fn, "cc", None))
        self._record(me, reads, writes)

    def finish(self, eng, bufs):
        waits = self._deps(eng, bufs, ())
        self.streams[eng].append((waits, None, None, 0))

    def emit(self, block, sems):
        def mk(e):
            def body(engh):
                for waits, fn, key, inc in self.streams[e]:
                    for k, v in waits:
                        engh.wait_ge(sems[k], v)
                    if fn is not None:
                        ins = fn(engh)
                        if inc is None:
                            ins.then_inc(sems[key])
                        else:
                            ins.then_inc(sems[key], inc)
            return body
        block.sync(mk("sync"))
        block.scalar(mk("scalar"))
        block.vector(mk("vector"))
        block.gpsimd(mk("gpsimd"))
        block.tensor(mk("tensor"))


def t5_bucket_np(dist):
    dist = np.maximum(dist, 0)
    d = np.maximum(dist, 16).astype(np.float32)
    large = 16 + (np.log(d / np.float32(16)) / np.float32(math.log(2048 / 16)) * np.float32(16)).astype(np.int32)
    large = np.minimum(large, 31)
    return np.where(dist < 16, dist, large)


def build(depth=DEPTH):
    nc = bass.Bass("TRN2", target_bir_lowering=False)
    P = Prog()
    L = depth

    def din(name, shape, dt=F32):
        return nc.dram_tensor(name, list(shape), dt, kind="ExternalInput").ap()

    def dscr(name, shape, dt):
        return nc.dram_tensor(name, list(shape), dt)

    xT_in = din("xT", [D, NT])
    w_in_a = din("w_in", [L, D, N_IN])
    w_br_a = din("w_br", [L, D, D])
    w_out_a = din("w_out", [L, D, D])
    normw_in = din("normw", [128, L * 16])
    fnormw_in = din("fnormw", [128, 16])
    foxb_in = din("foxb", [128, L * 4])
    lq_in = din("lq", [128, L * 4 * 64])
    subln_in = din("subln", [128, L])
    gt_in = din("gt", [12, 128, GL])
    mneg_in = din("mneg", [128, GL], BF16)
    cdil_in = din("cdil", [128, GL], BF16)
    b31_in = din("b31", [128, 12])
    mt_in = din("mt", [128, 2432], BF16)
    sel_in = din("sel", [128, 16 * 64])
    ident_in = din("ident", [128, 128], BF16)
    tri_in = din("tri", [128, 128])
    yT_out = nc.dram_tensor("yT", [D, NT], F32, kind="ExternalOutput").ap()

    wf_in = [w_in_a[l] for l in range(L)]
    wf_br = [w_br_a[l] for l in range(L)]
    wf_out = [w_out_a[l] for l in range(L)]
    xs_dd = [dscr("xs_d%d" % i, [D, NT], F32).ap() for i in range(2)]
    hT_d = dscr("hT_d", [128, 16, NT], BF16).ap()
    qs_d = dscr("qs_d", [24, 128, NT], BF16).ap()
    yT_d = dscr("yT_d", [16, 128, NT], BF16).ap()
    mT_d = dscr("mT_d", [16, 128, NT], BF16).ap()
    kT_loc = [dscr("kT_loc%d" % q, [128, NT], BF16) for q in range(17)]
    kT_all = [dscr("kT_all%d" % q, [512, NT], BF16) for q in range(17)]
    v_loc = [dscr("v_loc%d" % q, [128, 2048], BF16) for q in range(16)]
    v_all = [dscr("v_all%d" % q, [512, 2048], BF16) for q in range(16)]
    fa_loc = dscr("fa_loc", [128, 64], F32)
    fa_all = dscr("fa_all", [512, 64], F32)
    ao_d = dscr("ao_d", [16, 128, NT], F32).ap()
    A_d = dscr("A_d", [16, 128, S], BF16).ap()

    B = {}

    def buf(name):
        if name not in B:
            B[name] = Buf()
        return B[name]

    groups = [[0, 1, 2, 3], [4, 5, 6, 7]]
    es = ExitStack()
    with es:
        def sb(name, shape, dt):
            return es.enter_context(nc.sbuf_tensor("sb_" + name, list(shape), dt))

        arena = sb("arena", [128, 16, NT], BF16)
        kT_s = sb("kT_s", [128, S], BF16)
        v_s = sb("v_s", [128, 64, 128], BF16)
        wb = [sb("wb%d" % i, [128, 16, 256], BF16) for i in range(2)]
        G_s = sb("G_s", [128, GL], F32)
        Gadd = sb("Gadd", [128, GL], BF16)
        qT_s = sb("qT_s", [128, NT], BF16)
        pT = [sb("pT%d" % i, [128, 512], BF16) for i in range(4)]
        tmpf = [sb("tmpf%d" % i, [128, 512], F32) for i in range(2)]
        rbuf = [sb("rbuf%d" % i, [128, 512], F32) for i in range(2)]
        rbufI = rbuf + [sb("rbufI%d" % i, [128, 512], F32) for i in range(2)]
        rbufI_names = ["rbuf0", "rbuf1", "rbufI0", "rbufI1"]
        ot = [sb("ot%d" % i, [128, 512], F32) for i in range(5)]
        stg = [sb("stg%d" % i, [128, 512], BF16) for i in range(4)]
        bm = sb("bm", [128, 16, 64], F32)
        negcum = sb("negcum", [128, 4, 64], F32)
        totf = sb("totf", [128, 4, 64], F32)
        inclf = sb("inclf", [128, 4, 64], F32)
        agf = sb("agf", [128, 4, 64], F32)
        fa4 = sb("fa4", [128, 4, 64], F32)
        cqref = sb("cqref", [128, 4, 16], F32)
        seltmp = sb("seltmp", [128, 16, 64], F32)
        sel_s = sb("sel_s", [128, 16, 64], F32)
        ones1 = sb("ones1", [128, 64], F32)
        ff_s = sb("ff_s", [128, 16, 4], F32)
        fa_s = sb("fa_s", [128, 16, 4], F32)
        wI_s = sb("wI_s", [128, 16, 16], F32)
        qi_s = [sb("qi_s%d" % i, [128, 8, 128], BF16) for i in range(2)]
        ident = sb("ident", [128, 128], BF16)
        onesb = sb("onesb", [128, 128], BF16)
        tri = sb("tri", [128, 128], F32)
        onesf = sb("onesf", [128, 128], F32)
        mt_s = sb("mt_s", [128, 2432], BF16)
        normw = sb("normw", [128, L * 16], F32)
        fnormw = sb("fnormw", [128, 16], F32)
        foxb = sb("foxb", [128, L * 4], F32)
        lq_s = sb("lq_s", [128, L * 4 * 64], F32)
        subln = sb("subln", [128, L], F32)
        b31 = sb("b31", [128, 12], F32)
        sm = sb("sm", [128, 64], F32)
        whalf = sb("whalf", [128, NBIS], F32)
        pw2 = sb("pw2", [128, NBIS], F32)
        epsc = sb("epsc", [128, 1], F32)
        onec = sb("onec", [128, 1], F32)

        ps = [es.enter_context(nc.psum_tensor("ps%d" % i, [128, 512], F32)) for i in range(8)]
        sems = {e: es.enter_context(nc.semaphore("s_" + e)) for e in Prog.ENG}
        for i in range(P.ndma):
            sems["dma%d" % i] = es.enter_context(nc.semaphore("s_dma%d" % i))
        sems["cc"] = es.enter_context(nc.semaphore("s_cc"))
        block = es.enter_context(nc.Block())
        print("sbuf bytes remaining", nc.sbuf_bytes_remaining)

        Bps = [buf("ps%d" % i) for i in range(8)]
        rr = {"stg": 0, "pT": 0, "tmpf": 0, "rbuf": 0, "psL": 0, "psA": 0, "wb": 0, "q": 0, "ev": 0, "oz": 0, "psU": 0, "yb": 0, "rbufI": 0}

        def nxt(k, n):
            v = rr[k] % n
            rr[k] += 1
            return v

        def ld(eng, dst, src, name):
            P.dma(eng, lambda e: e.dma_start(out=dst, in_=src), writes=[buf(name)])
        ld("sync", normw[:], normw_in, "normw")
        ld("sync", fnormw[:], fnormw_in, "fnormw")
        ld("sync", foxb[:], foxb_in, "foxb")
        ld("sync", lq_s[:], lq_in, "lq")
        ld("sync", subln[:], subln_in, "subln")
        ld("sync", b31[:], b31_in, "b31")
        ld("sync", mt_s[:], mt_in, "mt")
        ld("sync", sel_s[:].rearrange("p a b -> p (a b)"), sel_in, "sel")
        ld("sync", ident[:], ident_in, "ident")
        ld("sync", tri[:], tri_in, "tri")
        P.op("vector", lambda e: e.memset(onesb[:], 1.0), writes=[buf("onesb")])
        P.op("vector", lambda e: e.memset(onesf[:], 1.0), writes=[buf("onesf")])
        P.op("vector", lambda e: e.memset(ones1[:], 1.0), writes=[buf("ones1")])
        P.op("vector", lambda e: e.memset(epsc[:], 1e-6), writes=[buf("epsc")])
        P.op("vector", lambda e: e.memset(onec[:], 1.0), writes=[buf("onec")])
        for i in range(NBIS):
            P.op("vector", lambda e, i=i: e.memset(pw2[:, i:i + 1], 2.0 ** -(i + 1)), writes=[buf("pw2")])

        def load_w(wfull, bname, col0, ncols, dup=False):
            i = nxt("wb", 2)
            t = wb[i]
            bw = buf("wb%d" % i)
            src = wfull[:, col0:col0 + ncols].rearrange("(k p) n -> p k n", p=128)
            P.dma("gpsimd", lambda e: e.dma_start(out=t[:, :, 0:ncols], in_=src), reads=[buf(bname)], writes=[bw])
            if dup:
                P.dma("gpsimd", lambda e: e.dma_start(out=t[:, :, ncols:2 * ncols], in_=src), reads=[buf(bname)], writes=[bw])
            return t, bw

        hT = arena
        Bar = [buf("arena%d" % j) for j in range(4)]

        def rstd_from_ssq(ps_ap, dst, n, width, bps, bdst):
            P.op("scalar", lambda e: e.activation(out=dst[:, :width], in_=ps_ap, func=AF.Ln, bias=epsc[:, 0:1], scale=1.0 / n),
                 reads=[bps, buf("epsc")], writes=[bdst])
            P.op("scalar", lambda e: e.activation(out=dst[:, :width], in_=dst[:, :width], func=AF.Exp, scale=-0.5),
                 reads=[bdst], writes=[bdst])

        def evac(dst_ap, src_ap, scale, reads, writes):
            i = nxt("ev", 2)
            if i == 0:
                P.op("scalar", lambda e: e.mul(dst_ap, src_ap, float(scale)), reads=reads, writes=writes)
            else:
                P.op("vector", lambda e: e.tensor_scalar(out=dst_ap, in0=src_ap, scalar1=float(scale), scalar2=None, op0=ALU.mult), reads=reads, writes=writes)

        def do_layer(layer):
            Bwin = "wf_in%d" % layer
            Wl = wf_in[layer]
            xsrc = xT_in if layer == 0 else xs_dd[(layer - 1) % 2]
            xs_d = xs_dd[layer % 2]
            Bx = buf("xin") if layer == 0 else buf("xs_d%d" % ((layer - 1) % 2))
            Bxo = buf("xs_d%d" % (layer % 2))
            for j in range(4):
                xt = arena
                pss = ps[6]
                for k in range(16):
                    o = ot[k % 2]
                    bo = buf("ot%d" % (k % 2))
                    P.dma("sync", lambda e, k=k, j=j, o=o: e.dma_start(out=o[:], in_=xsrc[k * 128:(k + 1) * 128, j * 512:(j + 1) * 512]), reads=[Bx], writes=[bo])
                    s_ = stg[k % 2]
                    bs_ = buf("stg%d" % (k % 2))
                    P.op("scalar", lambda e, o=o, s_=s_: e.activation(out=s_[:], in_=o[:], func=AF.Square), reads=[bo], writes=[bs_])
                    P.op("tensor", lambda e, s_=s_, k=k: e.matmul(pss[:, :], lhsT=onesb[:], rhs=s_[:], start=(k == 0), stop=(k == 15)),
                         reads=[bs_, buf("onesb")], writes=[Bps[6]])
                rstd_from_ssq(pss[:, :], ot[4], D, 512, Bps[6], buf("ot4"))
                for k in range(16):
                    o = ot[k % 2]
                    bo = buf("ot%d" % (k % 2))
                    P.dma("sync", lambda e, k=k, j=j, o=o: e.dma_start(out=o[:], in_=xsrc[k * 128:(k + 1) * 128, j * 512:(j + 1) * 512]), reads=[Bx], writes=[bo])
                    P.op("vector", lambda e, k=k, j=j, o=o: e.scalar_tensor_tensor(out=hT[:, k, j * 512:(j + 1) * 512], in0=o[:], scalar=normw[:, layer * 16 + k:layer * 16 + k + 1],
                                                                                   in1=ot[4][:], op0=ALU.mult, op1=ALU.mult),
                         reads=[bo, buf("ot4"), buf("normw")], writes=Bar, acc=True)
            P.dma("sync", lambda e: e.dma_start(out=hT_d, in_=hT[:]), reads=Bar, writes=[buf("hT_d")])

            def fm_group(col0, ncols, dest_fn, scale, dup=False):
                t, bw = load_w(Wl, Bwin, col0, ncols, dup=dup)
                nc_eff = 2 * ncols if dup else ncols
                for q in range((nc_eff + 127) // 128):
                    m = min(128, nc_eff - q * 128)
                    for j in range(4):
                        pi = 4 + nxt("psA", 2)
                        for k in range(16):
                            P.op("tensor", lambda e, pi=pi, k=k, q=q, j=j, m=m: e.matmul(ps[pi][:m, :], lhsT=t[:, k, q * 128:q * 128 + m], rhs=hT[:, k, j * 512:(j + 1) * 512],
                                                                                        start=(k == 0), stop=(k == 15)),
                                 reads=[bw] + Bar, writes=[Bps[pi]])
                        si = nxt("stg", 4)
                        evac(stg[si][:m, :], ps[pi][:m, :], scale, [Bps[pi]], [buf("stg%d" % si)])
                        dst, bd = dest_fn(q, j, m)
                        P.dma("sync", lambda e, dst=dst, si=si, m=m: e.dma_start(out=dst, in_=stg[si][:m, :]), reads=[buf("stg%d" % si)], writes=[bd], acc=True)

            def qdest(base):
                return lambda q, j, m: (qs_d[base + q, 0:m, j * 512:(j + 1) * 512], buf("qs_d"))

            def kdest(base):
                return lambda q, j, m: (kT_loc[(base + q) * 128:(base + q) * 128 + m, j * 512:(j + 1) * 512], buf("kT_loc"))

            def tm_group(col0, ncols, dest_fn, kind):
                t, bw = load_w(Wl, Bwin, col0, ncols)
                for blk in range(16):
                    pi = 4 + nxt("psA", 2)
                    for k in range(16):
                        P.op("tensor", lambda e, pi=pi, k=k, blk=blk: e.matmul(ps[pi][:, 0:ncols], lhsT=hT[:, k, blk * 128:(blk + 1) * 128], rhs=t[:, k, 0:ncols],
                                                                                start=(k == 0), stop=(k == 15)),
                             reads=[bw] + Bar, writes=[Bps[pi]])
                    dest_fn(blk, pi)

            for (mx, c0, s_q) in ((0, C_DSA, S128), (1, C_FOX, S128), (2, C_DIFF, S64), (3, C_DIL, S128)):
                for hh in range(2):
                    fm_group(c0 + hh * 256, 256, (lambda q, j, m, mx=mx, hh=hh: (qs_d[mx * 4 + hh * 2 + q, 0:m, j * 512:(j + 1) * 512], buf("qs_d"))), s_q)
                for hh in range(2):
                    fm_group(c0 + 512 + hh * 256, 256, (lambda q, j, m, mx=mx, hh=hh: (kT_loc[mx * 4 + hh * 2 + q][0:m, j * 512:(j + 1) * 512], buf("kT_loc"))), 1.0)
                for hh in range(2):
                    def vdest(blk, pi, mx=mx, hh=hh):
                        si = nxt("stg", 4)
                        evac(stg[si][:, 0:256], ps[pi][:, 0:256], 1.0, [Bps[pi]], [buf("stg%d" % si)])
                        P.dma("sync", lambda e, si=si: e.dma_start(out=v_loc[blk][:, mx * 512 + hh * 256:mx * 512 + hh * 256 + 256], in_=stg[si][:, 0:256]),
                              reads=[buf("stg%d" % si)], writes=[buf("v_loc")], acc=True)
                    tm_group(c0 + 1024 + hh * 256, 256, vdest, "v")
            for hh in range(4):
                fm_group(C_IQ + hh * 256, 256, (lambda q, j, m, hh=hh: (qs_d[16 + hh * 2 + q, 0:m, j * 512:(j + 1) * 512], buf("qs_d"))), 1.0)
            fm_group(C_IK, 64, (lambda q, j, m: (kT_loc[16][0:m, j * 512:(j + 1) * 512], buf("kT_loc"))), 1.0, dup=True)

            def wdest(blk, pi):
                P.op("vector", lambda e: e.tensor_copy(out=wI_s[:, blk, :], in_=ps[pi][:, 0:16]), reads=[Bps[pi]], writes=[buf("wI")], acc=True)
            tm_group(C_IW, 16, wdest, "w")

            def fdest(blk, pi):
                P.op("vector", lambda e: e.tensor_scalar(out=ff_s[:, blk, :], in0=ps[pi][:, 0:4], scalar1=1.0, scalar2=None, op0=ALU.mult), reads=[Bps[pi]], writes=[buf("ff")], acc=True)
            tm_group(C_FF, 4, fdest, "f")

            for h in range(4):
                P.op("vector", lambda e, h=h: e.tensor_scalar(out=fa_s[:, :, h], in0=ff_s[:, :, h], scalar1=foxb[:, layer * 4 + h:layer * 4 + h + 1], scalar2=None, op0=ALU.add),
                     reads=[buf("ff"), buf("foxb")], writes=[buf("fa")])
            P.op("scalar", lambda e: e.activation(out=fa_s[:], in_=fa_s[:], func=AF.Exp, scale=-1.0), reads=[buf("fa")], writes=[buf("fa")])
            P.op("scalar", lambda e: e.activation(out=fa_s[:], in_=fa_s[:], func=AF.Ln, bias=onec[:, 0:1], scale=1.0), reads=[buf("fa"), buf("onec")], writes=[buf("fa")])
            P.op("vector", lambda e: e.tensor_scalar(out=fa_s[:], in0=fa_s[:], scalar1=-1.0, scalar2=None, op0=ALU.mult), reads=[buf("fa")], writes=[buf("fa")])
            P.dma("sync", lambda e: e.dma_start(out=fa_loc[:, :], in_=fa_s[:].rearrange("p a b -> p (a b)")), reads=[buf("fa")], writes=[buf("fa_loc")])

            for q in range(17):
                P.cc(lambda e, q=q: e.collective_compute("AllGather", ALU.bypass, replica_groups=groups, ins=[kT_loc[q].ap().opt()], outs=[kT_all[q].ap().opt()]),
                     reads=[buf("kT_loc")], writes=[buf("kT_all%d" % q)])
            for q in range(16):
                P.cc(lambda e, q=q: e.collective_compute("AllGather", ALU.bypass, replica_groups=groups, ins=[v_loc[q].ap().opt()], outs=[v_all[q].ap().opt()]),
                     reads=[buf("v_loc")], writes=[buf("v_all%d" % q)])
            P.cc(lambda e: e.collective_compute("AllGather", ALU.bypass, replica_groups=groups, ins=[fa_loc.ap().opt()], outs=[fa_all.ap().opt()]),
                 reads=[buf("fa_loc")], writes=[buf("fa_all")])

            P.dma("sync", lambda e: e.dma_start(out=fa4[:], in_=fa_all.ap().rearrange("(c p) f -> p c f", p=128)), reads=[buf("fa_all")], writes=[buf("fa4")])
            for c in range(4):
                for h in range(4):
                    src = fa4[:, c, :].rearrange("p (j u h) -> p j u h", j=4, u=4, h=4)[:, :, :, h]
                    dst = agf[:, h, :].rearrange("p (j c u) -> p j c u", j=4, c=4, u=4)[:, :, c, :]
                    P.op("vector", lambda e, src=src, dst=dst: e.tensor_copy(out=dst, in_=src), reads=[buf("fa4")], writes=[buf("agf")], acc=True)
            agf2 = agf[:].rearrange("p a b -> p (a b)")
            P.op("tensor", lambda e: e.matmul(ps[6][:, 0:256], lhsT=tri[:], rhs=agf2, start=True, stop=True), reads=[buf("agf"), buf("tri")], writes=[Bps[6]])
            P.op("tensor", lambda e: e.matmul(ps[7][:, 0:256], lhsT=onesf[:], rhs=agf2, start=True, stop=True), reads=[buf("agf"), buf("onesf")], writes=[Bps[7]])
            P.op("vector", lambda e: e.tensor_copy(out=totf[:].rearrange("p a b -> p (a b)"), in_=ps[7][:, 0:256]), reads=[Bps[7]], writes=[buf("totf")])
            for h in range(4):
                P.op("vector", lambda e, h=h: e.tensor_tensor_scan(out=inclf[:, h, :], data0=ones1[:, :], data1=totf[:, h, :], initial=0.0, op0=ALU.mult, op1=ALU.add),
                     reads=[buf("totf"), buf("ones1")], writes=[buf("inclf")])
            P.op("vector", lambda e: e.tensor_tensor(out=negcum[:], in0=totf[:], in1=inclf[:], op=ALU.subtract), reads=[buf("totf"), buf("inclf")], writes=[buf("negcum")])
            P.op("vector", lambda e: e.tensor_tensor(out=negcum[:].rearrange("p a b -> p (a b)"), in0=negcum[:].rearrange("p a b -> p (a b)"), in1=ps[6][:, 0:256], op=ALU.subtract),
                 reads=[buf("negcum"), Bps[6]], writes=[buf("negcum")])
            for h in range(4):
                for blk in range(16):
                    P.op("vector", lambda e, h=h, blk=blk: e.tensor_tensor(out=seltmp[:, blk, :], in0=sel_s[:, blk, :], in1=totf[:, h, :], op=ALU.mult),
                         reads=[buf("sel"), buf("totf")], writes=[buf("seltmp")], acc=True)
                P.op("vector", lambda e, h=h: e.tensor_reduce(out=cqref[:, h, :], in_=seltmp[:], axis=AX.X, op=ALU.add), reads=[buf("seltmp")], writes=[buf("cqref")])

            lam0 = 0.8 - 0.6 * math.exp(-0.3 * layer)
            lb = layer * 256
            P.op("vector", lambda e: e.tensor_tensor(out=tmpf[0][:, 0:64], in0=lq_s[:, lb:lb + 64], in1=lq_s[:, lb + 64:lb + 128], op=ALU.mult), reads=[buf("lq")], writes=[buf("tmpf0")])
            P.op("vector", lambda e: e.tensor_reduce(out=sm[:, 0:1], in_=tmpf[0][:, 0:64], axis=AX.X, op=ALU.add), reads=[buf("tmpf0")], writes=[buf("sm")])
            P.op("vector", lambda e: e.tensor_tensor(out=tmpf[0][:, 0:64], in0=lq_s[:, lb + 128:lb + 192], in1=lq_s[:, lb + 192:lb + 256], op=ALU.mult), reads=[buf("lq")], writes=[buf("tmpf0")])
            P.op("vector", lambda e: e.tensor_reduce(out=sm[:, 1:2], in_=tmpf[0][:, 0:64], axis=AX.X, op=ALU.add), reads=[buf("tmpf0")], writes=[buf("sm")])
            P.op("scalar", lambda e: e.activation(out=sm[:, 2:4], in_=sm[:, 0:2], func=AF.Exp), reads=[buf("sm")], writes=[buf("sm")])
            P.op("vector", lambda e: e.tensor_tensor(out=sm[:, 4:5], in0=sm[:, 3:4], in1=sm[:, 2:3], op=ALU.subtract), reads=[buf("sm")], writes=[buf("sm")])
            P.op("vector", lambda e: e.tensor_scalar(out=sm[:, 5:6], in0=sm[:, 4:5], scalar1=-lam0, scalar2=None, op0=ALU.add), reads=[buf("sm")], writes=[buf("sm")])
            P.op("vector", lambda e: e.tensor_scalar(out=sm[:, 6:7], in0=subln[:, layer:layer + 1], scalar1=1.0 - lam0, scalar2=None, op0=ALU.mult), reads=[buf("subln")], writes=[buf("sm")])

            P.op("vector", lambda e: e.memset(sm[:, 40:41], 0.0),
                 writes=[buf("kT_s"), buf("v_s"), buf("fence")] + [buf("yb%d" % i) for i in range(4)] + [buf("acc%d" % i) for i in range(8)])
            def load_kv(mx, hh):
                ch = mx * 4 + hh
                src = kT_all[ch].ap().rearrange("(c r) (j i) -> r j c i", c=4, i=512)
                P.dma("sync", lambda e: e.dma_start(out=kT_s[:].rearrange("p (j c i) -> p j c i", j=4, c=4, i=512), in_=src), reads=[buf("kT_all%d" % ch)], writes=[buf("kT_s")])
                col = mx * 512 + hh * 128
                for c in range(4):
                    for blk in range(16):
                        srcv = v_all[blk][c * 128:(c + 1) * 128, col:col + 128]
                        kb = 16 * (blk // 4) + 4 * c + (blk % 4)
                        dstv = v_s[:, kb, :]
                        P.dma("gpsimd" if (blk % 2) else "sync", lambda e, srcv=srcv, dstv=dstv: e.dma_start(out=dstv, in_=srcv), reads=[buf("v_all%d" % blk)], writes=[buf("v_s")], acc=True)
                P.dma("sync", lambda e: e.dma_start(out=qT_s[:], in_=qs_d[ch]), reads=[buf("qs_d")], writes=[buf("qT_s")])

            def load_G(kind, gi):
                if kind == "mask":
                    P.dma("sync", lambda e: e.dma_start(out=Gadd[:], in_=mneg_in), writes=[buf("Gadd")])
                    return
                P.dma("sync", lambda e: e.dma_start(out=G_s[:], in_=gt_in[gi]), writes=[buf("G_s")])
                P.dma("sync", lambda e: e.dma_start(out=Gadd[:], in_=(mneg_in if kind == "bias" else cdil_in)), writes=[buf("Gadd")])
                P.op("gpsimd", lambda e: e.tensor_tensor(out=Gadd[:], in0=G_s[:], in1=Gadd[:], op=ALU.add), reads=[buf("Gadd"), buf("G_s")], writes=[buf("Gadd")])

            def indexer_gen():
                I_s = arena[:].rearrange("p a b -> p (a b)")[:, 0:16384].bitcast(F32)
                At_s = arena[:].rearrange("p a b -> p (a b)")[:, 16384:24576]
                ki_s = arena[:].rearrange("p a b -> p (a b)")[:, 24576:32768]
                BI = [Bar[0], Bar[1]]
                srck = kT_all[16].ap().rearrange("(c r) (j i) -> r j c i", c=4, i=512)
                P.dma("sync", lambda e: e.dma_start(out=ki_s.rearrange("p (j c i) -> p j c i", j=4, c=4, i=512), in_=srck), reads=[buf("kT_all16")], writes=[Bar[3]])
                for blk in range(16):
                    j, u = blk // 4, blk % 4
                    nkc = 4 * j + 4
                    nk = nkc * 512
                    qi = qi_s[blk % 2]
                    bq = buf("qi%d" % (blk % 2))
                    P.dma("sync", lambda e, qi=qi, blk=blk: e.dma_start(out=qi[:], in_=qs_d[16:24, :, blk * 128:(blk + 1) * 128].rearrange("m p t -> p m t")), reads=[buf("qs_d")], writes=[bq])
                    for kc in range(nkc):
                        for ih in range(16):
                            pi = 6 + nxt("psA", 2)
                            r0 = (ih % 2) * 64
                            P.op("tensor", lambda e, pi=pi, ih=ih, r0=r0, kc=kc, qi=qi: e.matmul(ps[pi][:, :], lhsT=qi[r0:r0 + 64, ih // 2, :], rhs=ki_s[r0:r0 + 64, kc * 512:(kc + 1) * 512],
                                                                                                  start=True, stop=True),
                                 reads=[bq, Bar[3]], writes=[Bps[pi]])
                            ri = nxt("rbufI", 4)
                            P.op("scalar", lambda e, pi=pi, ri=ri: e.activation(out=rbufI[ri][:], in_=ps[pi][:, :], func=AF.Relu), reads=[Bps[pi]], writes=[buf(rbufI_names[ri])])
                            if ih == 0:
                                P.op("vector", lambda e, ri=ri, kc=kc, blk=blk: e.tensor_scalar(out=I_s[:, kc * 512:(kc + 1) * 512], in0=rbufI[ri][:], scalar1=wI_s[:, blk, 0:1], scalar2=None, op0=ALU.mult),
                                     reads=[buf(rbufI_names[ri]), buf("wI")], writes=BI)
                            else:
                                P.op("vector", lambda e, ri=ri, kc=kc, blk=blk, ih=ih: e.scalar_tensor_tensor(out=I_s[:, kc * 512:(kc + 1) * 512], in0=rbufI[ri][:], scalar=wI_s[:, blk, ih:ih + 1],
                                                                                                              in1=I_s[:, kc * 512:(kc + 1) * 512], op0=ALU.mult, op1=ALU.add),
                                     reads=[buf(rbufI_names[ri]), buf("wI")] + BI, writes=BI)
                            yield
                    P.op("vector", lambda e, nk=nk: e.tensor_reduce(out=sm[:, 8:9], in_=I_s[:, 0:nk], axis=AX.X, op=ALU.min), reads=BI, writes=[buf("smI")])
                    for kc in range(4 * j, nkc):
                        v0 = 512 * kc - 2048 * j - 128 * u + 384
                        P.op("vector", lambda e, kc=kc, v0=v0: e.tensor_tensor(out=I_s[:, kc * 512:(kc + 1) * 512], in0=I_s[:, kc * 512:(kc + 1) * 512], in1=mt_s[:, v0:v0 + 512], op=ALU.add),
                             reads=BI + [buf("mt")], writes=BI)
                    P.op("vector", lambda e, nk=nk: e.tensor_reduce(out=sm[:, 9:10], in_=I_s[:, 0:nk], axis=AX.X, op=ALU.max), reads=BI, writes=[buf("smI")])
                    P.op("vector", lambda e: e.tensor_scalar(out=sm[:, 10:11], in0=sm[:, 8:9], scalar1=-1.0, scalar2=None, op0=ALU.add), reads=[buf("smI")], writes=[buf("smI")])
                    P.op("vector", lambda e: e.tensor_tensor(out=sm[:, 11:12], in0=sm[:, 9:10], in1=sm[:, 8:9], op=ALU.subtract), reads=[buf("smI")], writes=[buf("smI")])
                    P.op("vector", lambda e: e.tensor_scalar(out=sm[:, 11:12], in0=sm[:, 11:12], scalar1=2.0, scalar2=None, op0=ALU.add), reads=[buf("smI")], writes=[buf("smI")])
                    P.op("vector", lambda e: e.tensor_scalar(out=whalf[:], in0=pw2[:], scalar1=sm[:, 11:12], scalar2=None, op0=ALU.mult), reads=[buf("smI"), buf("pw2")], writes=[buf("whalf")])
                    yield
                    for it in range(NBIS):
                        P.op("vector", lambda e, it=it: e.tensor_tensor(out=sm[:, 12:13], in0=sm[:, 10:11], in1=whalf[:, it:it + 1], op=ALU.add), reads=[buf("smI"), buf("whalf")], writes=[buf("smI")])
                        npc = nk // 1024
                        for pc in range(npc):
                            cdst = 13 if (pc % 2 == (npc - 1) % 2) else 16
                            csrc = 29 - cdst
                            P.op("vector", lambda e, pc=pc, cdst=cdst, csrc=csrc: e.tensor_scalar(out=At_s[:, pc * 1024:(pc + 1) * 1024], in0=I_s[:, pc * 1024:(pc + 1) * 1024], scalar1=sm[:, 12:13],
                                                                                                   scalar2=(None if pc == 0 else sm[:, csrc:csrc + 1]), op0=ALU.is_ge, op1=ALU.add,
                                                                                                   accum_out=sm[:, cdst:cdst + 1]),
                                 reads=BI + [buf("smI")], writes=[Bar[2], buf("smI")])
                            yield
                        P.op("vector", lambda e, it=it: e.tensor_scalar(out=sm[:, 14:15], in0=sm[:, 13:14], scalar1=TOPK - 0.5, scalar2=whalf[:, it:it + 1], op0=ALU.is_ge, op1=ALU.mult),
                             reads=[buf("smI"), buf("whalf")], writes=[buf("smI")])
                        P.op("vector", lambda e: e.tensor_tensor(out=sm[:, 10:11], in0=sm[:, 10:11], in1=sm[:, 14:15], op=ALU.add), reads=[buf("smI")], writes=[buf("smI")])
                        yield
                    P.op("vector", lambda e, nk=nk: e.tensor_scalar(out=At_s[:, 0:nk], in0=I_s[:, 0:nk], scalar1=sm[:, 10:11], scalar2=NEG, op0=ALU.is_lt, op1=ALU.mult),
                         reads=BI + [buf("smI")], writes=[Bar[2]])
                    P.dma("sync", lambda e, blk=blk, nk=nk: e.dma_start(out=A_d[blk, :, 0:nk], in_=At_s[:, 0:nk]), reads=[Bar[2]], writes=[buf("A_d")], acc=True)
                    yield

            idx_it = indexer_gen()

            def idx_step(k):
                alive = True
                for _ in range(k):
                    try:
                        next(idx_it)
                    except StopIteration:
                        alive = False
                        break
                return alive

            LB = (0, 1)

            def flash(j, krows, kb_lo, mode, h, gi, dsa_A=None):
                kbs = list(range(kb_lo, 16 * j + 16))
                ob = 2 + 2 * nxt("oz", 2)
                zb = ob + 1
                pend = None

                def pvz(pi, kb, first, last):
                    bp = buf("pT%d" % pi)
                    P.op("tensor", lambda e: e.matmul(ps[ob][:, :], lhsT=v_s[:, kb, :], rhs=pT[pi][:], start=first, stop=last),
                         reads=[buf("v_s"), bp], writes=[Bps[ob]])
                    P.op("tensor", lambda e: e.matmul(ps[zb][:, :], lhsT=onesb[:], rhs=pT[pi][:], start=first, stop=last),
                         reads=[buf("onesb"), bp], writes=[Bps[zb]])
                for n, kb in enumerate(kbs):
                    ds_ = 2048 * j - 128 * kb
                    need_add = (ds_ < 128) if mode == "fox" else (ds_ < DFAR)
                    li = LB[nxt("psL", 2)]
                    Ls = ps[li]
                    P.op("tensor", lambda e, Ls=Ls, kb=kb: e.matmul(Ls[:, :], lhsT=kT_s[krows[0]:krows[1], kb * 128:(kb + 1) * 128], rhs=qT_s[krows[0]:krows[1], j * 512:(j + 1) * 512],
                                                                      start=True, stop=(dsa_A is None and not need_add)),
                         reads=[buf("kT_s"), buf("qT_s")], writes=[Bps[li]])
                    if dsa_A is not None:
                        At, bA, off = dsa_A(kb)
                        for u in range(4):
                            P.op("tensor", lambda e, Ls=Ls, u=u, At=At, off=off: e.matmul(Ls[:, u * 128:(u + 1) * 128], lhsT=At[:, u, off:off + 128], rhs=ident[:], start=False, stop=(not need_add),
                                                                                          skip_group_check=True),
                                 reads=[bA, buf("ident")], writes=[Bps[li]])
                    src, bsrc = Ls, Bps[li]
                    if need_add:
                        u0 = min(ds_, DFAR) + GOFF
                        P.op("tensor", lambda e, Ls=Ls, u0=u0: e.matmul(Ls[:, :], lhsT=ident[:], rhs=Gadd[:, u0:u0 + 512], start=False, stop=True, skip_group_check=True),
                             reads=[buf("Gadd"), buf("ident")], writes=[Bps[li]])
                    pi = nxt("pT", 4)
                    bp = buf("pT%d" % pi)
                    if mode == "fox":
                        for u in range(4):
                            P.op("scalar", lambda e, pi=pi, src=src, u=u, kb=kb: e.activation(out=pT[pi][:, u * 128:(u + 1) * 128], in_=src[:, u * 128:(u + 1) * 128], func=AF.Exp,
                                                                                                bias=bm[:, 4 * j + u, kb:kb + 1], scale=1.0),
                                 reads=[bsrc, buf("bm")], writes=[bp], acc=(u > 0))
                    elif need_add:
                        P.op("scalar", lambda e, pi=pi, src=src: e.activation(out=pT[pi][:], in_=src[:, :], func=AF.Exp), reads=[bsrc], writes=[bp])
                    else:
                        P.op("scalar", lambda e, pi=pi, src=src: e.activation(out=pT[pi][:], in_=src[:, :], func=AF.Exp, bias=b31[:, gi:gi + 1], scale=1.0),
                             reads=[bsrc, buf("b31")], writes=[bp])
                    if pend is not None:
                        pvz(*pend)
                    pend = (pi, kb, n == 0, n == len(kbs) - 1)
                    if dsa_A is None:
                        idx_step(2)
                pvz(*pend)
                return ob, zb

            def normalize(dst, bdst, oz):
                ob, zb = oz
                P.op("vector", lambda e: e.reciprocal(out=ot[4][:], in_=ps[zb][:, :]), reads=[Bps[zb]], writes=[buf("ot4")])
                P.op("vector", lambda e: e.tensor_tensor(out=dst[:], in0=ps[ob][:, :], in1=ot[4][:], op=ALU.mult), reads=[Bps[ob], buf("ot4")], writes=[bdst])

            def store_o(src, bsrc, ch, j):
                P.dma("sync", lambda e: e.dma_start(out=ao_d[ch, :, j * 512:(j + 1) * 512], in_=src[:]), reads=[bsrc], writes=[buf("ao_d")], acc=True)

            load_G("mask", 0)
            for h in range(4):
                load_kv(1, h)
                for blk in range(16):
                    P.op("vector", lambda e, h=h, blk=blk: e.tensor_scalar(out=bm[:, blk, :], in0=negcum[:, h, :], scalar1=cqref[:, h, blk:blk + 1], scalar2=None, op0=ALU.add),
                         reads=[buf("negcum"), buf("cqref")], writes=[buf("bm")], acc=True)
                for j in range(4):
                    oz = flash(j, (0, 128), 0, "fox", h, 0)
                    normalize(ot[0], buf("ot0"), oz)
                    store_o(ot[0], buf("ot0"), 4 + h, j)
            for h in range(4):
                load_G("dil", 8 + h)
                load_kv(3, h)
                for j in range(4):
                    kb_lo = max(0, 16 * j - 16)
                    oz = flash(j, (0, 128), kb_lo, "dil", h, 8 + h)
                    normalize(ot[0], buf("ot0"), oz)
                    store_o(ot[0], buf("ot0"), 12 + h, j)
            for h in range(4):
                load_G("bias", 4 + h)
                load_kv(2, h)
                for j in range(4):
                    oz = flash(j, (0, 64), 0, "bias", h, 4 + h)
                    normalize(ot[0], buf("ot0"), oz)
                    oz = flash(j, (64, 128), 0, "bias", h, 4 + h)
                    normalize(ot[1], buf("ot1"), oz)
                    P.op("vector", lambda e: e.scalar_tensor_tensor(out=ot[2][:], in0=ot[1][:], scalar=sm[:, 5:6], in1=ot[0][:], op0=ALU.mult, op1=ALU.add),
                         reads=[buf("ot0"), buf("ot1"), buf("sm")], writes=[buf("ot2")])
                    si = nxt("stg", 4)
                    P.op("scalar", lambda e, si=si: e.activation(out=stg[si][:], in_=ot[2][:], func=AF.Square), reads=[buf("ot2")], writes=[buf("stg%d" % si)])
                    P.op("tensor", lambda e, si=si: e.matmul(ps[6][:, :], lhsT=onesb[:], rhs=stg[si][:], start=True, stop=True), reads=[buf("stg%d" % si), buf("onesb")], writes=[Bps[6]])
                    rstd_from_ssq(ps[6][:, :], ot[3], 128, 512, Bps[6], buf("ot3"))
                    P.op("vector", lambda e: e.scalar_tensor_tensor(out=ot[0][:], in0=ot[2][:], scalar=sm[:, 6:7], in1=ot[3][:], op0=ALU.mult, op1=ALU.mult),
                         reads=[buf("ot2"), buf("ot3"), buf("sm")], writes=[buf("ot0")])
                    store_o(ot[0], buf("ot0"), 8 + h, j)

            while idx_step(64):
                pass
            Apc = [arena[:].rearrange("p a b -> p (a b)")[:, i * 8192:(i + 1) * 8192].rearrange("p (u s) -> p u s", u=4) for i in range(2)]
            for h in range(4):
                load_G("bias", h)
                load_kv(0, h)
                for j in range(4):
                    state = {}

                    def dsa_A(kb, j=j, state=state):
                        pc = kb // 16
                        if state.get("pc") != pc:
                            ai = nxt("q", 2)
                            state["pc"], state["ai"] = pc, ai
                            for u in range(4):
                                P.dma("sync", lambda e, ai=ai, u=u, pc=pc: e.dma_start(out=Apc[ai][:, u, :], in_=A_d[4 * j + u, :, pc * 2048:(pc + 1) * 2048]), reads=[buf("A_d")], writes=[Bar[ai]], acc=True)
                        ai = state["ai"]
                        return Apc[ai], Bar[ai], (kb % 16) * 128
                    oz = flash(j, (0, 128), 0, "bias", h, h, dsa_A=dsa_A)
                    normalize(ot[0], buf("ot0"), oz)
                    store_o(ot[0], buf("ot0"), h, j)

            P.op("vector", lambda e: e.memset(sm[:, 40:41], 0.0),
                 writes=[buf("kT_s"), buf("v_s"), buf("fence")] + [buf("yb%d" % i) for i in range(4)] + [buf("acc%d" % i) for i in range(8)])
            Wbr, Wout = wf_br[layer], wf_out[layer]
            Bbr, Bout = "wf_br%d" % layer, "wf_out%d" % layer
            P.dma("sync", lambda e: e.dma_start(out=hT[:], in_=hT_d), reads=[buf("hT_d")], writes=Bar)
            ybufs = [kT_s[:, i * 2048:(i + 1) * 2048].rearrange("p (k t) -> p k t", k=4) for i in range(4)]
            accs = [v_s[:].rearrange("p a b -> p (a b)")[:, i * 1024:(i + 1) * 1024].bitcast(F32) for i in range(8)]
            mbuf = G_s[:].bitcast(BF16)[:, 0:8192].rearrange("p (k t) -> p k t", k=16)
            for zg in range(8):
                t, bw = load_w(Wl, Bwin, C_Z + zg * 256, 256)
                for q in range(2):
                    ch = zg * 2 + q
                    for j in range(4):
                        pi = 4 + nxt("psA", 2)
                        for k in range(16):
                            P.op("tensor", lambda e, pi=pi, k=k, q=q, t=t, j=j: e.matmul(ps[pi][:, :], lhsT=t[:, k, q * 128:(q + 1) * 128], rhs=hT[:, k, j * 512:(j + 1) * 512], start=(k == 0), stop=(k == 15)),
                                 reads=[bw] + Bar, writes=[Bps[pi]])
                        ti = nxt("tmpf", 2)
                        P.op("scalar", lambda e, pi=pi, ti=ti: e.activation(out=tmpf[ti][:], in_=ps[pi][:, :], func=AF.Silu), reads=[Bps[pi]], writes=[buf("tmpf%d" % ti)])
                        oi = nxt("rbuf", 2)
                        P.dma("sync", lambda e, oi=oi, ch=ch, j=j: e.dma_start(out=rbuf[oi][:], in_=ao_d[ch, :, j * 512:(j + 1) * 512]), reads=[buf("ao_d")], writes=[buf("rbuf%d" % oi)])
                        si = nxt("stg", 4)
                        P.op("vector", lambda e, ti=ti, oi=oi, si=si: e.tensor_tensor(out=stg[si][:], in0=tmpf[ti][:], in1=rbuf[oi][:], op=ALU.mult),
                             reads=[buf("tmpf%d" % ti), buf("rbuf%d" % oi)], writes=[buf("stg%d" % si)])
                        P.dma("sync", lambda e, si=si, ch=ch, j=j: e.dma_start(out=yT_d[ch, :, j * 512:(j + 1) * 512], in_=stg[si][:]), reads=[buf("stg%d" % si)], writes=[buf("yT_d")], acc=True)
            for ng in range(8):
                for b in range(4):
                    tg, bwg = load_w(Wl, Bwin, C_G + b * 2048 + ng * 256, 256)
                    i2 = nxt("wb", 2)
                    tb_, bwb = wb[i2], buf("wb%d" % i2)
                    srcb = Wbr[b * 512:(b + 1) * 512, ng * 256:(ng + 1) * 256].rearrange("(k p) n -> p k n", p=128)
                    P.dma("gpsimd", lambda e, tb_=tb_, srcb=srcb: e.dma_start(out=tb_[:, 0:4, :], in_=srcb), reads=[buf(Bbr)], writes=[bwb])
                    for j in range(4):
                        yi = nxt("yb", 4)
                        yb, byb = ybufs[yi], buf("yb%d" % yi)
                        P.dma("sync", lambda e, yb=yb, b=b, j=j: e.dma_start(out=yb, in_=yT_d[b * 4:(b + 1) * 4, :, j * 512:(j + 1) * 512].rearrange("k p t -> p k t")), reads=[buf("yT_d")], writes=[byb])
                        for q in range(2):
                            pg = 4 + nxt("psA", 2)
                            for k in range(16):
                                P.op("tensor", lambda e, pg=pg, k=k, q=q, tg=tg, j=j: e.matmul(ps[pg][:, :], lhsT=tg[:, k, q * 128:(q + 1) * 128], rhs=hT[:, k, j * 512:(j + 1) * 512], start=(k == 0), stop=(k == 15)),
                                     reads=[bwg] + Bar, writes=[Bps[pg]])
                            pu = 6 + nxt("psU", 2)
                            for k in range(4):
                                P.op("tensor", lambda e, pu=pu, k=k, q=q, tb_=tb_, yb=yb: e.matmul(ps[pu][:, :], lhsT=tb_[:, k, q * 128:(q + 1) * 128], rhs=yb[:, k, :], start=(k == 0), stop=(k == 3)),
                                     reads=[bwb, byb], writes=[Bps[pu]])
                            ti = nxt("tmpf", 2)
                            P.op("scalar", lambda e, pg=pg, ti=ti: e.activation(out=tmpf[ti][:], in_=ps[pg][:, :], func=AF.Sigmoid), reads=[Bps[pg]], writes=[buf("tmpf%d" % ti)])
                            ai = q * 4 + j
                            acc, bacc = accs[ai], buf("acc%d" % ai)
                            if b == 0:
                                P.op("vector", lambda e, ti=ti, pu=pu, acc=acc: e.tensor_tensor(out=acc, in0=ps[pu][:, :], in1=tmpf[ti][:], op=ALU.mult),
                                     reads=[Bps[pu], buf("tmpf%d" % ti)], writes=[bacc])
                            else:
                                P.op("vector", lambda e, ti=ti, pu=pu: e.tensor_tensor(out=tmpf[ti][:], in0=ps[pu][:, :], in1=tmpf[ti][:], op=ALU.mult),
                                     reads=[Bps[pu], buf("tmpf%d" % ti)], writes=[buf("tmpf%d" % ti)])
                                P.op("gpsimd", lambda e, ti=ti, acc=acc: e.tensor_tensor(out=acc, in0=acc, in1=tmpf[ti][:], op=ALU.add),
                                     reads=[buf("tmpf%d" % ti), bacc], writes=[bacc])
                            if b == 3:
                                n = ng * 2 + q
                                si = nxt("stg", 4)
                                P.op("scalar", lambda e, acc=acc, si=si: e.activation(out=stg[si][:], in_=acc, func=AF.Copy), reads=[bacc], writes=[buf("stg%d" % si)])
                                P.dma("sync", lambda e, si=si, n=n, j=j: e.dma_start(out=mT_d[n, :, j * 512:(j + 1) * 512], in_=stg[si][:]), reads=[buf("stg%d" % si)], writes=[buf("mT_d")], acc=True)
            for j in range(4):
                P.dma("sync", lambda e, j=j: e.dma_start(out=mbuf, in_=mT_d[:, :, j * 512:(j + 1) * 512].rearrange("k p t -> p k t")), reads=[buf("mT_d")], writes=[buf("G_s")])
                for ng in range(8):
                    to, bwo = load_w(Wout, Bout, ng * 256, 256)
                    for q in range(2):
                        n = ng * 2 + q
                        po = 4 + nxt("psA", 2)
                        for k in range(16):
                            P.op("tensor", lambda e, po=po, k=k, q=q, to=to: e.matmul(ps[po][:, :], lhsT=to[:, k, q * 128:(q + 1) * 128], rhs=mbuf[:, k, :], start=(k == 0), stop=(k == 15)),
                                 reads=[bwo, buf("G_s")], writes=[Bps[po]])
                        oi = nxt("rbuf", 2)
                        P.dma("sync", lambda e, oi=oi, n=n, j=j: e.dma_start(out=rbuf[oi][:], in_=xsrc[n * 128:(n + 1) * 128, j * 512:(j + 1) * 512]), reads=[Bx], writes=[buf("rbuf%d" % oi)])
                        P.op("vector", lambda e, oi=oi, po=po: e.tensor_tensor(out=rbuf[oi][:], in0=ps[po][:, :], in1=rbuf[oi][:], op=ALU.add),
                             reads=[Bps[po], buf("rbuf%d" % oi)], writes=[buf("rbuf%d" % oi)])
                        P.dma("sync", lambda e, oi=oi, n=n, j=j: e.dma_start(out=xs_d[n * 128:(n + 1) * 128, j * 512:(j + 1) * 512], in_=rbuf[oi][:]), reads=[buf("rbuf%d" % oi)], writes=[Bxo], acc=True)

        for layer_ in range(L):
            do_layer(layer_)

        xs_d = xs_dd[(L - 1) % 2]
        Bx = buf("xs_d%d" % ((L - 1) % 2))
        Bout_ = buf("yT")
        for j in range(4):
            for k in range(16):
                o = ot[k % 2]
                bo = buf("ot%d" % (k % 2))
                P.dma("sync", lambda e, k=k, j=j, o=o: e.dma_start(out=o[:], in_=xs_d[k * 128:(k + 1) * 128, j * 512:(j + 1) * 512]), reads=[Bx], writes=[bo])
                s_ = stg[k % 2]
                bs_ = buf("stg%d" % (k % 2))
                P.op("scalar", lambda e, o=o, s_=s_: e.activation(out=s_[:], in_=o[:], func=AF.Square), reads=[bo], writes=[bs_])
                P.op("tensor", lambda e, s_=s_, k=k: e.matmul(ps[6][:, :], lhsT=onesb[:], rhs=s_[:], start=(k == 0), stop=(k == 15)), reads=[bs_, buf("onesb")], writes=[Bps[6]])
            rstd_from_ssq(ps[6][:, :], ot[4], D, 512, Bps[6], buf("ot4"))
            for k in range(16):
                o = ot[k % 2]
                bo = buf("ot%d" % (k % 2))
                P.dma("sync", lambda e, k=k, j=j, o=o: e.dma_start(out=o[:], in_=xs_d[k * 128:(k + 1) * 128, j * 512:(j + 1) * 512]), reads=[Bx], writes=[bo])
                ri = nxt("rbuf", 2)
                P.op("vector", lambda e, k=k, o=o, ri=ri: e.scalar_tensor_tensor(out=rbuf[ri][:], in0=o[:], scalar=fnormw[:, k:k + 1], in1=ot[4][:], op0=ALU.mult, op1=ALU.mult),
                     reads=[bo, buf("ot4"), buf("fnormw")], writes=[buf("rbuf%d" % ri)])
                P.dma("sync", lambda e, k=k, j=j, ri=ri: e.dma_start(out=yT_out[k * 128:(k + 1) * 128, j * 512:(j + 1) * 512], in_=rbuf[ri][:]), reads=[buf("rbuf%d" % ri)], writes=[Bout_], acc=True)
        P.finish("sync", [Bout_])
        P.emit(block, sems)
    print("ops", P.nops)
    return nc


def host_prep(inputs, depth=DEPTH):
    x = np.asarray(inputs["x"], np.float32)
    L = depth
    rel = np.asarray(inputs["rel_bias"], np.float32)
    maps = []
    sl = np.arange(128)[:, None]
    uu = np.arange(GL)[None, :]
    import ml_dtypes
    bf = ml_dtypes.bfloat16
    ident = np.eye(128, dtype=np.float32).astype(bf)
    tri = (np.arange(128)[:, None] <= np.arange(128)[None, :]).astype(np.float32)
    normw = np.ascontiguousarray(np.asarray(inputs["norm_w"], np.float32)[:L].reshape(L, 16, 128).transpose(2, 0, 1).reshape(128, L * 16))
    fnormw = np.ascontiguousarray(np.asarray(inputs["final_norm_w"], np.float32).reshape(16, 128).T)
    foxb = np.ascontiguousarray(np.broadcast_to(np.asarray(inputs["fox_b_f"], np.float32)[:L].reshape(1, L * 4), (128, L * 4)))
    lq = np.stack([np.asarray(inputs[k], np.float32)[:L] for k in ("diff_lq1", "diff_lk1", "diff_lq2", "diff_lk2")], axis=1)
    lq = np.ascontiguousarray(np.broadcast_to(lq.reshape(1, L * 256), (128, L * 256)))
    subln = np.ascontiguousarray(np.asarray(inputs["diff_subln_w"], np.float32)[:L].T)
    b31 = np.ascontiguousarray(np.broadcast_to(rel[31:32, :], (128, 12)))
    w_in = np.asarray(inputs["w_in"], np.float32)
    w_br = np.asarray(inputs["w_branch"], np.float32).reshape(DEPTH, 2048, 2048)
    w_out = np.asarray(inputs["w_out"], np.float32)
    for core in range(8):
        b, c = core // 4, core % 4
        toks = np.concatenate([np.arange(512 * (4 * j + c), 512 * (4 * j + c + 1)) for j in range(4)])
        xT = np.ascontiguousarray(x[b, toks, :].T)
        dist = uu - GOFF + 512 * c - sl
        bidx = t5_bucket_np(dist)
        gt = np.ascontiguousarray(rel[bidx, :].transpose(2, 0, 1))
        mneg = np.where(dist >= 0, 0.0, NEG).astype(np.float32).astype(bf)
        nval = ((dist >= 0) & (dist <= 128)).astype(np.int32) + ((dist >= 0) & (dist % 4 == 0) & (dist <= 512)).astype(np.int32) \
            + ((dist >= 0) & (dist % 16 == 0) & (dist <= 2048)).astype(np.int32)
        cdil = np.where(nval > 0, np.log(np.maximum(nval, 1).astype(np.float32)), NEG).astype(np.float32).astype(bf)
        vv = np.arange(2432)[None, :]
        mt = np.where((vv - 384 - 512 * c - sl) > 0, -1e9, 0.0).astype(np.float32).astype(bf)
        blk = np.arange(16)
        kb_own = 16 * (blk // 4) + 4 * c + (blk % 4)
        sel = (np.arange(64)[None, :] <= kb_own[:, None]).astype(np.float32)
        sel = np.ascontiguousarray(np.broadcast_to(sel.reshape(1, 1024), (128, 1024)))
        maps.append({
            "xT": xT,
            "w_in": w_in[:L], "w_br": w_br[:L], "w_out": w_out[:L],
            "normw": normw, "fnormw": fnormw, "foxb": foxb, "lq": lq, "subln": subln,
            "gt": gt, "mneg": mneg, "cdil": cdil, "b31": b31, "mt": mt, "sel": sel, "ident": ident, "tri": tri,
        })
    return maps


def run(inputs, depth=DEPTH):
    nc = build(depth)
    maps = host_prep(inputs, depth)
    res = run_bass_kernel_spmd(nc, maps, core_ids=list(range(8)))
    out = np.zeros((2, S, D), np.float32)
    for core in range(8):
        b, c = core // 4, core % 4
        toks = np.concatenate([np.arange(512 * (4 * j + c), 512 * (4 * j + c + 1)) for j in range(4)])
        out[b, toks, :] = res.results[core]["yT"].T
    return out


def kernel(**inputs):
    return run(inputs, DEPTH)
```

```python
import math
from contextlib import ExitStack

import numpy as np
import concourse.bass as bass
import concourse.mybir as mybir
from concourse.bass_utils import run_bass_kernel_spmd

F32 = mybir.dt.float32
BF16 = mybir.dt.bfloat16
ALU = mybir.AluOpType
AF = mybir.ActivationFunctionType
AX = mybir.AxisListType

D = 2048
S = 8192
NT = 2048
DEPTH = 4
N_IN = 17492
NEG = -30000.0
GL = 4608
GOFF = 1920
DFAR = 2176
C_Z, C_G = 0, 2048
C_DSA, C_IQ, C_IK, C_IW, C_FOX, C_FF, C_DIFF, C_DIL = 10240, 11776, 12800, 12864, 12880, 14416, 14420, 15956
S128 = 128 ** -0.5
S64 = 64 ** -0.5
TOPK = 256
NBIS = 18


class Buf:
    __slots__ = ("w", "r")

    def __init__(self):
        self.w = {}
        self.r = {}


class Prog:
    ENG = ("sync", "scalar", "vector", "gpsimd", "tensor")

    def __init__(self, ndma=24):
        self.streams = {e: [] for e in self.ENG}
        self.count = {e: 0 for e in self.ENG}
        self.seen = {e: {} for e in self.ENG}
        self.ndma = ndma
        self.dma_i = 0
        self.dma_cnt = [0] * ndma
        self.cc_cnt = 0
        self.nops = 0

    def _deps(self, eng, reads, writes, acc=False):
        deps = {}

        def add(k, v):
            if deps.get(k, 0) < v:
                deps[k] = v
        for b in reads:
            for k, v in b.w.items():
                add(k, v)
        for b in writes:
            if not acc:
                for k, v in b.w.items():
                    add(k, v)
            for k, v in b.r.items():
                add(k, v)
        waits = []
        for k, v in deps.items():
            if k == eng and eng == "tensor":
                continue
            if self.seen[eng].get(k, 0) < v:
                self.seen[eng][k] = v
                waits.append((k, v))
        return waits

    def _record(self, me, reads, writes, acc=False):
        for b in reads:
            if b.r.get(me[0], 0) < me[1]:
                b.r[me[0]] = me[1]
        for b in writes:
            if acc:
                b.w[me[0]] = me[1]
            else:
                b.w = {me[0]: me[1]}
                b.r = {}
        self.nops += 1

    def op(self, eng, fn, reads=(), writes=(), acc=False):
        waits = self._deps(eng, reads, writes, acc)
        self.count[eng] += 1
        me = (eng, self.count[eng])
        self.streams[eng].append((waits, fn, eng, 1))
        self._record(me, reads, writes, acc)

    def dma(self, eng, fn, reads=(), writes=(), acc=False):
        i = self.dma_i % self.ndma
        self.dma_i += 1
        key = "dma%d" % i
        waits = self._deps(eng, reads, writes, acc)
        prev = self.dma_cnt[i]
        if prev and self.seen[eng].get(key, 0) < prev:
            self.seen[eng][key] = prev
            waits.append((key, prev))
        self.dma_cnt[i] += 16
        me = (key, self.dma_cnt[i])
        self.streams[eng].append((waits, fn, key, 16))
        self._record(me, reads, writes, acc)

    def cc(self, fn, reads=(), writes=()):
        eng = "gpsimd"
        waits = self._deps(eng, reads, writes)
        self.cc_cnt += 1
        me = ("cc", self.cc_cnt)
        self.streams[eng].append((waits, fn, "cc", None))
        self._record(me, reads, writes)

    def finish(self, eng, bufs):
        waits = self._deps(eng, bufs, ())
        self.streams[eng].append((waits, None, None, 0))

    def emit(self, block, sems):
        def mk(e):
            def body(engh):
                for waits, fn, key, inc in self.streams[e]:
                    for k, v in waits:
                        engh.wait_ge(sems[k], v)
                    if fn is not None:
                        ins = fn(engh)
                        if inc is None:
                            ins.then_inc(sems[key])
                        else:
                            ins.then_inc(sems[key], inc)
            return body
        block.sync(mk("sync"))
        block.scalar(mk("scalar"))
        block.vector(mk("vector"))
        block.gpsimd(mk("gpsimd"))
        block.tensor(mk("tensor"))


def t5_bucket_np(dist):
    dist = np.maximum(dist, 0)
    d = np.maximum(dist, 16).astype(np.float32)
    large = 16 + (np.log(d / np.float32(16)) / np.float32(math.log(2048 / 16)) * np.float32(16)).astype(np.int32)
    large = np.minimum(large, 31)
    return np.where(dist < 16, dist, large)


def build(depth=DEPTH):
    nc = bass.Bass("TRN2", target_bir_lowering=False)
    P = Prog()
    L = depth

    def din(name, shape, dt=F32):
        return nc.dram_tensor(name, list(shape), dt, kind="ExternalInput").ap()

    def dscr(name, shape, dt):
        return nc.dram_tensor(name, list(shape), dt)

    xT_in = din("xT", [D, NT])
    w_in_a = din("w_in", [L, D, N_IN])
    w_br_a = din("w_br", [L, D, D])
    w_out_a = din("w_out", [L, D, D])
    normw_in = din("normw", [128, L * 16])
    fnormw_in = din("fnormw", [128, 16])
    foxb_in = din("foxb", [128, L * 4])
    lq_in = din("lq", [128, L * 4 * 64])
    subln_in = din("subln", [128, L])
    gt_in = din("gt", [12, 128, GL])
    mneg_in = din("mneg", [128, GL], BF16)
    cdil_in = din("cdil", [128, GL], BF16)
    b31_in = din("b31", [128, 12])
    mt_in = din("mt", [128, 2432], BF16)
    sel_in = din("sel", [128, 16 * 64])
    ident_in = din("ident", [128, 128], BF16)
    tri_in = din("tri", [128, 128])
    yT_out = nc.dram_tensor("yT", [D, NT], F32, kind="ExternalOutput").ap()

    wf_in = [w_in_a[l] for l in range(L)]
    wf_br = [w_br_a[l] for l in range(L)]
    wf_out = [w_out_a[l] for l in range(L)]
    xs_dd = [dscr("xs_d%d" % i, [D, NT], F32).ap() for i in range(2)]
    hT_d = dscr("hT_d", [128, 16, NT], BF16).ap()
    qs_d = dscr("qs_d", [24, 128, NT], BF16).ap()
    yT_d = dscr("yT_d", [16, 128, NT], BF16).ap()
    mT_d = dscr("mT_d", [16, 128, NT], BF16).ap()
    kT_loc = [dscr("kT_loc%d" % q, [128, NT], BF16) for q in range(17)]
    kT_all = [dscr("kT_all%d" % q, [512, NT], BF16) for q in range(17)]
    v_loc = [dscr("v_loc%d" % q, [128, 2048], BF16) for q in range(16)]
    v_all = [dscr("v_all%d" % q, [512, 2048], BF16) for q in range(16)]
    fa_loc = dscr("fa_loc", [128, 64], F32)
    fa_all = dscr("fa_all", [512, 64], F32)
    ao_d = dscr("ao_d", [16, 128, NT], F32).ap()
    A_d = dscr("A_d", [16, 128, S], BF16).ap()

    B = {}

    def buf(name):
        if name not in B:
            B[name] = Buf()
        return B[name]

    groups = [[0, 1, 2, 3], [4, 5, 6, 7]]
    es = ExitStack()
    with es:
        def sb(name, shape, dt):
            return es.enter_context(nc.sbuf_tensor("sb_" + name, list(shape), dt))

        arena = sb("arena", [128, 16, NT], BF16)
        kT_s = sb("kT_s", [128, S], BF16)
        v_s = sb("v_s", [128, 64, 128], BF16)
        wb = [sb("wb%d" % i, [128, 16, 256], BF16) for i in range(2)]
        G_s = sb("G_s", [128, GL], F32)
        Gadd = sb("Gadd", [128, GL], BF16)
        qT_s = sb("qT_s", [128, NT], BF16)
        pT = [sb("pT%d" % i, [128, 512], BF16) for i in range(4)]
        tmpf = [sb("tmpf%d" % i, [128, 512], F32) for i in range(2)]
        rbuf = [sb("rbuf%d" % i, [128, 512], F32) for i in range(2)]
        rbufI = rbuf + [sb("rbufI%d" % i, [128, 512], F32) for i in range(2)]
        rbufI_names = ["rbuf0", "rbuf1", "rbufI0", "rbufI1"]
        ot = [sb("ot%d" % i, [128, 512], F32) for i in range(5)]
        stg = [sb("stg%d" % i, [128, 512], BF16) for i in range(4)]
        bm = sb("bm", [128, 16, 64], F32)
        negcum = sb("negcum", [128, 4, 64], F32)
        totf = sb("totf", [128, 4, 64], F32)
        inclf = sb("inclf", [128, 4, 64], F32)
        agf = sb("agf", [128, 4, 64], F32)
        fa4 = sb("fa4", [128, 4, 64], F32)
        cqref = sb("cqref", [128, 4, 16], F32)
        seltmp = sb("seltmp", [128, 16, 64], F32)
        sel_s = sb("sel_s", [128, 16, 64], F32)
        ones1 = sb("ones1", [128, 64], F32)
        ff_s = sb("ff_s", [128, 16, 4], F32)
        fa_s = sb("fa_s", [128, 16, 4], F32)
        wI_s = sb("wI_s", [128, 16, 16], F32)
        qi_s = [sb("qi_s%d" % i, [128, 8, 128], BF16) for i in range(2)]
        ident = sb("ident", [128, 128], BF16)
        onesb = sb("onesb", [128, 128], BF16)
        tri = sb("tri", [128, 128], F32)
        onesf = sb("onesf", [128, 128], F32)
        mt_s = sb("mt_s", [128, 2432], BF16)
        normw = sb("normw", [128, L * 16], F32)
        fnormw = sb("fnormw", [128, 16], F32)
        foxb = sb("foxb", [128, L * 4], F32)
        lq_s = sb("lq_s", [128, L * 4 * 64], F32)
        subln = sb("subln", [128, L], F32)
        b31 = sb("b31", [128, 12], F32)
        sm = sb("sm", [128, 64], F32)
        whalf = sb("whalf", [128, NBIS], F32)
        pw2 = sb("pw2", [128, NBIS], F32)
        epsc = sb("epsc", [128, 1], F32)
        onec = sb("onec", [128, 1], F32)

        ps = [es.enter_context(nc.psum_tensor("ps%d" % i, [128, 512], F32)) for i in range(8)]
        sems = {e: es.enter_context(nc.semaphore("s_" + e)) for e in Prog.ENG}
        for i in range(P.ndma):
            sems["dma%d" % i] = es.enter_context(nc.semaphore("s_dma%d" % i))
        sems["cc"] = es.enter_context(nc.semaphore("s_cc"))
        block = es.enter_context(nc.Block())
        print("sbuf bytes remaining", nc.sbuf_bytes_remaining)

        Bps = [buf("ps%d" % i) for i in range(8)]
        rr = {"stg": 0, "pT": 0, "tmpf": 0, "rbuf": 0, "psL": 0, "psA": 0, "wb": 0, "q": 0, "ev": 0, "oz": 0, "psU": 0, "yb": 0, "rbufI": 0, "brb": 0}

        def nxt(k, n):
            v = rr[k] % n
            rr[k] += 1
            return v

        def ld(eng, dst, src, name):
            P.dma(eng, lambda e: e.dma_start(out=dst, in_=src), writes=[buf(name)])
        ld("sync", normw[:], normw_in, "normw")
        ld("sync", fnormw[:], fnormw_in, "fnormw")
        ld("sync", foxb[:], foxb_in, "foxb")
        ld("sync", lq_s[:], lq_in, "lq")
        ld("sync", subln[:], subln_in, "subln")
        ld("sync", b31[:], b31_in, "b31")
        ld("sync", mt_s[:], mt_in, "mt")
        ld("sync", sel_s[:].rearrange("p a b -> p (a b)"), sel_in, "sel")
        ld("sync", ident[:], ident_in, "ident")
        ld("sync", tri[:], tri_in, "tri")
        P.op("vector", lambda e: e.memset(onesb[:], 1.0), writes=[buf("onesb")])
        P.op("vector", lambda e: e.memset(onesf[:], 1.0), writes=[buf("onesf")])
        P.op("vector", lambda e: e.memset(ones1[:], 1.0), writes=[buf("ones1")])
        P.op("vector", lambda e: e.memset(epsc[:], 1e-6), writes=[buf("epsc")])
        P.op("vector", lambda e: e.memset(onec[:], 1.0), writes=[buf("onec")])
        for i in range(NBIS):
            P.op("vector", lambda e, i=i: e.memset(pw2[:, i:i + 1], 2.0 ** -(i + 1)), writes=[buf("pw2")])

        def load_w(wfull, bname, col0, ncols, dup=False):
            i = nxt("wb", 2)
            t = wb[i]
            bw = buf("wb%d" % i)
            src = wfull[:, col0:col0 + ncols].rearrange("(k p) n -> p k n", p=128)
            P.dma("gpsimd", lambda e: e.dma_start(out=t[:, :, 0:ncols], in_=src), reads=[buf(bname)], writes=[bw])
            if dup:
                P.dma("gpsimd", lambda e: e.dma_start(out=t[:, :, ncols:2 * ncols], in_=src), reads=[buf(bname)], writes=[bw])
            return t, bw

        hT = arena
        Bar = [buf("arena%d" % j) for j in range(4)]

        def rstd_from_ssq(ps_ap, dst, n, width, bps, bdst):
            P.op("scalar", lambda e: e.activation(out=dst[:, :width], in_=ps_ap, func=AF.Ln, bias=epsc[:, 0:1], scale=1.0 / n),
                 reads=[bps, buf("epsc")], writes=[bdst])
            P.op("scalar", lambda e: e.activation(out=dst[:, :width], in_=dst[:, :width], func=AF.Exp, scale=-0.5),
                 reads=[bdst], writes=[bdst])

        def evac(dst_ap, src_ap, scale, reads, writes):
            i = nxt("ev", 2)
            if i == 0:
                P.op("scalar", lambda e: e.mul(dst_ap, src_ap, float(scale)), reads=reads, writes=writes)
            else:
                P.op("vector", lambda e: e.tensor_scalar(out=dst_ap, in0=src_ap, scalar1=float(scale), scalar2=None, op0=ALU.mult), reads=reads, writes=writes)

        def do_layer(layer):
            Bwin = "wf_in%d" % layer
            Wl = wf_in[layer]
            xsrc = xT_in if layer == 0 else xs_dd[(layer - 1) % 2]
            xs_d = xs_dd[layer % 2]
            Bx = buf("xin") if layer == 0 else buf("xs_d%d" % ((layer - 1) % 2))
            Bxo = buf("xs_d%d" % (layer % 2))
            for j in range(4):
                xt = arena
                pss = ps[6]
                for k in range(16):
                    o = ot[k % 2]
                    bo = buf("ot%d" % (k % 2))
                    P.dma("sync", lambda e, k=k, j=j, o=o: e.dma_start(out=o[:], in_=xsrc[k * 128:(k + 1) * 128, j * 512:(j + 1) * 512]), reads=[Bx], writes=[bo])
                    s_ = stg[k % 2]
                    bs_ = buf("stg%d" % (k % 2))
                    P.op("scalar", lambda e, o=o, s_=s_: e.activation(out=s_[:], in_=o[:], func=AF.Square), reads=[bo], writes=[bs_])
                    P.op("tensor", lambda e, s_=s_, k=k: e.matmul(pss[:, :], lhsT=onesb[:], rhs=s_[:], start=(k == 0), stop=(k == 15)),
                         reads=[bs_, buf("onesb")], writes=[Bps[6]])
                rstd_from_ssq(pss[:, :], ot[4], D, 512, Bps[6], buf("ot4"))
                for k in range(16):
                    o = ot[k % 2]
                    bo = buf("ot%d" % (k % 2))
                    P.dma("sync", lambda e, k=k, j=j, o=o: e.dma_start(out=o[:], in_=xsrc[k * 128:(k + 1) * 128, j * 512:(j + 1) * 512]), reads=[Bx], writes=[bo])
                    P.op("vector", lambda e, k=k, j=j, o=o: e.scalar_tensor_tensor(out=hT[:, k, j * 512:(j + 1) * 512], in0=o[:], scalar=normw[:, layer * 16 + k:layer * 16 + k + 1],
                                                                                   in1=ot[4][:], op0=ALU.mult, op1=ALU.mult),
                         reads=[bo, buf("ot4"), buf("normw")], writes=Bar, acc=True)
            P.dma("sync", lambda e: e.dma_start(out=hT_d, in_=hT[:]), reads=Bar, writes=[buf("hT_d")])

            def fm_group(col0, ncols, dest_fn, scale, dup=False):
                t, bw = load_w(Wl, Bwin, col0, ncols, dup=dup)
                nc_eff = 2 * ncols if dup else ncols
                for q in range((nc_eff + 127) // 128):
                    m = min(128, nc_eff - q * 128)
                    for j in range(4):
                        pi = 4 + nxt("psA", 2)
                        for k in range(16):
                            P.op("tensor", lambda e, pi=pi, k=k, q=q, j=j, m=m: e.matmul(ps[pi][:m, :], lhsT=t[:, k, q * 128:q * 128 + m], rhs=hT[:, k, j * 512:(j + 1) * 512],
                                                                                        start=(k == 0), stop=(k == 15)),
                                 reads=[bw] + Bar, writes=[Bps[pi]])
                        si = nxt("stg", 4)
                        evac(stg[si][:m, :], ps[pi][:m, :], scale, [Bps[pi]], [buf("stg%d" % si)])
                        dst, bd = dest_fn(q, j, m)
                        P.dma("sync", lambda e, dst=dst, si=si, m=m: e.dma_start(out=dst, in_=stg[si][:m, :]), reads=[buf("stg%d" % si)], writes=[bd], acc=True)

            def qdest(base):
                return lambda q, j, m: (qs_d[base + q, 0:m, j * 512:(j + 1) * 512], buf("qs_d"))

            def kdest(base):
                return lambda q, j, m: (kT_loc[(base + q) * 128:(base + q) * 128 + m, j * 512:(j + 1) * 512], buf("kT_loc"))

            def tm_group(col0, ncols, dest_fn, kind):
                t, bw = load_w(Wl, Bwin, col0, ncols)
                for blk in range(16):
                    pi = 4 + nxt("psA", 2)
                    for k in range(16):
                        P.op("tensor", lambda e, pi=pi, k=k, blk=blk: e.matmul(ps[pi][:, 0:ncols], lhsT=hT[:, k, blk * 128:(blk + 1) * 128], rhs=t[:, k, 0:ncols],
                                                                                start=(k == 0), stop=(k == 15)),
                             reads=[bw] + Bar, writes=[Bps[pi]])
                    dest_fn(blk, pi)

            MIX = ((0, C_DSA, S128), (1, C_FOX, S128), (2, C_DIFF, S64), (3, C_DIL, S128))
            for (mx, c0, s_q) in MIX:
                for hh in range(2):
                    def vdest(blk, pi, mx=mx, hh=hh):
                        si = nxt("stg", 4)
                        evac(stg[si][:, 0:256], ps[pi][:, 0:256], 1.0, [Bps[pi]], [buf("stg%d" % si)])
                        P.dma("sync", lambda e, si=si: e.dma_start(out=v_loc[blk][:, mx * 512 + hh * 256:mx * 512 + hh * 256 + 256], in_=stg[si][:, 0:256]),
                              reads=[buf("stg%d" % si)], writes=[buf("v_loc%d" % blk)], acc=True)
                    tm_group(c0 + 1024 + hh * 256, 256, vdest, "v")
            for (mx, c0, s_q) in MIX:
                for hh in range(2):
                    fm_group(c0 + 512 + hh * 256, 256, (lambda q, j, m, mx=mx, hh=hh: (kT_loc[mx * 4 + hh * 2 + q][0:m, j * 512:(j + 1) * 512], buf("kT_loc%d" % (mx * 4 + hh * 2 + q)))), 1.0)
            fm_group(C_IK, 64, (lambda q, j, m: (kT_loc[16][0:m, j * 512:(j + 1) * 512], buf("kT_loc16"))), 1.0, dup=True)
            for q in range(16):
                P.cc(lambda e, q=q: e.collective_compute("AllGather", ALU.bypass, replica_groups=groups, ins=[v_loc[q].ap().opt()], outs=[v_all[q].ap().opt()]),
                     reads=[buf("v_loc%d" % q)], writes=[buf("v_all%d" % q)])
            for hh in range(2):
                fm_group(C_IQ + hh * 256, 256, (lambda q, j, m, hh=hh: (qs_d[16 + hh * 2 + q, 0:m, j * 512:(j + 1) * 512], buf("qs_d"))), 1.0)
            for q in range(17):
                P.cc(lambda e, q=q: e.collective_compute("AllGather", ALU.bypass, replica_groups=groups, ins=[kT_loc[q].ap().opt()], outs=[kT_all[q].ap().opt()]),
                     reads=[buf("kT_loc%d" % q)], writes=[buf("kT_all%d" % q)])
            for hh in range(2, 4):
                fm_group(C_IQ + hh * 256, 256, (lambda q, j, m, hh=hh: (qs_d[16 + hh * 2 + q, 0:m, j * 512:(j + 1) * 512], buf("qs_d"))), 1.0)
            for (mx, c0, s_q) in MIX:
                for hh in range(2):
                    fm_group(c0 + hh * 256, 256, (lambda q, j, m, mx=mx, hh=hh: (qs_d[mx * 4 + hh * 2 + q, 0:m, j * 512:(j + 1) * 512], buf("qs_d"))), s_q)

            def wdest(blk, pi):
                P.op("vector", lambda e: e.tensor_copy(out=wI_s[:, blk, :], in_=ps[pi][:, 0:16]), reads=[Bps[pi]], writes=[buf("wI")], acc=True)
            tm_group(C_IW, 16, wdest, "w")

            def fdest(blk, pi):
                P.op("vector", lambda e: e.tensor_scalar(out=ff_s[:, blk, :], in0=ps[pi][:, 0:4], scalar1=1.0, scalar2=None, op0=ALU.mult), reads=[Bps[pi]], writes=[buf("ff")], acc=True)
            tm_group(C_FF, 4, fdest, "f")

            for h in range(4):
                P.op("vector", lambda e, h=h: e.tensor_scalar(out=fa_s[:, :, h], in0=ff_s[:, :, h], scalar1=foxb[:, layer * 4 + h:layer * 4 + h + 1], scalar2=None, op0=ALU.add),
                     reads=[buf("ff"), buf("foxb")], writes=[buf("fa")])
            P.op("scalar", lambda e: e.activation(out=fa_s[:], in_=fa_s[:], func=AF.Exp, scale=-1.0), reads=[buf("fa")], writes=[buf("fa")])
            P.op("scalar", lambda e: e.activation(out=fa_s[:], in_=fa_s[:], func=AF.Ln, bias=onec[:, 0:1], scale=1.0), reads=[buf("fa"), buf("onec")], writes=[buf("fa")])
            P.op("vector", lambda e: e.tensor_scalar(out=fa_s[:], in0=fa_s[:], scalar1=-1.0, scalar2=None, op0=ALU.mult), reads=[buf("fa")], writes=[buf("fa")])
            P.dma("sync", lambda e: e.dma_start(out=fa_loc[:, :], in_=fa_s[:].rearrange("p a b -> p (a b)")), reads=[buf("fa")], writes=[buf("fa_loc")])

            P.cc(lambda e: e.collective_compute("AllGather", ALU.bypass, replica_groups=groups, ins=[fa_loc.ap().opt()], outs=[fa_all.ap().opt()]),
                 reads=[buf("fa_loc")], writes=[buf("fa_all")])

            P.dma("sync", lambda e: e.dma_start(out=fa4[:], in_=fa_all.ap().rearrange("(c p) f -> p c f", p=128)), reads=[buf("fa_all")], writes=[buf("fa4")])
            for c in range(4):
                for h in range(4):
                    src = fa4[:, c, :].rearrange("p (j u h) -> p j u h", j=4, u=4, h=4)[:, :, :, h]
                    dst = agf[:, h, :].rearrange("p (j c u) -> p j c u", j=4, c=4, u=4)[:, :, c, :]
                    P.op("vector", lambda e, src=src, dst=dst: e.tensor_copy(out=dst, in_=src), reads=[buf("fa4")], writes=[buf("agf")], acc=True)
            agf2 = agf[:].rearrange("p a b -> p (a b)")
            P.op("tensor", lambda e: e.matmul(ps[6][:, 0:256], lhsT=tri[:], rhs=agf2, start=True, stop=True), reads=[buf("agf"), buf("tri")], writes=[Bps[6]])
            P.op("tensor", lambda e: e.matmul(ps[7][:, 0:256], lhsT=onesf[:], rhs=agf2, start=True, stop=True), reads=[buf("agf"), buf("onesf")], writes=[Bps[7]])
            P.op("vector", lambda e: e.tensor_copy(out=totf[:].rearrange("p a b -> p (a b)"), in_=ps[7][:, 0:256]), reads=[Bps[7]], writes=[buf("totf")])
            for h in range(4):
                P.op("vector", lambda e, h=h: e.tensor_tensor_scan(out=inclf[:, h, :], data0=ones1[:, :], data1=totf[:, h, :], initial=0.0, op0=ALU.mult, op1=ALU.add),
                     reads=[buf("totf"), buf("ones1")], writes=[buf("inclf")])
            P.op("vector", lambda e: e.tensor_tensor(out=negcum[:], in0=totf[:], in1=inclf[:], op=ALU.subtract), reads=[buf("totf"), buf("inclf")], writes=[buf("negcum")])
            P.op("vector", lambda e: e.tensor_tensor(out=negcum[:].rearrange("p a b -> p (a b)"), in0=negcum[:].rearrange("p a b -> p (a b)"), in1=ps[6][:, 0:256], op=ALU.subtract),
                 reads=[buf("negcum"), Bps[6]], writes=[buf("negcum")])
            for h in range(4):
                for blk in range(16):
                    P.op("vector", lambda e, h=h, blk=blk: e.tensor_tensor(out=seltmp[:, blk, :], in0=sel_s[:, blk, :], in1=totf[:, h, :], op=ALU.mult),
                         reads=[buf("sel"), buf("totf")], writes=[buf("seltmp")], acc=True)
                P.op("vector", lambda e, h=h: e.tensor_reduce(out=cqref[:, h, :], in_=seltmp[:], axis=AX.X, op=ALU.add), reads=[buf("seltmp")], writes=[buf("cqref")])

            lam0 = 0.8 - 0.6 * math.exp(-0.3 * layer)
            lb = layer * 256
            P.op("vector", lambda e: e.tensor_tensor(out=tmpf[0][:, 0:64], in0=lq_s[:, lb:lb + 64], in1=lq_s[:, lb + 64:lb + 128], op=ALU.mult), reads=[buf("lq")], writes=[buf("tmpf0")])
            P.op("vector", lambda e: e.tensor_reduce(out=sm[:, 0:1], in_=tmpf[0][:, 0:64], axis=AX.X, op=ALU.add), reads=[buf("tmpf0")], writes=[buf("sm")])
            P.op("vector", lambda e: e.tensor_tensor(out=tmpf[0][:, 0:64], in0=lq_s[:, lb + 128:lb + 192], in1=lq_s[:, lb + 192:lb + 256], op=ALU.mult), reads=[buf("lq")], writes=[buf("tmpf0")])
            P.op("vector", lambda e: e.tensor_reduce(out=sm[:, 1:2], in_=tmpf[0][:, 0:64], axis=AX.X, op=ALU.add), reads=[buf("tmpf0")], writes=[buf("sm")])
            P.op("scalar", lambda e: e.activation(out=sm[:, 2:4], in_=sm[:, 0:2], func=AF.Exp), reads=[buf("sm")], writes=[buf("sm")])
            P.op("vector", lambda e: e.tensor_tensor(out=sm[:, 4:5], in0=sm[:, 3:4], in1=sm[:, 2:3], op=ALU.subtract), reads=[buf("sm")], writes=[buf("sm")])
            P.op("vector", lambda e: e.tensor_scalar(out=sm[:, 5:6], in0=sm[:, 4:5], scalar1=-lam0, scalar2=None, op0=ALU.add), reads=[buf("sm")], writes=[buf("sm")])
            P.op("vector", lambda e: e.tensor_scalar(out=sm[:, 6:7], in0=subln[:, layer:layer + 1], scalar1=1.0 - lam0, scalar2=None, op0=ALU.mult), reads=[buf("subln")], writes=[buf("sm")])

            P.op("vector", lambda e: e.memset(sm[:, 40:41], 0.0),
                 writes=[buf("kT_s"), buf("v_s"), buf("qT_s"), buf("brb0"), buf("brb1"), buf("fence")] + [buf("yb%d" % i) for i in range(4)] + [buf("acc%d" % i) for i in range(8)])
            def load_kv(mx, hh):
                ch = mx * 4 + hh
                src = kT_all[ch].ap().rearrange("(c r) (j i) -> r j c i", c=4, i=512)
                P.dma("sync", lambda e: e.dma_start(out=kT_s[:].rearrange("p (j c i) -> p j c i", j=4, c=4, i=512), in_=src), reads=[buf("kT_all%d" % ch)], writes=[buf("kT_s")])
                col = mx * 512 + hh * 128
                for c in range(4):
                    for blk in range(16):
                        srcv = v_all[blk][c * 128:(c + 1) * 128, col:col + 128]
                        kb = 16 * (blk // 4) + 4 * c + (blk % 4)
                        dstv = v_s[:, kb, :]
                        P.dma("gpsimd" if (blk % 2) else "sync", lambda e, srcv=srcv, dstv=dstv: e.dma_start(out=dstv, in_=srcv), reads=[buf("v_all%d" % blk)], writes=[buf("v_s")], acc=True)
                P.dma("sync", lambda e: e.dma_start(out=qT_s[:], in_=qs_d[ch]), reads=[buf("qs_d")], writes=[buf("qT_s")])

            def load_G(kind, gi):
                if kind == "mask":
                    P.dma("sync", lambda e: e.dma_start(out=Gadd[:], in_=mneg_in), writes=[buf("Gadd")])
                    return
                P.dma("sync", lambda e: e.dma_start(out=G_s[:], in_=gt_in[gi]), writes=[buf("G_s")])
                P.dma("sync", lambda e: e.dma_start(out=Gadd[:], in_=(mneg_in if kind == "bias" else cdil_in)), writes=[buf("Gadd")])
                P.op("gpsimd", lambda e: e.tensor_tensor(out=Gadd[:], in0=G_s[:], in1=Gadd[:], op=ALU.add), reads=[buf("Gadd"), buf("G_s")], writes=[buf("Gadd")])

            def indexer_gen():
                I_s = arena[:].rearrange("p a b -> p (a b)")[:, 0:16384].bitcast(F32)
                At_s = arena[:].rearrange("p a b -> p (a b)")[:, 16384:24576]
                ki_s = arena[:].rearrange("p a b -> p (a b)")[:, 24576:32768]
                BI = [Bar[0], Bar[1]]
                srck = kT_all[16].ap().rearrange("(c r) (j i) -> r j c i", c=4, i=512)
                P.dma("sync", lambda e: e.dma_start(out=ki_s.rearrange("p (j c i) -> p j c i", j=4, c=4, i=512), in_=srck), reads=[buf("kT_all16")], writes=[Bar[3]])
                for blk in range(16):
                    j, u = blk // 4, blk % 4
                    nkc = 4 * j + 4
                    nk = nkc * 512
                    qi = qi_s[blk % 2]
                    bq = buf("qi%d" % (blk % 2))
                    P.dma("sync", lambda e, qi=qi, blk=blk: e.dma_start(out=qi[:], in_=qs_d[16:24, :, blk * 128:(blk + 1) * 128].rearrange("m p t -> p m t")), reads=[buf("qs_d")], writes=[bq])
                    for kc in range(nkc):
                        for ih in range(16):
                            pi = 6 + nxt("psA", 2)
                            r0 = (ih % 2) * 64
                            P.op("tensor", lambda e, pi=pi, ih=ih, r0=r0, kc=kc, qi=qi: e.matmul(ps[pi][:, :], lhsT=qi[r0:r0 + 64, ih // 2, :], rhs=ki_s[r0:r0 + 64, kc * 512:(kc + 1) * 512],
                                                                                                  start=True, stop=True),
                                 reads=[bq, Bar[3]], writes=[Bps[pi]])
                            ri = nxt("rbufI", 4)
                            P.op("scalar", lambda e, pi=pi, ri=ri: e.activation(out=rbufI[ri][:], in_=ps[pi][:, :], func=AF.Relu), reads=[Bps[pi]], writes=[buf(rbufI_names[ri])])
                            if ih == 0:
                                P.op("vector", lambda e, ri=ri, kc=kc, blk=blk: e.tensor_scalar(out=I_s[:, kc * 512:(kc + 1) * 512], in0=rbufI[ri][:], scalar1=wI_s[:, blk, 0:1], scalar2=None, op0=ALU.mult),
                                     reads=[buf(rbufI_names[ri]), buf("wI")], writes=BI)
                            else:
                                P.op("vector", lambda e, ri=ri, kc=kc, blk=blk, ih=ih: e.scalar_tensor_tensor(out=I_s[:, kc * 512:(kc + 1) * 512], in0=rbufI[ri][:], scalar=wI_s[:, blk, ih:ih + 1],
                                                                                                              in1=I_s[:, kc * 512:(kc + 1) * 512], op0=ALU.mult, op1=ALU.add),
                                     reads=[buf(rbufI_names[ri]), buf("wI")] + BI, writes=BI)
                            yield
                    P.op("vector", lambda e, nk=nk: e.tensor_reduce(out=sm[:, 8:9], in_=I_s[:, 0:nk], axis=AX.X, op=ALU.min), reads=BI, writes=[buf("smI")])
                    for kc in range(4 * j, nkc):
                        v0 = 512 * kc - 2048 * j - 128 * u + 384
                        P.op("vector", lambda e, kc=kc, v0=v0: e.tensor_tensor(out=I_s[:, kc * 512:(kc + 1) * 512], in0=I_s[:, kc * 512:(kc + 1) * 512], in1=mt_s[:, v0:v0 + 512], op=ALU.add),
                             reads=BI + [buf("mt")], writes=BI)
                    P.op("vector", lambda e, nk=nk: e.tensor_reduce(out=sm[:, 9:10], in_=I_s[:, 0:nk], axis=AX.X, op=ALU.max), reads=BI, writes=[buf("smI")])
                    P.op("vector", lambda e: e.tensor_scalar(out=sm[:, 10:11], in0=sm[:, 8:9], scalar1=-1.0, scalar2=None, op0=ALU.add), reads=[buf("smI")], writes=[buf("smI")])
                    P.op("vector", lambda e: e.tensor_tensor(out=sm[:, 11:12], in0=sm[:, 9:10], in1=sm[:, 8:9], op=ALU.subtract), reads=[buf("smI")], writes=[buf("smI")])
                    P.op("vector", lambda e: e.tensor_scalar(out=sm[:, 11:12], in0=sm[:, 11:12], scalar1=2.0, scalar2=None, op0=ALU.add), reads=[buf("smI")], writes=[buf("smI")])
                    P.op("vector", lambda e: e.tensor_scalar(out=whalf[:], in0=pw2[:], scalar1=sm[:, 11:12], scalar2=None, op0=ALU.mult), reads=[buf("smI"), buf("pw2")], writes=[buf("whalf")])
                    yield
                    for it in range(NBIS):
                        P.op("vector", lambda e, it=it: e.tensor_tensor(out=sm[:, 12:13], in0=sm[:, 10:11], in1=whalf[:, it:it + 1], op=ALU.add), reads=[buf("smI"), buf("whalf")], writes=[buf("smI")])
                        npc = nk // 1024
                        for pc in range(npc):
                            cdst = 13 if (pc % 2 == (npc - 1) % 2) else 16
                            csrc = 29 - cdst
                            P.op("vector", lambda e, pc=pc, cdst=cdst, csrc=csrc: e.tensor_scalar(out=At_s[:, pc * 1024:(pc + 1) * 1024], in0=I_s[:, pc * 1024:(pc + 1) * 1024], scalar1=sm[:, 12:13],
                                                                                                   scalar2=(None if pc == 0 else sm[:, csrc:csrc + 1]), op0=ALU.is_ge, op1=ALU.add,
                                                                                                   accum_out=sm[:, cdst:cdst + 1]),
                                 reads=BI + [buf("smI")], writes=[Bar[2], buf("smI")])
                            yield
                        P.op("vector", lambda e, it=it: e.tensor_scalar(out=sm[:, 14:15], in0=sm[:, 13:14], scalar1=TOPK - 0.5, scalar2=whalf[:, it:it + 1], op0=ALU.is_ge, op1=ALU.mult),
                             reads=[buf("smI"), buf("whalf")], writes=[buf("smI")])
                        P.op("vector", lambda e: e.tensor_tensor(out=sm[:, 10:11], in0=sm[:, 10:11], in1=sm[:, 14:15], op=ALU.add), reads=[buf("smI")], writes=[buf("smI")])
                        yield
                    P.op("vector", lambda e, nk=nk: e.tensor_scalar(out=At_s[:, 0:nk], in0=I_s[:, 0:nk], scalar1=sm[:, 10:11], scalar2=NEG, op0=ALU.is_lt, op1=ALU.mult),
                         reads=BI + [buf("smI")], writes=[Bar[2]])
                    P.dma("sync", lambda e, blk=blk, nk=nk: e.dma_start(out=A_d[blk, :, 0:nk], in_=At_s[:, 0:nk]), reads=[Bar[2]], writes=[buf("A_d")], acc=True)
                    yield

            idx_it = indexer_gen()

            def idx_step(k):
                alive = True
                for _ in range(k):
                    try:
                        next(idx_it)
                    except StopIteration:
                        alive = False
                        break
                return alive

            LB = (0, 1)

            def flash(j, krows, kb_lo, mode, h, gi, dsa_A=None):
                kbs = list(range(kb_lo, 16 * j + 16))
                ob = 2 + 2 * nxt("oz", 2)
                zb = ob + 1
                pend = None

                def pvz(pi, kb, first, last):
                    bp = buf("pT%d" % pi)
                    P.op("tensor", lambda e: e.matmul(ps[ob][:, :], lhsT=v_s[:, kb, :], rhs=pT[pi][:], start=first, stop=last),
                         reads=[buf("v_s"), bp], writes=[Bps[ob]])
                    P.op("tensor", lambda e: e.matmul(ps[zb][:, :], lhsT=onesb[:], rhs=pT[pi][:], start=first, stop=last),
                         reads=[buf("onesb"), bp], writes=[Bps[zb]])
                for n, kb in enumerate(kbs):
                    ds_ = 2048 * j - 128 * kb
                    need_add = (ds_ < 128) if mode == "fox" else (ds_ < DFAR)
                    li = LB[nxt("psL", 2)]
                    Ls = ps[li]
                    P.op("tensor", lambda e, Ls=Ls, kb=kb: e.matmul(Ls[:, :], lhsT=kT_s[krows[0]:krows[1], kb * 128:(kb + 1) * 128], rhs=qT_s[krows[0]:krows[1], j * 512:(j + 1) * 512],
                                                                      start=True, stop=(dsa_A is None and not need_add)),
                         reads=[buf("kT_s"), buf("qT_s")], writes=[Bps[li]])
                    if dsa_A is not None:
                        At, bA, off = dsa_A(kb)
                        for u in range(4):
                            P.op("tensor", lambda e, Ls=Ls, u=u, At=At, off=off: e.matmul(Ls[:, u * 128:(u + 1) * 128], lhsT=At[:, u, off:off + 128], rhs=ident[:], start=False, stop=(not need_add),
                                                                                          skip_group_check=True),
                                 reads=[bA, buf("ident")], writes=[Bps[li]])
                    src, bsrc = Ls, Bps[li]
                    if need_add:
                        u0 = min(ds_, DFAR) + GOFF
                        P.op("tensor", lambda e, Ls=Ls, u0=u0: e.matmul(Ls[:, :], lhsT=ident[:], rhs=Gadd[:, u0:u0 + 512], start=False, stop=True, skip_group_check=True),
                             reads=[buf("Gadd"), buf("ident")], writes=[Bps[li]])
                    pi = nxt("pT", 4)
                    bp = buf("pT%d" % pi)
                    if mode == "fox":
                        for u in range(4):
                            P.op("scalar", lambda e, pi=pi, src=src, u=u, kb=kb: e.activation(out=pT[pi][:, u * 128:(u + 1) * 128], in_=src[:, u * 128:(u + 1) * 128], func=AF.Exp,
                                                                                                bias=bm[:, 4 * j + u, kb:kb + 1], scale=1.0),
                                 reads=[bsrc, buf("bm")], writes=[bp], acc=(u > 0))
                    elif need_add:
                        P.op("scalar", lambda e, pi=pi, src=src: e.activation(out=pT[pi][:], in_=src[:, :], func=AF.Exp), reads=[bsrc], writes=[bp])
                    else:
                        P.op("scalar", lambda e, pi=pi, src=src: e.activation(out=pT[pi][:], in_=src[:, :], func=AF.Exp, bias=b31[:, gi:gi + 1], scale=1.0),
                             reads=[bsrc, buf("b31")], writes=[bp])
                    if pend is not None:
                        pvz(*pend)
                    pend = (pi, kb, n == 0, n == len(kbs) - 1)
                    if dsa_A is None:
                        idx_step(2)
                pvz(*pend)
                return ob, zb

            def normalize(dst, bdst, oz):
                ob, zb = oz
                P.op("vector", lambda e: e.reciprocal(out=ot[4][:], in_=ps[zb][:, :]), reads=[Bps[zb]], writes=[buf("ot4")])
                P.op("vector", lambda e: e.tensor_tensor(out=dst[:], in0=ps[ob][:, :], in1=ot[4][:], op=ALU.mult), reads=[Bps[ob], buf("ot4")], writes=[bdst])

            def store_o(src, bsrc, ch, j):
                P.dma("sync", lambda e: e.dma_start(out=ao_d[ch, :, j * 512:(j + 1) * 512], in_=src[:]), reads=[bsrc], writes=[buf("ao_d")], acc=True)

            load_G("mask", 0)
            for h in range(4):
                load_kv(1, h)
                for blk in range(16):
                    P.op("vector", lambda e, h=h, blk=blk: e.tensor_scalar(out=bm[:, blk, :], in0=negcum[:, h, :], scalar1=cqref[:, h, blk:blk + 1], scalar2=None, op0=ALU.add),
                         reads=[buf("negcum"), buf("cqref")], writes=[buf("bm")], acc=True)
                for j in range(4):
                    oz = flash(j, (0, 128), 0, "fox", h, 0)
                    normalize(ot[0], buf("ot0"), oz)
                    store_o(ot[0], buf("ot0"), 4 + h, j)
            for h in range(4):
                load_G("dil", 8 + h)
                load_kv(3, h)
                for j in range(4):
                    kb_lo = max(0, 16 * j - 16)
                    oz = flash(j, (0, 128), kb_lo, "dil", h, 8 + h)
                    normalize(ot[0], buf("ot0"), oz)
                    store_o(ot[0], buf("ot0"), 12 + h, j)
            for h in range(4):
                load_G("bias", 4 + h)
                load_kv(2, h)
                for j in range(4):
                    oz = flash(j, (0, 64), 0, "bias", h, 4 + h)
                    normalize(ot[0], buf("ot0"), oz)
                    oz = flash(j, (64, 128), 0, "bias", h, 4 + h)
                    normalize(ot[1], buf("ot1"), oz)
                    P.op("vector", lambda e: e.scalar_tensor_tensor(out=ot[2][:], in0=ot[1][:], scalar=sm[:, 5:6], in1=ot[0][:], op0=ALU.mult, op1=ALU.add),
                         reads=[buf("ot0"), buf("ot1"), buf("sm")], writes=[buf("ot2")])
                    si = nxt("stg", 4)
                    P.op("scalar", lambda e, si=si: e.activation(out=stg[si][:], in_=ot[2][:], func=AF.Square), reads=[buf("ot2")], writes=[buf("stg%d" % si)])
                    P.op("tensor", lambda e, si=si: e.matmul(ps[6][:, :], lhsT=onesb[:], rhs=stg[si][:], start=True, stop=True), reads=[buf("stg%d" % si), buf("onesb")], writes=[Bps[6]])
                    rstd_from_ssq(ps[6][:, :], ot[3], 128, 512, Bps[6], buf("ot3"))
                    P.op("vector", lambda e: e.scalar_tensor_tensor(out=ot[0][:], in0=ot[2][:], scalar=sm[:, 6:7], in1=ot[3][:], op0=ALU.mult, op1=ALU.mult),
                         reads=[buf("ot2"), buf("ot3"), buf("sm")], writes=[buf("ot0")])
                    store_o(ot[0], buf("ot0"), 8 + h, j)

            while idx_step(64):
                pass
            Apc = [arena[:].rearrange("p a b -> p (a b)")[:, i * 8192:(i + 1) * 8192].rearrange("p (u s) -> p u s", u=4) for i in range(2)]
            for h in range(4):
                load_G("bias", h)
                load_kv(0, h)
                for j in range(4):
                    state = {}

                    def dsa_A(kb, j=j, state=state):
                        pc = kb // 16
                        if state.get("pc") != pc:
                            ai = nxt("q", 2)
                            state["pc"], state["ai"] = pc, ai
                            for u in range(4):
                                P.dma("sync", lambda e, ai=ai, u=u, pc=pc: e.dma_start(out=Apc[ai][:, u, :], in_=A_d[4 * j + u, :, pc * 2048:(pc + 1) * 2048]), reads=[buf("A_d")], writes=[Bar[ai]], acc=True)
                        ai = state["ai"]
                        return Apc[ai], Bar[ai], (kb % 16) * 128
                    oz = flash(j, (0, 128), 0, "bias", h, h, dsa_A=dsa_A)
                    normalize(ot[0], buf("ot0"), oz)
                    store_o(ot[0], buf("ot0"), h, j)

            P.op("vector", lambda e: e.memset(sm[:, 40:41], 0.0),
                 writes=[buf("kT_s"), buf("v_s"), buf("qT_s"), buf("brb0"), buf("brb1"), buf("fence")] + [buf("yb%d" % i) for i in range(4)] + [buf("acc%d" % i) for i in range(8)])
            Wbr, Wout = wf_br[layer], wf_out[layer]
            Bbr, Bout = "wf_br%d" % layer, "wf_out%d" % layer
            P.dma("sync", lambda e: e.dma_start(out=hT[:], in_=hT_d), reads=[buf("hT_d")], writes=Bar)
            ybufs = [kT_s[:, i * 2048:(i + 1) * 2048].rearrange("p (k t) -> p k t", k=4) for i in range(4)]
            accs = [v_s[:].rearrange("p a b -> p (a b)")[:, i * 1024:(i + 1) * 1024].bitcast(F32) for i in range(8)]
            mbuf = G_s[:].bitcast(BF16)[:, 0:8192].rearrange("p (k t) -> p k t", k=16)
            for zg in range(8):
                t, bw = load_w(Wl, Bwin, C_Z + zg * 256, 256)
                for q in range(2):
                    ch = zg * 2 + q
                    for j in range(4):
                        pi = 4 + nxt("psA", 2)
                        for k in range(16):
                            P.op("tensor", lambda e, pi=pi, k=k, q=q, t=t, j=j: e.matmul(ps[pi][:, :], lhsT=t[:, k, q * 128:(q + 1) * 128], rhs=hT[:, k, j * 512:(j + 1) * 512], start=(k == 0), stop=(k == 15)),
                                 reads=[bw] + Bar, writes=[Bps[pi]])
                        ti = nxt("tmpf", 2)
                        P.op("scalar", lambda e, pi=pi, ti=ti: e.activation(out=tmpf[ti][:], in_=ps[pi][:, :], func=AF.Silu), reads=[Bps[pi]], writes=[buf("tmpf%d" % ti)])
                        oi = nxt("rbuf", 2)
                        P.dma("sync", lambda e, oi=oi, ch=ch, j=j: e.dma_start(out=rbuf[oi][:], in_=ao_d[ch, :, j * 512:(j + 1) * 512]), reads=[buf("ao_d")], writes=[buf("rbuf%d" % oi)])
                        si = nxt("stg", 4)
                        P.op("vector", lambda e, ti=ti, oi=oi, si=si: e.tensor_tensor(out=stg[si][:], in0=tmpf[ti][:], in1=rbuf[oi][:], op=ALU.mult),
                             reads=[buf("tmpf%d" % ti), buf("rbuf%d" % oi)], writes=[buf("stg%d" % si)])
                        P.dma("sync", lambda e, si=si, ch=ch, j=j: e.dma_start(out=yT_d[ch, :, j * 512:(j + 1) * 512], in_=stg[si][:]), reads=[buf("stg%d" % si)], writes=[buf("yT_d")], acc=True)
            for ng in range(8):
                for b in range(4):
                    tg, bwg = load_w(Wl, Bwin, C_G + b * 2048 + ng * 256, 256)
                    i2 = nxt("brb", 2)
                    tb_ = qT_s[:, i2 * 1024:(i2 + 1) * 1024].rearrange("p (k n) -> p k n", k=4)
                    bwb = buf("brb%d" % i2)
                    srcb = Wbr[b * 512:(b + 1) * 512, ng * 256:(ng + 1) * 256].rearrange("(k p) n -> p k n", p=128)
                    P.dma("gpsimd", lambda e, tb_=tb_, srcb=srcb: e.dma_start(out=tb_, in_=srcb), reads=[buf(Bbr)], writes=[bwb])
                    for j in range(4):
                        yi = nxt("yb", 4)
                        yb, byb = ybufs[yi], buf("yb%d" % yi)
                        P.dma("sync", lambda e, yb=yb, b=b, j=j: e.dma_start(out=yb, in_=yT_d[b * 4:(b + 1) * 4, :, j * 512:(j + 1) * 512].rearrange("k p t -> p k t")), reads=[buf("yT_d")], writes=[byb])
                        for q in range(2):
                            pg = 4 + nxt("psA", 2)
                            for k in range(16):
                                P.op("tensor", lambda e, pg=pg, k=k, q=q, tg=tg, j=j: e.matmul(ps[pg][:, :], lhsT=tg[:, k, q * 128:(q + 1) * 128], rhs=hT[:, k, j * 512:(j + 1) * 512], start=(k == 0), stop=(k == 15)),
                                     reads=[bwg] + Bar, writes=[Bps[pg]])
                            pu = 6 + nxt("psU", 2)
                            for k in range(4):
                                P.op("tensor", lambda e, pu=pu, k=k, q=q, tb_=tb_, yb=yb: e.matmul(ps[pu][:, :], lhsT=tb_[:, k, q * 128:(q + 1) * 128], rhs=yb[:, k, :], start=(k == 0), stop=(k == 3)),
                                     reads=[bwb, byb], writes=[Bps[pu]])
                            ti = nxt("tmpf", 2)
                            P.op("scalar", lambda e, pg=pg, ti=ti: e.activation(out=tmpf[ti][:], in_=ps[pg][:, :], func=AF.Sigmoid), reads=[Bps[pg]], writes=[buf("tmpf%d" % ti)])
                            ai = q * 4 + j
                            acc, bacc = accs[ai], buf("acc%d" % ai)
                            if b == 0:
                                P.op("vector", lambda e, ti=ti, pu=pu, acc=acc: e.tensor_tensor(out=acc, in0=ps[pu][:, :], in1=tmpf[ti][:], op=ALU.mult),
                                     reads=[Bps[pu], buf("tmpf%d" % ti)], writes=[bacc])
                            else:
                                P.op("vector", lambda e, ti=ti, pu=pu: e.tensor_tensor(out=tmpf[ti][:], in0=ps[pu][:, :], in1=tmpf[ti][:], op=ALU.mult),
                                     reads=[Bps[pu], buf("tmpf%d" % ti)], writes=[buf("tmpf%d" % ti)])
                                P.op("gpsimd", lambda e, ti=ti, acc=acc: e.tensor_tensor(out=acc, in0=acc, in1=tmpf[ti][:], op=ALU.add),
                                     reads=[buf("tmpf%d" % ti), bacc], writes=[bacc])
                            if b == 3:
                                n = ng * 2 + q
                                si = nxt("stg", 4)
                                P.op("scalar", lambda e, acc=acc, si=si: e.activation(out=stg[si][:], in_=acc, func=AF.Copy), reads=[bacc], writes=[buf("stg%d" % si)])
                                P.dma("sync", lambda e, si=si, n=n, j=j: e.dma_start(out=mT_d[n, :, j * 512:(j + 1) * 512], in_=stg[si][:]), reads=[buf("stg%d" % si)], writes=[buf("mT_d")], acc=True)
            for j in range(4):
                P.dma("sync", lambda e, j=j: e.dma_start(out=mbuf, in_=mT_d[:, :, j * 512:(j + 1) * 512].rearrange("k p t -> p k t")), reads=[buf("mT_d")], writes=[buf("G_s")])
                for ng in range(8):
                    to, bwo = load_w(Wout, Bout, ng * 256, 256)
                    for q in range(2):
                        n = ng * 2 + q
                        po = 4 + nxt("psA", 2)
                        for k in range(16):
                            P.op("tensor", lambda e, po=po, k=k, q=q, to=to: e.matmul(ps[po][:, :], lhsT=to[:, k, q * 128:(q + 1) * 128], rhs=mbuf[:, k, :], start=(k == 0), stop=(k == 15)),
                                 reads=[bwo, buf("G_s")], writes=[Bps[po]])
                        oi = nxt("rbuf", 2)
                        P.dma("sync", lambda e, oi=oi, n=n, j=j: e.dma_start(out=rbuf[oi][:], in_=xsrc[n * 128:(n + 1) * 128, j * 512:(j + 1) * 512]), reads=[Bx], writes=[buf("rbuf%d" % oi)])
                        P.op("vector", lambda e, oi=oi, po=po: e.tensor_tensor(out=rbuf[oi][:], in0=ps[po][:, :], in1=rbuf[oi][:], op=ALU.add),
                             reads=[Bps[po], buf("rbuf%d" % oi)], writes=[buf("rbuf%d" % oi)])
                        P.dma("sync", lambda e, oi=oi, n=n, j=j: e.dma_start(out=xs_d[n * 128:(n + 1) * 128, j * 512:(j + 1) * 512], in_=rbuf[oi][:]), reads=[buf("rbuf%d" % oi)], writes=[Bxo], acc=True)

        for layer_ in range(L):
            do_layer(layer_)

        xs_d = xs_dd[(L - 1) % 2]
        Bx = buf("xs_d%d" % ((L - 1) % 2))
        Bout_ = buf("yT")
        for j in range(4):
            for k in range(16):
                o = ot[k % 2]
                bo = buf("ot%d" % (k % 2))
                P.dma("sync", lambda e, k=k, j=j, o=o: e.dma_start(out=o[:], in_=xs_d[k * 128:(k + 1) * 128, j * 512:(j + 1) * 512]), reads=[Bx], writes=[bo])
                s_ = stg[k % 2]
                bs_ = buf("stg%d" % (k % 2))
                P.op("scalar", lambda e, o=o, s_=s_: e.activation(out=s_[:], in_=o[:], func=AF.Square), reads=[bo], writes=[bs_])
                P.op("tensor", lambda e, s_=s_, k=k: e.matmul(ps[6][:, :], lhsT=onesb[:], rhs=s_[:], start=(k == 0), stop=(k == 15)), reads=[bs_, buf("onesb")], writes=[Bps[6]])
            rstd_from_ssq(ps[6][:, :], ot[4], D, 512, Bps[6], buf("ot4"))
            for k in range(16):
                o = ot[k % 2]
                bo = buf("ot%d" % (k % 2))
                P.dma("sync", lambda e, k=k, j=j, o=o: e.dma_start(out=o[:], in_=xs_d[k * 128:(k + 1) * 128, j * 512:(j + 1) * 512]), reads=[Bx], writes=[bo])
                ri = nxt("rbuf", 2)
                P.op("vector", lambda e, k=k, o=o, ri=ri: e.scalar_tensor_tensor(out=rbuf[ri][:], in0=o[:], scalar=fnormw[:, k:k + 1], in1=ot[4][:], op0=ALU.mult, op1=ALU.mult),
                     reads=[bo, buf("ot4"), buf("fnormw")], writes=[buf("rbuf%d" % ri)])
                P.dma("sync", lambda e, k=k, j=j, ri=ri: e.dma_start(out=yT_out[k * 128:(k + 1) * 128, j * 512:(j + 1) * 512], in_=rbuf[ri][:]), reads=[buf("rbuf%d" % ri)], writes=[Bout_], acc=True)
        P.finish("sync", [Bout_])
        P.emit(block, sems)
    print("ops", P.nops)
    return nc


def host_prep(inputs, depth=DEPTH):
    x = np.asarray(inputs["x"], np.float32)
    L = depth
    rel = np.asarray(inputs["rel_bias"], np.float32)
    maps = []
    sl = np.arange(128)[:, None]
    uu = np.arange(GL)[None, :]
    import ml_dtypes
    bf = ml_dtypes.bfloat16
    ident = np.eye(128, dtype=np.float32).astype(bf)
    tri = (np.arange(128)[:, None] <= np.arange(128)[None, :]).astype(np.float32)
    normw = np.ascontiguousarray(np.asarray(inputs["norm_w"], np.float32)[:L].reshape(L, 16, 128).transpose(2, 0, 1).reshape(128, L * 16))
    fnormw = np.ascontiguousarray(np.asarray(inputs["final_norm_w"], np.float32).reshape(16, 128).T)
    foxb = np.ascontiguousarray(np.broadcast_to(np.asarray(inputs["fox_b_f"], np.float32)[:L].reshape(1, L * 4), (128, L * 4)))
    lq = np.stack([np.asarray(inputs[k], np.float32)[:L] for k in ("diff_lq1", "diff_lk1", "diff_lq2", "diff_lk2")], axis=1)
    lq = np.ascontiguousarray(np.broadcast_to(lq.reshape(1, L * 256), (128, L * 256)))
    subln = np.ascontiguousarray(np.asarray(inputs["diff_subln_w"], np.float32)[:L].T)
    b31 = np.ascontiguousarray(np.broadcast_to(rel[31:32, :], (128, 12)))
    w_in = np.asarray(inputs["w_in"], np.float32)
    w_br = np.asarray(inputs["w_branch"], np.float32).reshape(DEPTH, 2048, 2048)
    w_out = np.asarray(inputs["w_out"], np.float32)
    for core in range(8):
        b, c = core // 4, core % 4
        toks = np.concatenate([np.arange(512 * (4 * j + c), 512 * (4 * j + c + 1)) for j in range(4)])
        xT = np.ascontiguousarray(x[b, toks, :].T)
        dist = uu - GOFF + 512 * c - sl
        bidx = t5_bucket_np(dist)
        gt = np.ascontiguousarray(rel[bidx, :].transpose(2, 0, 1))
        mneg = np.where(dist >= 0, 0.0, NEG).astype(np.float32).astype(bf)
        nval = ((dist >= 0) & (dist <= 128)).astype(np.int32) + ((dist >= 0) & (dist % 4 == 0) & (dist <= 512)).astype(np.int32) \
            + ((dist >= 0) & (dist % 16 == 0) & (dist <= 2048)).astype(np.int32)
        cdil = np.where(nval > 0, np.log(np.maximum(nval, 1).astype(np.float32)), NEG).astype(np.float32).astype(bf)
        vv = np.arange(2432)[None, :]
        mt = np.where((vv - 384 - 512 * c - sl) > 0, -1e9, 0.0).astype(np.float32).astype(bf)
        blk = np.arange(16)
        kb_own = 16 * (blk // 4) + 4 * c + (blk % 4)
        sel = (np.arange(64)[None, :] <= kb_own[:, None]).astype(np.float32)
        sel = np.ascontiguousarray(np.broadcast_to(sel.reshape(1, 1024), (128, 1024)))
        maps.append({
            "xT": xT,
            "w_in": w_in[:L], "w_br": w_br[:L], "w_out": w_out[:L],
            "normw": normw, "fnormw": fnormw, "foxb": foxb, "lq": lq, "subln": subln,
            "gt": gt, "mneg": mneg, "cdil": cdil, "b31": b31, "mt": mt, "sel": sel, "ident": ident, "tri": tri,
        })
    return maps


def run(inputs, depth=DEPTH):
    nc = build(depth)
    maps = host_prep(inputs, depth)
    res = run_bass_kernel_spmd(nc, maps, core_ids=list(range(8)))
    out = np.zeros((2, S, D), np.float32)
    for core in range(8):
        b, c = core // 4, core % 4
        toks = np.concatenate([np.arange(512 * (4 * j + c), 512 * (4 * j + c + 1)) for j in range(4)])
        out[b, toks, :] = res.results[core]["yT"].T
    return out


def kernel(**inputs):
    return run(inputs, DEPTH)
```

```python
import math
from contextlib import ExitStack

import numpy as np
import concourse.bass as bass
import concourse.mybir as mybir
from concourse.bass_utils import run_bass_kernel_spmd

F32 = mybir.dt.float32
BF16 = mybir.dt.bfloat16
ALU = mybir.AluOpType
AF = mybir.ActivationFunctionType
AX = mybir.AxisListType

D = 2048
S = 8192
NT = 2048
DEPTH = 4
N_IN = 17492
NEG = -30000.0
GL = 4608
GOFF = 1920
DFAR = 2176
C_Z, C_G = 0, 2048
C_DSA, C_IQ, C_IK, C_IW, C_FOX, C_FF, C_DIFF, C_DIL = 10240, 11776, 12800, 12864, 12880, 14416, 14420, 15956
S128 = 128 ** -0.5
S64 = 64 ** -0.5
TOPK = 256
NBIS = 18


class Buf:
    __slots__ = ("w", "r")

    def __init__(self):
        self.w = {}
        self.r = {}


class Prog:
    ENG = ("sync", "scalar", "vector", "gpsimd", "tensor")

    def __init__(self, ndma=24):
        self.streams = {e: [] for e in self.ENG}
        self.count = {e: 0 for e in self.ENG}
        self.seen = {e: {} for e in self.ENG}
        self.ndma = ndma
        self.dma_i = 0
        self.dma_cnt = [0] * ndma
        self.cc_cnt = 0
        self.nops = 0

    def _deps(self, eng, reads, writes, acc=False):
        deps = {}

        def add(k, v):
            if deps.get(k, 0) < v:
                deps[k] = v
        for b in reads:
            for k, v in b.w.items():
                add(k, v)
        for b in writes:
            if not acc:
                for k, v in b.w.items():
                    add(k, v)
            for k, v in b.r.items():
                add(k, v)
        waits = []
        for k, v in deps.items():
            if k == eng and eng == "tensor":
                continue
            if self.seen[eng].get(k, 0) < v:
                self.seen[eng][k] = v
                waits.append((k, v))
        return waits

    def _record(self, me, reads, writes, acc=False):
        for b in reads:
            if b.r.get(me[0], 0) < me[1]:
                b.r[me[0]] = me[1]
        for b in writes:
            if acc:
                b.w[me[0]] = me[1]
            else:
                b.w = {me[0]: me[1]}
                b.r = {}
        self.nops += 1

    def op(self, eng, fn, reads=(), writes=(), acc=False):
        waits = self._deps(eng, reads, writes, acc)
        self.count[eng] += 1
        me = (eng, self.count[eng])
        self.streams[eng].append((waits, fn, eng, 1))
        self._record(me, reads, writes, acc)

    def dma(self, eng, fn, reads=(), writes=(), acc=False):
        i = self.dma_i % self.ndma
        self.dma_i += 1
        key = "dma%d" % i
        waits = self._deps(eng, reads, writes, acc)
        prev = self.dma_cnt[i]
        if prev and self.seen[eng].get(key, 0) < prev:
            self.seen[eng][key] = prev
            waits.append((key, prev))
        self.dma_cnt[i] += 16
        me = (key, self.dma_cnt[i])
        self.streams[eng].append((waits, fn, key, 16))
        self._record(me, reads, writes, acc)

    def cc(self, fn, reads=(), writes=()):
        eng = "gpsimd"
        waits = self._deps(eng, reads, writes)
        self.cc_cnt += 1
        me = ("cc", self.cc_cnt)
        self.streams[eng].append((waits, fn, "cc", None))
        self._record(me, reads, writes)

    def finish(self, eng, bufs):
        waits = self._deps(eng, bufs, ())
        self.streams[eng].append((waits, None, None, 0))

    def emit(self, block, sems):
        def mk(e):
            def body(engh):
                for waits, fn, key, inc in self.streams[e]:
                    for k, v in waits:
                        engh.wait_ge(sems[k], v)
                    if fn is not None:
                        ins = fn(engh)
                        if inc is None:
                            ins.then_inc(sems[key])
                        else:
                            ins.then_inc(sems[key], inc)
            return body
        block.sync(mk("sync"))
        block.scalar(mk("scalar"))
        block.vector(mk("vector"))
        block.gpsimd(mk("gpsimd"))
        block.tensor(mk("tensor"))


def t5_bucket_np(dist):
    dist = np.maximum(dist, 0)
    d = np.maximum(dist, 16).astype(np.float32)
    large = 16 + (np.log(d / np.float32(16)) / np.float32(math.log(2048 / 16)) * np.float32(16)).astype(np.int32)
    large = np.minimum(large, 31)
    return np.where(dist < 16, dist, large)


def build(depth=DEPTH):
    nc = bass.Bass("TRN2", target_bir_lowering=False)
    P = Prog()
    L = depth

    def din(name, shape, dt=F32):
        return nc.dram_tensor(name, list(shape), dt, kind="ExternalInput").ap()

    def dscr(name, shape, dt):
        return nc.dram_tensor(name, list(shape), dt)

    xT_in = din("xT", [D, NT])
    w_in_a = din("w_in", [L, D, N_IN])
    w_br_a = din("w_br", [L, D, D])
    w_out_a = din("w_out", [L, D, D])
    normw_in = din("normw", [128, L * 16])
    fnormw_in = din("fnormw", [128, 16])
    foxb_in = din("foxb", [128, L * 4])
    lq_in = din("lq", [128, L * 4 * 64])
    subln_in = din("subln", [128, L])
    gt_in = din("gt", [12, 128, GL])
    mneg_in = din("mneg", [128, GL], BF16)
    cdil_in = din("cdil", [128, GL], BF16)
    b31_in = din("b31", [128, 12])
    mt_in = din("mt", [128, 2432], BF16)
    sel_in = din("sel", [128, 16 * 64])
    ident_in = din("ident", [128, 128], BF16)
    tri_in = din("tri", [128, 128])
    yT_out = nc.dram_tensor("yT", [D, NT], F32, kind="ExternalOutput").ap()

    wf_in = [w_in_a[l] for l in range(L)]
    wf_br = [w_br_a[l] for l in range(L)]
    wf_out = [w_out_a[l] for l in range(L)]
    xs_dd = [dscr("xs_d%d" % i, [D, NT], F32).ap() for i in range(2)]
    hT_d = dscr("hT_d", [128, 16, NT], BF16).ap()
    qs_d = dscr("qs_d", [24, 128, NT], BF16).ap()
    yT_d = dscr("yT_d", [16, 128, NT], BF16).ap()
    mT_d = dscr("mT_d", [16, 128, NT], BF16).ap()
    kT_loc = [dscr("kT_loc%d" % q, [128, NT], BF16) for q in range(17)]
    kT_all = [dscr("kT_all%d" % q, [512, NT], BF16) for q in range(17)]
    v_loc = [dscr("v_loc%d" % q, [128, 2048], BF16) for q in range(16)]
    v_all = [dscr("v_all%d" % q, [512, 2048], BF16) for q in range(16)]
    fa_loc = dscr("fa_loc", [128, 64], F32)
    fa_all = dscr("fa_all", [512, 64], F32)
    ao_d = dscr("ao_d", [16, 128, NT], F32).ap()
    A_d = dscr("A_d", [16, 128, S], BF16).ap()

    B = {}

    def buf(name):
        if name not in B:
            B[name] = Buf()
        return B[name]

    groups = [[0, 1, 2, 3], [4, 5, 6, 7]]
    es = ExitStack()
    with es:
        def sb(name, shape, dt):
            return es.enter_context(nc.sbuf_tensor("sb_" + name, list(shape), dt))

        arena = sb("arena", [128, 16, NT], BF16)
        kT_s = sb("kT_s", [128, S], BF16)
        v_s = sb("v_s", [128, 64, 128], BF16)
        wb = [sb("wb%d" % i, [128, 16, 256], BF16) for i in range(2)]
        G_s = sb("G_s", [128, GL], F32)
        Gadd = sb("Gadd", [128, GL], BF16)
        qT_s = sb("qT_s", [128, NT], BF16)
        pT = [sb("pT%d" % i, [128, 512], BF16) for i in range(4)]
        tmpf = [sb("tmpf%d" % i, [128, 512], F32) for i in range(2)]
        rbuf = [sb("rbuf%d" % i, [128, 512], F32) for i in range(2)]
        rbufI = rbuf + [sb("rbufI%d" % i, [128, 512], F32) for i in range(2)]
        rbufI_names = ["rbuf0", "rbuf1", "rbufI0", "rbufI1"]
        ot = [sb("ot%d" % i, [128, 512], F32) for i in range(5)]
        stg = [sb("stg%d" % i, [128, 512], BF16) for i in range(4)]
        bm = sb("bm", [128, 16, 64], F32)
        negcum = sb("negcum", [128, 4, 64], F32)
        totf = sb("totf", [128, 4, 64], F32)
        inclf = sb("inclf", [128, 4, 64], F32)
        agf = sb("agf", [128, 4, 64], F32)
        fa4 = sb("fa4", [128, 4, 64], F32)
        cqref = sb("cqref", [128, 4, 16], F32)
        seltmp = sb("seltmp", [128, 16, 64], F32)
        sel_s = sb("sel_s", [128, 16, 64], F32)
        ones1 = sb("ones1", [128, 64], F32)
        ff_s = sb("ff_s", [128, 16, 4], F32)
        fa_s = sb("fa_s", [128, 16, 4], F32)
        wI_s = sb("wI_s", [128, 16, 16], F32)
        qi_s = [sb("qi_s%d" % i, [128, 8, 128], BF16) for i in range(2)]
        ident = sb("ident", [128, 128], BF16)
        onesb = sb("onesb", [128, 128], BF16)
        tri = sb("tri", [128, 128], F32)
        onesf = sb("onesf", [128, 128], F32)
        mt_s = sb("mt_s", [128, 2432], BF16)
        normw = sb("normw", [128, L * 16], F32)
        fnormw = sb("fnormw", [128, 16], F32)
        foxb = sb("foxb", [128, L * 4], F32)
        lq_s = sb("lq_s", [128, L * 4 * 64], F32)
        subln = sb("subln", [128, L], F32)
        b31 = sb("b31", [128, 12], F32)
        sm = sb("sm", [128, 64], F32)
        whalf = sb("whalf", [128, NBIS], F32)
        pw2 = sb("pw2", [128, NBIS], F32)
        epsc = sb("epsc", [128, 1], F32)
        onec = sb("onec", [128, 1], F32)

        ps = [es.enter_context(nc.psum_tensor("ps%d" % i, [128, 512], F32)) for i in range(8)]
        sems = {e: es.enter_context(nc.semaphore("s_" + e)) for e in Prog.ENG}
        for i in range(P.ndma):
            sems["dma%d" % i] = es.enter_context(nc.semaphore("s_dma%d" % i))
        sems["cc"] = es.enter_context(nc.semaphore("s_cc"))
        block = es.enter_context(nc.Block())
        print("sbuf bytes remaining", nc.sbuf_bytes_remaining)

        Bps = [buf("ps%d" % i) for i in range(8)]
        rr = {"stg": 0, "pT": 0, "tmpf": 0, "rbuf": 0, "psL": 0, "psA": 0, "wb": 0, "q": 0, "ev": 0, "oz": 0, "psU": 0, "yb": 0, "rbufI": 0, "brb": 0}

        def nxt(k, n):
            v = rr[k] % n
            rr[k] += 1
            return v

        def ld(eng, dst, src, name):
            P.dma(eng, lambda e: e.dma_start(out=dst, in_=src), writes=[buf(name)])
        ld("sync", normw[:], normw_in, "normw")
        ld("sync", fnormw[:], fnormw_in, "fnormw")
        ld("sync", foxb[:], foxb_in, "foxb")
        ld("sync", lq_s[:], lq_in, "lq")
        ld("sync", subln[:], subln_in, "subln")
        ld("sync", b31[:], b31_in, "b31")
        ld("sync", mt_s[:], mt_in, "mt")
        ld("sync", sel_s[:].rearrange("p a b -> p (a b)"), sel_in, "sel")
        ld("sync", ident[:], ident_in, "ident")
        ld("sync", tri[:], tri_in, "tri")
        P.op("vector", lambda e: e.memset(onesb[:], 1.0), writes=[buf("onesb")])
        P.op("vector", lambda e: e.memset(onesf[:], 1.0), writes=[buf("onesf")])
        P.op("vector", lambda e: e.memset(ones1[:], 1.0), writes=[buf("ones1")])
        P.op("vector", lambda e: e.memset(epsc[:], 1e-6), writes=[buf("epsc")])
        P.op("vector", lambda e: e.memset(onec[:], 1.0), writes=[buf("onec")])
        for i in range(NBIS):
            P.op("vector", lambda e, i=i: e.memset(pw2[:, i:i + 1], 2.0 ** -(i + 1)), writes=[buf("pw2")])

        def load_w(wfull, bname, col0, ncols, dup=False):
            i = nxt("wb", 2)
            t = wb[i]
            bw = buf("wb%d" % i)
            src = wfull[:, col0:col0 + ncols].rearrange("(k p) n -> p k n", p=128)
            P.dma("gpsimd", lambda e: e.dma_start(out=t[:, :, 0:ncols], in_=src), reads=[buf(bname)], writes=[bw])
            if dup:
                P.dma("gpsimd", lambda e: e.dma_start(out=t[:, :, ncols:2 * ncols], in_=src), reads=[buf(bname)], writes=[bw])
            return t, bw

        hT = arena
        Bar = [buf("arena%d" % j) for j in range(4)]

        def rstd_from_ssq(ps_ap, dst, n, width, bps, bdst):
            P.op("scalar", lambda e: e.activation(out=dst[:, :width], in_=ps_ap, func=AF.Ln, bias=epsc[:, 0:1], scale=1.0 / n),
                 reads=[bps, buf("epsc")], writes=[bdst])
            P.op("scalar", lambda e: e.activation(out=dst[:, :width], in_=dst[:, :width], func=AF.Exp, scale=-0.5),
                 reads=[bdst], writes=[bdst])

        def evac(dst_ap, src_ap, scale, reads, writes):
            i = nxt("ev", 2)
            if i == 0:
                P.op("scalar", lambda e: e.mul(dst_ap, src_ap, float(scale)), reads=reads, writes=writes)
            else:
                P.op("vector", lambda e: e.tensor_scalar(out=dst_ap, in0=src_ap, scalar1=float(scale), scalar2=None, op0=ALU.mult), reads=reads, writes=writes)

        def do_layer(layer):
            Bwin = "wf_in%d" % layer
            Wl = wf_in[layer]
            xsrc = xT_in if layer == 0 else xs_dd[(layer - 1) % 2]
            xs_d = xs_dd[layer % 2]
            Bx = buf("xin") if layer == 0 else buf("xs_d%d" % ((layer - 1) % 2))
            Bxo = buf("xs_d%d" % (layer % 2))
            for j in range(4):
                xt = arena
                pss = ps[6]
                for k in range(16):
                    o = ot[k % 2]
                    bo = buf("ot%d" % (k % 2))
                    P.dma("sync", lambda e, k=k, j=j, o=o: e.dma_start(out=o[:], in_=xsrc[k * 128:(k + 1) * 128, j * 512:(j + 1) * 512]), reads=[Bx], writes=[bo])
                    s_ = stg[k % 2]
                    bs_ = buf("stg%d" % (k % 2))
                    P.op("scalar", lambda e, o=o, s_=s_: e.activation(out=s_[:], in_=o[:], func=AF.Square), reads=[bo], writes=[bs_])
                    P.op("tensor", lambda e, s_=s_, k=k: e.matmul(pss[:, :], lhsT=onesb[:], rhs=s_[:], start=(k == 0), stop=(k == 15)),
                         reads=[bs_, buf("onesb")], writes=[Bps[6]])
                rstd_from_ssq(pss[:, :], ot[4], D, 512, Bps[6], buf("ot4"))
                for k in range(16):
                    o = ot[k % 2]
                    bo = buf("ot%d" % (k % 2))
                    P.dma("sync", lambda e, k=k, j=j, o=o: e.dma_start(out=o[:], in_=xsrc[k * 128:(k + 1) * 128, j * 512:(j + 1) * 512]), reads=[Bx], writes=[bo])
                    P.op("vector", lambda e, k=k, j=j, o=o: e.scalar_tensor_tensor(out=hT[:, k, j * 512:(j + 1) * 512], in0=o[:], scalar=normw[:, layer * 16 + k:layer * 16 + k + 1],
                                                                                   in1=ot[4][:], op0=ALU.mult, op1=ALU.mult),
                         reads=[bo, buf("ot4"), buf("normw")], writes=Bar, acc=True)
            P.dma("sync", lambda e: e.dma_start(out=hT_d, in_=hT[:]), reads=Bar, writes=[buf("hT_d")])

            def fm_group(col0, ncols, dest_fn, scale, dup=False):
                t, bw = load_w(Wl, Bwin, col0, ncols, dup=dup)
                nc_eff = 2 * ncols if dup else ncols
                for q in range((nc_eff + 127) // 128):
                    m = min(128, nc_eff - q * 128)
                    for j in range(4):
                        pi = 4 + nxt("psA", 2)
                        for k in range(16):
                            P.op("tensor", lambda e, pi=pi, k=k, q=q, j=j, m=m: e.matmul(ps[pi][:m, :], lhsT=t[:, k, q * 128:q * 128 + m], rhs=hT[:, k, j * 512:(j + 1) * 512],
                                                                                        start=(k == 0), stop=(k == 15)),
                                 reads=[bw] + Bar, writes=[Bps[pi]])
                        si = nxt("stg", 4)
                        evac(stg[si][:m, :], ps[pi][:m, :], scale, [Bps[pi]], [buf("stg%d" % si)])
                        dst, bd = dest_fn(q, j, m)
                        P.dma("sync", lambda e, dst=dst, si=si, m=m: e.dma_start(out=dst, in_=stg[si][:m, :]), reads=[buf("stg%d" % si)], writes=[bd], acc=True)

            def qdest(base):
                return lambda q, j, m: (qs_d[base + q, 0:m, j * 512:(j + 1) * 512], buf("qs_d"))

            def kdest(base):
                return lambda q, j, m: (kT_loc[(base + q) * 128:(base + q) * 128 + m, j * 512:(j + 1) * 512], buf("kT_loc"))

            def tm_group(col0, ncols, dest_fn, kind):
                t, bw = load_w(Wl, Bwin, col0, ncols)
                for blk in range(16):
                    pi = 4 + nxt("psA", 2)
                    for k in range(16):
                        P.op("tensor", lambda e, pi=pi, k=k, blk=blk: e.matmul(ps[pi][:, 0:ncols], lhsT=hT[:, k, blk * 128:(blk + 1) * 128], rhs=t[:, k, 0:ncols],
                                                                                start=(k == 0), stop=(k == 15)),
                             reads=[bw] + Bar, writes=[Bps[pi]])
                    dest_fn(blk, pi)

            MIX = ((0, C_DSA, S128), (1, C_FOX, S128), (2, C_DIFF, S64), (3, C_DIL, S128))
            for (mx, c0, s_q) in MIX:
                for hh in range(2):
                    def vdest(blk, pi, mx=mx, hh=hh):
                        si = nxt("stg", 4)
                        evac(stg[si][:, 0:256], ps[pi][:, 0:256], 1.0, [Bps[pi]], [buf("stg%d" % si)])
                        P.dma("sync", lambda e, si=si: e.dma_start(out=v_loc[blk][:, mx * 512 + hh * 256:mx * 512 + hh * 256 + 256], in_=stg[si][:, 0:256]),
                              reads=[buf("stg%d" % si)], writes=[buf("v_loc%d" % blk)], acc=True)
                    tm_group(c0 + 1024 + hh * 256, 256, vdest, "v")
            for (mx, c0, s_q) in MIX:
                for hh in range(2):
                    fm_group(c0 + 512 + hh * 256, 256, (lambda q, j, m, mx=mx, hh=hh: (kT_loc[mx * 4 + hh * 2 + q][0:m, j * 512:(j + 1) * 512], buf("kT_loc%d" % (mx * 4 + hh * 2 + q)))), 1.0)
            fm_group(C_IK, 64, (lambda q, j, m: (kT_loc[16][0:m, j * 512:(j + 1) * 512], buf("kT_loc16"))), 1.0, dup=True)
            for q in range(16):
                P.cc(lambda e, q=q: e.collective_compute("AllGather", ALU.bypass, replica_groups=groups, ins=[v_loc[q].ap().opt()], outs=[v_all[q].ap().opt()]),
                     reads=[buf("v_loc%d" % q)], writes=[buf("v_all%d" % q)])
            for hh in range(2):
                fm_group(C_IQ + hh * 256, 256, (lambda q, j, m, hh=hh: (qs_d[16 + hh * 2 + q, 0:m, j * 512:(j + 1) * 512], buf("qs_d"))), 1.0)
            for q in range(17):
                P.cc(lambda e, q=q: e.collective_compute("AllGather", ALU.bypass, replica_groups=groups, ins=[kT_loc[q].ap().opt()], outs=[kT_all[q].ap().opt()]),
                     reads=[buf("kT_loc%d" % q)], writes=[buf("kT_all%d" % q)])
            for hh in range(2, 4):
                fm_group(C_IQ + hh * 256, 256, (lambda q, j, m, hh=hh: (qs_d[16 + hh * 2 + q, 0:m, j * 512:(j + 1) * 512], buf("qs_d"))), 1.0)
            for (mx, c0, s_q) in MIX:
                for hh in range(2):
                    fm_group(c0 + hh * 256, 256, (lambda q, j, m, mx=mx, hh=hh: (qs_d[mx * 4 + hh * 2 + q, 0:m, j * 512:(j + 1) * 512], buf("qs_d"))), s_q)

            def wdest(blk, pi):
                P.op("vector", lambda e: e.tensor_copy(out=wI_s[:, blk, :], in_=ps[pi][:, 0:16]), reads=[Bps[pi]], writes=[buf("wI")], acc=True)
            tm_group(C_IW, 16, wdest, "w")

            def fdest(blk, pi):
                P.op("vector", lambda e: e.tensor_scalar(out=ff_s[:, blk, :], in0=ps[pi][:, 0:4], scalar1=1.0, scalar2=None, op0=ALU.mult), reads=[Bps[pi]], writes=[buf("ff")], acc=True)
            tm_group(C_FF, 4, fdest, "f")

            for h in range(4):
                P.op("vector", lambda e, h=h: e.tensor_scalar(out=fa_s[:, :, h], in0=ff_s[:, :, h], scalar1=foxb[:, layer * 4 + h:layer * 4 + h + 1], scalar2=None, op0=ALU.add),
                     reads=[buf("ff"), buf("foxb")], writes=[buf("fa")])
            P.op("scalar", lambda e: e.activation(out=fa_s[:], in_=fa_s[:], func=AF.Exp, scale=-1.0), reads=[buf("fa")], writes=[buf("fa")])
            P.op("scalar", lambda e: e.activation(out=fa_s[:], in_=fa_s[:], func=AF.Ln, bias=onec[:, 0:1], scale=1.0), reads=[buf("fa"), buf("onec")], writes=[buf("fa")])
            P.op("vector", lambda e: e.tensor_scalar(out=fa_s[:], in0=fa_s[:], scalar1=-1.0, scalar2=None, op0=ALU.mult), reads=[buf("fa")], writes=[buf("fa")])
            P.dma("sync", lambda e: e.dma_start(out=fa_loc[:, :], in_=fa_s[:].rearrange("p a b -> p (a b)")), reads=[buf("fa")], writes=[buf("fa_loc")])

            P.cc(lambda e: e.collective_compute("AllGather", ALU.bypass, replica_groups=groups, ins=[fa_loc.ap().opt()], outs=[fa_all.ap().opt()]),
                 reads=[buf("fa_loc")], writes=[buf("fa_all")])

            P.dma("sync", lambda e: e.dma_start(out=fa4[:], in_=fa_all.ap().rearrange("(c p) f -> p c f", p=128)), reads=[buf("fa_all")], writes=[buf("fa4")])
            for c in range(4):
                for h in range(4):
                    src = fa4[:, c, :].rearrange("p (j u h) -> p j u h", j=4, u=4, h=4)[:, :, :, h]
                    dst = agf[:, h, :].rearrange("p (j c u) -> p j c u", j=4, c=4, u=4)[:, :, c, :]
                    P.op("vector", lambda e, src=src, dst=dst: e.tensor_copy(out=dst, in_=src), reads=[buf("fa4")], writes=[buf("agf")], acc=True)
            agf2 = agf[:].rearrange("p a b -> p (a b)")
            P.op("tensor", lambda e: e.matmul(ps[6][:, 0:256], lhsT=tri[:], rhs=agf2, start=True, stop=True), reads=[buf("agf"), buf("tri")], writes=[Bps[6]])
            P.op("tensor", lambda e: e.matmul(ps[7][:, 0:256], lhsT=onesf[:], rhs=agf2, start=True, stop=True), reads=[buf("agf"), buf("onesf")], writes=[Bps[7]])
            P.op("vector", lambda e: e.tensor_copy(out=totf[:].rearrange("p a b -> p (a b)"), in_=ps[7][:, 0:256]), reads=[Bps[7]], writes=[buf("totf")])
            for h in range(4):
                P.op("vector", lambda e, h=h: e.tensor_tensor_scan(out=inclf[:, h, :], data0=ones1[:, :], data1=totf[:, h, :], initial=0.0, op0=ALU.mult, op1=ALU.add),
                     reads=[buf("totf"), buf("ones1")], writes=[buf("inclf")])
            P.op("vector", lambda e: e.tensor_tensor(out=negcum[:], in0=totf[:], in1=inclf[:], op=ALU.subtract), reads=[buf("totf"), buf("inclf")], writes=[buf("negcum")])
            P.op("vector", lambda e: e.tensor_tensor(out=negcum[:].rearrange("p a b -> p (a b)"), in0=negcum[:].rearrange("p a b -> p (a b)"), in1=ps[6][:, 0:256], op=ALU.subtract),
                 reads=[buf("negcum"), Bps[6]], writes=[buf("negcum")])
            for h in range(4):
                for blk in range(16):
                    P.op("vector", lambda e, h=h, blk=blk: e.tensor_tensor(out=seltmp[:, blk, :], in0=sel_s[:, blk, :], in1=totf[:, h, :], op=ALU.mult),
                         reads=[buf("sel"), buf("totf")], writes=[buf("seltmp")], acc=True)
                P.op("vector", lambda e, h=h: e.tensor_reduce(out=cqref[:, h, :], in_=seltmp[:], axis=AX.X, op=ALU.add), reads=[buf("seltmp")], writes=[buf("cqref")])

            lam0 = 0.8 - 0.6 * math.exp(-0.3 * layer)
            lb = layer * 256
            P.op("vector", lambda e: e.tensor_tensor(out=tmpf[0][:, 0:64], in0=lq_s[:, lb:lb + 64], in1=lq_s[:, lb + 64:lb + 128], op=ALU.mult), reads=[buf("lq")], writes=[buf("tmpf0")])
            P.op("vector", lambda e: e.tensor_reduce(out=sm[:, 0:1], in_=tmpf[0][:, 0:64], axis=AX.X, op=ALU.add), reads=[buf("tmpf0")], writes=[buf("sm")])
            P.op("vector", lambda e: e.tensor_tensor(out=tmpf[0][:, 0:64], in0=lq_s[:, lb + 128:lb + 192], in1=lq_s[:, lb + 192:lb + 256], op=ALU.mult), reads=[buf("lq")], writes=[buf("tmpf0")])
            P.op("vector", lambda e: e.tensor_reduce(out=sm[:, 1:2], in_=tmpf[0][:, 0:64], axis=AX.X, op=ALU.add), reads=[buf("tmpf0")], writes=[buf("sm")])
            P.op("scalar", lambda e: e.activation(out=sm[:, 2:4], in_=sm[:, 0:2], func=AF.Exp), reads=[buf("sm")], writes=[buf("sm")])
            P.op("vector", lambda e: e.tensor_tensor(out=sm[:, 4:5], in0=sm[:, 3:4], in1=sm[:, 2:3], op=ALU.subtract), reads=[buf("sm")], writes=[buf("sm")])
            P.op("vector", lambda e: e.tensor_scalar(out=sm[:, 5:6], in0=sm[:, 4:5], scalar1=-lam0, scalar2=None, op0=ALU.add), reads=[buf("sm")], writes=[buf("sm")])
            P.op("vector", lambda e: e.tensor_scalar(out=sm[:, 6:7], in0=subln[:, layer:layer + 1], scalar1=1.0 - lam0, scalar2=None, op0=ALU.mult), reads=[buf("subln")], writes=[buf("sm")])

            P.op("vector", lambda e: e.memset(sm[:, 40:41], 0.0),
                 writes=[buf("kT_s"), buf("v_s"), buf("qT_s"), buf("brb0"), buf("brb1"), buf("fence")] + [buf("yb%d" % i) for i in range(4)] + [buf("acc%d" % i) for i in range(8)])
            def load_kv(mx, hh):
                ch = mx * 4 + hh
                src = kT_all[ch].ap().rearrange("(c r) (j i) -> r j c i", c=4, i=512)
                P.dma("sync", lambda e: e.dma_start(out=kT_s[:].rearrange("p (j c i) -> p j c i", j=4, c=4, i=512), in_=src), reads=[buf("kT_all%d" % ch)], writes=[buf("kT_s")])
                col = mx * 512 + hh * 128
                for c in range(4):
                    for blk in range(16):
                        srcv = v_all[blk][c * 128:(c + 1) * 128, col:col + 128]
                        kb = 16 * (blk // 4) + 4 * c + (blk % 4)
                        dstv = v_s[:, kb, :]
                        P.dma("gpsimd" if (blk % 2) else "sync", lambda e, srcv=srcv, dstv=dstv: e.dma_start(out=dstv, in_=srcv), reads=[buf("v_all%d" % blk)], writes=[buf("v_s")], acc=True)
                P.dma("sync", lambda e: e.dma_start(out=qT_s[:], in_=qs_d[ch]), reads=[buf("qs_d")], writes=[buf("qT_s")])

            def load_G(kind, gi):
                if kind == "mask":
                    P.dma("sync", lambda e: e.dma_start(out=Gadd[:], in_=mneg_in), writes=[buf("Gadd")])
                    return
                P.dma("sync", lambda e: e.dma_start(out=G_s[:], in_=gt_in[gi]), writes=[buf("G_s")])
                P.dma("sync", lambda e: e.dma_start(out=Gadd[:], in_=(mneg_in if kind == "bias" else cdil_in)), writes=[buf("Gadd")])
                P.op("gpsimd", lambda e: e.tensor_tensor(out=Gadd[:], in0=G_s[:], in1=Gadd[:], op=ALU.add), reads=[buf("Gadd"), buf("G_s")], writes=[buf("Gadd")])

            def indexer_gen():
                I_s = arena[:].rearrange("p a b -> p (a b)")[:, 0:16384].bitcast(F32)
                At_s = arena[:].rearrange("p a b -> p (a b)")[:, 16384:24576]
                ki_s = arena[:].rearrange("p a b -> p (a b)")[:, 24576:32768]
                BI = [Bar[0], Bar[1]]
                srck = kT_all[16].ap().rearrange("(c r) (j i) -> r j c i", c=4, i=512)
                P.dma("sync", lambda e: e.dma_start(out=ki_s.rearrange("p (j c i) -> p j c i", j=4, c=4, i=512), in_=srck), reads=[buf("kT_all16")], writes=[Bar[3]])
                for blk in range(16):
                    j, u = blk // 4, blk % 4
                    nkc = 4 * j + 4
                    nk = nkc * 512
                    qi = qi_s[blk % 2]
                    bq = buf("qi%d" % (blk % 2))
                    P.dma("sync", lambda e, qi=qi, blk=blk: e.dma_start(out=qi[:], in_=qs_d[16:24, :, blk * 128:(blk + 1) * 128].rearrange("m p t -> p m t")), reads=[buf("qs_d")], writes=[bq])
                    for kc in range(nkc):
                        for ih in range(16):
                            pi = 6 + nxt("psA", 2)
                            r0 = (ih % 2) * 64
                            P.op("tensor", lambda e, pi=pi, ih=ih, r0=r0, kc=kc, qi=qi: e.matmul(ps[pi][:, :], lhsT=qi[r0:r0 + 64, ih // 2, :], rhs=ki_s[r0:r0 + 64, kc * 512:(kc + 1) * 512],
                                                                                                  start=True, stop=True),
                                 reads=[bq, Bar[3]], writes=[Bps[pi]])
                            ri = nxt("rbufI", 4)
                            P.op("scalar", lambda e, pi=pi, ri=ri: e.activation(out=rbufI[ri][:], in_=ps[pi][:, :], func=AF.Relu), reads=[Bps[pi]], writes=[buf(rbufI_names[ri])])
                            if ih == 0:
                                P.op("vector", lambda e, ri=ri, kc=kc, blk=blk: e.tensor_scalar(out=I_s[:, kc * 512:(kc + 1) * 512], in0=rbufI[ri][:], scalar1=wI_s[:, blk, 0:1], scalar2=None, op0=ALU.mult),
                                     reads=[buf(rbufI_names[ri]), buf("wI")], writes=BI)
                            else:
                                P.op("vector", lambda e, ri=ri, kc=kc, blk=blk, ih=ih: e.scalar_tensor_tensor(out=I_s[:, kc * 512:(kc + 1) * 512], in0=rbufI[ri][:], scalar=wI_s[:, blk, ih:ih + 1],
                                                                                                              in1=I_s[:, kc * 512:(kc + 1) * 512], op0=ALU.mult, op1=ALU.add),
                                     reads=[buf(rbufI_names[ri]), buf("wI")] + BI, writes=BI)
                            yield
                    P.op("vector", lambda e, nk=nk: e.tensor_reduce(out=sm[:, 8:9], in_=I_s[:, 0:nk], axis=AX.X, op=ALU.min), reads=BI, writes=[buf("smI")])
                    for kc in range(4 * j, nkc):
                        v0 = 512 * kc - 2048 * j - 128 * u + 384
                        P.op("vector", lambda e, kc=kc, v0=v0: e.tensor_tensor(out=I_s[:, kc * 512:(kc + 1) * 512], in0=I_s[:, kc * 512:(kc + 1) * 512], in1=mt_s[:, v0:v0 + 512], op=ALU.add),
                             reads=BI + [buf("mt")], writes=BI)
                    P.op("vector", lambda e, nk=nk: e.tensor_reduce(out=sm[:, 9:10], in_=I_s[:, 0:nk], axis=AX.X, op=ALU.max), reads=BI, writes=[buf("smI")])
                    P.op("vector", lambda e: e.tensor_scalar(out=sm[:, 10:11], in0=sm[:, 8:9], scalar1=-1.0, scalar2=None, op0=ALU.add), reads=[buf("smI")], writes=[buf("smI")])
                    P.op("vector", lambda e: e.tensor_tensor(out=sm[:, 11:12], in0=sm[:, 9:10], in1=sm[:, 8:9], op=ALU.subtract), reads=[buf("smI")], writes=[buf("smI")])
                    P.op("vector", lambda e: e.tensor_scalar(out=sm[:, 11:12], in0=sm[:, 11:12], scalar1=2.0, scalar2=None, op0=ALU.add), reads=[buf("smI")], writes=[buf("smI")])
                    P.op("vector", lambda e: e.tensor_scalar(out=whalf[:], in0=pw2[:], scalar1=sm[:, 11:12], scalar2=None, op0=ALU.mult), reads=[buf("smI"), buf("pw2")], writes=[buf("whalf")])
                    yield
                    for it in range(NBIS):
                        P.op("vector", lambda e, it=it: e.tensor_tensor(out=sm[:, 12:13], in0=sm[:, 10:11], in1=whalf[:, it:it + 1], op=ALU.add), reads=[buf("smI"), buf("whalf")], writes=[buf("smI")])
                        npc = nk // 1024
                        for pc in range(npc):
                            cdst = 13 if (pc % 2 == (npc - 1) % 2) else 16
                            csrc = 29 - cdst
                            P.op("vector", lambda e, pc=pc, cdst=cdst, csrc=csrc: e.tensor_scalar(out=At_s[:, pc * 1024:(pc + 1) * 1024], in0=I_s[:, pc * 1024:(pc + 1) * 1024], scalar1=sm[:, 12:13],
                                                                                                   scalar2=(None if pc == 0 else sm[:, csrc:csrc + 1]), op0=ALU.is_ge, op1=ALU.add,
                                                                                                   accum_out=sm[:, cdst:cdst + 1]),
                                 reads=BI + [buf("smI")], writes=[Bar[2], buf("smI")])
                            yield
                        P.op("vector", lambda e, it=it: e.tensor_scalar(out=sm[:, 14:15], in0=sm[:, 13:14], scalar1=TOPK - 0.5, scalar2=whalf[:, it:it + 1], op0=ALU.is_ge, op1=ALU.mult),
                             reads=[buf("smI"), buf("whalf")], writes=[buf("smI")])
                        P.op("vector", lambda e: e.tensor_tensor(out=sm[:, 10:11], in0=sm[:, 10:11], in1=sm[:, 14:15], op=ALU.add), reads=[buf("smI")], writes=[buf("smI")])
                        yield
                    P.op("vector", lambda e, nk=nk: e.tensor_scalar(out=At_s[:, 0:nk], in0=I_s[:, 0:nk], scalar1=sm[:, 10:11], scalar2=NEG, op0=ALU.is_lt, op1=ALU.mult),
                         reads=BI + [buf("smI")], writes=[Bar[2]])
                    P.dma("sync", lambda e, blk=blk, nk=nk: e.dma_start(out=A_d[blk, :, 0:nk], in_=At_s[:, 0:nk]), reads=[Bar[2]], writes=[buf("A_d")], acc=True)
                    yield

            idx_it = indexer_gen()

            def idx_step(k):
                alive = True
                for _ in range(k):
                    try:
                        next(idx_it)
                    except StopIteration:
                        alive = False
                        break
                return alive

            LB = (0, 1)

            def flash(j, krows, kb_lo, mode, h, gi, dsa_A=None):
                kbs = list(range(kb_lo, 16 * j + 16))
                ob = 2 + 2 * nxt("oz", 2)
                zb = ob + 1
                pend = None

                def pvz(pi, kb, first, last):
                    bp = buf("pT%d" % pi)
                    P.op("tensor", lambda e: e.matmul(ps[ob][:, :], lhsT=v_s[:, kb, :], rhs=pT[pi][:], start=first, stop=last),
                         reads=[buf("v_s"), bp], writes=[Bps[ob]])
                    P.op("tensor", lambda e: e.matmul(ps[zb][:, :], lhsT=onesb[:], rhs=pT[pi][:], start=first, stop=last),
                         reads=[buf("onesb"), bp], writes=[Bps[zb]])
                for n, kb in enumerate(kbs):
                    ds_ = 2048 * j - 128 * kb
                    need_add = (ds_ < 128) if mode == "fox" else (ds_ < DFAR)
                    li = LB[nxt("psL", 2)]
                    Ls = ps[li]
                    P.op("tensor", lambda e, Ls=Ls, kb=kb: e.matmul(Ls[:, :], lhsT=kT_s[krows[0]:krows[1], kb * 128:(kb + 1) * 128], rhs=qT_s[krows[0]:krows[1], j * 512:(j + 1) * 512],
                                                                      start=True, stop=(dsa_A is None and not need_add)),
                         reads=[buf("kT_s"), buf("qT_s")], writes=[Bps[li]])
                    if dsa_A is not None:
                        At, bA, off = dsa_A(kb)
                        for u in range(4):
                            P.op("tensor", lambda e, Ls=Ls, u=u, At=At, off=off: e.matmul(Ls[:, u * 128:(u + 1) * 128], lhsT=At[:, u, off:off + 128], rhs=ident[:], start=False, stop=(not need_add),
                                                                                          skip_group_check=True),
                                 reads=[bA, buf("ident")], writes=[Bps[li]])
                    src, bsrc = Ls, Bps[li]
                    if need_add:
                        u0 = min(ds_, DFAR) + GOFF
                        P.op("tensor", lambda e, Ls=Ls, u0=u0: e.matmul(Ls[:, :], lhsT=ident[:], rhs=Gadd[:, u0:u0 + 512], start=False, stop=True, skip_group_check=True),
                             reads=[buf("Gadd"), buf("ident")], writes=[Bps[li]])
                    pi = nxt("pT", 4)
                    bp = buf("pT%d" % pi)
                    if mode == "fox":
                        for u in range(4):
                            P.op("scalar", lambda e, pi=pi, src=src, u=u, kb=kb: e.activation(out=pT[pi][:, u * 128:(u + 1) * 128], in_=src[:, u * 128:(u + 1) * 128], func=AF.Exp,
                                                                                                bias=bm[:, 4 * j + u, kb:kb + 1], scale=1.0),
                                 reads=[bsrc, buf("bm")], writes=[bp], acc=(u > 0))
                    elif need_add:
                        P.op("scalar", lambda e, pi=pi, src=src: e.activation(out=pT[pi][:], in_=src[:, :], func=AF.Exp), reads=[bsrc], writes=[bp])
                    else:
                        P.op("scalar", lambda e, pi=pi, src=src: e.activation(out=pT[pi][:], in_=src[:, :], func=AF.Exp, bias=b31[:, gi:gi + 1], scale=1.0),
                             reads=[bsrc, buf("b31")], writes=[bp])
                    if pend is not None:
                        pvz(*pend)
                    pend = (pi, kb, n == 0, n == len(kbs) - 1)
                    if dsa_A is None:
                        idx_step(2)
                pvz(*pend)
                return ob, zb

            def normalize(dst, bdst, oz):
                ob, zb = oz
                P.op("vector", lambda e: e.reciprocal(out=ot[4][:], in_=ps[zb][:, :]), reads=[Bps[zb]], writes=[buf("ot4")])
                P.op("vector", lambda e: e.tensor_tensor(out=dst[:], in0=ps[ob][:, :], in1=ot[4][:], op=ALU.mult), reads=[Bps[ob], buf("ot4")], writes=[bdst])

            def store_o(src, bsrc, ch, j):
                P.dma("sync", lambda e: e.dma_start(out=ao_d[ch, :, j * 512:(j + 1) * 512], in_=src[:]), reads=[bsrc], writes=[buf("ao_d")], acc=True)

            load_G("mask", 0)
            for h in range(4):
                load_kv(1, h)
                for blk in range(16):
                    P.op("vector", lambda e, h=h, blk=blk: e.tensor_scalar(out=bm[:, blk, :], in0=negcum[:, h, :], scalar1=cqref[:, h, blk:blk + 1], scalar2=None, op0=ALU.add),
                         reads=[buf("negcum"), buf("cqref")], writes=[buf("bm")], acc=True)
                for j in range(4):
                    oz = flash(j, (0, 128), 0, "fox", h, 0)
                    normalize(ot[0], buf("ot0"), oz)
                    store_o(ot[0], buf("ot0"), 4 + h, j)
            for h in range(4):
                load_G("dil", 8 + h)
                load_kv(3, h)
                for j in range(4):
                    kb_lo = max(0, 16 * j - 16)
                    oz = flash(j, (0, 128), kb_lo, "dil", h, 8 + h)
                    normalize(ot[0], buf("ot0"), oz)
                    store_o(ot[0], buf("ot0"), 12 + h, j)
            for h in range(4):
                load_G("bias", 4 + h)
                load_kv(2, h)
                for j in range(4):
                    oz = flash(j, (0, 64), 0, "bias", h, 4 + h)
                    normalize(ot[0], buf("ot0"), oz)
                    oz = flash(j, (64, 128), 0, "bias", h, 4 + h)
                    normalize(ot[1], buf("ot1"), oz)
                    P.op("vector", lambda e: e.scalar_tensor_tensor(out=ot[2][:], in0=ot[1][:], scalar=sm[:, 5:6], in1=ot[0][:], op0=ALU.mult, op1=ALU.add),
                         reads=[buf("ot0"), buf("ot1"), buf("sm")], writes=[buf("ot2")])
                    si = nxt("stg", 4)
                    P.op("scalar", lambda e, si=si: e.activation(out=stg[si][:], in_=ot[2][:], func=AF.Square), reads=[buf("ot2")], writes=[buf("stg%d" % si)])
                    P.op("tensor", lambda e, si=si: e.matmul(ps[6][:, :], lhsT=onesb[:], rhs=stg[si][:], start=True, stop=True), reads=[buf("stg%d" % si), buf("onesb")], writes=[Bps[6]])
                    rstd_from_ssq(ps[6][:, :], ot[3], 128, 512, Bps[6], buf("ot3"))
                    P.op("vector", lambda e: e.scalar_tensor_tensor(out=ot[0][:], in0=ot[2][:], scalar=sm[:, 6:7], in1=ot[3][:], op0=ALU.mult, op1=ALU.mult),
                         reads=[buf("ot2"), buf("ot3"), buf("sm")], writes=[buf("ot0")])
                    store_o(ot[0], buf("ot0"), 8 + h, j)

            while idx_step(64):
                pass
            Apc = [arena[:].rearrange("p a b -> p (a b)")[:, i * 8192:(i + 1) * 8192].rearrange("p (u s) -> p u s", u=4) for i in range(2)]
            for h in range(4):
                load_G("bias", h)
                load_kv(0, h)
                for j in range(4):
                    state = {}

                    def dsa_A(kb, j=j, state=state):
                        pc = kb // 16
                        if state.get("pc") != pc:
                            ai = nxt("q", 2)
                            state["pc"], state["ai"] = pc, ai
                            for u in range(4):
                                P.dma("sync", lambda e, ai=ai, u=u, pc=pc: e.dma_start(out=Apc[ai][:, u, :], in_=A_d[4 * j + u, :, pc * 2048:(pc + 1) * 2048]), reads=[buf("A_d")], writes=[Bar[ai]], acc=True)
                        ai = state["ai"]
                        return Apc[ai], Bar[ai], (kb % 16) * 128
                    oz = flash(j, (0, 128), 0, "bias", h, h, dsa_A=dsa_A)
                    normalize(ot[0], buf("ot0"), oz)
                    store_o(ot[0], buf("ot0"), h, j)

            P.op("vector", lambda e: e.memset(sm[:, 40:41], 0.0),
                 writes=[buf("kT_s"), buf("v_s"), buf("qT_s"), buf("brb0"), buf("brb1"), buf("fence")] + [buf("yb%d" % i) for i in range(4)] + [buf("acc%d" % i) for i in range(8)])
            Wbr, Wout = wf_br[layer], wf_out[layer]
            Bbr, Bout = "wf_br%d" % layer, "wf_out%d" % layer
            P.dma("sync", lambda e: e.dma_start(out=hT[:], in_=hT_d), reads=[buf("hT_d")], writes=Bar)
            ybufs = [kT_s[:, i * 2048:(i + 1) * 2048].rearrange("p (k t) -> p k t", k=4) for i in range(4)]
            accs = [v_s[:].rearrange("p a b -> p (a b)")[:, i * 1024:(i + 1) * 1024].bitcast(F32) for i in range(8)]
            mbuf = G_s[:].bitcast(BF16)[:, 0:8192].rearrange("p (k t) -> p k t", k=16)
            for zg in range(8):
                t, bw = load_w(Wl, Bwin, C_Z + zg * 256, 256)
                for q in range(2):
                    ch = zg * 2 + q
                    for j in range(4):
                        pi = 4 + nxt("psA", 2)
                        for k in range(16):
                            P.op("tensor", lambda e, pi=pi, k=k, q=q, t=t, j=j: e.matmul(ps[pi][:, :], lhsT=t[:, k, q * 128:(q + 1) * 128], rhs=hT[:, k, j * 512:(j + 1) * 512], start=(k == 0), stop=(k == 15)),
                                 reads=[bw] + Bar, writes=[Bps[pi]])
                        ti = nxt("tmpf", 2)
                        P.op("scalar", lambda e, pi=pi, ti=ti: e.activation(out=tmpf[ti][:], in_=ps[pi][:, :], func=AF.Silu), reads=[Bps[pi]], writes=[buf("tmpf%d" % ti)])
                        oi = nxt("rbuf", 2)
                        P.dma("sync", lambda e, oi=oi, ch=ch, j=j: e.dma_start(out=rbuf[oi][:], in_=ao_d[ch, :, j * 512:(j + 1) * 512]), reads=[buf("ao_d")], writes=[buf("rbuf%d" % oi)])
                        si = nxt("stg", 4)
                        P.op("vector", lambda e, ti=ti, oi=oi, si=si: e.tensor_tensor(out=stg[si][:], in0=tmpf[ti][:], in1=rbuf[oi][:], op=ALU.mult),
                             reads=[buf("tmpf%d" % ti), buf("rbuf%d" % oi)], writes=[buf("stg%d" % si)])
                        P.dma("sync", lambda e, si=si, ch=ch, j=j: e.dma_start(out=yT_d[ch, :, j * 512:(j + 1) * 512], in_=stg[si][:]), reads=[buf("stg%d" % si)], writes=[buf("yT_d")], acc=True)
            for ng in range(8):
                for b in range(4):
                    tg, bwg = load_w(Wl, Bwin, C_G + b * 2048 + ng * 256, 256)
                    i2 = nxt("brb", 2)
                    tb_ = qT_s[:, i2 * 1024:(i2 + 1) * 1024].rearrange("p (k n) -> p k n", k=4)
                    bwb = buf("brb%d" % i2)
                    srcb = Wbr[b * 512:(b + 1) * 512, ng * 256:(ng + 1) * 256].rearrange("(k p) n -> p k n", p=128)
                    P.dma("gpsimd", lambda e, tb_=tb_, srcb=srcb: e.dma_start(out=tb_, in_=srcb), reads=[buf(Bbr)], writes=[bwb])
                    for j in range(4):
                        yi = nxt("yb", 4)
                        yb, byb = ybufs[yi], buf("yb%d" % yi)
                        P.dma("sync", lambda e, yb=yb, b=b, j=j: e.dma_start(out=yb, in_=yT_d[b * 4:(b + 1) * 4, :, j * 512:(j + 1) * 512].rearrange("k p t -> p k t")), reads=[buf("yT_d")], writes=[byb])
                        for q in range(2):
                            pg = 4 + nxt("psA", 2)
                            for k in range(16):
                                P.op("tensor", lambda e, pg=pg, k=k, q=q, tg=tg, j=j: e.matmul(ps[pg][:, :], lhsT=tg[:, k, q * 128:(q + 1) * 128], rhs=hT[:, k, j * 512:(j + 1) * 512], start=(k == 0), stop=(k == 15)),
                                     reads=[bwg] + Bar, writes=[Bps[pg]])
                            pu = 6 + nxt("psU", 2)
                            for k in range(4):
                                P.op("tensor", lambda e, pu=pu, k=k, q=q, tb_=tb_, yb=yb: e.matmul(ps[pu][:, :], lhsT=tb_[:, k, q * 128:(q + 1) * 128], rhs=yb[:, k, :], start=(k == 0), stop=(k == 3)),
                                     reads=[bwb, byb], writes=[Bps[pu]])
                            ti = nxt("tmpf", 2)
                            P.op("scalar", lambda e, pg=pg, ti=ti: e.activation(out=tmpf[ti][:], in_=ps[pg][:, :], func=AF.Sigmoid), reads=[Bps[pg]], writes=[buf("tmpf%d" % ti)])
                            ai = q * 4 + j
                            acc, bacc = accs[ai], buf("acc%d" % ai)
                            if b == 0:
                                P.op("vector", lambda e, ti=ti, pu=pu, acc=acc: e.tensor_tensor(out=acc, in0=ps[pu][:, :], in1=tmpf[ti][:], op=ALU.mult),
                                     reads=[Bps[pu], buf("tmpf%d" % ti)], writes=[bacc])
                            else:
                                P.op("vector", lambda e, ti=ti, pu=pu: e.tensor_tensor(out=tmpf[ti][:], in0=ps[pu][:, :], in1=tmpf[ti][:], op=ALU.mult),
                                     reads=[Bps[pu], buf("tmpf%d" % ti)], writes=[buf("tmpf%d" % ti)])
                                P.op("vector", lambda e, ti=ti, acc=acc: e.tensor_tensor(out=acc, in0=acc, in1=tmpf[ti][:], op=ALU.add),
                                     reads=[buf("tmpf%d" % ti), bacc], writes=[bacc])
                            if b == 3:
                                n = ng * 2 + q
                                si = nxt("stg", 4)
                                P.op("scalar", lambda e, acc=acc, si=si: e.activation(out=stg[si][:], in_=acc, func=AF.Copy), reads=[bacc], writes=[buf("stg%d" % si)])
                                P.dma("sync", lambda e, si=si, n=n, j=j: e.dma_start(out=mT_d[n, :, j * 512:(j + 1) * 512], in_=stg[si][:]), reads=[buf("stg%d" % si)], writes=[buf("mT_d")], acc=True)
            for j in range(4):
                P.dma("sync", lambda e, j=j: e.dma_start(out=mbuf, in_=mT_d[:, :, j * 512:(j + 1) * 512].rearrange("k p t -> p k t")), reads=[buf("mT_d")], writes=[buf("G_s")])
                for ng in range(8):
                    to, bwo = load_w(Wout, Bout, ng * 256, 256)
                    for q in range(2):
                        n = ng * 2 + q
                        po = 4 + nxt("psA", 2)
                        for k in range(16):
                            P.op("tensor", lambda e, po=po, k=k, q=q, to=to: e.matmul(ps[po][:, :], lhsT=to[:, k, q * 128:(q + 1) * 128], rhs=mbuf[:, k, :], start=(k == 0), stop=(k == 15)),
                                 reads=[bwo, buf("G_s")], writes=[Bps[po]])
                        oi = nxt("rbuf", 2)
                        P.dma("sync", lambda e, oi=oi, n=n, j=j: e.dma_start(out=rbuf[oi][:], in_=xsrc[n * 128:(n + 1) * 128, j * 512:(j + 1) * 512]), reads=[Bx], writes=[buf("rbuf%d" % oi)])
                        P.op("vector", lambda e, oi=oi, po=po: e.tensor_tensor(out=rbuf[oi][:], in0=ps[po][:, :], in1=rbuf[oi][:], op=ALU.add),
                             reads=[Bps[po], buf("rbuf%d" % oi)], writes=[buf("rbuf%d" % oi)])
                        P.dma("sync", lambda e, oi=oi, n=n, j=j: e.dma_start(out=xs_d[n * 128:(n + 1) * 128, j * 512:(j + 1) * 512], in_=rbuf[oi][:]), reads=[buf("rbuf%d" % oi)], writes=[Bxo], acc=True)

        for layer_ in range(L):
            do_layer(layer_)

        xs_d = xs_dd[(L - 1) % 2]
        Bx = buf("xs_d%d" % ((L - 1) % 2))
        Bout_ = buf("yT")
        for j in range(4):
            for k in range(16):
                o = ot[k % 2]
                bo = buf("ot%d" % (k % 2))
                P.dma("sync", lambda e, k=k, j=j, o=o: e.dma_start(out=o[:], in_=xs_d[k * 128:(k + 1) * 128, j * 512:(j + 1) * 512]), reads=[Bx], writes=[bo])
                s_ = stg[k % 2]
                bs_ = buf("stg%d" % (k % 2))
                P.op("scalar", lambda e, o=o, s_=s_: e.activation(out=s_[:], in_=o[:], func=AF.Square), reads=[bo], writes=[bs_])
                P.op("tensor", lambda e, s_=s_, k=k: e.matmul(ps[6][:, :], lhsT=onesb[:], rhs=s_[:], start=(k == 0), stop=(k == 15)), reads=[bs_, buf("onesb")], writes=[Bps[6]])
            rstd_from_ssq(ps[6][:, :], ot[4], D, 512, Bps[6], buf("ot4"))
            for k in range(16):
                o = ot[k % 2]
                bo = buf("ot%d" % (k % 2))
                P.dma("sync", lambda e, k=k, j=j, o=o: e.dma_start(out=o[:], in_=xs_d[k * 128:(k + 1) * 128, j * 512:(j + 1) * 512]), reads=[Bx], writes=[bo])
                ri = nxt("rbuf", 2)
                P.op("vector", lambda e, k=k, o=o, ri=ri: e.scalar_tensor_tensor(out=rbuf[ri][:], in0=o[:], scalar=fnormw[:, k:k + 1], in1=ot[4][:], op0=ALU.mult, op1=ALU.mult),
                     reads=[bo, buf("ot4"), buf("fnormw")], writes=[buf("rbuf%d" % ri)])
                P.dma("sync", lambda e, k=k, j=j, ri=ri: e.dma_start(out=yT_out[k * 128:(k + 1) * 128, j * 512:(j + 1) * 512], in_=rbuf[ri][:]), reads=[buf("rbuf%d" % ri)], writes=[Bout_], acc=True)
        P.finish("sync", [Bout_])
        P.emit(block, sems)
    print("ops", P.nops)
    return nc


def host_prep(inputs, depth=DEPTH):
    x = np.asarray(inputs["x"], np.float32)
    L = depth
    rel = np.asarray(inputs["rel_bias"], np.float32)
    maps = []
    sl = np.arange(128)[:, None]
    uu = np.arange(GL)[None, :]
    import ml_dtypes
    bf = ml_dtypes.bfloat16
    ident = np.eye(128, dtype=np.float32).astype(bf)
    tri = (np.arange(128)[:, None] <= np.arange(128)[None, :]).astype(np.float32)
    normw = np.ascontiguousarray(np.asarray(inputs["norm_w"], np.float32)[:L].reshape(L, 16, 128).transpose(2, 0, 1).reshape(128, L * 16))
    fnormw = np.ascontiguousarray(np.asarray(inputs["final_norm_w"], np.float32).reshape(16, 128).T)
    foxb = np.ascontiguousarray(np.broadcast_to(np.asarray(inputs["fox_b_f"], np.float32)[:L].reshape(1, L * 4), (128, L * 4)))
    lq = np.stack([np.asarray(inputs[k], np.float32)[:L] for k in ("diff_lq1", "diff_lk1", "diff_lq2", "diff_lk2")], axis=1)
    lq = np.ascontiguousarray(np.broadcast_to(lq.reshape(1, L * 256), (128, L * 256)))
    subln = np.ascontiguousarray(np.asarray(inputs["diff_subln_w"], np.float32)[:L].T)
    b31 = np.ascontiguousarray(np.broadcast_to(rel[31:32, :], (128, 12)))
    w_in = np.asarray(inputs["w_in"], np.float32)
    w_br = np.asarray(inputs["w_branch"], np.float32).reshape(DEPTH, 2048, 2048)
    w_out = np.asarray(inputs["w_out"], np.float32)
    for core in range(8):
        b, c = core // 4, core % 4
        toks = np.concatenate([np.arange(512 * (4 * j + c), 512 * (4 * j + c + 1)) for j in range(4)])
        xT = np.ascontiguousarray(x[b, toks, :].T)
        dist = uu - GOFF + 512 * c - sl
        bidx = t5_bucket_np(dist)
        gt = np.ascontiguousarray(rel[bidx, :].transpose(2, 0, 1))
        mneg = np.where(dist >= 0, 0.0, NEG).astype(np.float32).astype(bf)
        nval = ((dist >= 0) & (dist <= 128)).astype(np.int32) + ((dist >= 0) & (dist % 4 == 0) & (dist <= 512)).astype(np.int32) \
            + ((dist >= 0) & (dist % 16 == 0) & (dist <= 2048)).astype(np.int32)
        cdil = np.where(nval > 0, np.log(np.maximum(nval, 1).astype(np.float32)), NEG).astype(np.float32).astype(bf)
        vv = np.arange(2432)[None, :]
        mt = np.where((vv - 384 - 512 * c - sl) > 0, -1e9, 0.0).astype(np.float32).astype(bf)
        blk = np.arange(16)
        kb_own = 16 * (blk // 4) + 4 * c + (blk % 4)
        sel = (np.arange(64)[None, :] <= kb_own[:, None]).astype(np.float32)
        sel = np.ascontiguousarray(np.broadcast_to(sel.reshape(1, 1024), (128, 1024)))
        maps.append({
            "xT": xT,
            "w_in": w_in[:L], "w_br": w_br[:L], "w_out": w_out[:L],
            "normw": normw, "fnormw": fnormw, "foxb": foxb, "lq": lq, "subln": subln,
            "gt": gt, "mneg": mneg, "cdil": cdil, "b31": b31, "mt": mt, "sel": sel, "ident": ident, "tri": tri,
        })
    return maps


def run(inputs, depth=DEPTH):
    nc = build(depth)
    maps = host_prep(inputs, depth)
    res = run_bass_kernel_spmd(nc, maps, core_ids=list(range(8)))
    out = np.zeros((2, S, D), np.float32)
    for core in range(8):
        b, c = core // 4, core % 4
        toks = np.concatenate([np.arange(512 * (4 * j + c), 512 * (4 * j + c + 1)) for j in range(4)])
        out[b, toks, :] = res.results[core]["yT"].T
    return out


def kernel(**inputs):
    return run(inputs, DEPTH)
```
